# Optimizing a Trainium2 kernel written in Bass

```python
import math
import jax, jax.numpy as jnp
from jax import lax
import numpy as np

D_MODEL = 1024
BATCH = 8
SEQ = 4096
DEPTH = 4

GRID_W = 64
CTX_LEN = 256
N_MIXERS = 3
HY_ORDER = 2
HY_FILTER_WIDTH = 64
HY_EMB_DIM = 33
HY_INNER_MLPS = 2
HY_DECAY_TARGET = 1e-2
HY_SHORT_DECAY_PCT = 0.3
HY_LONG_DECAY_PCT = 1.5
RW_HEAD_DIM = 64
RW_HEADS = D_MODEL // RW_HEAD_DIM
RW_DECAY_LORA = max(32, int(round(1.8 * D_MODEL ** 0.5 / 32)) * 32)
RW_A_LORA = max(32, int(round(1.8 * D_MODEL ** 0.5 / 32)) * 32)
RW_GATE_LORA = max(32, int(round(0.6 * D_MODEL ** 0.8 / 32)) * 32)
RW_GN_EPS = 64e-5
FN_GROUPS = 8
FN_GROUP_DIM = D_MODEL // FN_GROUPS
N_EXPERTS = 16
EC_CAPACITY = 2
D_FF_EXPERT = 2 * D_MODEL
LN_EPS = 1e-5
ADALN_EPS = 1e-6
DEEPNORM_ALPHA = (2 * DEPTH) ** 0.25
DEEPNORM_BETA = (8 * DEPTH) ** -0.25

kernel_name = "hybrid_hyena_rwkv7_fnet_ecmoe_deepnorm"

F32 = jnp.float32


def _layer_norm(x, eps):
    xf = x.astype(F32)
    mu = jnp.mean(xf, -1, keepdims=True)
    var = jnp.mean(jnp.square(xf - mu), -1, keepdims=True)
    return (xf - mu) * lax.rsqrt(var + eps)


def _ln_affine(x, g, b):
    return (_layer_norm(x, LN_EPS) * g.astype(F32) + b.astype(F32)).astype(x.dtype)


def _modulate(x, shift, scale):
    return (_layer_norm(x, ADALN_EPS) * (1.0 + scale.astype(F32)) + shift.astype(F32)).astype(x.dtype)


def _grid_pos_embed(rows, dim):
    r_idx = jnp.repeat(jnp.arange(rows, dtype=F32), GRID_W)
    c_idx = jnp.tile(jnp.arange(GRID_W, dtype=F32), rows)
    quarter = dim // 4
    omega = 1.0 / (10000.0 ** (jnp.arange(quarter, dtype=F32) / quarter))
    def emb(p):
        a = p[:, None] * omega[None, :]
        return jnp.concatenate([jnp.sin(a), jnp.cos(a)], -1)
    return jnp.concatenate([emb(r_idx), emb(c_idx)], -1)


def _centred_conv3(u, w, b):
    up = jnp.pad(u, ((0, 0), (1, 1), (0, 0)))
    return up[:, :-2] * w[0] + up[:, 1:-1] * w[1] + up[:, 2:] * w[2] + b


def _hyena_filters(L, f_w1, f_b1, f_freq, f_w2, f_b2, f_w3):
    pos = jnp.arange(L, dtype=F32)
    t01 = pos / max(L - 1, 1)
    bands = (HY_EMB_DIM - 1) // 2
    f = jnp.linspace(1e-4, bands - 1, bands, dtype=F32)
    ang = f[None, :] * (2.0 * math.pi * pos / L)[:, None]
    z = jnp.concatenate([t01[:, None], jnp.cos(ang), -jnp.sin(ang)], -1)
    freq = f_freq.astype(F32)
    a = jnp.sin(freq * (z @ f_w1.astype(F32) + f_b1.astype(F32)))
    for n in range(HY_INNER_MLPS):
        a = jnp.sin(freq * (a @ f_w2[n].astype(F32) + f_b2[n].astype(F32)))
    h = (a @ f_w3.astype(F32)).reshape(L, 2, HY_ORDER, D_MODEL)
    max_decay = math.log(HY_DECAY_TARGET) / HY_SHORT_DECAY_PCT
    min_decay = math.log(HY_DECAY_TARGET) / HY_LONG_DECAY_PCT
    deltas = jnp.linspace(min_decay, max_decay, D_MODEL, dtype=F32)
    window = jnp.exp(-t01[:, None] * jnp.abs(deltas)[None, :])
    h = h * window[:, None, None, :]
    two_sided = jnp.concatenate([h[:, 0], jnp.zeros((1, HY_ORDER, D_MODEL), F32), h[:0:-1, 1]], 0)
    two_sided = two_sided / jnp.sum(jnp.abs(two_sided), 0, keepdims=True)
    return jnp.fft.rfft(two_sided, axis=0)


def _hyena(h, w_in, b_in, conv_w, conv_b, f_w1, f_b1, f_freq, f_w2, f_b2, f_w3, f_bias, w_out, b_out):
    _, L, _ = h.shape
    u = _centred_conv3(h @ w_in + b_in, conv_w, conv_b)
    v, x1, x2 = jnp.split(u, 3, -1)
    kf = _hyena_filters(L, f_w1, f_b1, f_freq, f_w2, f_b2, f_w3)
    def long_conv(zin, o):
        zf = zin.astype(F32)
        y = jnp.fft.irfft(jnp.fft.rfft(zf, n=2 * L, axis=1) * kf[None, :, o], n=2 * L, axis=1)[:, :L]
        return (y + zf * f_bias[o].astype(F32)).astype(h.dtype)
    z = x1 * long_conv(v, 0)
    z = x2 * long_conv(z, 1)
    return z @ w_out + b_out


def _heads(t):
    return t.astype(F32).reshape(t.shape[:-1] + (RW_HEADS, RW_HEAD_DIM))


def _token_shift_bidir(h):
    hp = jnp.pad(h, ((0, 0), (1, 1), (0, 0)))
    return 0.5 * (hp[:, :-2] + hp[:, 2:]) - h


def _rwkv_prep(h, mu, wr, wk, wv, w0, w1, w2, a0, a1, a2, k_k, k_a):
    B_, T, _ = h.shape
    xx = _token_shift_bidir(h)
    xr, xw, xk, xv, xa, xg = [h + xx * mu[n] for n in range(6)]
    k = xk @ wk
    lw = jnp.einsum('nbtr,nrc->nbtc', jnp.tanh(jnp.einsum('btc,ncr->nbtr', xw, w1)), w2).astype(F32) + w0[:, None, None, :].astype(F32)
    decay = jnp.exp(-jnp.exp(-jax.nn.softplus(-lw) - 0.5))
    a = jax.nn.sigmoid(jnp.einsum('nbtr,nrc->nbtc', jnp.einsum('btc,ncr->nbtr', xa, a1), a2).astype(F32) + a0[:, None, None, :].astype(F32))
    kk = (k * k_k).astype(F32).reshape(B_, T, RW_HEADS, RW_HEAD_DIM)
    kk = kk * lax.rsqrt(jnp.maximum(jnp.sum(kk * kk, -1, keepdims=True), 1e-24))
    kk = kk.reshape(B_, T, D_MODEL)
    k_dir = k.astype(F32)[None] * (1.0 + (a - 1.0) * k_a.astype(F32))
    return {"r": (xr @ wr).astype(F32), "v": (xv @ wv).astype(F32), "kk": kk, "decay": decay,
            "k": k_dir, "kka": kk[None] * a, "xg": xg}


def _bidir_seq(tc, tl, directional):
    if directional:
        fc, bc, fl, bl = tc[0], tc[1], tl[0], tl[1]
    else:
        fc, bc, fl, bl = tc, tc, tl, tl
    fwd = jnp.concatenate([fc, fl], 1)
    bwd = jnp.concatenate([jnp.flip(bc, 1), jnp.flip(bl, 1)], 1)
    return jnp.stack([fwd, bwd], 0)


def _wkv_scan(r, decay, k, v, kk, kka):
    def step(S, inp):
        r_t, w_t, k_t, v_t, kk_t, kka_t = inp
        sa = jnp.einsum('dbhvk,dbhk->dbhv', S, kk_t)
        S = S * w_t[..., None, :] - sa[..., :, None] * kka_t[..., None, :] + v_t[..., :, None] * k_t[..., None, :]
        return S, jnp.einsum('dbhvk,dbhk->dbhv', S, r_t)
    S0 = jnp.zeros(r.shape[:2] + (RW_HEADS, RW_HEAD_DIM, RW_HEAD_DIM), F32)
    xs = tuple(jnp.moveaxis(t, 2, 0) for t in (r, decay, k, v, kk, kka))
    _, y = lax.scan(step, S0, xs)
    return jnp.moveaxis(y, 0, 2)


def _rwkv_mixer(hc, hl, prep_params, g1, g2, r_k, gn_g, gn_b, w_o, ctx_out):
    pc = _rwkv_prep(hc, *prep_params)
    pl = _rwkv_prep(hl, *prep_params)
    n_ctx = hc.shape[1]
    def seq(name, directional):
        return _heads(_bidir_seq(pc[name], pl[name], directional))
    y = _wkv_scan(seq("r", False), seq("decay", True), seq("k", True), seq("v", False), seq("kk", False), seq("kka", True))
    def finish(y_dir, p, h):
        wkv = y_dir[0] + jnp.flip(y_dir[1], 1)
        m = jnp.mean(wkv, -1, keepdims=True)
        var = jnp.mean(jnp.square(wkv - m), -1, keepdims=True)
        gn = (wkv - m) * lax.rsqrt(var + RW_GN_EPS) * _heads(gn_g) + _heads(gn_b)
        bonus = jnp.sum(_heads(p["r"])[None] * _heads(p["k"]) * _heads(r_k), axis=(0, -1))[..., None] * _heads(p["v"])
        g = jax.nn.sigmoid(p["xg"] @ g1) @ g2
        o = (gn + bonus).reshape(h.shape).astype(h.dtype) * g
        return o @ w_o
    yl = finish(y[:, :, n_ctx:], pl, hl)
    yc = finish(y[:, :, :n_ctx], pc, hc) if ctx_out else None
    return yc, yl


def _fourier(h, w_o, b_o):
    B_, T, _ = h.shape
    hg = h.astype(F32).reshape(B_, T, FN_GROUPS, FN_GROUP_DIM)
    mixed = jnp.real(jnp.fft.fft2(hg, axes=(1, 3), norm="ortho")).reshape(B_, T, D_MODEL)
    return mixed.astype(h.dtype) @ w_o + b_o


def _expert_choice_moe(h, w_router, w1, w3, w2):
    B_, T, _ = h.shape
    cap = EC_CAPACITY * T // N_EXPERTS
    aff = jax.nn.softmax((h @ w_router).astype(F32), -1)
    gate, idx = lax.top_k(jnp.swapaxes(aff, 1, 2), cap)
    bidx = jnp.arange(B_)[:, None, None]
    xe = h[bidx, idx]
    he = jax.nn.silu(jnp.einsum('becd,edf->becf', xe, w1)) * jnp.einsum('becd,edf->becf', xe, w3)
    ye = jnp.einsum('becf,efd->becd', he, w2) * gate[..., None].astype(h.dtype)
    return jnp.zeros_like(h).at[bidx, idx].add(ye)


def setup_inputs(seed: int = 0) -> dict:
    key = jax.random.key(seed)
    keys = iter(jax.random.split(key, 64))
    def nrm(shape, scale):
        return scale * jax.random.normal(next(keys), shape, F32)
    def unif(shape, lo, hi):
        return jax.random.uniform(next(keys), shape, F32, lo, hi)
    D, E, F = D_MODEL, N_EXPERTS, D_FF_EXPERT
    n_a = len(range(0, DEPTH, N_MIXERS))
    n_b = len(range(1, DEPTH, N_MIXERS))
    n_c = len(range(2, DEPTH, N_MIXERS))
    FW = HY_FILTER_WIDTH
    return {
        "x": nrm((BATCH, SEQ, D), 1.0),
        "c": nrm((BATCH, D), 1.0),
        "ctx": nrm((BATCH, CTX_LEN, D), 1.0),
        "c_ctx": nrm((D,), 1.0),
        "mod_w": nrm((DEPTH, D, 6 * D), 0.25 * D ** -0.5),
        "mod_b": nrm((DEPTH, 6 * D), 0.02),
        "ln_g": 1.0 + nrm((DEPTH, 2, D), 0.02),
        "ln_b": nrm((DEPTH, 2, D), 0.02),
        "moe_router": nrm((DEPTH, D, E), D ** -0.5),
        "moe_w1": nrm((DEPTH, E, D, F), D ** -0.5),
        "moe_w3": nrm((DEPTH, E, D, F), D ** -0.5),
        "moe_w2": nrm((DEPTH, E, F, D), DEEPNORM_BETA * F ** -0.5),
        "hy_w_in": nrm((n_a, D, 3 * D), D ** -0.5),
        "hy_b_in": nrm((n_a, 3 * D), 0.02),
        "hy_conv_w": nrm((n_a, 3, 3 * D), 3 ** -0.5),
        "hy_conv_b": nrm((n_a, 3 * D), 0.02),
        "hy_f_w1": nrm((n_a, HY_EMB_DIM, FW), HY_EMB_DIM ** -0.5),
        "hy_f_b1": nrm((n_a, FW), 0.1),
        "hy_f_freq": 1.0 + nrm((n_a, FW), 0.02),
        "hy_f_w2": nrm((n_a, HY_INNER_MLPS, FW, FW), FW ** -0.5),
        "hy_f_b2": nrm((n_a, HY_INNER_MLPS, FW), 0.1),
        "hy_f_w3": nrm((n_a, FW, 2 * HY_ORDER * D), FW ** -0.5),
        "hy_f_bias": nrm((n_a, HY_ORDER, D), 1.0),
        "hy_w_out": nrm((n_a, D, D), DEEPNORM_BETA * D ** -0.5),
        "hy_b_out": nrm((n_a, D), 0.02),
        "rw_mu": unif((n_b, 6, D), 0.0, 1.0),
        "rw_wr": nrm((n_b, D, D), D ** -0.5),
        "rw_wk": nrm((n_b, D, D), D ** -0.5),
        "rw_wv": nrm((n_b, D, D), D ** -0.5),
        "rw_w0": unif((n_b, 2, D), -6.0, 1.0),
        "rw_w1": nrm((n_b, 2, D, RW_DECAY_LORA), D ** -0.5),
        "rw_w2": nrm((n_b, 2, RW_DECAY_LORA, D), 0.1 * RW_DECAY_LORA ** -0.5),
        "rw_a0": nrm((n_b, 2, D), 0.1),
        "rw_a1": nrm((n_b, 2, D, RW_A_LORA), D ** -0.5),
        "rw_a2": nrm((n_b, 2, RW_A_LORA, D), 0.1 * RW_A_LORA ** -0.5),
        "rw_kk": 0.85 + nrm((n_b, D), 0.02),
        "rw_ka": 1.0 + nrm((n_b, D), 0.02),
        "rw_g1": nrm((n_b, D, RW_GATE_LORA), D ** -0.5),
        "rw_g2": nrm((n_b, RW_GATE_LORA, D), RW_GATE_LORA ** -0.5),
        "rw_rk": nrm((n_b, D), 0.1),
        "rw_gn_g": 1.0 + nrm((n_b, D), 0.02),
        "rw_gn_b": nrm((n_b, D), 0.02),
        "rw_wo": nrm((n_b, D, D), DEEPNORM_BETA * D ** -0.5),
        "fn_wo": nrm((n_c, D, D), DEEPNORM_BETA * D ** -0.5),
        "fn_bo": nrm((n_c, D), 0.02),
    }


def reference(x, c, ctx, c_ctx, mod_w, mod_b, ln_g, ln_b, moe_router, moe_w1, moe_w3, moe_w2,
              hy_w_in, hy_b_in, hy_conv_w, hy_conv_b, hy_f_w1, hy_f_b1, hy_f_freq, hy_f_w2, hy_f_b2, hy_f_w3,
              hy_f_bias, hy_w_out, hy_b_out,
              rw_mu, rw_wr, rw_wk, rw_wv, rw_w0, rw_w1, rw_w2, rw_a0, rw_a1, rw_a2, rw_kk, rw_ka,
              rw_g1, rw_g2, rw_rk, rw_gn_g, rw_gn_b, rw_wo,
              fn_wo, fn_bo):
    n_lat = x.shape[1]
    rows = n_lat // GRID_W
    x = x + _grid_pos_embed(rows, D_MODEL).astype(x.dtype)[None]
    xc = ctx
    readers = [i for i in range(DEPTH) if i % N_MIXERS == 1]
    last_reader = readers[-1] if readers else -1
    for i in range(DEPTH):
        kind, j = i % N_MIXERS, i // N_MIXERS
        ctx_in = i <= last_reader
        ctx_full = i < last_reader
        m_l = jnp.split((jax.nn.silu(c) @ mod_w[i] + mod_b[i])[:, None, :], 6, -1)
        hl = _modulate(x, m_l[0], m_l[1])
        if ctx_in:
            m_c = jnp.split((jax.nn.silu(c_ctx) @ mod_w[i] + mod_b[i])[None, None, :], 6, -1)
            hc = _modulate(xc, m_c[0], m_c[1])
        if kind == 0:
            hy = (hy_w_in[j], hy_b_in[j], hy_conv_w[j], hy_conv_b[j], hy_f_w1[j], hy_f_b1[j], hy_f_freq[j],
                  hy_f_w2[j], hy_f_b2[j], hy_f_w3[j], hy_f_bias[j], hy_w_out[j], hy_b_out[j])
            yl = _hyena(hl, *hy)
            yc = _hyena(hc, *hy) if ctx_full else None
        elif kind == 1:
            prep = (rw_mu[j], rw_wr[j], rw_wk[j], rw_wv[j], rw_w0[j], rw_w1[j], rw_w2[j],
                    rw_a0[j], rw_a1[j], rw_a2[j], rw_kk[j], rw_ka[j])
            yc, yl = _rwkv_mixer(hc, hl, prep, rw_g1[j], rw_g2[j], rw_rk[j], rw_gn_g[j], rw_gn_b[j], rw_wo[j], ctx_full)
        else:
            yl = _fourier(hl, fn_wo[j], fn_bo[j])
            yc = _fourier(hc, fn_wo[j], fn_bo[j]) if ctx_full else None
        x = _ln_affine(DEEPNORM_ALPHA * x + (1.0 + m_l[2]) * yl, ln_g[i, 0], ln_b[i, 0])
        hl = _modulate(x, m_l[3], m_l[4])
        x = _ln_affine(DEEPNORM_ALPHA * x + (1.0 + m_l[5]) * _expert_choice_moe(hl, moe_router[i], moe_w1[i], moe_w3[i], moe_w2[i]),
                       ln_g[i, 1], ln_b[i, 1])
        if ctx_full:
            xc = _ln_affine(DEEPNORM_ALPHA * xc + (1.0 + m_c[2]) * yc, ln_g[i, 0], ln_b[i, 0])
            hc = _modulate(xc, m_c[3], m_c[4])
            xc = _ln_affine(DEEPNORM_ALPHA * xc + (1.0 + m_c[5]) * _expert_choice_moe(hc, moe_router[i], moe_w1[i], moe_w3[i], moe_w2[i]),
                            ln_g[i, 1], ln_b[i, 1])
    return x
```

```python
import math
import numpy as np
from contextlib import ExitStack
import concourse.bass as bass
import concourse.mybir as mybir
from concourse.bass_utils import run_bass_kernel_spmd

F32 = mybir.dt.float32
I32 = mybir.dt.int32
U32 = mybir.dt.uint32
AF = mybir.ActivationFunctionType
ALU = mybir.AluOpType
AX = mybir.AxisListType

NCORES = 8
D = 1024
T = 4096
TC = 256
DEPTH = 4
NE = 16
FF = 2048
ALPHA = (2 * DEPTH) ** 0.25
BW = 1024


class Buf:
    def __init__(self, name, ap=None, nsub=0):
        self.name = name
        self.t = ap
        self.w = []
        self.r = []
        self.kids = [Buf(name + str(i), ap) for i in range(nsub)]

    def __getitem__(self, idx):
        return self.t[idx]

    def ch(self, i):
        return self.kids[i]

    def leaves(self):
        if not self.kids:
            return [self]
        out = []
        for c in self.kids:
            out += c.leaves()
        return out


def _lv(bufs):
    out = []
    for b in bufs:
        out += b.leaves()
    return out


class KB:
    def __init__(self, nc, n_dma_sems=88):
        self.nc = nc
        self.eng = {"pe": nc.tensor, "dve": nc.vector, "act": nc.scalar,
                    "pool": nc.gpsimd, "sp": nc.sync}
        self.esem = {}
        self.cnt = {}
        self.semh = {}
        self.seen = {k: {} for k in self.eng}
        for k in self.eng:
            nm = "e_" + k
            self.semh[nm] = nc.alloc_semaphore(name=nm)
            self.esem[k] = nm
            self.cnt[nm] = 0
        self.free_dma = []
        for i in range(n_dma_sems):
            nm = "d%d" % i
            self.semh[nm] = nc.alloc_semaphore(name=nm)
            self.cnt[nm] = 0
            self.free_dma.append(nm)
        self.phase_sems = []
        self.stack = None
        self.rr = 0
        self.pstack = ExitStack()
        self.semreg = {}

    def begin(self):
        self.stack = ExitStack()
        self.phase_sems = []

    def end(self):
        self.barrier()
        self.stack.close()
        self.stack = None
        for nm in self.phase_sems:
            self.semreg.pop(nm, None)
        self.free_dma = self.phase_sems + self.free_dma
        self.phase_sems = []

    def dsem(self, persistent=False):
        nm = self.free_dma.pop()
        if not persistent:
            self.phase_sems.append(nm)
        return nm

    def tile(self, shape, dtype=F32, name=None, persistent=False, nsub=0):
        st = self.pstack if persistent else self.stack
        t = st.enter_context(self.nc.sbuf_tensor(list(shape), dtype))
        return Buf(name or "t", t, nsub)

    def ptile(self, shape, dtype=F32, name=None, nsub=0):
        t = self.stack.enter_context(self.nc.psum_tensor(list(shape), dtype))
        return Buf(name or "p", t, nsub)

    def _wait(self, ek, toks):
        need = {}
        for (s, v) in toks:
            if v > need.get(s, 0):
                need[s] = v
        for s, v in need.items():
            if self.seen[ek].get(s, 0) >= v:
                continue
            self.eng[ek].wait_ge(self.semh[s], v)
            self.seen[ek][s] = v

    def barrier(self):
        toks = [(s, c) for s, c in self.cnt.items() if c > 0]
        for ek in self.eng:
            self._wait(ek, toks)

    def op(self, ek, fn, reads=(), writes=(), accum=False):
        reads = _lv(reads)
        writes = _lv(writes)
        toks = []
        es = self.esem[ek]
        for b in reads:
            toks += b.w
        for b in writes:
            if accum:
                toks += [t for t in b.w if t[0] != es]
            else:
                toks += b.w
            toks += b.r
        self._wait(ek, toks)
        ins = fn(self.eng[ek])
        self.cnt[es] += 1
        tok = (es, self.cnt[es])
        ins.then_inc(self.semh[es], 1)
        for b in reads:
            b.r.append(tok)
        for b in writes:
            b.w = [tok]
            b.r = []
        return ins

    def dma(self, out_ap, in_ap, reads=(), writes=(), sem=None, q=None, fn=None, **kw):
        reads = _lv(reads)
        writes = _lv(writes)
        if q is None:
            q = ("sp", "act")[self.rr % 2]
            self.rr += 1
        toks = []
        for b in reads:
            toks += b.w
        for b in writes:
            toks += [t for t in b.w if t[0] != sem]
            toks += b.r
        self._wait(q, toks)
        if fn is not None:
            ins = fn(self.eng[q])
        else:
            ins = self.eng[q].dma_start(out=out_ap, in_=in_ap, **kw)
        self.cnt[sem] += 16
        tok = (sem, self.cnt[sem])
        ins.then_inc(self.semh[sem], 16)
        reg = self.semreg.setdefault(sem, {})
        for b in reg.values():
            b.w = [tok if t[0] == sem else t for t in b.w]
            b.r = [tok if t[0] == sem else t for t in b.r]
        for b in reads:
            b.r.append(tok)
            reg[id(b)] = b
        for b in writes:
            b.w = [tok]
            b.r = []
            reg[id(b)] = b
        return ins


def _pv_cols():
    cols = {}
    off = 0

    def add(name, n):
        nonlocal off
        cols[name] = (off, n // 128)
        off += n // 128
    for i in range(DEPTH):
        add("mod_b%d" % i, 6 * D)
        for j in range(2):
            add("ln_g%d_%d" % (i, j), D)
            add("ln_b%d_%d" % (i, j), D)
    for j in range(2):
        add("hy_b_out%d" % j, D)
    for n in range(6):
        add("rw_mu%d" % n, D)
    add("fn_bo", D)
    return cols, off


PV_COLS, PV_N = _pv_cols()


def blob_spec(layers):
    sp = {"misc": []}
    m = sp["misc"]
    m.append(("ident", (128, 128)))
    m.append(("pv", (128, 1024)))
    m.append(("pos", (T, D)))
    m.append(("mod_w", (DEPTH * D, 6 * D)))
    m.append(("moe_router", (DEPTH * D, NE)))
    for i in layers:
        sp["moe%d" % i] = [("w1", (NE * D, FF)), ("w3", (NE * D, FF)), ("w2", (NE * FF, D))]
    if 0 in layers or 3 in layers:
        h = [("w_in", (2 * D, 3 * D)), ("b_in", (2, 3 * D)), ("conv_w", (6, 3 * D)), ("conv_b", (2, 3 * D)),
             ("f_w1", (66, 64)), ("hyv", (128, 4)), ("f_w2", (256, 64)), ("f_w3", (128, 4096)),
             ("f_bias", (4, D)), ("w_out", (2 * D, D)),
             ("C2", (64, 64)), ("S2", (64, 64)), ("nS2", (64, 64)), ("RA", (64, 128)), ("RB", (64, 128))]
        for L in (T, TC):
            NH = L // 64
            N1 = 2 * NH
            h += [("zT_%d" % L, (33, L)), ("win_%d" % L, (L, D)), ("F1_%d" % L, (NH, 2 * N1)),
                  ("c_%d" % L, (64, N1)), ("s_%d" % L, (64, N1)), ("cT_%d" % L, (N1, 64)), ("sT_%d" % L, (N1, 64)),
                  ("C1_%d" % L, (N1, NH)), ("nS1_%d" % L, (N1, NH))]
        sp["hy"] = h
    if 1 in layers:
        sp["rw"] = [("wr", (D, D)), ("wk", (D, D)), ("wv", (D, D)), ("wo", (D, D)),
                    ("w1cat", (D, 128)), ("a1cat", (D, 128)), ("g1pad", (D, 256)),
                    ("w2pad0", (128, D)), ("w2pad1", (128, D)), ("a2pad0", (128, D)), ("a2pad1", (128, D)),
                    ("g2pad", (256, D)), ("rows", (9, D)),
                    ("TRI0", (128, 128)), ("TRI1", (128, 128)), ("CHK", (128, 2)),
                    ("MASK0", (64, 512)), ("MASK1", (64, 512)), ("IDN", (64, 64))]
    if 2 in layers:
        sp["fnet"] = [("fn_cs", (128, 256)), ("fn_wo", (D, D)), ("fn_nct", (T, T)), ("fn_nst", (T, T))]
    return sp


def blob_layout(spec):
    lay = {}
    for piece, items in spec.items():
        off = 0
        ent = {}
        for name, (R, C) in items:
            n = R * C
            rows = (n + BW - 1) // BW
            ent[name] = (off, rows, R, C)
            off += rows
        tot = (off + 8 * 16 - 1) // (8 * 16) * (8 * 16)
        lay[piece] = (tot, ent)
    return lay


def grid_pos_embed():
    rows = T // 64
    r_idx = np.repeat(np.arange(rows, dtype=np.float32), 64)
    c_idx = np.tile(np.arange(64, dtype=np.float32), rows)
    quarter = D // 4
    omega = (1.0 / (10000.0 ** (np.arange(quarter, dtype=np.float32) / np.float32(quarter)))).astype(np.float32)

    def emb(p):
        a = (p[:, None] * omega[None, :]).astype(np.float32)
        return np.concatenate([np.sin(a), np.cos(a)], -1)
    return np.concatenate([emb(r_idx), emb(c_idx)], -1).astype(np.float32)


def hy_const_tables():
    out = {}
    n2 = np.arange(64)
    a2 = 2 * np.pi * (np.outer(n2, n2) % 64) / 64.0
    C2, S2 = np.cos(a2), np.sin(a2)
    out["C2"], out["S2"], out["nS2"] = C2, S2, -S2
    out["RA"] = np.concatenate([C2, S2], 1)
    out["RB"] = np.concatenate([-S2, C2], 1)
    for L in (T, TC):
        NH = L // 64
        N1 = 2 * NH
        N = 64 * N1
        a1 = 2 * np.pi * (np.outer(np.arange(NH), np.arange(N1)) % N1) / N1
        out["F1_%d" % L] = np.concatenate([np.cos(a1), -np.sin(a1)], 1)
        at = 2 * np.pi * (np.outer(n2, np.arange(N1)) % N) / N
        out["c_%d" % L], out["s_%d" % L] = np.cos(at), np.sin(at)
        out["cT_%d" % L], out["sT_%d" % L] = np.cos(at).T, np.sin(at).T
        ai = 2 * np.pi * (np.outer(np.arange(N1), np.arange(NH)) % N1) / N1
        out["C1_%d" % L], out["nS1_%d" % L] = np.cos(ai), -np.sin(ai)
        pos = np.arange(L, dtype=np.float32)
        t01 = (pos / np.float32(max(L - 1, 1))).astype(np.float32)
        f = np.linspace(1e-4, 15, 16, dtype=np.float32)
        ang = (f[None, :] * (np.float32(2.0 * math.pi) * pos / np.float32(L))[:, None]).astype(np.float32)
        z = np.concatenate([t01[:, None], np.cos(ang), -np.sin(ang)], -1).astype(np.float32)
        out["zT_%d" % L] = z.T
        deltas = np.linspace(math.log(1e-2) / 1.5, math.log(1e-2) / 0.3, D, dtype=np.float32)
        out["win_%d" % L] = np.exp(-t01[:, None] * np.abs(deltas)[None, :]).astype(np.float32)
    return {k_: np.ascontiguousarray(v, dtype=np.float32) for k_, v in out.items()}


def rw_const_tables():
    out = {}
    i = np.arange(128)
    same = (i[:, None] // 64) == (i[None, :] // 64)
    out["TRI0"] = (same & (i[:, None] <= i[None, :])).astype(np.float32)
    out["TRI1"] = (same & (i[:, None] >= i[None, :])).astype(np.float32)
    out["CHK"] = np.stack([(i < 64), (i >= 64)], 1).astype(np.float32)
    a = np.arange(64)
    for n in range(2):
        before = (a[:, None] < a[None, :]) if n == 0 else (a[:, None] > a[None, :])
        ateq = before | (a[:, None] == a[None, :])
        m = np.concatenate([-before.astype(np.float32), ateq.astype(np.float32), before.astype(np.float32),
                            ateq.astype(np.float32), -before.T.astype(np.float32), np.zeros((64, 192), np.float32)], 1)
        out["MASK%d" % n] = m
    out["IDN"] = np.eye(64, dtype=np.float32)
    return out


def pack_host(inputs, layers):
    spec = blob_spec(layers)
    lay = blob_layout(spec)
    src = {}
    src["ident"] = np.eye(128, dtype=np.float32)
    pv = np.zeros((128, 1024), np.float32)

    def putv(name, v):
        c0, n = PV_COLS[name]
        pv[:, c0:c0 + n] = np.asarray(v, np.float32).reshape(n, 128).T
    for i in range(DEPTH):
        putv("mod_b%d" % i, inputs["mod_b"][i])
        for j in range(2):
            putv("ln_g%d_%d" % (i, j), inputs["ln_g"][i, j])
            putv("ln_b%d_%d" % (i, j), inputs["ln_b"][i, j])
    for j in range(2):
        putv("hy_b_out%d" % j, inputs["hy_b_out"][j])
    for n in range(6):
        putv("rw_mu%d" % n, inputs["rw_mu"][0, n])
    putv("fn_bo", inputs["fn_bo"][0])
    src["pv"] = pv
    src["pos"] = grid_pos_embed()
    src["mod_w"] = inputs["mod_w"].reshape(DEPTH * D, 6 * D)
    src["moe_router"] = inputs["moe_router"].reshape(DEPTH * D, NE)
    if 0 in layers or 3 in layers:
        src["w_in"] = inputs["hy_w_in"].reshape(2 * D, 3 * D)
        src["b_in"] = inputs["hy_b_in"]
        src["conv_w"] = inputs["hy_conv_w"].reshape(6, 3 * D)
        src["conv_b"] = inputs["hy_conv_b"]
        src["f_w1"] = inputs["hy_f_w1"].reshape(66, 64)
        src["hyv"] = np.concatenate([np.stack([inputs["hy_f_b1"][j], inputs["hy_f_freq"][j],
                                               inputs["hy_f_b2"][j, 0], inputs["hy_f_b2"][j, 1]], 1) for j in range(2)], 0)
        src["f_w2"] = inputs["hy_f_w2"].reshape(256, 64)
        src["f_w3"] = inputs["hy_f_w3"].reshape(128, 4096)
        src["f_bias"] = inputs["hy_f_bias"].reshape(4, D)
        src["w_out"] = inputs["hy_w_out"].reshape(2 * D, D)
        src.update(hy_const_tables())
    if 1 in layers:
        src["wr"], src["wk"], src["wv"], src["wo"] = inputs["rw_wr"][0], inputs["rw_wk"][0], inputs["rw_wv"][0], inputs["rw_wo"][0]
        src["w1cat"] = np.concatenate([inputs["rw_w1"][0, 0], inputs["rw_w1"][0, 1]], 1)
        src["a1cat"] = np.concatenate([inputs["rw_a1"][0, 0], inputs["rw_a1"][0, 1]], 1)
        g1 = np.zeros((D, 256), np.float32); g1[:, :160] = inputs["rw_g1"][0]
        g2 = np.zeros((256, D), np.float32); g2[:160] = inputs["rw_g2"][0]
        src["g1pad"], src["g2pad"] = g1, g2
        for n in range(2):
            w2 = np.zeros((128, D), np.float32); w2[n * 64:(n + 1) * 64] = inputs["rw_w2"][0, n]
            a2 = np.zeros((128, D), np.float32); a2[n * 64:(n + 1) * 64] = inputs["rw_a2"][0, n]
            src["w2pad%d" % n], src["a2pad%d" % n] = w2, a2
        src["rows"] = np.stack([inputs["rw_w0"][0, 0], inputs["rw_w0"][0, 1], inputs["rw_a0"][0, 0], inputs["rw_a0"][0, 1],
                                inputs["rw_kk"][0], inputs["rw_ka"][0], inputs["rw_rk"][0], inputs["rw_gn_g"][0], inputs["rw_gn_b"][0]], 0)
        src.update(rw_const_tables())
    if 2 in layers:
        dk = np.outer(np.arange(128), np.arange(128)) % 128
        ang = 2.0 * np.pi * dk / 128.0
        nrm = 1.0 / math.sqrt(T * 128.0)
        src["fn_cs"] = np.concatenate([-np.cos(ang) * nrm, np.sin(ang) * nrm], 1).astype(np.float32)
        tk = (np.outer(np.arange(T, dtype=np.int64), np.arange(T, dtype=np.int64)) % T).astype(np.float64)
        src["fn_nct"] = (-np.cos(2.0 * np.pi * tk / T)).astype(np.float32)
        src["fn_nst"] = (-np.sin(2.0 * np.pi * tk / T)).astype(np.float32)
        del tk
        src["fn_wo"] = inputs["fn_wo"][0]
    blobs = {}
    for piece, (tot, ent) in lay.items():
        flat = np.zeros((tot, BW), np.float32)
        for name, (off, rows, R, C) in ent.items():
            if piece.startswith("moe"):
                i = int(piece[3:])
                a = {"w1": inputs["moe_w1"][i], "w3": inputs["moe_w3"][i], "w2": inputs["moe_w2"][i]}[name]
            else:
                a = src[name]
            a = np.ascontiguousarray(a, dtype=np.float32).reshape(-1)
            flat[off:off + rows].reshape(-1)[:a.size] = a
        blobs[piece] = flat
    return blobs, lay


class Prog:
    def __init__(self, layers, dbg=(), test=None):
        self.test = test
        import os
        self.stop_at = int(os.environ.get("STOP_AT", "0"))
        self.layers = layers
        self.dbg = list(dbg)
        nc = bass.Bass("TRN2", target_bir_lowering=False)
        self.nc = nc
        self.k = KB(nc)
        self.lay = blob_layout(blob_spec(layers))
        self.dram = {}
        self.build()

    def dt(self, name, shape, kind="Internal", dtype=F32):
        t = self.nc.dram_tensor(name, list(shape), dtype, kind=kind)
        self.dram[name] = t
        return t.ap()

    def W(self, piece, name):
        tot, ent = self.lay[piece]
        off, rows, R, C = ent[name]
        g = self.gath[piece]
        v = g[off:off + rows, :]
        if C == BW:
            return v[:R, :]
        if C < BW:
            return v.rearrange("r (a c) -> (r a) c", c=C)[:R, :]
        return v.rearrange("(r a) c -> r (a c)", a=C // BW)[:R, :]

    def build(self):
        nc, k = self.nc, self.k
        self.x_in = self.dt("x", [T, D], kind="ExternalInput")
        self.c_in = self.dt("c", [1, D], kind="ExternalInput")
        self.ctx_in = self.dt("ctx", [TC, D], kind="ExternalInput")
        self.cctx_in = self.dt("c_ctx", [1, D], kind="ExternalInput")
        self.shard = {}
        self.gath = {}
        self.bounce = {}
        for piece, (tot, ent) in self.lay.items():
            self.gath[piece] = self.dt("blob_" + piece, [tot, BW], kind="ExternalInput")
        self.out = self.dt("out", [T, D], kind="ExternalOutput")

        self.ident = k.tile([128, 128], persistent=True)
        self.pv = k.tile([128, 1024], persistent=True)
        self.ones = k.tile([128, 128], persistent=True)
        self.modv = k.tile([128, DEPTH, 48, 2], persistent=True)
        self.modp = k.tile([128, DEPTH, 48, 2], persistent=True)
        self.idxT = k.tile([128, 4, NE], I32, persistent=True)
        self.gateT = k.tile([128, 4, NE], persistent=True)
        self.phase_consts()
        if self.test == "rw":
            injc = self.dt("injc", [TC, D], kind="ExternalInput")
            injl = self.dt("inj", [T, D], kind="ExternalInput")
            HCF = self.dt("HCF", [D, TC]); HLF = self.dt("HLF", [D, T])
            Y = self.dt("YR", [D, T])
            self.transpose_in(injc, HCF, TC)
            self.transpose_in(injl, HLF, T)
            self.rwkv(HCF, HLF, Y)
            self.final_dbg()
            return
        if self.test in ("hy", "hyc"):
            L = T if self.test == "hy" else TC
            inj = self.dt("inj", [L, D], kind="ExternalInput")
            HFM = self.dt("HFM", [D, L])
            Y = self.dt("YH", [D, L])
            self.transpose_in(inj, HFM, L)
            self.hyena(0, HFM, L, Y, "t")
            self.final_dbg()
            return
        if self.test == "fnet":
            inj = self.dt("inj", [T, D], kind="ExternalInput")
            HFM = self.dt("HFM", [D, T])
            Y = self.dt("YF", [D, T])
            self.transpose_in(inj, HFM, T)
            self.fnet(HFM, Y)
            self.final_dbg()
            return
        if self.test == "moe":
            inj = self.dt("inj", [T, D], kind="ExternalInput")
            HFM = self.dt("HFM", [D, T])
            YM = self.dt("YM", [T, D])
            self.transpose_in(inj, HFM, T)
            self.moe(0, HFM, inj, YM, T)
            self.final_dbg()
            return
        self.phase_modvec()
        self.XT = self.dt("XT", [D, T])
        self.XCT = self.dt("XCT", [D, TC])
        XT, XCT = self.XT, self.XCT
        self.transpose_in(self.x_in, XT, T, add=self.W("misc", "pos"))
        self.transpose_in(self.ctx_in, XCT, TC)
        HL = self.dt("HL", [D, T]); HC = self.dt("HC", [D, TC])
        YL = self.dt("YL", [D, T]); YC = self.dt("YC", [D, TC])
        HL2 = self.dt("HL2", [D, T]); HL2TM = self.dt("HL2TM", [T, D])
        HC2 = self.dt("HC2", [D, TC]); HC2TM = self.dt("HC2TM", [TC, D])
        YM = self.dt("YM", [T, D]); YMT = self.dt("YMT", [D, T])
        YMC = self.dt("YMC", [TC, D]); YMCT = self.dt("YMCT", [D, TC])
        self.ln_pass(XT, T, layer=0, first=True, H_out=HL, modj=0, which=0)
        self.ln_pass(XCT, TC, layer=0, first=True, H_out=HC, modj=0, which=1)
        for i in range(DEPTH):
            if i not in self.layers:
                break
            kind = i % 3
            ctx_full = (i == 0)
            if kind == 0:
                self.hyena(i // 3, HL, T, YL, "l%d" % i)
                if ctx_full:
                    self.hyena(i // 3, HC, TC, YC, "c%d" % i)
            elif kind == 1:
                self.rwkv(HC, HL, YL)
            else:
                self.fnet(HL, YL)
            self.ln_pass(XT, T, layer=i, Y=YL, ymul=lambda c, i=i: self.mv(i, 2, c, 0, plus1=True), lnj=0,
                         X_out=XT, H_out=HL2, H_tm=HL2TM, modj=3, which=0)
            self.moe(i, HL2, HL2TM, YM, T)
            self.transpose_in(YM, YMT, T)
            last = (i == DEPTH - 1)
            self.ln_pass(XT, T, layer=i, Y=YMT, ymul=lambda c, i=i: self.mv(i, 5, c, 0, plus1=True), lnj=1,
                         X_out=None if last else XT, X_tm=self.out if last else None,
                         H_out=None if last else HL, do_mod=not last, modj=0, which=0, mod_layer=i + 1)
            if ctx_full:
                self.ln_pass(XCT, TC, layer=i, Y=YC, ymul=lambda c, i=i: self.mv(i, 2, c, 1, plus1=True), lnj=0,
                             X_out=XCT, H_out=HC2, H_tm=HC2TM, modj=3, which=1)
                self.moe(i, HC2, HC2TM, YMC, TC)
                self.transpose_in(YMC, YMCT, TC)
                self.ln_pass(XCT, TC, layer=i, Y=YMCT, ymul=lambda c, i=i: self.mv(i, 5, c, 1, plus1=True), lnj=1,
                             X_out=XCT, H_out=HC, modj=0, which=1, mod_layer=i + 1)
        self.final_dbg()

    def phase_consts(self):
        k = self.k
        k.begin()
        s = k.dsem()
        k.dma(self.ident[:], self.W("misc", "ident"), writes=[self.ident], sem=s)
        k.dma(self.pv[:], self.W("misc", "pv"), writes=[self.pv], sem=s)
        k.op("dve", lambda e: e.memset(self.ones[:], 1.0), writes=[self.ones])
        k.end()

    def pvc(self, name, c=None):
        c0, n = PV_COLS[name]
        if c is None:
            return self.pv[:, c0:c0 + n]
        return self.pv[:, c0 + c:c0 + c + 1]

    def phase_modvec(self):
        k = self.k
        k.begin()
        sc = k.tile([128, 8, 2])
        s = k.dsem()
        if True:
            k.dma(sc[:, :, 0], self.c_in.rearrange("o (c p) -> p (o c)", p=128), writes=[sc], sem=s, allow_slow_non_contiguous=True)
            k.dma(sc[:, :, 1], self.cctx_in.rearrange("o (c p) -> p (o c)", p=128), writes=[sc], sem=s, allow_slow_non_contiguous=True)
        k.op("act", lambda e: e.activation(out=sc[:], in_=sc[:], func=AF.Silu), reads=[sc], writes=[sc])
        mw = self.W("misc", "mod_w")
        wb = [k.tile([128, 8, 1536]) for _ in range(2)]
        ws = [k.dsem() for _ in range(2)]
        ps = [k.ptile([128, 12, 2]) for _ in range(2)]
        it = 0
        for i in range(DEPTH):
            for g in range(4):
                b = wb[it % 2]
                src = mw[i * D:(i + 1) * D, g * 1536:(g + 1) * 1536].rearrange("(c p) n -> p c n", p=128)
                k.dma(b[:], src, writes=[b], sem=ws[it % 2])
                p = ps[it % 2]
                for j in range(12):
                    for c in range(8):
                        k.op("pe", lambda e, j=j, c=c, b=b, p=p: e.matmul(
                            p[:, j, :], b[:, c, j * 128:(j + 1) * 128], sc[:, c, :],
                            start=(c == 0), stop=(c == 7)),
                            reads=[b, sc], writes=[p], accum=True)
                c0, _ = PV_COLS["mod_b%d" % i]
                bias = self.pv[:, c0 + g * 12:c0 + (g + 1) * 12]
                k.op("dve", lambda e, p=p, i=i, g=g, bias=bias: e.tensor_tensor(
                    out=self.modv[:, i, g * 12:(g + 1) * 12, :], in0=p[:],
                    in1=bias.unsqueeze(2).to_broadcast([128, 12, 2]), op=ALU.add),
                    reads=[p, self.pv], writes=[self.modv])
                it += 1
        k.op("dve", lambda e: e.tensor_scalar(out=self.modp[:], in0=self.modv[:], scalar1=1.0,
                                              scalar2=None, op0=ALU.add),
             reads=[self.modv], writes=[self.modp])
        k.end()

    def mv(self, layer, j, c, which=0, plus1=False):
        t = self.modp if plus1 else self.modv
        return t[:, layer, j * 8 + c, which:which + 1]

    def transpose_in(self, src, dstT, Tn, add=None):
        k = self.k
        k.begin()
        nt = Tn // 128
        grp = min(4, nt)
        xin = [k.tile([128, D]) for _ in range(2)]
        ain = [k.tile([128, D]) for _ in range(2)] if add is not None else None
        sx = [k.dsem() for _ in range(2)]
        sa = [k.dsem() for _ in range(2)]
        so = [k.dsem() for _ in range(2)]
        pst = [k.ptile([128, 4, 128]) for _ in range(4)]
        outb = [k.tile([128, 8, 128 * grp], nsub=2 * grp) for _ in range(2)]
        dv = dstT.rearrange("(c p) t -> p c t", p=128)
        pi = 0
        for t in range(nt):
            xb = xin[t % 2]
            k.dma(xb[:], src[t * 128:(t + 1) * 128, :], writes=[xb], sem=sx[t % 2])
            if add is not None:
                ab = ain[t % 2]
                k.dma(ab[:], add[t * 128:(t + 1) * 128, :], writes=[ab], sem=sa[t % 2])
                k.op("dve", lambda e, xb=xb, ab=ab: e.tensor_tensor(out=xb[:], in0=xb[:], in1=ab[:], op=ALU.add),
                     reads=[xb, ab], writes=[xb])
            ob = outb[(t // grp) % 2]
            tt = t % grp
            for h in range(2):
                p = pst[pi % 4]
                pi += 1
                for cc in range(4):
                    c = h * 4 + cc
                    k.op("pe", lambda e, p=p, cc=cc, c=c, xb=xb: e.transpose(
                        p[:, cc, :], xb[:, c * 128:(c + 1) * 128], self.ident[:]),
                        reads=[xb, self.ident], writes=[p], accum=True)
                dst = ob[:, h * 4:(h + 1) * 4, tt * 128:(tt + 1) * 128]
                if h == 0:
                    k.op("act", lambda e, p=p, dst=dst: e.copy(out=dst, in_=p[:]),
                         reads=[p], writes=[ob.ch(h * grp + tt)])
                else:
                    k.op("dve", lambda e, p=p, dst=dst: e.tensor_copy(out=dst, in_=p[:]),
                         reads=[p], writes=[ob.ch(h * grp + tt)])
            if tt == grp - 1:
                g = t // grp
                k.dma(dv[:, :, g * 128 * grp:(g + 1) * 128 * grp], ob[:], reads=[ob], sem=so[g % 2])
        k.end()

    def ln_pass(self, X, Tn, layer, first=False, Y=None, ymul=None, lnj=0, which=0,
                X_out=None, H_out=None, H_tm=None, X_tm=None, modj=0, do_mod=True, Y_tm=False, mod_layer=None):
        k = self.k
        if mod_layer is None:
            mod_layer = layer
        k.begin()
        TT = min(512, Tn)
        ntile = Tn // TT
        xv = X.rearrange("(c p) t -> p c t", p=128)
        xb = [k.tile([128, 8, TT], nsub=8) for _ in range(2)]
        sxs = [k.dsem() for _ in range(2)]
        if not first:
            yv = Y.rearrange("(c p) t -> p c t", p=128)
            yb = [k.tile([128, 8, TT], nsub=8) for _ in range(2)]
            sys_ = [k.dsem() for _ in range(2)]
        sq = [k.tile([128, 8, TT], nsub=8) for _ in range(2)]
        hb = [k.tile([128, 8, TT], nsub=8) for _ in range(2)] if do_mod else None
        so = [k.dsem() for _ in range(2)]
        so2 = [k.dsem() for _ in range(2)]
        st = [k.tile([128, 4, TT], nsub=3) for _ in range(2)]
        ps1 = [k.ptile([128, TT]) for _ in range(2)]
        ps2 = [k.ptile([128, TT]) for _ in range(2)]
        if X_tm is not None or H_tm is not None:
            pst = [k.ptile([128, 4, 128]) for _ in range(2)]
            tmb = [k.tile([128, D], nsub=2) for _ in range(2)]
            stm = [k.dsem() for _ in range(2)]
        self._tmi = 0

        def stats_norm(zb, sqb, sb, p1, p2, eps):
            for c in range(8):
                k.op("act", lambda e, c=c: e.activation(out=sqb[:, c, :], in_=zb[:, c, :], func=AF.Square),
                     reads=[zb.ch(c)], writes=[sqb.ch(c)])
            for c in range(8):
                k.op("pe", lambda e, c=c: e.matmul(p1[:], self.ones[:], zb[:, c, :], start=(c == 0), stop=(c == 7)),
                     reads=[self.ones, zb.ch(c)], writes=[p1], accum=True)
            for c in range(8):
                k.op("pe", lambda e, c=c: e.matmul(p2[:], self.ones[:], sqb[:, c, :], start=(c == 0), stop=(c == 7)),
                     reads=[self.ones, sqb.ch(c)], writes=[p2], accum=True)
            k.op("dve", lambda e: e.tensor_scalar(out=sb[:, 0, :], in0=p1[:], scalar1=1.0 / D, scalar2=None, op0=ALU.mult),
                 reads=[p1], writes=[sb.ch(0)])
            k.op("dve", lambda e: e.tensor_tensor(out=sb[:, 2, :], in0=sb[:, 0, :], in1=sb[:, 0, :], op=ALU.mult),
                 reads=[sb.ch(0)], writes=[sb.ch(2)])
            k.op("dve", lambda e: e.scalar_tensor_tensor(out=sb[:, 1, :], in0=p2[:], scalar=1.0 / D, in1=sb[:, 2, :],
                                                         op0=ALU.mult, op1=ALU.subtract),
                 reads=[p2, sb.ch(2)], writes=[sb.ch(1)])
            k.op("dve", lambda e: e.tensor_scalar(out=sb[:, 1, :], in0=sb[:, 1, :], scalar1=eps, scalar2=None,
                                                  op0=ALU.add),
                 reads=[sb.ch(1)], writes=[sb.ch(1)])
            k.op("act", lambda e: e.sqrt(out=sb[:, 1, :], in_=sb[:, 1, :]),
                 reads=[sb.ch(1)], writes=[sb.ch(1)])
            k.op("dve", lambda e: e.reciprocal(out=sb[:, 1, :], in_=sb[:, 1, :]),
                 reads=[sb.ch(1)], writes=[sb.ch(1)])
            for c in range(8):
                eng = "dve" if c % 2 == 0 else "pool"
                k.op(eng, lambda e, c=c: e.tensor_tensor(out=zb[:, c, :], in0=zb[:, c, :], in1=sb[:, 0, :], op=ALU.subtract),
                     reads=[zb.ch(c), sb.ch(0)], writes=[zb.ch(c)])
                k.op(eng, lambda e, c=c: e.tensor_tensor(out=zb[:, c, :], in0=zb[:, c, :], in1=sb[:, 1, :], op=ALU.mult),
                     reads=[zb.ch(c), sb.ch(1)], writes=[zb.ch(c)])

        def emit_tm(srcb, dst_tm, t0):
            for q in range(TT // 128):
                i = self._tmi
                self._tmi += 1
                tb = tmb[i % 2]
                for h in range(2):
                    p = pst[h]
                    for cc in range(4):
                        c = h * 4 + cc
                        k.op("pe", lambda e, p=p, cc=cc, c=c, q=q: e.transpose(
                            p[:, cc, :], srcb[:, c, q * 128:(q + 1) * 128], self.ident[:]),
                            reads=[srcb.ch(c), self.ident], writes=[p], accum=True)
                    if h == 0:
                        k.op("act", lambda e, p=p, tb=tb, h=h: e.copy(
                            out=tb[:, h * 512:(h + 1) * 512], in_=p[:].rearrange("p a b -> p (a b)")),
                            reads=[p], writes=[tb.ch(h)])
                    else:
                        k.op("dve", lambda e, p=p, tb=tb, h=h: e.tensor_copy(
                            out=tb[:, h * 512:(h + 1) * 512], in_=p[:].rearrange("p a b -> p (a b)")),
                            reads=[p], writes=[tb.ch(h)])
                k.dma(dst_tm[t0 + q * 128:t0 + (q + 1) * 128, :], tb[:], reads=[tb], sem=stm[i % 2])

        for t in range(ntile):
            zb = xb[t % 2]
            sl = slice(t * TT, (t + 1) * TT)
            k.dma(zb[:], xv[:, :, sl], writes=[zb], sem=sxs[t % 2])
            sqb, sb, p1, p2 = sq[t % 2], st[t % 2], ps1[t % 2], ps2[t % 2]
            if not first:
                ybb = yb[t % 2]
                if Y_tm:
                    raise NotImplementedError
                k.dma(ybb[:], yv[:, :, sl], writes=[ybb], sem=sys_[t % 2])
                for c in range(8):
                    k.op("act", lambda e, c=c: e.mul(out=zb[:, c, :], in_=zb[:, c, :], mul=float(ALPHA)),
                         reads=[zb.ch(c)], writes=[zb.ch(c)])
                    k.op("dve", lambda e, c=c: e.scalar_tensor_tensor(
                        out=zb[:, c, :], in0=ybb[:, c, :], scalar=ymul(c), in1=zb[:, c, :],
                        op0=ALU.mult, op1=ALU.add), reads=[ybb.ch(c), zb.ch(c), self.modp], writes=[zb.ch(c)])
                stats_norm(zb, sqb, sb, p1, p2, 1e-5)
                gc0, _ = PV_COLS["ln_g%d_%d" % (layer, lnj)]
                bc0, _ = PV_COLS["ln_b%d_%d" % (layer, lnj)]
                for c in range(8):
                    k.op("act", lambda e, c=c: e.activation(
                        out=zb[:, c, :], in_=zb[:, c, :], func=AF.Identity,
                        scale=self.pv[:, gc0 + c:gc0 + c + 1], bias=self.pv[:, bc0 + c:bc0 + c + 1]),
                        reads=[zb.ch(c), self.pv], writes=[zb.ch(c)])
                if X_out is not None:
                    k.dma(X_out.rearrange("(c p) t -> p c t", p=128)[:, :, sl], zb[:], reads=[zb], sem=so[t % 2])
                if X_tm is not None:
                    emit_tm(zb, X_tm, t * TT)
            if do_mod:
                h = hb[t % 2]
                for c in range(8):
                    k.op("pool", lambda e, c=c: e.tensor_copy(out=h[:, c, :], in_=zb[:, c, :]),
                         reads=[zb.ch(c)], writes=[h.ch(c)])
                stats_norm(h, sqb, sb, p1, p2, 1e-6)
                for c in range(8):
                    k.op("act", lambda e, c=c: e.activation(
                        out=h[:, c, :], in_=h[:, c, :], func=AF.Identity,
                        scale=self.mv(mod_layer, modj + 1, c, which, plus1=True),
                        bias=self.mv(mod_layer, modj, c, which)),
                        reads=[h.ch(c), self.modv, self.modp], writes=[h.ch(c)])
                if H_out is not None:
                    k.dma(H_out.rearrange("(c p) t -> p c t", p=128)[:, :, sl], h[:], reads=[h], sem=so2[t % 2])
                if H_tm is not None:
                    emit_tm(h, H_tm, t * TT)
        k.end()

    def moe(self, layer, HFM, HTM, YM, Tn):
        k, nc = self.k, self.nc
        cap = 2 * Tn // NE
        JP = min(128, cap)
        nch = cap // JP
        ntt = Tn // 128
        piece = "moe%d" % layer
        W1, W3, W2 = self.W(piece, "w1"), self.W(piece, "w3"), self.W(piece, "w2")
        idxT, gateT = self.idxT, self.gateT
        k.begin()
        rw = k.tile([128, 8, NE])
        s = k.dsem()
        rsrc = self.W("misc", "moe_router")[layer * D:(layer + 1) * D, :].rearrange("(c p) e -> p c e", p=128)
        k.dma(rw[:], rsrc, writes=[rw], sem=s)
        zt = k.tile([128, D])
        k.op("pool", lambda e: e.memset(zt[:], 0.0), writes=[zt])
        sz = k.dsem()
        for tt in range(ntt):
            k.dma(YM[tt * 128:(tt + 1) * 128, :], zt[:], reads=[zt], sem=sz)
        TT = min(512, Tn)
        hb = [k.tile([128, 8, TT]) for _ in range(2)]
        sh = [k.dsem() for _ in range(2)]
        hv = HFM.rearrange("(c p) t -> p c t", p=128)
        lg = k.ptile([128, ntt, NE])
        for t in range(Tn // TT):
            b = hb[t % 2]
            k.dma(b[:], hv[:, :, t * TT:(t + 1) * TT], writes=[b], sem=sh[t % 2])
            for q in range(TT // 128):
                tt = t * (TT // 128) + q
                for c in range(8):
                    k.op("pe", lambda e, b=b, c=c, q=q, tt=tt: e.matmul(
                        lg[:, tt, :], b[:, c, q * 128:(q + 1) * 128], rw[:, c, :], start=(c == 0), stop=(c == 7)),
                        reads=[b, rw], writes=[lg], accum=True)
        aff = k.tile([128, ntt, NE])
        mx = k.tile([128, ntt])
        k.op("dve", lambda e: e.tensor_reduce(out=mx[:], in_=lg[:], axis=AX.X, op=ALU.max), reads=[lg], writes=[mx])
        k.op("dve", lambda e: e.tensor_tensor(out=aff[:], in0=lg[:], in1=mx[:].unsqueeze(2).to_broadcast([128, ntt, NE]),
                                              op=ALU.subtract), reads=[lg, mx], writes=[aff])
        k.op("act", lambda e: e.activation(out=aff[:], in_=aff[:], func=AF.Exp), reads=[aff], writes=[aff])
        k.op("dve", lambda e: e.tensor_reduce(out=mx[:], in_=aff[:], axis=AX.X, op=ALU.add), reads=[aff], writes=[mx])
        k.op("dve", lambda e: e.reciprocal(out=mx[:], in_=mx[:]), reads=[mx], writes=[mx])
        k.op("dve", lambda e: e.tensor_tensor(out=aff[:], in0=aff[:], in1=mx[:].unsqueeze(2).to_broadcast([128, ntt, NE]),
                                              op=ALU.mult), reads=[aff, mx], writes=[aff])
        affT = k.tile([NE, Tn])
        pT = [k.ptile([NE, 4, 128]) for _ in range(2)]
        ng = (ntt + 3) // 4
        for g in range(ng):
            p = pT[g % 2]
            n = min(4, ntt - g * 4)
            for q in range(n):
                tt = g * 4 + q
                k.op("pe", lambda e, p=p, q=q, tt=tt: e.transpose(p[:, q, :], aff[:, tt, :], self.ident[:]),
                     reads=[aff, self.ident], writes=[p], accum=True)
            k.op("act", lambda e, p=p, g=g, n=n: e.copy(
                out=affT[:, g * 512:g * 512 + n * 128], in_=p[:, 0:n, :].rearrange("p a b -> p (a b)")),
                reads=[p], writes=[affT], accum=True)
        work = k.tile([NE, Tn])
        vals = k.tile([NE, cap])
        idxu = k.tile([NE, cap], U32)
        k.op("dve", lambda e: e.tensor_copy(out=work[:], in_=affT[:]), reads=[affT], writes=[work])
        for r in range(cap // 8):
            sl = slice(r * 8, (r + 1) * 8)
            k.op("dve", lambda e, sl=sl: e.max(out=vals[:, sl], in_=work[:]), reads=[work], writes=[vals], accum=True)
            k.op("dve", lambda e, sl=sl: e.max_index(out=idxu[:, sl], in_max=vals[:, sl], in_values=work[:]),
                 reads=[work, vals], writes=[idxu], accum=True)
            k.op("dve", lambda e, sl=sl: e.match_replace(out=work[:], in_to_replace=vals[:, sl], in_values=work[:],
                                                         imm_value=-1.0), reads=[vals, work], writes=[work])
        idxf = k.tile([NE, cap])
        k.op("dve", lambda e: e.tensor_copy(out=idxf[:], in_=idxu[:]), reads=[idxu], writes=[idxf])
        pI = k.ptile([128, nch, NE])
        pG = k.ptile([128, nch, NE])
        for ch in range(nch):
            k.op("pe", lambda e, ch=ch: e.transpose(pI[:JP, ch, :], idxf[:, ch * JP:(ch + 1) * JP], self.ident[:NE, :NE]),
                 reads=[idxf, self.ident], writes=[pI], accum=True)
            k.op("pe", lambda e, ch=ch: e.transpose(pG[:JP, ch, :], vals[:, ch * JP:(ch + 1) * JP], self.ident[:NE, :NE]),
                 reads=[vals, self.ident], writes=[pG], accum=True)
        k.op("dve", lambda e: e.tensor_copy(out=idxT[:JP, :nch, :], in_=pI[:JP]), reads=[pI], writes=[idxT])
        k.op("dve", lambda e: e.tensor_copy(out=gateT[:JP, :nch, :], in_=pG[:JP]), reads=[pG], writes=[gateT])
        k.end()
        k.begin()
        Xe = k.tile([128, nch, D], nsub=nch)
        XeT = k.tile([128, 8, cap], nsub=8)
        heT = k.tile([128, 16, cap], nsub=16)
        Ye = k.tile([128, nch, D], nsub=nch)
        w1t = [k.tile([128, 8, 256]) for _ in range(2)]
        w3t = [k.tile([128, 8, 256]) for _ in range(2)]
        w2t = [k.tile([128, 16, 256]) for _ in range(2)]
        tmp = [k.tile([128, cap]) for _ in range(2)]
        s1 = [k.dsem() for _ in range(2)]
        s3 = [k.dsem() for _ in range(2)]
        s2 = [k.dsem() for _ in range(2)]
        sg = k.dsem()
        ss = k.dsem()
        ph1 = [k.ptile([128, cap]) for _ in range(2)]
        ph3 = [k.ptile([128, cap]) for _ in range(2)]
        py = [k.ptile([128, 512]) for _ in range(2)]
        pt = [k.ptile([128, 4, 128]) for _ in range(2)]
        i1 = i2 = ip = iy = 0
        for ex in range(NE):
            for ch in range(nch):
                k.dma(None, None, reads=[idxT], writes=[Xe.ch(ch)], sem=sg, q="pool",
                      fn=lambda e, ch=ch, ex=ex: e.indirect_dma_start(
                          out=Xe[:JP, ch, :], out_offset=None, in_=HTM[:, :],
                          in_offset=bass.IndirectOffsetOnAxis(ap=idxT[:JP, ch, ex:ex + 1], axis=0)))
            for dc in range(8):
                for ch in range(nch):
                    if ch % 4 == 0:
                        p = pt[ip % 2]
                        ip += 1
                    k.op("pe", lambda e, p=p, ch=ch, dc=dc: e.transpose(
                        p[:, ch % 4, :JP], Xe[:JP, ch, dc * 128:(dc + 1) * 128], self.ident[:JP, :JP]),
                        reads=[Xe.ch(ch), self.ident], writes=[p], accum=True)
                    if ch % 4 == 3 or ch == nch - 1:
                        c0 = (ch // 4) * 4
                        n = ch - c0 + 1
                        eng = "act" if dc % 2 == 0 else "dve"
                        if eng == "act":
                            k.op("act", lambda e, p=p, dc=dc, c0=c0, n=n: e.copy(
                                out=XeT[:, dc, c0 * JP:(c0 + n) * JP].rearrange("p (a b) -> p a b", a=n),
                                in_=p[:, 0:n, :JP]), reads=[p], writes=[XeT.ch(dc)])
                        else:
                            k.op("dve", lambda e, p=p, dc=dc, c0=c0, n=n: e.tensor_copy(
                                out=XeT[:, dc, c0 * JP:(c0 + n) * JP].rearrange("p (a b) -> p a b", a=n),
                                in_=p[:, 0:n, :JP]), reads=[p], writes=[XeT.ch(dc)])
            for g in range(8):
                a1, a3 = w1t[i1 % 2], w3t[i1 % 2]
                r0 = ex * D
                k.dma(a1[:], W1[r0:r0 + D, g * 256:(g + 1) * 256].rearrange("(c p) n -> p c n", p=128),
                      writes=[a1], sem=s1[i1 % 2], q="sp")
                k.dma(a3[:], W3[r0:r0 + D, g * 256:(g + 1) * 256].rearrange("(c p) n -> p c n", p=128),
                      writes=[a3], sem=s3[i1 % 2], q="act")
                i1 += 1
                for fl in range(2):
                    fc = g * 2 + fl
                    p1, p3 = ph1[fc % 2], ph3[fc % 2]
                    for dc in range(8):
                        k.op("pe", lambda e, p1=p1, a1=a1, dc=dc, fl=fl: e.matmul(
                            p1[:], a1[:, dc, fl * 128:(fl + 1) * 128], XeT[:, dc, :], start=(dc == 0), stop=(dc == 7)),
                            reads=[a1, XeT.ch(dc)], writes=[p1], accum=True)
                    for dc in range(8):
                        k.op("pe", lambda e, p3=p3, a3=a3, dc=dc, fl=fl: e.matmul(
                            p3[:], a3[:, dc, fl * 128:(fl + 1) * 128], XeT[:, dc, :], start=(dc == 0), stop=(dc == 7)),
                            reads=[a3, XeT.ch(dc)], writes=[p3], accum=True)
                    tb = tmp[fc % 2]
                    k.op("act", lambda e, tb=tb, p1=p1: e.activation(out=tb[:], in_=p1[:], func=AF.Silu),
                         reads=[p1], writes=[tb])
                    k.op("dve", lambda e, tb=tb, p3=p3, fc=fc: e.tensor_tensor(
                        out=heT[:, fc, :], in0=tb[:], in1=p3[:], op=ALU.mult),
                        reads=[tb, p3], writes=[heT.ch(fc)])
            for q in range(4):
                a2 = w2t[i2 % 2]
                r0 = ex * FF
                k.dma(a2[:], W2[r0:r0 + FF, q * 256:(q + 1) * 256].rearrange("(c p) n -> p c n", p=128),
                      writes=[a2], sem=s2[i2 % 2], q=("sp", "act")[i2 % 2])
                i2 += 1
                for ch in range(nch):
                    p = py[iy % 2]
                    iy += 1
                    for fc in range(16):
                        k.op("pe", lambda e, p=p, a2=a2, fc=fc, ch=ch: e.matmul(
                            p[:JP, 0:256], heT[:, fc, ch * JP:(ch + 1) * JP], a2[:, fc, :],
                            start=(fc == 0), stop=(fc == 15)),
                            reads=[a2, heT.ch(fc)], writes=[p], accum=True)
                    k.op("act", lambda e, p=p, ch=ch, q=q, ex=ex: e.activation(
                        out=Ye[:JP, ch, q * 256:(q + 1) * 256], in_=p[:JP, 0:256], func=AF.Identity,
                        scale=gateT[:JP, ch, ex:ex + 1]),
                        reads=[p, gateT], writes=[Ye.ch(ch)], accum=True)
            for ch in range(nch):
                k._wait("pool", [(ss, k.cnt[ss])])
                k.dma(None, None, reads=[idxT, Ye.ch(ch)], sem=ss, q="pool",
                      fn=lambda e, ch=ch, ex=ex: e.indirect_dma_start(
                          out=YM[:, :], out_offset=bass.IndirectOffsetOnAxis(ap=idxT[:JP, ch, ex:ex + 1], axis=0),
                          in_=Ye[:JP, ch, :], in_offset=None, compute_op=ALU.add))
        k.end()

    def gemm(self, XT, Wap, K, N, Tn, out, bias_pv=None, act=None, out_tm=False, bias_row=None):
        k = self.k
        k.begin()
        KC = K // 128
        wt = k.tile([128, KC, N])
        sw = k.dsem()
        for kc in range(KC):
            k.dma(wt[:, kc, :], Wap[kc * 128:(kc + 1) * 128, :], writes=[wt], sem=sw)
        TT = 512 if Tn % 512 == 0 else 256
        xv = XT.rearrange("(c p) t -> p c t", p=128)
        xb = [k.tile([128, KC, TT]) for _ in range(2)]
        sx = [k.dsem() for _ in range(2)]
        so = [k.dsem() for _ in range(2)]
        ps = [k.ptile([128, 512]) for _ in range(4)]
        func = act if act is not None else AF.Identity
        ip = io = 0
        if not out_tm:
            ov = out.rearrange("(c p) t -> p c t", p=128)
            ob = [k.tile([128, 4, TT], nsub=4) for _ in range(2)]
            for t in range(Tn // TT):
                b = xb[t % 2]
                k.dma(b[:], xv[:, :, t * TT:(t + 1) * TT], writes=[b], sem=sx[t % 2])
                for n in range(N // 128):
                    p = ps[ip % 4]
                    ip += 1
                    for kc in range(KC):
                        k.op("pe", lambda e, p=p, kc=kc, n=n, b=b: e.matmul(
                            p[:, :TT], wt[:, kc, n * 128:(n + 1) * 128], b[:, kc, :], start=(kc == 0), stop=(kc == KC - 1)),
                            reads=[wt, b], writes=[p], accum=True)
                    o = ob[io % 2]
                    if bias_pv is not None:
                        c0, _ = PV_COLS[bias_pv]
                        k.op("act", lambda e, p=p, o=o, n=n, c0=c0: e.activation(
                            out=o[:, n % 4, :], in_=p[:, :TT], func=func, bias=self.pv[:, c0 + n:c0 + n + 1]),
                            reads=[p, self.pv], writes=[o.ch(n % 4)])
                    else:
                        k.op("act", lambda e, p=p, o=o, n=n: e.activation(out=o[:, n % 4, :], in_=p[:, :TT], func=func),
                             reads=[p], writes=[o.ch(n % 4)])
                    if n % 4 == 3 or n == N // 128 - 1:
                        n0 = (n // 4) * 4
                        k.dma(ov[:, n0:n + 1, t * TT:(t + 1) * TT], o[:, 0:n - n0 + 1, :], reads=[o], sem=so[io % 2])
                        io += 1
        else:
            brow = None
            if bias_row is not None:
                brow = k.tile([128, N])
                sb_ = k.dsem()
                k.dma(brow[:], bias_row.partition_broadcast(128), writes=[brow], sem=sb_)
            ob = [k.tile([128, 512]) for _ in range(2)]
            for t in range(Tn // TT):
                b = xb[t % 2]
                k.dma(b[:], xv[:, :, t * TT:(t + 1) * TT], writes=[b], sem=sx[t % 2])
                for q in range(TT // 128):
                    for n in range(N // 512):
                        p = ps[ip % 4]
                        ip += 1
                        for kc in range(KC):
                            k.op("pe", lambda e, p=p, kc=kc, n=n, b=b, q=q: e.matmul(
                                p[:], b[:, kc, q * 128:(q + 1) * 128], wt[:, kc, n * 512:(n + 1) * 512],
                                start=(kc == 0), stop=(kc == KC - 1)), reads=[wt, b], writes=[p], accum=True)
                        o = ob[io % 2]
                        if brow is not None:
                            k.op("dve", lambda e, p=p, o=o, n=n: e.tensor_tensor(
                                out=o[:], in0=p[:], in1=brow[:, n * 512:(n + 1) * 512], op=ALU.add),
                                reads=[p, brow], writes=[o])
                            if act is not None:
                                k.op("act", lambda e, o=o: e.activation(out=o[:], in_=o[:], func=act), reads=[o], writes=[o])
                        else:
                            k.op("act", lambda e, p=p, o=o: e.activation(out=o[:], in_=p[:], func=func), reads=[p], writes=[o])
                        r0 = t * TT + q * 128
                        k.dma(out[r0:r0 + 128, n * 512:(n + 1) * 512], o[:], reads=[o], sem=so[io % 2])
                        io += 1
        k.end()

    def fnet(self, HL, Y):
        k = self.k
        PQ = self.dt("fn_PQ", [2, T, D])
        MX = self.dt("fn_MX", [D, T])
        k.begin()
        cs = k.tile([128, 256])
        s0 = k.dsem()
        k.dma(cs[:], self.W("fnet", "fn_cs"), writes=[cs], sem=s0)
        hv = HL.rearrange("(c p) t -> p c t", p=128)
        hb = [k.tile([128, 8, 512]) for _ in range(2)]
        sh = [k.dsem() for _ in range(2)]
        ps = [k.ptile([128, 2, 256]) for _ in range(4)]
        pb = [k.tile([128, D], nsub=4) for _ in range(2)]
        qb = [k.tile([128, D], nsub=4) for _ in range(2)]
        so = [k.dsem() for _ in range(2)]
        ip = io = 0
        for t in range(T // 512):
            b = hb[t % 2]
            k.dma(b[:], hv[:, :, t * 512:(t + 1) * 512], writes=[b], sem=sh[t % 2])
            for q in range(4):
                po, qo = pb[io % 2], qb[io % 2]
                for g2 in range(4):
                    p = ps[ip % 4]
                    ip += 1
                    for gl in range(2):
                        g = g2 * 2 + gl
                        k.op("pe", lambda e, p=p, gl=gl, g=g, b=b, q=q: e.matmul(
                            p[:, gl, :], b[:, g, q * 128:(q + 1) * 128], cs[:], start=True, stop=True),
                            reads=[b, cs], writes=[p], accum=True)
                    if g2 % 2 == 0:
                        k.op("act", lambda e, p=p, po=po, g2=g2: e.copy(
                            out=po[:, g2 * 256:(g2 + 1) * 256].rearrange("p (a b) -> p a b", a=2), in_=p[:, :, 0:128]),
                            reads=[p], writes=[po.ch(g2)])
                        k.op("act", lambda e, p=p, qo=qo, g2=g2: e.copy(
                            out=qo[:, g2 * 256:(g2 + 1) * 256].rearrange("p (a b) -> p a b", a=2), in_=p[:, :, 128:256]),
                            reads=[p], writes=[qo.ch(g2)])
                    else:
                        k.op("dve", lambda e, p=p, po=po, g2=g2: e.tensor_copy(
                            out=po[:, g2 * 256:(g2 + 1) * 256].rearrange("p (a b) -> p a b", a=2), in_=p[:, :, 0:128]),
                            reads=[p], writes=[po.ch(g2)])
                        k.op("dve", lambda e, p=p, qo=qo, g2=g2: e.tensor_copy(
                            out=qo[:, g2 * 256:(g2 + 1) * 256].rearrange("p (a b) -> p a b", a=2), in_=p[:, :, 128:256]),
                            reads=[p], writes=[qo.ch(g2)])
                r0 = t * 512 + q * 128
                k.dma(PQ[0, r0:r0 + 128, :], po[:], reads=[po], sem=so[io % 2])
                k.dma(PQ[1, r0:r0 + 128, :], qo[:], reads=[qo], sem=so[io % 2])
                io += 1
        k.end()
        if self.test == "fnet" and self.stop_at == 1:
            return
        k.begin()
        CT, ST = self.W("fnet", "fn_nct"), self.W("fnet", "fn_nst")
        ps = [k.ptile([128, 512]) for _ in range(8)]
        pt = [k.tile([128, 512]) for _ in range(3)]
        qt = [k.tile([128, 512]) for _ in range(3)]
        ct = [k.tile([128, 512]) for _ in range(3)]
        st_ = [k.tile([128, 512]) for _ in range(3)]
        sp_ = [k.dsem() for _ in range(3)]
        sq_ = [k.dsem() for _ in range(3)]
        sc_ = [k.dsem() for _ in range(3)]
        ss_ = [k.dsem() for _ in range(3)]
        ob = [k.tile([128, 4, 512], nsub=4) for _ in range(2)]
        so = [k.dsem() for _ in range(2)]
        mv_ = MX.rearrange("(c p) t -> p c t", p=128)
        it = 0
        io = 0
        for kb in range(T // 512):
            for half in range(2):
                pp = [ps[(io % 2) * 4 + j] for j in range(4)]
                for tt in range(T // 128):
                    i = it % 3
                    it += 1
                    k.dma(pt[i][:], PQ[0, tt * 128:(tt + 1) * 128, half * 512:(half + 1) * 512], writes=[pt[i]], sem=sp_[i], q="sp")
                    k.dma(qt[i][:], PQ[1, tt * 128:(tt + 1) * 128, half * 512:(half + 1) * 512], writes=[qt[i]], sem=sq_[i], q="act")
                    k.dma(ct[i][:], CT[tt * 128:(tt + 1) * 128, kb * 512:(kb + 1) * 512], writes=[ct[i]], sem=sc_[i], q="sp")
                    k.dma(st_[i][:], ST[tt * 128:(tt + 1) * 128, kb * 512:(kb + 1) * 512], writes=[st_[i]], sem=ss_[i], q="act")
                    for j in range(4):
                        k.op("pe", lambda e, j=j, i=i, tt=tt: e.matmul(
                            pp[j][:], pt[i][:, j * 128:(j + 1) * 128], ct[i][:], start=(tt == 0), stop=False),
                            reads=[pt[i], ct[i]], writes=[pp[j]], accum=True)
                        k.op("pe", lambda e, j=j, i=i, tt=tt: e.matmul(
                            pp[j][:], qt[i][:, j * 128:(j + 1) * 128], st_[i][:], start=False, stop=(tt == T // 128 - 1)),
                            reads=[qt[i], st_[i]], writes=[pp[j]], accum=True)
                o = ob[io % 2]
                for j in range(4):
                    if j % 2 == 0:
                        k.op("act", lambda e, j=j, o=o: e.copy(out=o[:, j, :], in_=pp[j][:]), reads=[pp[j]], writes=[o.ch(j)])
                    else:
                        k.op("dve", lambda e, j=j, o=o: e.tensor_copy(out=o[:, j, :], in_=pp[j][:]), reads=[pp[j]], writes=[o.ch(j)])
                k.dma(mv_[:, half * 4:(half + 1) * 4, kb * 512:(kb + 1) * 512], o[:], reads=[o], sem=so[io % 2])
                io += 1
        k.end()
        if self.test == "fnet" and self.stop_at == 2:
            return
        self.gemm(MX, self.W("fnet", "fn_wo"), D, D, T, Y, bias_pv="fn_bo")

    def hy_tabs(self, L, names):
        k = self.k
        NH = L // 64
        N1 = 2 * NH
        shp = {"F1": (NH, 2 * N1), "c": (64, N1), "s": (64, N1), "cT": (N1, 64), "sT": (N1, 64),
               "C2": (64, 64), "S2": (64, 64), "nS2": (64, 64), "RA": (64, 128), "RB": (64, 128),
               "C1": (N1, NH), "nS1": (N1, NH)}
        out = {}
        sm = k.dsem()
        for nm in names:
            r, c = shp[nm]
            t = k.tile([max(r, 1), c])
            key = nm if nm in ("C2", "S2", "nS2", "RA", "RB") else "%s_%d" % (nm, L)
            k.dma(t[:], self.W("hy", key), writes=[t], sem=sm)
            out[nm] = t
        return out

    def hy_filters(self, j, L, HF, RN):
        k = self.k
        N = 2 * L
        k.begin()
        s0 = k.dsem()
        zT = k.tile([33, L])
        w1 = k.tile([33, 64])
        w2 = k.tile([64, 2, 64])
        w3 = k.tile([64, 4096])
        hv = k.tile([64, 4])
        k.dma(zT[:], self.W("hy", "zT_%d" % L), writes=[zT], sem=s0)
        k.dma(w1[:], self.W("hy", "f_w1")[j * 33:(j + 1) * 33, :], writes=[w1], sem=s0)
        for n in range(2):
            k.dma(w2[:, n, :], self.W("hy", "f_w2")[(j * 2 + n) * 64:(j * 2 + n + 1) * 64, :], writes=[w2], sem=s0)
        k.dma(w3[:], self.W("hy", "f_w3")[j * 64:(j + 1) * 64, :], writes=[w3], sem=s0)
        k.dma(hv[:], self.W("hy", "hyv")[j * 64:(j + 1) * 64, :], writes=[hv], sem=s0)
        a = [k.tile([64, L]) for _ in range(3)]
        ps = [k.ptile([128, 512]) for _ in range(2)]
        tmp = [k.tile([64, 512]) for _ in range(2)]
        tmpi = [k.tile([64, 512], I32) for _ in range(2)]
        tmpf = [k.tile([64, 512]) for _ in range(2)]
        CT = min(512, L)
        it = 0
        for layer in range(3):
            for ct in range(L // CT):
                p = ps[it % 2]
                tb = tmp[it % 2]
                it += 1
                sl = slice(ct * CT, (ct + 1) * CT)
                if layer == 0:
                    k.op("pe", lambda e, p=p, sl=sl: e.matmul(p[:64, :CT], w1[:, :], zT[:, sl], start=True, stop=True),
                         reads=[w1, zT], writes=[p])
                else:
                    src = a[layer - 1]
                    k.op("pe", lambda e, p=p, sl=sl, src=src, layer=layer: e.matmul(
                        p[:64, :CT], w2[:, layer - 1, :], src[:, sl], start=True, stop=True),
                        reads=[w2, src], writes=[p])
                bcol = hv[:, 0:1] if layer == 0 else hv[:, 1 + layer:2 + layer]
                k.op("dve", lambda e, p=p, tb=tb, bcol=bcol: e.tensor_scalar(
                    out=tb[:, :CT], in0=p[:64, :CT], scalar1=bcol, scalar2=hv[:, 1:2], op0=ALU.add, op1=ALU.mult),
                    reads=[p, hv], writes=[tb])
                k.op("dve", lambda e, tb=tb: e.tensor_scalar(
                    out=tb[:, :CT], in0=tb[:, :CT], scalar1=1.0 / (2 * math.pi), scalar2=16.0, op0=ALU.mult, op1=ALU.add),
                    reads=[tb], writes=[tb])
                ti, tf = tmpi[it % 2], tmpf[it % 2]
                k.op("dve", lambda e, tb=tb, ti=ti: e.tensor_copy(out=ti[:, :CT], in_=tb[:, :CT]), reads=[tb], writes=[ti])
                k.op("dve", lambda e, tf=tf, ti=ti: e.tensor_copy(out=tf[:, :CT], in_=ti[:, :CT]), reads=[ti], writes=[tf])
                k.op("dve", lambda e, tb=tb, tf=tf: e.tensor_tensor(out=tb[:, :CT], in0=tb[:, :CT], in1=tf[:, :CT], op=ALU.subtract),
                     reads=[tb, tf], writes=[tb])
                k.op("dve", lambda e, tb=tb, tf=tf: e.scalar_tensor_tensor(
                    out=tf[:, :CT], in0=tb[:, :CT], scalar=0.5, in1=tb[:, :CT], op0=ALU.is_gt, op1=ALU.subtract),
                    reads=[tb], writes=[tf])
                dst = a[layer]
                k.op("act", lambda e, tf=tf, dst=dst, sl=sl: e.activation(
                    out=dst[:, sl], in_=tf[:, :CT], func=AF.Sin, scale=-2 * math.pi),
                    reads=[tf], writes=[dst], accum=True)
        a3 = a[2]
        win = [k.tile([128, 512]) for _ in range(2)]
        sw = [k.dsem() for _ in range(2)]
        hs = [k.tile([128, 512]) for _ in range(2)]
        ha = [k.tile([128, 512]) for _ in range(2)]
        so = [k.dsem() for _ in range(2)]
        pn = k.ptile([128, 512])
        nrm = k.tile([128, 8, 512], nsub=8)
        WIN = self.W("hy", "win_%d" % L)
        LT = min(128, L)
        nlt = L // LT
        it = 0
        for ct in range(8):
            dcol = (ct % 2) * 512
            for lt in range(nlt):
                i = it % 2
                it += 1
                p = ps[i]
                k.dma(win[i][:LT, :], WIN[lt * LT:(lt + 1) * LT, dcol:dcol + 512], writes=[win[i]], sem=sw[i])
                k.op("pe", lambda e, p=p, lt=lt, ct=ct: e.matmul(
                    p[:LT, :], a3[:, lt * LT:(lt + 1) * LT], w3[:, ct * 512:(ct + 1) * 512], start=True, stop=True),
                    reads=[a3, w3], writes=[p])
                k.op("dve", lambda e, p=p, i=i: e.tensor_tensor(out=hs[i][:LT, :], in0=p[:LT, :], in1=win[i][:LT, :], op=ALU.mult),
                     reads=[p, win[i]], writes=[hs[i]])
                if ct >= 4 and lt == 0:
                    k.op("dve", lambda e, i=i: e.memset(hs[i][0:1, :], 0.0), reads=[hs[i]], writes=[hs[i]])
                k.dma(HF[lt * LT:(lt + 1) * LT, ct * 512:(ct + 1) * 512], hs[i][:LT, :], reads=[hs[i]], sem=so[i])
                k.op("act", lambda e, i=i: e.activation(out=ha[i][:LT, :], in_=hs[i][:LT, :], func=AF.Abs),
                     reads=[hs[i]], writes=[ha[i]])
                k.op("pe", lambda e, i=i, lt=lt: e.matmul(pn[:, :], self.ones[:LT, :], ha[i][:LT, :],
                                                          start=(lt == 0), stop=(lt == nlt - 1)),
                     reads=[self.ones, ha[i]], writes=[pn], accum=True)
            k.op("act", lambda e, ct=ct: e.copy(out=nrm[:, ct, :], in_=pn[:, :]), reads=[pn], writes=[nrm.ch(ct)])
        rn = k.tile([128, 8, 512], nsub=8)
        for q in range(4):
            k.op("dve", lambda e, q=q: e.tensor_tensor(out=rn[:, q, :], in0=nrm[:, q, :], in1=nrm[:, q + 4, :], op=ALU.add),
                 reads=[nrm.ch(q), nrm.ch(q + 4)], writes=[rn.ch(q)])
            k.op("dve", lambda e, q=q: e.tensor_scalar(out=rn[:, q, :], in0=rn[:, q, :], scalar1=float(N), scalar2=None, op0=ALU.mult),
                 reads=[rn.ch(q)], writes=[rn.ch(q)])
            k.op("dve", lambda e, q=q: e.reciprocal(out=rn[:, q, :], in_=rn[:, q, :]), reads=[rn.ch(q)], writes=[rn.ch(q)])
            k.op("pool", lambda e, q=q: e.tensor_copy(out=rn[:, q + 4, :], in_=rn[:, q, :]), reads=[rn.ch(q)], writes=[rn.ch(q + 4)])
        sr = k.dsem()
        k.dma(RN[0:1, :], rn[0:1, :, :].rearrange("p a b -> p (a b)"), reads=[rn], sem=sr)
        k.end()

    def _fft_fwd(self, L, tb, zs, g0, G, A, Bt, X):
        k = self.k
        NH = L // 64
        N1 = 2 * NH
        for g in range(G):
            k.op("pe", lambda e, g=g: e.matmul(A[:64, g, 0:2 * N1], zs[:NH, :, g0 + g], tb["F1"][:NH, :],
                                               start=True, stop=True),
                 reads=[zs, tb["F1"]], writes=[A], accum=True)
        Ar, Ai = A[:64, :G, 0:N1], A[:64, :G, N1:2 * N1]
        cb = tb["c"][:, :].unsqueeze(1).to_broadcast([64, G, N1])
        sb = tb["s"][:, :].unsqueeze(1).to_broadcast([64, G, N1])
        t1, t2, t3, t4, Br, Bi = Bt
        v = lambda t: t[:64, :G * N1].rearrange("p (g n) -> p g n", g=G)
        k.op("dve", lambda e: e.tensor_tensor(out=v(t1), in0=Ar, in1=cb, op=ALU.mult), reads=[A, tb["c"]], writes=[t1])
        k.op("dve", lambda e: e.tensor_tensor(out=v(t2), in0=Ai, in1=sb, op=ALU.mult), reads=[A, tb["s"]], writes=[t2])
        k.op("dve", lambda e: e.tensor_tensor(out=v(t3), in0=Ai, in1=cb, op=ALU.mult), reads=[A, tb["c"]], writes=[t3])
        k.op("dve", lambda e: e.tensor_tensor(out=v(t4), in0=Ar, in1=sb, op=ALU.mult), reads=[A, tb["s"]], writes=[t4])
        k.op("pool", lambda e: e.tensor_tensor(out=Br[:64, :G * N1], in0=t1[:64, :G * N1], in1=t2[:64, :G * N1], op=ALU.add),
             reads=[t1, t2], writes=[Br])
        k.op("pool", lambda e: e.tensor_tensor(out=Bi[:64, :G * N1], in0=t3[:64, :G * N1], in1=t4[:64, :G * N1], op=ALU.subtract),
             reads=[t3, t4], writes=[Bi])
        W_ = G * N1
        k.op("pe", lambda e: e.matmul(X["r"][:64, :W_], tb["C2"][:, :], Br[:64, :W_], start=True, stop=False),
             reads=[tb["C2"], Br], writes=[X["r"]], accum=True)
        k.op("pe", lambda e: e.matmul(X["r"][:64, :W_], tb["S2"][:, :], Bi[:64, :W_], start=False, stop=True),
             reads=[tb["S2"], Bi], writes=[X["r"]], accum=True)
        k.op("pe", lambda e: e.matmul(X["i"][:64, :W_], tb["C2"][:, :], Bi[:64, :W_], start=True, stop=False),
             reads=[tb["C2"], Bi], writes=[X["i"]], accum=True)
        k.op("pe", lambda e: e.matmul(X["i"][:64, :W_], tb["nS2"][:, :], Br[:64, :W_], start=False, stop=True),
             reads=[tb["nS2"], Br], writes=[X["i"]], accum=True)

    def hy_filter_fft(self, L, HF, RN, SPEC):
        k = self.k
        NH = L // 64
        N1 = 2 * NH
        G = 4
        k.begin()
        tb = self.hy_tabs(L, ["F1", "c", "s", "C2", "S2", "nS2"])
        rnb = k.tile([64, 4096])
        s0 = k.dsem()
        k.dma(rnb[:], RN[0:1, :].partition_broadcast(64), writes=[rnb], sem=s0)
        zs = [k.tile([64, 64, 128]) for _ in range(2)]
        sz = [k.dsem() for _ in range(2)]
        A = k.ptile([128, G, 256])
        X = {"r": k.ptile([128, 512]), "i": k.ptile([128, 512])}
        Bt = [k.tile([64, 512]) for _ in range(6)]
        xo = [[k.tile([64, 512]) for _ in range(2)] for _ in range(2)]
        so = [k.dsem() for _ in range(2)]
        hv = HF.rearrange("(a b) c -> a b c", b=64)
        io = 0
        for cb in range(32):
            z = zs[cb % 2]
            k.dma(z[:NH, :, :], hv[:, :, cb * 128:(cb + 1) * 128], writes=[z], sem=sz[cb % 2])
            for sb in range(128 // G):
                g0 = sb * G
                ch0 = cb * 128 + g0
                self._fft_fwd(L, tb, z, g0, G, A, Bt, X)
                rv = rnb[:, ch0:ch0 + G].unsqueeze(2).to_broadcast([64, G, N1])
                for ri, nm in enumerate(("r", "i")):
                    o = xo[io % 2][ri]
                    k.op("dve", lambda e, o=o, nm=nm, rv=rv: e.tensor_tensor(
                        out=o[:, :G * N1].rearrange("p (g n) -> p g n", g=G),
                        in0=X[nm][:64, :G * N1].rearrange("p (g n) -> p g n", g=G), in1=rv, op=ALU.mult),
                        reads=[X[nm], rnb], writes=[o])
                    k.dma(SPEC[ri, :, ch0:ch0 + G, :], o[:, :G * N1].rearrange("p (g n) -> p g n", g=G),
                          reads=[o], sem=so[io % 2])
                io += 1
        k.end()

    def hy_conv(self, L, Z, zc0, XG, xc0, SPEC, o, bias_ap, OUT):
        k = self.k
        NH = L // 64
        N1 = 2 * NH
        G = 4
        k.begin()
        tb = self.hy_tabs(L, ["F1", "c", "s", "C2", "S2", "nS2", "RA", "RB", "cT", "sT", "C1", "nS1"])
        brow = k.tile([64, 1024])
        s0 = k.dsem()
        k.dma(brow[:], bias_ap.partition_broadcast(64), writes=[brow], sem=s0)
        zs = [k.tile([64, 64, 128]) for _ in range(2)]
        xs = [k.tile([64, 64, 128]) for _ in range(2)]
        sz = [k.dsem() for _ in range(2)]
        sx = [k.dsem() for _ in range(2)]
        so = [k.dsem() for _ in range(2)]
        A = k.ptile([128, G, 256])
        X = {"r": k.ptile([128, 512]), "i": k.ptile([128, 512])}
        Zp = k.ptile([128, G, 128])
        yp = k.ptile([128, 512])
        Bt = [k.tile([64, 512]) for _ in range(6)]
        Kt = [[k.tile([64, 512]) for _ in range(4)] for _ in range(2)]
        sk = [k.dsem() for _ in range(2)]
        Kc = [k.tile([64, 512]) for _ in range(2)]
        Yt = [k.tile([64, 512]) for _ in range(6)]
        Zt = [k.tile([128, G * 64]) for _ in range(6)]
        et = [k.tile([64, 64, G]) for _ in range(2)]
        zv = Z.rearrange("(a b) c -> a b c", b=64)
        xv = XG.rearrange("(a b) c -> a b c", b=64)
        ov = OUT.rearrange("(a b) c -> a b c", b=64)
        W_ = G * N1
        ik = 0
        for cb in range(8):
            z, x = zs[cb % 2], xs[cb % 2]
            k.dma(z[:NH, :, :], zv[:, :, zc0 + cb * 128:zc0 + (cb + 1) * 128], writes=[z], sem=sz[cb % 2])
            k.dma(x[:NH, :, :], xv[:, :, xc0 + cb * 128:xc0 + (cb + 1) * 128], writes=[x], sem=sx[cb % 2])
            for sb in range(128 // G):
                g0 = sb * G
                d0 = cb * 128 + g0
                kt = Kt[ik % 2]
                for q, (ri, dr) in enumerate(((0, 0), (1, 0), (0, 1), (1, 1))):
                    ch0 = dr * 2048 + o * 1024 + d0
                    k.dma(kt[q][:, :W_].rearrange("p (g n) -> p g n", g=G), SPEC[ri, :, ch0:ch0 + G, :],
                          writes=[kt[q]], sem=sk[ik % 2])
                ik += 1
                self._fft_fwd(L, tb, z, g0, G, A, Bt, X)
                Kr, Ki = Kc
                k.op("pool", lambda e, kt=kt: e.tensor_tensor(out=Kr[:, :W_], in0=kt[0][:, :W_], in1=kt[2][:, :W_], op=ALU.add),
                     reads=[kt[0], kt[2]], writes=[Kr])
                k.op("pool", lambda e, kt=kt: e.tensor_tensor(out=Ki[:, :W_], in0=kt[1][:, :W_], in1=kt[3][:, :W_], op=ALU.subtract),
                     reads=[kt[1], kt[3]], writes=[Ki])
                y1, y2, y3, y4, Yr, Yi = Yt
                k.op("dve", lambda e: e.tensor_tensor(out=y1[:, :W_], in0=X["r"][:64, :W_], in1=Kr[:, :W_], op=ALU.mult), reads=[X["r"], Kr], writes=[y1])
                k.op("dve", lambda e: e.tensor_tensor(out=y2[:, :W_], in0=X["i"][:64, :W_], in1=Ki[:, :W_], op=ALU.mult), reads=[X["i"], Ki], writes=[y2])
                k.op("dve", lambda e: e.tensor_tensor(out=y3[:, :W_], in0=X["r"][:64, :W_], in1=Ki[:, :W_], op=ALU.mult), reads=[X["r"], Ki], writes=[y3])
                k.op("dve", lambda e: e.tensor_tensor(out=y4[:, :W_], in0=X["i"][:64, :W_], in1=Kr[:, :W_], op=ALU.mult), reads=[X["i"], Kr], writes=[y4])
                k.op("pool", lambda e: e.tensor_tensor(out=Yr[:, :W_], in0=y1[:, :W_], in1=y2[:, :W_], op=ALU.subtract), reads=[y1, y2], writes=[Yr])
                k.op("pool", lambda e: e.tensor_tensor(out=Yi[:, :W_], in0=y3[:, :W_], in1=y4[:, :W_], op=ALU.add), reads=[y3, y4], writes=[Yi])
                for g in range(G):
                    k.op("pe", lambda e, g=g: e.matmul(Zp[:N1, g, :], Yr[:, g * N1:(g + 1) * N1], tb["RA"][:, :], start=True, stop=False),
                         reads=[Yr, tb["RA"]], writes=[Zp], accum=True)
                    k.op("pe", lambda e, g=g: e.matmul(Zp[:N1, g, :], Yi[:, g * N1:(g + 1) * N1], tb["RB"][:, :], start=False, stop=True),
                         reads=[Yi, tb["RB"]], writes=[Zp], accum=True)
                Zr, Zi = Zp[:N1, :, 0:64], Zp[:N1, :, 64:128]
                cT = tb["cT"][:N1, :].unsqueeze(1).to_broadcast([N1, G, 64])
                sT = tb["sT"][:N1, :].unsqueeze(1).to_broadcast([N1, G, 64])
                u1, u2, u3, u4, Zr2, Zi2 = Zt
                v = lambda t: t[:N1, :].rearrange("p (g n) -> p g n", g=G)
                k.op("dve", lambda e: e.tensor_tensor(out=v(u1), in0=Zr, in1=cT, op=ALU.mult), reads=[Zp, tb["cT"]], writes=[u1])
                k.op("dve", lambda e: e.tensor_tensor(out=v(u2), in0=Zi, in1=sT, op=ALU.mult), reads=[Zp, tb["sT"]], writes=[u2])
                k.op("dve", lambda e: e.tensor_tensor(out=v(u3), in0=Zr, in1=sT, op=ALU.mult), reads=[Zp, tb["sT"]], writes=[u3])
                k.op("dve", lambda e: e.tensor_tensor(out=v(u4), in0=Zi, in1=cT, op=ALU.mult), reads=[Zp, tb["cT"]], writes=[u4])
                k.op("pool", lambda e: e.tensor_tensor(out=Zr2[:N1, :], in0=u1[:N1, :], in1=u2[:N1, :], op=ALU.subtract), reads=[u1, u2], writes=[Zr2])
                k.op("pool", lambda e: e.tensor_tensor(out=Zi2[:N1, :], in0=u3[:N1, :], in1=u4[:N1, :], op=ALU.add), reads=[u3, u4], writes=[Zi2])
                k.op("pe", lambda e: e.matmul(yp[:NH, :G * 64], tb["C1"][:N1, :NH], Zr2[:N1, :], start=True, stop=False),
                     reads=[tb["C1"], Zr2], writes=[yp], accum=True)
                k.op("pe", lambda e: e.matmul(yp[:NH, :G * 64], tb["nS1"][:N1, :NH], Zi2[:N1, :], start=False, stop=True),
                     reads=[tb["nS1"], Zi2], writes=[yp], accum=True)
                e1, e2 = et
                bv = brow[:NH, d0:d0 + G].unsqueeze(1).to_broadcast([NH, 64, G])
                k.op("pool", lambda e, z=z, g0=g0, bv=bv: e.tensor_tensor(out=e1[:NH, :, :], in0=z[:NH, :, g0:g0 + G], in1=bv, op=ALU.mult),
                     reads=[z, brow], writes=[e1])
                k.op("dve", lambda e: e.tensor_tensor(out=e2[:NH, :, :], in0=yp[:NH, :G * 64].rearrange("p (g n) -> p n g", g=G),
                                                      in1=e1[:NH, :, :], op=ALU.add), reads=[yp, e1], writes=[e2])
                k.op("pool", lambda e, x=x, g0=g0: e.tensor_tensor(out=x[:NH, :, g0:g0 + G], in0=x[:NH, :, g0:g0 + G], in1=e2[:NH, :, :], op=ALU.mult),
                     reads=[x, e2], writes=[x])
            k.dma(ov[:, :, cb * 128:(cb + 1) * 128], x[:NH, :, :], reads=[x], sem=so[cb % 2])
        k.end()

    def hy_conv3(self, U, L, j, UC):
        k = self.k
        k.begin()
        C3 = 3 * D
        wr = k.tile([128, 3, C3])
        br = k.tile([128, C3])
        s0 = k.dsem()
        for r in range(3):
            k.dma(wr[:, r, :], self.W("hy", "conv_w")[j * 3 + r:j * 3 + r + 1, :].partition_broadcast(128), writes=[wr], sem=s0)
        k.dma(br[:], self.W("hy", "conv_b")[j:j + 1, :].partition_broadcast(128), writes=[br], sem=s0)
        ub = [[k.tile([128, C3]) for _ in range(3)] for _ in range(2)]
        su = [[k.dsem() for _ in range(3)] for _ in range(2)]
        so = [k.dsem() for _ in range(2)]
        nt = L // 128
        for t in range(nt):
            u0, u1, u2 = ub[t % 2]
            s_ = su[t % 2]
            t0 = t * 128
            if t == 0:
                k.op("pool", lambda e, u0=u0: e.memset(u0[:], 0.0), writes=[u0])
                k.dma(u0[1:128, :], U[0:127, :], writes=[u0], sem=s_[0])
            else:
                k.dma(u0[:], U[t0 - 1:t0 + 127, :], writes=[u0], sem=s_[0])
            k.dma(u1[:], U[t0:t0 + 128, :], writes=[u1], sem=s_[1])
            if t == nt - 1:
                k.op("pool", lambda e, u2=u2: e.memset(u2[:], 0.0), writes=[u2])
                k.dma(u2[0:127, :], U[t0 + 1:t0 + 128, :], writes=[u2], sem=s_[2])
            else:
                k.dma(u2[:], U[t0 + 1:t0 + 129, :], writes=[u2], sem=s_[2])
            k.op("pool", lambda e, u0=u0: e.tensor_tensor(out=u0[:], in0=u0[:], in1=wr[:, 0, :], op=ALU.mult), reads=[u0, wr], writes=[u0])
            k.op("dve", lambda e, u1=u1: e.tensor_tensor(out=u1[:], in0=u1[:], in1=wr[:, 1, :], op=ALU.mult), reads=[u1, wr], writes=[u1])
            k.op("pool", lambda e, u2=u2: e.tensor_tensor(out=u2[:], in0=u2[:], in1=wr[:, 2, :], op=ALU.mult), reads=[u2, wr], writes=[u2])
            k.op("dve", lambda e, u0=u0, u1=u1: e.tensor_tensor(out=u1[:], in0=u1[:], in1=u0[:], op=ALU.add), reads=[u0, u1], writes=[u1])
            k.op("pool", lambda e, u2=u2: e.tensor_tensor(out=u2[:], in0=u2[:], in1=br[:], op=ALU.add), reads=[u2, br], writes=[u2])
            k.op("dve", lambda e, u1=u1, u2=u2: e.tensor_tensor(out=u1[:], in0=u1[:], in1=u2[:], op=ALU.add), reads=[u1, u2], writes=[u1])
            k.dma(UC[t0:t0 + 128, :], u1[:], reads=[u1], sem=so[t % 2])
        k.end()

    def hyena(self, j, HLFM, L, Y, tag):
        N1 = L // 32
        U = self.dt("hyU" + tag, [L, 3 * D])
        UC = self.dt("hyUC" + tag, [L, 3 * D])
        HF = self.dt("hyHF" + tag, [L, 4096])
        RN = self.dt("hyRN" + tag, [1, 4096])
        SPEC = self.dt("hySP" + tag, [2, 64, 4096, N1])
        Z1 = self.dt("hyZ1" + tag, [L, D])
        Z2 = self.dt("hyZ2" + tag, [L, D])
        Z2T = self.dt("hyZ2T" + tag, [D, L])
        self.hy_filters(j, L, HF, RN)
        if self.stop_at == 1:
            return
        self.hy_filter_fft(L, HF, RN, SPEC)
        if self.stop_at == 2:
            return
        self.gemm(HLFM, self.W("hy", "w_in")[j * D:(j + 1) * D, :], D, 3 * D, L, U, out_tm=True,
                  bias_row=self.W("hy", "b_in")[j:j + 1, :])
        self.hy_conv3(U, L, j, UC)
        if self.stop_at == 3:
            return
        fb = self.W("hy", "f_bias")
        self.hy_conv(L, UC, 0, UC, D, SPEC, 0, fb[j * 2:j * 2 + 1, :], Z1)
        if self.stop_at == 4:
            return
        self.hy_conv(L, Z1, 0, UC, 2 * D, SPEC, 1, fb[j * 2 + 1:j * 2 + 2, :], Z2)
        self.transpose_in(Z2, Z2T, L)
        self.gemm(Z2T, self.W("hy", "w_out")[j * D:(j + 1) * D, :], D, D, L, Y, bias_pv="hy_b_out%d" % j)

    def rwkv(self, HC, HL, Y):
        k = self.k
        S = TC + T
        NT = S // 128
        NCH = S // 64
        H = 16
        Wr = lambda nm: self.W("rw", nm)
        rows = Wr("rows")
        XN = self.dt("rwXN", [6, D, S])
        k.begin()
        hh = [k.tile([128, T + 2]) for _ in range(2)]
        sh = [k.dsem() for _ in range(2)]
        xx = k.tile([128, T])
        xo = [k.tile([128, T]) for _ in range(2)]
        so = [k.dsem() for _ in range(2)]
        it = io = 0
        for (src, Tn, c0) in ((HC, TC, 0), (HL, T, TC)):
            for c in range(8):
                hb = hh[it % 2]
                it += 1
                k.op("pool", lambda e, hb=hb: e.memset(hb[:], 0.0), writes=[hb])
                k.dma(hb[:, 1:Tn + 1], src[c * 128:(c + 1) * 128, :], writes=[hb], sem=sh[it % 2])
                k.op("dve", lambda e, hb=hb, Tn=Tn: e.tensor_tensor(out=xx[:, :Tn], in0=hb[:, 0:Tn], in1=hb[:, 2:Tn + 2], op=ALU.add),
                     reads=[hb], writes=[xx])
                k.op("dve", lambda e, hb=hb, Tn=Tn: e.scalar_tensor_tensor(out=xx[:, :Tn], in0=xx[:, :Tn], scalar=0.5, in1=hb[:, 1:Tn + 1],
                                                                       op0=ALU.mult, op1=ALU.subtract), reads=[hb, xx], writes=[xx])
                for n in range(6):
                    o = xo[io % 2]
                    mu = self.pvc("rw_mu%d" % n, c)
                    eng = "dve"
                    k.op(eng, lambda e, o=o, hb=hb, Tn=Tn, mu=mu: e.scalar_tensor_tensor(
                        out=o[:, :Tn], in0=xx[:, :Tn], scalar=mu, in1=hb[:, 1:Tn + 1], op0=ALU.mult, op1=ALU.add),
                        reads=[xx, hb, self.pv], writes=[o])
                    k.dma(XN[n, c * 128:(c + 1) * 128, c0:c0 + Tn], o[:, :Tn], reads=[o], sem=so[io % 2])
                    io += 1
        k.end()
        Rm = self.dt("rwR", [S, D]); Km = self.dt("rwK", [S, D]); Vm = self.dt("rwV", [S, D])
        SW = [self.dt("rwSW%d" % n, [S, D]) for n in range(2)]
        Am = [self.dt("rwA%d" % n, [S, D]) for n in range(2)]
        Gm = self.dt("rwG", [S, D])
        HW = self.dt("rwHW", [128, S]); HA = self.dt("rwHA", [128, S]); HG = self.dt("rwHG", [256, S])
        self.gemm(XN[0], Wr("wr"), D, D, S, Rm, out_tm=True)
        self.gemm(XN[2], Wr("wk"), D, D, S, Km, out_tm=True)
        self.gemm(XN[3], Wr("wv"), D, D, S, Vm, out_tm=True)
        self.gemm(XN[1], Wr("w1cat"), D, 128, S, HW, act=AF.Tanh)
        self.gemm(XN[4], Wr("a1cat"), D, 128, S, HA)
        self.gemm(XN[5], Wr("g1pad"), D, 256, S, HG, act=AF.Sigmoid)
        for n in range(2):
            self.gemm(HW, Wr("w2pad%d" % n), 128, D, S, SW[n], out_tm=True, bias_row=rows[n:n + 1, :], act=AF.Sigmoid)
            self.gemm(HA, Wr("a2pad%d" % n), 128, D, S, Am[n], out_tm=True, bias_row=rows[2 + n:3 + n, :], act=AF.Sigmoid)
        self.gemm(HG, Wr("g2pad"), 256, D, S, Gm, out_tm=True)
        XT = self.dt("rwXT", [2, NT, 64, H, 4, 128])
        KD = self.dt("rwKD", [2, S, D]); NAD = self.dt("rwNAD", [2, S, D])
        PCf = self.dt("rwPC", [2, NCH, 64, H])
        BON = self.dt("rwBON", [S, D])
        k.begin()
        s0 = k.dsem()
        rw = k.tile([128, 3, D])
        for i_, r_ in enumerate((4, 5, 6)):
            k.dma(rw[:, i_, :], rows[r_:r_ + 1, :].partition_broadcast(128), writes=[rw], sem=s0)
        tri = [k.tile([128, 128]) for _ in range(2)]
        for n in range(2):
            k.dma(tri[n][:], Wr("TRI%d" % n), writes=[tri[n]], sem=s0)
        chk = k.tile([128, 2])
        k.dma(chk[:], Wr("CHK"), writes=[chk], sem=s0)
        inp = {nm: k.tile([128, D]) for nm in ("r", "k", "v", "sw0", "sw1", "a0", "a1")}
        sin = {nm: k.dsem() for nm in inp}
        srcs = {"r": Rm, "k": Km, "v": Vm, "sw0": SW[0], "sw1": SW[1], "a0": Am[0], "a1": Am[1]}
        wk_ = {nm: k.tile([128, D]) for nm in ("kkq", "kk", "t1", "t2", "lw", "kdir", "kd", "nad", "kt", "rt", "ksum")}
        sm = k.tile([128, H])
        pl = k.ptile([128, 2, 512])
        ptp = [k.ptile([64, 4, 128]) for _ in range(2)]
        ppc = k.ptile([64, H, 2])
        xts = k.tile([64, H, 4, 128])
        pcs = k.tile([64, 2, H])
        sxo = k.dsem(); sko = k.dsem(); sno = k.dsem(); spo = k.dsem(); sbo = k.dsem()
        v3 = lambda t: t[:].rearrange("p (h c) -> p h c", h=H)
        itp = 0
        for tt in range(NT):
            r0 = tt * 128
            for nm in inp:
                k.dma(inp[nm][:], srcs[nm][r0:r0 + 128, :], writes=[inp[nm]], sem=sin[nm])
            R_, K_, V_ = inp["r"], inp["k"], inp["v"]
            kkq, kk, t1, t2, lw, kdir, kd, nad, kt, rt, ksum = (wk_[n_] for n_ in ("kkq", "kk", "t1", "t2", "lw", "kdir", "kd", "nad", "kt", "rt", "ksum"))
            k.op("dve", lambda e: e.tensor_tensor(out=kkq[:], in0=K_[:], in1=rw[:, 0, :], op=ALU.mult), reads=[K_, rw], writes=[kkq])
            k.op("pool", lambda e: e.tensor_tensor(out=t1[:], in0=kkq[:], in1=kkq[:], op=ALU.mult), reads=[kkq], writes=[t1])
            k.op("dve", lambda e: e.tensor_reduce(out=sm[:], in_=v3(t1), axis=AX.X, op=ALU.add), reads=[t1], writes=[sm])
            k.op("dve", lambda e: e.tensor_scalar(out=sm[:], in0=sm[:], scalar1=1e-24, scalar2=None, op0=ALU.max), reads=[sm], writes=[sm])
            k.op("act", lambda e: e.sqrt(out=sm[:], in_=sm[:]), reads=[sm], writes=[sm])
            k.op("dve", lambda e: e.reciprocal(out=sm[:], in_=sm[:]), reads=[sm], writes=[sm])
            k.op("dve", lambda e: e.tensor_tensor(out=v3(kk), in0=v3(kkq), in1=sm[:].unsqueeze(2).to_broadcast([128, H, 64]), op=ALU.mult),
                 reads=[kkq, sm], writes=[kk])
            for n in range(2):
                SWt, At = inp["sw%d" % n], inp["a%d" % n]
                k.op("act", lambda e, SWt=SWt: e.mul(out=lw[:], in_=SWt[:], mul=-0.6065306597126334), reads=[SWt], writes=[lw])
                k.op("dve", lambda e, At=At: e.scalar_tensor_tensor(out=t1[:], in0=At[:], scalar=-1.0, in1=rw[:, 1, :], op0=ALU.add, op1=ALU.mult),
                     reads=[At, rw], writes=[t1])
                k.op("dve", lambda e: e.scalar_tensor_tensor(out=kdir[:], in0=t1[:], scalar=1.0, in1=K_[:], op0=ALU.add, op1=ALU.mult),
                     reads=[t1, K_], writes=[kdir])
                if n == 0:
                    k.op("pool", lambda e: e.tensor_copy(out=ksum[:], in_=kdir[:]), reads=[kdir], writes=[ksum])
                else:
                    k.op("pool", lambda e: e.tensor_tensor(out=ksum[:], in0=ksum[:], in1=kdir[:], op=ALU.add), reads=[kdir, ksum], writes=[ksum])
                k.op("pool", lambda e, At=At: e.tensor_tensor(out=nad[:], in0=kk[:], in1=At[:], op=ALU.mult), reads=[kk, At], writes=[nad])
                for hf in range(2):
                    k.op("pe", lambda e, hf=hf, n=n: e.matmul(pl[:, hf, :], tri[n][:], lw[:, hf * 512:(hf + 1) * 512], start=True, stop=True),
                         reads=[tri[n], lw], writes=[pl], accum=True)
                plv = pl[:].rearrange("p a b -> p (a b)")
                k.op("dve", lambda e: e.tensor_tensor(out=t2[:], in0=plv, in1=lw[:], op=ALU.subtract), reads=[pl, lw], writes=[t2])
                k.op("act", lambda e: e.activation(out=t2[:], in_=t2[:], func=AF.Exp), reads=[t2], writes=[t2])
                k.op("pool", lambda e: e.tensor_tensor(out=kt[:], in0=kk[:], in1=t2[:], op=ALU.mult), reads=[kk, t2], writes=[kt])
                k.op("act", lambda e: e.activation(out=t1[:], in_=plv, func=AF.Exp), reads=[pl], writes=[t1])
                k.op("dve", lambda e: e.tensor_tensor(out=rt[:], in0=R_[:], in1=t1[:], op=ALU.mult), reads=[R_, t1], writes=[rt])
                k.op("act", lambda e: e.activation(out=t2[:], in_=plv, func=AF.Exp, scale=-1.0), reads=[pl], writes=[t2])
                k.op("dve", lambda e: e.tensor_tensor(out=kd[:], in0=kdir[:], in1=t2[:], op=ALU.mult), reads=[kdir, t2], writes=[kd])
                k.op("dve", lambda e: e.scalar_tensor_tensor(out=nad[:], in0=nad[:], scalar=-1.0, in1=t2[:], op0=ALU.mult, op1=ALU.mult),
                     reads=[nad, t2], writes=[nad])
                k.dma(KD[n, r0:r0 + 128, :], kd[:], reads=[kd], sem=sko)
                k.dma(NAD[n, r0:r0 + 128, :], nad[:], reads=[nad], sem=sno)
                for h in range(H):
                    k.op("pe", lambda e, h=h: e.matmul(ppc[:, h, :], lw[:, h * 64:(h + 1) * 64], chk[:], start=True, stop=True),
                         reads=[lw, chk], writes=[ppc], accum=True)
                k.op("act", lambda e: e.activation(out=pcs[:].rearrange("p c h -> p h c"), in_=ppc[:], func=AF.Exp), reads=[ppc], writes=[pcs])
                for cc in range(2):
                    k.dma(PCf[n, tt * 2 + cc, :, :], pcs[:, cc, :], reads=[pcs], sem=spo)
                for q, srct in enumerate((kt, rt, kd, nad)):
                    for hg in range(4):
                        p = ptp[itp % 2]
                        itp += 1
                        for hl in range(4):
                            h = hg * 4 + hl
                            k.op("pe", lambda e, p=p, hl=hl, h=h, srct=srct: e.transpose(p[:, hl, :], srct[:, h * 64:(h + 1) * 64], self.ident[:]),
                                 reads=[srct, self.ident], writes=[p], accum=True)
                        if itp % 2 == 0:
                            k.op("act", lambda e, p=p, hg=hg, q=q: e.copy(out=xts[:, hg * 4:(hg + 1) * 4, q, :], in_=p[:]), reads=[p], writes=[xts], accum=True)
                        else:
                            k.op("dve", lambda e, p=p, hg=hg, q=q: e.tensor_copy(out=xts[:, hg * 4:(hg + 1) * 4, q, :], in_=p[:]), reads=[p], writes=[xts], accum=True)
                k._wait("sp", [(k.esem["act"], k.cnt[k.esem["act"]]), (k.esem["dve"], k.cnt[k.esem["dve"]])])
                k.dma(XT[n, tt], xts[:], reads=[xts], sem=sxo, q="sp")
            k.op("dve", lambda e: e.tensor_tensor(out=t1[:], in0=ksum[:], in1=rw[:, 2, :], op=ALU.mult), reads=[ksum, rw], writes=[t1])
            k.op("dve", lambda e: e.tensor_tensor(out=t1[:], in0=t1[:], in1=R_[:], op=ALU.mult), reads=[t1, R_], writes=[t1])
            k.op("dve", lambda e: e.tensor_reduce(out=sm[:], in_=v3(t1), axis=AX.X, op=ALU.add), reads=[t1], writes=[sm])
            k.op("dve", lambda e: e.tensor_tensor(out=v3(t2), in0=v3(V_), in1=sm[:].unsqueeze(2).to_broadcast([128, H, 64]), op=ALU.mult),
                 reads=[V_, sm], writes=[t2])
            k.dma(BON[r0:r0 + 128, :], t2[:], reads=[t2], sem=sbo)
        k.end()
        if self.stop_at == 3:
            return
        CH = self.dt("rwCH", [2, NCH, 64, H, 256])
        k.begin()
        s0 = k.dsem()
        msk = [k.tile([64, 512]) for _ in range(2)]
        for n in range(2):
            k.dma(msk[n][:], Wr("MASK%d" % n), writes=[msk[n]], sem=s0)
        idn = k.tile([64, 64])
        k.dma(idn[:], Wr("IDN"), writes=[idn], sem=s0)
        xt = [k.tile([64, H, 4, 128]) for _ in range(2)]
        sx = [k.dsem() for _ in range(2)]
        sc = [k.tile([64, H, 320]) for _ in range(2)]
        psc = k.ptile([64, 2, 512])
        pN = k.ptile([64, H, 64]); pL = k.ptile([64, H, 64]); pX = k.ptile([64, H, 64])
        Nb = [k.tile([64, H, 64]) for _ in range(2)]
        Lb = [k.tile([64, H, 64]) for _ in range(2)]
        ILb = k.tile([64, H, 64])
        Xb = [k.tile([64, H, 64]) for _ in range(2)]
        so1 = [k.dsem() for _ in range(2)]
        so2 = [k.dsem() for _ in range(2)]
        idb = idn[:].unsqueeze(1).to_broadcast([64, H, 64])
        ci = 0
        for tt in range(NT):
            for n in range(2):
                x = xt[(tt * 2 + n) % 2]
                k.dma(x[:], XT[n, tt], writes=[x], sem=sx[(tt * 2 + n) % 2])
                for cc in range(2):
                    cs = slice(cc * 64, (cc + 1) * 64)
                    s_ = sc[ci % 2]
                    for hp in range(8):
                        for hl in range(2):
                            h = hp * 2 + hl
                            k.op("pe", lambda e, hl=hl, h=h, x=x, cs=cs: e.matmul(
                                psc[:, hl, 0:128].rearrange("p (a b) -> p a b", a=2), x[:, h, 3, cs], x[:, h, 0:2, cs], start=True, stop=True),
                                reads=[x], writes=[psc], accum=True)
                            k.op("pe", lambda e, hl=hl, h=h, x=x, cs=cs: e.matmul(
                                psc[:, hl, 128:256].rearrange("p (a b) -> p a b", a=2), x[:, h, 2, cs], x[:, h, 0:2, cs], start=True, stop=True),
                                reads=[x], writes=[psc], accum=True)
                            k.op("pe", lambda e, hl=hl, h=h, x=x, cs=cs: e.matmul(
                                psc[:, hl, 256:320], x[:, h, 0, cs], x[:, h, 3, cs], start=True, stop=True),
                                reads=[x], writes=[psc], accum=True)
                        k.op("dve", lambda e, hp=hp, s_=s_, n=n: e.tensor_tensor(
                            out=s_[:, hp * 2:hp * 2 + 2, :], in0=psc[:, :, 0:320],
                            in1=msk[n][:, 0:320].unsqueeze(1).to_broadcast([64, 2, 320]), op=ALU.mult),
                            reads=[psc, msk[n]], writes=[s_], accum=True)
                    N2, L2 = s_[:, :, 0:64], s_[:, :, 256:320]
                    Xc = Xb[0]
                    k.op("pool", lambda e, Xc=Xc, N2=N2: e.tensor_tensor(out=Xc[:], in0=idb, in1=N2, op=ALU.subtract), reads=[idn, s_], writes=[Xc])
                    Ncur, Lcur, Nbuf, Lbuf = N2, L2, s_, s_
                    for j in range(1, 6):
                        Ln = Lb[j % 2]
                        for h in range(H):
                            k.op("pe", lambda e, h=h, Ncur=Ncur, Lcur=Lcur: e.matmul(pL[:, h, :], Ncur[:, h, :], Lcur[:, h, :], start=True, stop=True),
                                 reads=[Nbuf, Lbuf], writes=[pL], accum=True)
                        if j < 5:
                            Nn = Nb[j % 2]
                            for h in range(H):
                                k.op("pe", lambda e, h=h, Ncur=Ncur, Lcur=Lcur: e.matmul(pN[:, h, :], Lcur[:, h, :], Ncur[:, h, :], start=True, stop=True),
                                     reads=[Nbuf, Lbuf], writes=[pN], accum=True)
                            k.op("act", lambda e, Nn=Nn: e.copy(out=Nn[:], in_=pN[:]), reads=[pN], writes=[Nn])
                        k.op("dve", lambda e: e.tensor_tensor(out=ILb[:], in0=pL[:], in1=idb, op=ALU.add), reads=[pL, idn], writes=[ILb])
                        if j < 5:
                            k.op("dve", lambda e, Ln=Ln: e.tensor_copy(out=Ln[:], in_=pL[:]), reads=[pL], writes=[Ln])
                        Xn = Xb[j % 2]
                        for h in range(H):
                            k.op("pe", lambda e, h=h, Xc=Xc: e.matmul(pX[:, h, :], ILb[:, h, :], Xc[:, h, :], start=True, stop=True),
                                 reads=[ILb, Xc], writes=[pX], accum=True)
                        k.op("act", lambda e, Xn=Xn: e.copy(out=Xn[:], in_=pX[:]), reads=[pX], writes=[Xn])
                        Xc = Xn
                        if j < 5:
                            Ncur, Lcur, Nbuf, Lbuf = Nn[:], Ln[:], Nn, Ln
                    chn = tt * 2 + cc
                    k.dma(CH[n, chn, :, :, 0:64], Xc[:], reads=[Xc], sem=so1[ci % 2])
                    k.dma(CH[n, chn, :, :, 64:256], s_[:, :, 64:256], reads=[s_], sem=so2[ci % 2])
                    ci += 1
        k.end()
        if self.stop_at == 4:
            return
        YW = self.dt("rwYW", [2, S, D])
        k.begin()
        M = [k.tile([64, H, 64]) for _ in range(2)]
        for n in range(2):
            k.op("pool", lambda e, n=n: e.memset(M[n][:], 0.0), writes=[M[n]])
        ktrt = [k.tile([64, H, 2, 64]) for _ in range(2)]
        cht = [k.tile([64, H, 256]) for _ in range(2)]
        kdt = [k.tile([64, D]) for _ in range(2)]
        nadt = [k.tile([64, D]) for _ in range(2)]
        vt = [k.tile([64, D]) for _ in range(2)]
        pct = [k.tile([64, H]) for _ in range(2)]
        sl_ = [[k.dsem() for _ in range(6)] for _ in range(2)]
        W0s = k.tile([64, H, 64]); Us = k.tile([64, H, 64])
        Ys = [k.tile([64, H, 64]) for _ in range(2)]
        sy = [k.dsem() for _ in range(2)]
        pW = k.ptile([64, H, 64]); pU = k.ptile([64, H, 64]); pY = k.ptile([64, H, 64]); pM = k.ptile([64, H, 64])
        order = {0: list(range(NCH)), 1: list(range(TC // 64 - 1, -1, -1)) + list(range(NCH - 1, TC // 64 - 1, -1))}
        for step in range(NCH):
            for n in range(2):
                chn = order[n][step]
                tt, cc = chn // 2, chn % 2
                cs = slice(cc * 64, (cc + 1) * 64)
                r0 = chn * 64
                b = n
                sl = sl_[b]
                k.dma(ktrt[b][:], XT[n, tt, :, :, 0:2, cs], writes=[ktrt[b]], sem=sl[0], q="sp")
                k.dma(cht[b][:], CH[n, chn], writes=[cht[b]], sem=sl[1], q="act")
                k.dma(kdt[b][:], KD[n, r0:r0 + 64, :], writes=[kdt[b]], sem=sl[2], q="sp")
                k.dma(nadt[b][:], NAD[n, r0:r0 + 64, :], writes=[nadt[b]], sem=sl[3], q="act")
                k.dma(vt[b][:], Vm[r0:r0 + 64, :], writes=[vt[b]], sem=sl[4], q="sp")
                k.dma(pct[b][:], PCf[n, chn], writes=[pct[b]], sem=sl[5], q="act")
                kr, ch_, kd_, nad_, v_, pc_, Mn = ktrt[b], cht[b], kdt[b], nadt[b], vt[b], pct[b], M[n]
                for h in range(H):
                    hs = slice(h * 64, (h + 1) * 64)
                    k.op("pe", lambda e, h=h: e.matmul(pW[:, h, :], kr[:, h, 0, :], Mn[:, h, :], start=True, stop=False),
                         reads=[kr, Mn], writes=[pW], accum=True)
                    k.op("pe", lambda e, h=h, hs=hs: e.matmul(pW[:, h, :], ch_[:, h, 128:192], v_[:, hs], start=False, stop=True),
                         reads=[ch_, v_], writes=[pW], accum=True)
                k.op("act", lambda e: e.copy(out=W0s[:], in_=pW[:]), reads=[pW], writes=[W0s])
                for h in range(H):
                    k.op("pe", lambda e, h=h: e.matmul(pU[:, h, :], ch_[:, h, 0:64], W0s[:, h, :], start=True, stop=True),
                         reads=[ch_, W0s], writes=[pU], accum=True)
                k.op("dve", lambda e: e.tensor_copy(out=Us[:], in_=pU[:]), reads=[pU], writes=[Us])
                if chn >= TC // 64:
                    ys = Ys[n]
                    for h in range(H):
                        hs = slice(h * 64, (h + 1) * 64)
                        k.op("pe", lambda e, h=h: e.matmul(pY[:, h, :], kr[:, h, 1, :], Mn[:, h, :], start=True, stop=False),
                             reads=[kr, Mn], writes=[pY], accum=True)
                        k.op("pe", lambda e, h=h, hs=hs: e.matmul(pY[:, h, :], ch_[:, h, 192:256], v_[:, hs], start=False, stop=False),
                             reads=[ch_, v_], writes=[pY], accum=True)
                        k.op("pe", lambda e, h=h: e.matmul(pY[:, h, :], ch_[:, h, 64:128], Us[:, h, :], start=False, stop=True),
                             reads=[ch_, Us], writes=[pY], accum=True)
                    k.op("act", lambda e, ys=ys: e.copy(out=ys[:], in_=pY[:]), reads=[pY], writes=[ys])
                    k.dma(YW[n, r0:r0 + 64, :], ys[:].rearrange("p h c -> p (h c)"), reads=[ys], sem=sy[n])
                for h in range(H):
                    hs = slice(h * 64, (h + 1) * 64)
                    k.op("pe", lambda e, h=h, hs=hs: e.matmul(pM[:, h, :], kd_[:, hs], v_[:, hs], start=True, stop=False),
                         reads=[kd_, v_], writes=[pM], accum=True)
                    k.op("pe", lambda e, h=h, hs=hs: e.matmul(pM[:, h, :], nad_[:, hs], Us[:, h, :], start=False, stop=True),
                         reads=[nad_, Us], writes=[pM], accum=True)
                k.op("dve", lambda e, Mn=Mn: e.tensor_tensor(out=Mn[:], in0=Mn[:], in1=pM[:], op=ALU.add), reads=[Mn, pM], writes=[Mn])
                k.op("dve", lambda e, Mn=Mn, pc_=pc_: e.tensor_tensor(out=Mn[:], in0=Mn[:], in1=pc_[:].unsqueeze(2).to_broadcast([64, H, 64]), op=ALU.mult),
                     reads=[Mn, pc_], writes=[Mn])
        k.end()
        if self.stop_at == 5:
            return
        O = self.dt("rwO", [T, D])
        k.begin()
        s0 = k.dsem()
        gw = k.tile([128, 2, D])
        for i_, r_ in enumerate((7, 8)):
            k.dma(gw[:, i_, :], rows[r_:r_ + 1, :].partition_broadcast(128), writes=[gw], sem=s0)
        ya = [k.tile([128, D]) for _ in range(2)]
        yb_ = [k.tile([128, D]) for _ in range(2)]
        bo = [k.tile([128, D]) for _ in range(2)]
        gt = [k.tile([128, D]) for _ in range(2)]
        ss = [[k.dsem() for _ in range(4)] for _ in range(2)]
        sq_ = k.tile([128, D])
        st1 = k.tile([128, H]); st2 = k.tile([128, H])
        so = [k.dsem() for _ in range(2)]
        for t in range(T // 128):
            b = t % 2
            r0 = TC + t * 128
            k.dma(ya[b][:], YW[0, r0:r0 + 128, :], writes=[ya[b]], sem=ss[b][0])
            k.dma(yb_[b][:], YW[1, r0:r0 + 128, :], writes=[yb_[b]], sem=ss[b][1])
            k.dma(bo[b][:], BON[r0:r0 + 128, :], writes=[bo[b]], sem=ss[b][2])
            k.dma(gt[b][:], Gm[r0:r0 + 128, :], writes=[gt[b]], sem=ss[b][3])
            w = ya[b]
            w3_ = w[:].rearrange("p (h c) -> p h c", h=H)
            k.op("dve", lambda e, w=w, b=b: e.tensor_tensor(out=w[:], in0=w[:], in1=yb_[b][:], op=ALU.add), reads=[w, yb_[b]], writes=[w])
            k.op("dve", lambda e, w3_=w3_: e.tensor_reduce(out=st1[:], in_=w3_, axis=AX.X, op=ALU.add), reads=[w], writes=[st1])
            k.op("dve", lambda e: e.tensor_scalar(out=st1[:], in0=st1[:], scalar1=1.0 / 64, scalar2=None, op0=ALU.mult), reads=[st1], writes=[st1])
            k.op("dve", lambda e, w3_=w3_: e.tensor_tensor(out=w3_, in0=w3_, in1=st1[:].unsqueeze(2).to_broadcast([128, H, 64]), op=ALU.subtract),
                 reads=[w, st1], writes=[w])
            k.op("pool", lambda e, w=w: e.tensor_tensor(out=sq_[:], in0=w[:], in1=w[:], op=ALU.mult), reads=[w], writes=[sq_])
            k.op("dve", lambda e: e.tensor_reduce(out=st2[:], in_=sq_[:].rearrange("p (h c) -> p h c", h=H), axis=AX.X, op=ALU.add), reads=[sq_], writes=[st2])
            k.op("dve", lambda e: e.tensor_scalar(out=st2[:], in0=st2[:], scalar1=1.0 / 64, scalar2=64e-5, op0=ALU.mult, op1=ALU.add), reads=[st2], writes=[st2])
            k.op("act", lambda e: e.sqrt(out=st2[:], in_=st2[:]), reads=[st2], writes=[st2])
            k.op("dve", lambda e: e.reciprocal(out=st2[:], in_=st2[:]), reads=[st2], writes=[st2])
            k.op("dve", lambda e, w3_=w3_: e.tensor_tensor(out=w3_, in0=w3_, in1=st2[:].unsqueeze(2).to_broadcast([128, H, 64]), op=ALU.mult),
                 reads=[w, st2], writes=[w])
            k.op("pool", lambda e, w=w: e.tensor_tensor(out=w[:], in0=w[:], in1=gw[:, 0, :], op=ALU.mult), reads=[w, gw], writes=[w])
            k.op("pool", lambda e, w=w: e.tensor_tensor(out=w[:], in0=w[:], in1=gw[:, 1, :], op=ALU.add), reads=[w, gw], writes=[w])
            k.op("dve", lambda e, w=w, b=b: e.tensor_tensor(out=w[:], in0=w[:], in1=bo[b][:], op=ALU.add), reads=[w, bo[b]], writes=[w])
            k.op("dve", lambda e, w=w, b=b: e.tensor_tensor(out=w[:], in0=w[:], in1=gt[b][:], op=ALU.mult), reads=[w, gt[b]], writes=[w])
            k.dma(O[t * 128:(t + 1) * 128, :], w[:], reads=[w], sem=so[b])
        k.end()
        OT = self.dt("rwOT", [D, T])
        self.transpose_in(O, OT, T)
        self.gemm(OT, Wr("wo"), D, D, T, Y)

    def final_dbg(self):
        k = self.k
        k.begin()
        s = k.dsem()
        for name in self.dbg:
            if name == "modv":
                o = self.dt("dbg_modv", [128, DEPTH * 48 * 2], kind="ExternalOutput")
                k.dma(o, self.modv[:].rearrange("p a b c -> p (a b c)"), reads=[self.modv], sem=s)
                continue
            src = self.dram[name]
            o = self.dt("dbg_" + name, list(src.shape), kind="ExternalOutput")
            k.dma(o, src.ap(), sem=s, q="sp")
        k.end()


_CACHE = {}


def run(inputs, layers=(0, 1, 2, 3), dbg=(), cores=NCORES, test=None, extra=None):
    key = (tuple(layers), tuple(dbg), test)
    if key not in _CACHE:
        _CACHE[key] = Prog(list(layers), dbg, test)
    prog = _CACHE[key]
    blobs, lay = pack_host(inputs, list(layers))
    in_maps = []
    for c in range(cores):
        m = {"x": np.ascontiguousarray(inputs["x"][c]),
             "c": np.ascontiguousarray(inputs["c"][c:c + 1]),
             "ctx": np.ascontiguousarray(inputs["ctx"][c]),
             "c_ctx": np.ascontiguousarray(inputs["c_ctx"][None, :])}
        for piece, flat in blobs.items():
            m["blob_" + piece] = flat
        if extra:
            m.update(extra)
        in_maps.append(m)
    res = run_bass_kernel_spmd(prog.nc, in_maps, core_ids=list(range(cores)))
    return res.results


def kernel(**inputs):
    res = run(inputs)
    return np.stack([r["out"] for r in res], 0).astype(np.float32)
```

```python
import math
import numpy as np
from contextlib import ExitStack
import concourse.bass as bass
import concourse.mybir as mybir
from concourse.bass_utils import run_bass_kernel_spmd

F32 = mybir.dt.float32
F32R = mybir.dt.float32r
I32 = mybir.dt.int32
U32 = mybir.dt.uint32
AF = mybir.ActivationFunctionType
ALU = mybir.AluOpType
AX = mybir.AxisListType

NCORES = 8
D = 1024
T = 4096
TC = 256
DEPTH = 4
NE = 16
FF = 2048
ALPHA = (2 * DEPTH) ** 0.25
BW = 1024


class Buf:
    def __init__(self, name, ap=None, nsub=0):
        self.name = name
        self.t = ap
        self.w = []
        self.r = []
        self.kids = [Buf(name + str(i), ap) for i in range(nsub)]

    def __getitem__(self, idx):
        return self.t[idx]

    def ch(self, i):
        return self.kids[i]

    def leaves(self):
        if not self.kids:
            return [self]
        out = []
        for c in self.kids:
            out += c.leaves()
        return out


def _lv(bufs):
    out = []
    for b in bufs:
        out += b.leaves()
    return out


class KB:
    def __init__(self, nc, n_dma_sems=88):
        self.nc = nc
        self.eng = {"pe": nc.tensor, "dve": nc.vector, "act": nc.scalar,
                    "pool": nc.gpsimd, "sp": nc.sync}
        self.esem = {}
        self.cnt = {}
        self.semh = {}
        self.seen = {k: {} for k in self.eng}
        for k in self.eng:
            nm = "e_" + k
            self.semh[nm] = nc.alloc_semaphore(name=nm)
            self.esem[k] = nm
            self.cnt[nm] = 0
        self.free_dma = []
        for i in range(n_dma_sems):
            nm = "d%d" % i
            self.semh[nm] = nc.alloc_semaphore(name=nm)
            self.cnt[nm] = 0
            self.free_dma.append(nm)
        self.phase_sems = []
        self.stack = None
        self.rr = 0
        self.pstack = ExitStack()
        self.semreg = {}

    def begin(self):
        self.stack = ExitStack()
        self.phase_sems = []

    def end(self):
        self.barrier()
        self.stack.close()
        self.stack = None
        for nm in self.phase_sems:
            self.semreg.pop(nm, None)
        self.free_dma = self.phase_sems + self.free_dma
        self.phase_sems = []

    def dsem(self, persistent=False):
        nm = self.free_dma.pop()
        if not persistent:
            self.phase_sems.append(nm)
        return nm

    def tile(self, shape, dtype=F32, name=None, persistent=False, nsub=0):
        st = self.pstack if persistent else self.stack
        t = st.enter_context(self.nc.sbuf_tensor(list(shape), dtype))
        return Buf(name or "t", t, nsub)

    def ptile(self, shape, dtype=F32, name=None, nsub=0):
        t = self.stack.enter_context(self.nc.psum_tensor(list(shape), dtype))
        return Buf(name or "p", t, nsub)

    def _wait(self, ek, toks):
        need = {}
        for (s, v) in toks:
            if v > need.get(s, 0):
                need[s] = v
        for s, v in need.items():
            if self.seen[ek].get(s, 0) >= v:
                continue
            self.eng[ek].wait_ge(self.semh[s], v)
            self.seen[ek][s] = v

    def rnd(self, ek, dst_ap, src_ap, reads, writes):
        if ek == "act":
            return self.op("act", lambda e: e.copy(out=dst_ap, in_=src_ap), reads=reads, writes=writes)
        return self.op(ek, lambda e: e.tensor_copy(out=dst_ap, in_=src_ap), reads=reads, writes=writes)

    def barrier(self):
        toks = [(s, c) for s, c in self.cnt.items() if c > 0]
        for ek in self.eng:
            self._wait(ek, toks)

    def op(self, ek, fn, reads=(), writes=(), accum=False):
        reads = _lv(reads)
        writes = _lv(writes)
        toks = []
        es = self.esem[ek]
        for b in reads:
            toks += b.w
        for b in writes:
            if accum:
                toks += [t for t in b.w if t[0] != es]
            else:
                toks += b.w
            toks += b.r
        self._wait(ek, toks)
        ins = fn(self.eng[ek])
        self.cnt[es] += 1
        tok = (es, self.cnt[es])
        ins.then_inc(self.semh[es], 1)
        for b in reads:
            b.r.append(tok)
        for b in writes:
            b.w = [tok]
            b.r = []
        return ins

    def dma(self, out_ap, in_ap, reads=(), writes=(), sem=None, q=None, fn=None, **kw):
        reads = _lv(reads)
        writes = _lv(writes)
        if q is None:
            q = ("sp", "act")[self.rr % 2]
            self.rr += 1
        toks = []
        for b in reads:
            toks += b.w
        for b in writes:
            toks += [t for t in b.w if t[0] != sem]
            toks += b.r
        self._wait(q, toks)
        if fn is not None:
            ins = fn(self.eng[q])
        else:
            ins = self.eng[q].dma_start(out=out_ap, in_=in_ap, **kw)
        self.cnt[sem] += 16
        tok = (sem, self.cnt[sem])
        ins.then_inc(self.semh[sem], 16)
        reg = self.semreg.setdefault(sem, {})
        for b in reg.values():
            b.w = [tok if t[0] == sem else t for t in b.w]
            b.r = [tok if t[0] == sem else t for t in b.r]
        for b in reads:
            b.r.append(tok)
            reg[id(b)] = b
        for b in writes:
            b.w = [tok]
            b.r = []
            reg[id(b)] = b
        return ins


def _pv_cols():
    cols = {}
    off = 0

    def add(name, n):
        nonlocal off
        cols[name] = (off, n // 128)
        off += n // 128
    for i in range(DEPTH):
        add("mod_b%d" % i, 6 * D)
        for j in range(2):
            add("ln_g%d_%d" % (i, j), D)
            add("ln_b%d_%d" % (i, j), D)
    for j in range(2):
        add("hy_b_out%d" % j, D)
    for n in range(6):
        add("rw_mu%d" % n, D)
    add("fn_bo", D)
    return cols, off


PV_COLS, PV_N = _pv_cols()


def blob_spec(layers):
    sp = {"misc": []}
    m = sp["misc"]
    m.append(("ident", (128, 128)))
    m.append(("pv", (128, 1024)))
    m.append(("pos", (T, D)))
    m.append(("mod_w", (DEPTH * D, 6 * D)))
    m.append(("moe_router", (DEPTH * D, NE)))
    for i in layers:
        sp["moe%d" % i] = [("w1", (NE * D, FF)), ("w3", (NE * D, FF)), ("w2", (NE * FF, D))]
    if 0 in layers or 3 in layers:
        h = [("w_in", (2 * D, 3 * D)), ("b_in", (2, 3 * D)), ("conv_w", (6, 3 * D)), ("conv_b", (2, 3 * D)),
             ("f_w1", (66, 64)), ("hyv", (128, 4)), ("f_w2", (256, 64)), ("f_w3", (128, 4096)),
             ("f_bias", (4, D)), ("w_out", (2 * D, D)),
             ("C2", (64, 64)), ("S2", (64, 64)), ("nS2", (64, 64)), ("RA", (64, 128)), ("RB", (64, 128))]
        for L in (T, TC):
            NH = L // 64
            N1 = 2 * NH
            h += [("zT_%d" % L, (33, L)), ("win_%d" % L, (L, D)), ("F1_%d" % L, (NH, 2 * N1)),
                  ("c_%d" % L, (64, N1)), ("s_%d" % L, (64, N1)), ("cT_%d" % L, (N1, 64)), ("sT_%d" % L, (N1, 64)),
                  ("C1_%d" % L, (N1, NH)), ("nS1_%d" % L, (N1, NH))]
        sp["hy"] = h
    if 1 in layers:
        sp["rw"] = [("wr", (D, D)), ("wk", (D, D)), ("wv", (D, D)), ("wo", (D, D)),
                    ("w1cat", (D, 128)), ("a1cat", (D, 128)), ("g1pad", (D, 256)),
                    ("w2pad0", (128, D)), ("w2pad1", (128, D)), ("a2pad0", (128, D)), ("a2pad1", (128, D)),
                    ("g2pad", (256, D)), ("rows", (9, D)),
                    ("TRI0", (128, 128)), ("TRI1", (128, 128)), ("CHK", (128, 2)),
                    ("MASK0", (64, 512)), ("MASK1", (64, 512)), ("IDN", (64, 64))]
    if 2 in layers:
        sp["fnet"] = [("fn_cs", (128, 256)), ("fn_wo", (D, D)), ("fn_nct", (T, T)), ("fn_nst", (T, T))]
    return sp


def blob_layout(spec):
    lay = {}
    for piece, items in spec.items():
        off = 0
        ent = {}
        for name, (R, C) in items:
            n = R * C
            rows = (n + BW - 1) // BW
            ent[name] = (off, rows, R, C)
            off += rows
        tot = (off + 8 * 16 - 1) // (8 * 16) * (8 * 16)
        lay[piece] = (tot, ent)
    return lay


def grid_pos_embed():
    rows = T // 64
    r_idx = np.repeat(np.arange(rows, dtype=np.float32), 64)
    c_idx = np.tile(np.arange(64, dtype=np.float32), rows)
    quarter = D // 4
    omega = (1.0 / (10000.0 ** (np.arange(quarter, dtype=np.float32) / np.float32(quarter)))).astype(np.float32)

    def emb(p):
        a = (p[:, None] * omega[None, :]).astype(np.float32)
        return np.concatenate([np.sin(a), np.cos(a)], -1)
    return np.concatenate([emb(r_idx), emb(c_idx)], -1).astype(np.float32)


def hy_const_tables():
    out = {}
    n2 = np.arange(64)
    a2 = 2 * np.pi * (np.outer(n2, n2) % 64) / 64.0
    C2, S2 = np.cos(a2), np.sin(a2)
    out["C2"], out["S2"], out["nS2"] = C2, S2, -S2
    out["RA"] = np.concatenate([C2, S2], 1)
    out["RB"] = np.concatenate([-S2, C2], 1)
    for L in (T, TC):
        NH = L // 64
        N1 = 2 * NH
        N = 64 * N1
        a1 = 2 * np.pi * (np.outer(np.arange(NH), np.arange(N1)) % N1) / N1
        out["F1_%d" % L] = np.concatenate([np.cos(a1), -np.sin(a1)], 1)
        at = 2 * np.pi * (np.outer(n2, np.arange(N1)) % N) / N
        out["c_%d" % L], out["s_%d" % L] = np.cos(at), np.sin(at)
        out["cT_%d" % L], out["sT_%d" % L] = np.cos(at).T, np.sin(at).T
        ai = 2 * np.pi * (np.outer(np.arange(N1), np.arange(NH)) % N1) / N1
        out["C1_%d" % L], out["nS1_%d" % L] = np.cos(ai), -np.sin(ai)
        pos = np.arange(L, dtype=np.float32)
        t01 = (pos / np.float32(max(L - 1, 1))).astype(np.float32)
        f = np.linspace(1e-4, 15, 16, dtype=np.float32)
        ang = (f[None, :] * (np.float32(2.0 * math.pi) * pos / np.float32(L))[:, None]).astype(np.float32)
        z = np.concatenate([t01[:, None], np.cos(ang), -np.sin(ang)], -1).astype(np.float32)
        out["zT_%d" % L] = z.T
        deltas = np.linspace(math.log(1e-2) / 1.5, math.log(1e-2) / 0.3, D, dtype=np.float32)
        out["win_%d" % L] = np.exp(-t01[:, None] * np.abs(deltas)[None, :]).astype(np.float32)
    return {k_: np.ascontiguousarray(v, dtype=np.float32) for k_, v in out.items()}


def rw_const_tables():
    out = {}
    i = np.arange(128)
    same = (i[:, None] // 64) == (i[None, :] // 64)
    out["TRI0"] = (same & (i[:, None] <= i[None, :])).astype(np.float32)
    out["TRI1"] = (same & (i[:, None] >= i[None, :])).astype(np.float32)
    out["CHK"] = np.stack([(i < 64), (i >= 64)], 1).astype(np.float32)
    a = np.arange(64)
    for n in range(2):
        before = (a[:, None] < a[None, :]) if n == 0 else (a[:, None] > a[None, :])
        ateq = before | (a[:, None] == a[None, :])
        m = np.concatenate([-before.astype(np.float32), ateq.astype(np.float32), before.astype(np.float32),
                            ateq.astype(np.float32), -before.T.astype(np.float32), np.zeros((64, 192), np.float32)], 1)
        out["MASK%d" % n] = m
    out["IDN"] = np.eye(64, dtype=np.float32)
    return out


def pack_host(inputs, layers):
    spec = blob_spec(layers)
    lay = blob_layout(spec)
    src = {}
    src["ident"] = np.eye(128, dtype=np.float32)
    pv = np.zeros((128, 1024), np.float32)

    def putv(name, v):
        c0, n = PV_COLS[name]
        pv[:, c0:c0 + n] = np.asarray(v, np.float32).reshape(n, 128).T
    for i in range(DEPTH):
        putv("mod_b%d" % i, inputs["mod_b"][i])
        for j in range(2):
            putv("ln_g%d_%d" % (i, j), inputs["ln_g"][i, j])
            putv("ln_b%d_%d" % (i, j), inputs["ln_b"][i, j])
    for j in range(2):
        putv("hy_b_out%d" % j, inputs["hy_b_out"][j])
    for n in range(6):
        putv("rw_mu%d" % n, inputs["rw_mu"][0, n])
    putv("fn_bo", inputs["fn_bo"][0])
    src["pv"] = pv
    src["pos"] = grid_pos_embed()
    src["mod_w"] = inputs["mod_w"].reshape(DEPTH * D, 6 * D)
    src["moe_router"] = inputs["moe_router"].reshape(DEPTH * D, NE)
    if 0 in layers or 3 in layers:
        src["w_in"] = inputs["hy_w_in"].reshape(2 * D, 3 * D)
        src["b_in"] = inputs["hy_b_in"]
        src["conv_w"] = inputs["hy_conv_w"].reshape(6, 3 * D)
        src["conv_b"] = inputs["hy_conv_b"]
        src["f_w1"] = inputs["hy_f_w1"].reshape(66, 64)
        src["hyv"] = np.concatenate([np.stack([inputs["hy_f_b1"][j], inputs["hy_f_freq"][j],
                                               inputs["hy_f_b2"][j, 0], inputs["hy_f_b2"][j, 1]], 1) for j in range(2)], 0)
        src["f_w2"] = inputs["hy_f_w2"].reshape(256, 64)
        src["f_w3"] = inputs["hy_f_w3"].reshape(128, 4096)
        src["f_bias"] = inputs["hy_f_bias"].reshape(4, D)
        src["w_out"] = inputs["hy_w_out"].reshape(2 * D, D)
        src.update(hy_const_tables())
    if 1 in layers:
        src["wr"], src["wk"], src["wv"], src["wo"] = inputs["rw_wr"][0], inputs["rw_wk"][0], inputs["rw_wv"][0], inputs["rw_wo"][0]
        src["w1cat"] = np.concatenate([inputs["rw_w1"][0, 0], inputs["rw_w1"][0, 1]], 1)
        src["a1cat"] = np.concatenate([inputs["rw_a1"][0, 0], inputs["rw_a1"][0, 1]], 1)
        g1 = np.zeros((D, 256), np.float32); g1[:, :160] = inputs["rw_g1"][0]
        g2 = np.zeros((256, D), np.float32); g2[:160] = inputs["rw_g2"][0]
        src["g1pad"], src["g2pad"] = g1, g2
        for n in range(2):
            w2 = np.zeros((128, D), np.float32); w2[n * 64:(n + 1) * 64] = inputs["rw_w2"][0, n]
            a2 = np.zeros((128, D), np.float32); a2[n * 64:(n + 1) * 64] = inputs["rw_a2"][0, n]
            src["w2pad%d" % n], src["a2pad%d" % n] = w2, a2
        src["rows"] = np.stack([inputs["rw_w0"][0, 0], inputs["rw_w0"][0, 1], inputs["rw_a0"][0, 0], inputs["rw_a0"][0, 1],
                                inputs["rw_kk"][0], inputs["rw_ka"][0], inputs["rw_rk"][0], inputs["rw_gn_g"][0], inputs["rw_gn_b"][0]], 0)
        src.update(rw_const_tables())
    if 2 in layers:
        dk = np.outer(np.arange(128), np.arange(128)) % 128
        ang = 2.0 * np.pi * dk / 128.0
        nrm = 1.0 / math.sqrt(T * 128.0)
        src["fn_cs"] = np.concatenate([-np.cos(ang) * nrm, np.sin(ang) * nrm], 1).astype(np.float32)
        tk = (np.outer(np.arange(T, dtype=np.int64), np.arange(T, dtype=np.int64)) % T).astype(np.float64)
        src["fn_nct"] = (-np.cos(2.0 * np.pi * tk / T)).astype(np.float32)
        src["fn_nst"] = (-np.sin(2.0 * np.pi * tk / T)).astype(np.float32)
        del tk
        src["fn_wo"] = inputs["fn_wo"][0]
    blobs = {}
    for piece, (tot, ent) in lay.items():
        flat = np.zeros((tot, BW), np.float32)
        for name, (off, rows, R, C) in ent.items():
            if piece.startswith("moe"):
                i = int(piece[3:])
                a = {"w1": inputs["moe_w1"][i], "w3": inputs["moe_w3"][i], "w2": inputs["moe_w2"][i]}[name]
            else:
                a = src[name]
            a = np.ascontiguousarray(a, dtype=np.float32).reshape(-1)
            flat[off:off + rows].reshape(-1)[:a.size] = a
        blobs[piece] = flat
    return blobs, lay


class Prog:
    def __init__(self, layers, dbg=(), test=None):
        self.test = test
        import os
        self.stop_at = int(os.environ.get("STOP_AT", "0"))
        self.layers = layers
        self.dbg = list(dbg)
        nc = bass.Bass("TRN2", target_bir_lowering=False)
        self.nc = nc
        self.k = KB(nc)
        self.lay = blob_layout(blob_spec(layers))
        self.dram = {}
        self.build()

    def dt(self, name, shape, kind="Internal", dtype=F32):
        t = self.nc.dram_tensor(name, list(shape), dtype, kind=kind)
        self.dram[name] = t
        return t.ap()

    def W(self, piece, name):
        tot, ent = self.lay[piece]
        off, rows, R, C = ent[name]
        g = self.gath[piece]
        v = g[off:off + rows, :]
        if C == BW:
            return v[:R, :]
        if C < BW:
            return v.rearrange("r (a c) -> (r a) c", c=C)[:R, :]
        return v.rearrange("(r a) c -> r (a c)", a=C // BW)[:R, :]

    def build(self):
        nc, k = self.nc, self.k
        self.x_in = self.dt("x", [T, D], kind="ExternalInput")
        self.c_in = self.dt("c", [1, D], kind="ExternalInput")
        self.ctx_in = self.dt("ctx", [TC, D], kind="ExternalInput")
        self.cctx_in = self.dt("c_ctx", [1, D], kind="ExternalInput")
        self.shard = {}
        self.gath = {}
        self.bounce = {}
        for piece, (tot, ent) in self.lay.items():
            self.gath[piece] = self.dt("blob_" + piece, [tot, BW], kind="ExternalInput")
        self.out = self.dt("out", [T, D], kind="ExternalOutput")

        self.ident = k.tile([128, 128], persistent=True)
        self.pv = k.tile([128, 1024], persistent=True)
        self.ones = k.tile([128, 128], persistent=True)
        self.modv = k.tile([128, DEPTH, 48, 2], persistent=True)
        self.modp = k.tile([128, DEPTH, 48, 2], persistent=True)
        self.idxT = k.tile([128, 4, NE], I32, persistent=True)
        self.gateT = k.tile([128, 4, NE], persistent=True)
        self.phase_consts()
        if self.test == "rw":
            injc = self.dt("injc", [TC, D], kind="ExternalInput")
            injl = self.dt("inj", [T, D], kind="ExternalInput")
            HCF = self.dt("HCF", [D, TC]); HLF = self.dt("HLF", [D, T])
            Y = self.dt("YR", [D, T])
            self.transpose_in(injc, HCF, TC)
            self.transpose_in(injl, HLF, T)
            self.rwkv(HCF, HLF, Y)
            self.final_dbg()
            return
        if self.test in ("hy", "hyc"):
            L = T if self.test == "hy" else TC
            inj = self.dt("inj", [L, D], kind="ExternalInput")
            HFM = self.dt("HFM", [D, L])
            Y = self.dt("YH", [D, L])
            self.transpose_in(inj, HFM, L)
            self.hyena(0, HFM, L, Y, "t")
            self.final_dbg()
            return
        if self.test == "fnet":
            inj = self.dt("inj", [T, D], kind="ExternalInput")
            HFM = self.dt("HFM", [D, T])
            Y = self.dt("YF", [D, T])
            self.transpose_in(inj, HFM, T)
            self.fnet(HFM, Y)
            self.final_dbg()
            return
        if self.test == "moe":
            inj = self.dt("inj", [T, D], kind="ExternalInput")
            HFM = self.dt("HFM", [D, T])
            YM = self.dt("YM", [T, D])
            self.transpose_in(inj, HFM, T)
            self.moe(0, HFM, inj, YM, T)
            self.final_dbg()
            return
        self.phase_modvec()
        self.XT = self.dt("XT", [D, T])
        self.XCT = self.dt("XCT", [D, TC])
        XT, XCT = self.XT, self.XCT
        self.transpose_in(self.x_in, XT, T, add=self.W("misc", "pos"))
        self.transpose_in(self.ctx_in, XCT, TC)
        HL = self.dt("HL", [D, T]); HC = self.dt("HC", [D, TC])
        YL = self.dt("YL", [D, T]); YC = self.dt("YC", [D, TC])
        HL2 = self.dt("HL2", [D, T]); HL2TM = self.dt("HL2TM", [T, D])
        HC2 = self.dt("HC2", [D, TC]); HC2TM = self.dt("HC2TM", [TC, D])
        YM = self.dt("YM", [T, D]); YMT = self.dt("YMT", [D, T])
        YMC = self.dt("YMC", [TC, D]); YMCT = self.dt("YMCT", [D, TC])
        self.ln_pass(XT, T, layer=0, first=True, H_out=HL, modj=0, which=0)
        self.ln_pass(XCT, TC, layer=0, first=True, H_out=HC, modj=0, which=1)
        for i in range(DEPTH):
            if i not in self.layers:
                break
            kind = i % 3
            ctx_full = (i == 0)
            if kind == 0:
                self.hyena(i // 3, HL, T, YL, "l%d" % i)
                if ctx_full:
                    self.hyena(i // 3, HC, TC, YC, "c%d" % i)
            elif kind == 1:
                self.rwkv(HC, HL, YL)
            else:
                self.fnet(HL, YL)
            self.ln_pass(XT, T, layer=i, Y=YL, ymul=lambda c, i=i: self.mv(i, 2, c, 0, plus1=True), lnj=0,
                         X_out=XT, H_out=HL2, H_tm=HL2TM, modj=3, which=0)
            self.moe(i, HL2, HL2TM, YM, T)
            self.transpose_in(YM, YMT, T)
            last = (i == DEPTH - 1)
            self.ln_pass(XT, T, layer=i, Y=YMT, ymul=lambda c, i=i: self.mv(i, 5, c, 0, plus1=True), lnj=1,
                         X_out=None if last else XT, X_tm=self.out if last else None,
                         H_out=None if last else HL, do_mod=not last, modj=0, which=0, mod_layer=i + 1)
            if ctx_full:
                self.ln_pass(XCT, TC, layer=i, Y=YC, ymul=lambda c, i=i: self.mv(i, 2, c, 1, plus1=True), lnj=0,
                             X_out=XCT, H_out=HC2, H_tm=HC2TM, modj=3, which=1)
                self.moe(i, HC2, HC2TM, YMC, TC)
                self.transpose_in(YMC, YMCT, TC)
                self.ln_pass(XCT, TC, layer=i, Y=YMCT, ymul=lambda c, i=i: self.mv(i, 5, c, 1, plus1=True), lnj=1,
                             X_out=XCT, H_out=HC, modj=0, which=1, mod_layer=i + 1)
        self.final_dbg()

    def phase_consts(self):
        k = self.k
        k.begin()
        s = k.dsem()
        k.dma(self.ident[:], self.W("misc", "ident"), writes=[self.ident], sem=s)
        k.dma(self.pv[:], self.W("misc", "pv"), writes=[self.pv], sem=s)
        k.op("dve", lambda e: e.memset(self.ones[:], 1.0), writes=[self.ones])
        k.end()

    def pvc(self, name, c=None):
        c0, n = PV_COLS[name]
        if c is None:
            return self.pv[:, c0:c0 + n]
        return self.pv[:, c0 + c:c0 + c + 1]

    def phase_modvec(self):
        k = self.k
        k.begin()
        sc = k.tile([128, 8, 2])
        s = k.dsem()
        if True:
            k.dma(sc[:, :, 0], self.c_in.rearrange("o (c p) -> p (o c)", p=128), writes=[sc], sem=s, allow_slow_non_contiguous=True)
            k.dma(sc[:, :, 1], self.cctx_in.rearrange("o (c p) -> p (o c)", p=128), writes=[sc], sem=s, allow_slow_non_contiguous=True)
        k.op("act", lambda e: e.activation(out=sc[:], in_=sc[:], func=AF.Silu), reads=[sc], writes=[sc])
        mw = self.W("misc", "mod_w")
        wb = [k.tile([128, 8, 1536]) for _ in range(2)]
        ws = [k.dsem() for _ in range(2)]
        ps = [k.ptile([128, 12, 2]) for _ in range(2)]
        it = 0
        for i in range(DEPTH):
            for g in range(4):
                b = wb[it % 2]
                src = mw[i * D:(i + 1) * D, g * 1536:(g + 1) * 1536].rearrange("(c p) n -> p c n", p=128)
                k.dma(b[:], src, writes=[b], sem=ws[it % 2])
                p = ps[it % 2]
                for j in range(12):
                    for c in range(8):
                        k.op("pe", lambda e, j=j, c=c, b=b, p=p: e.matmul(
                            p[:, j, :], b[:, c, j * 128:(j + 1) * 128], sc[:, c, :],
                            start=(c == 0), stop=(c == 7)),
                            reads=[b, sc], writes=[p], accum=True)
                c0, _ = PV_COLS["mod_b%d" % i]
                bias = self.pv[:, c0 + g * 12:c0 + (g + 1) * 12]
                k.op("dve", lambda e, p=p, i=i, g=g, bias=bias: e.tensor_tensor(
                    out=self.modv[:, i, g * 12:(g + 1) * 12, :], in0=p[:],
                    in1=bias.unsqueeze(2).to_broadcast([128, 12, 2]), op=ALU.add),
                    reads=[p, self.pv], writes=[self.modv])
                it += 1
        k.op("dve", lambda e: e.tensor_scalar(out=self.modp[:], in0=self.modv[:], scalar1=1.0,
                                              scalar2=None, op0=ALU.add),
             reads=[self.modv], writes=[self.modp])
        k.end()

    def mv(self, layer, j, c, which=0, plus1=False):
        t = self.modp if plus1 else self.modv
        return t[:, layer, j * 8 + c, which:which + 1]

    def transpose_in(self, src, dstT, Tn, add=None):
        k = self.k
        k.begin()
        nt = Tn // 128
        grp = min(4, nt)
        xin = [k.tile([128, D]) for _ in range(2)]
        ain = [k.tile([128, D]) for _ in range(2)] if add is not None else None
        sx = [k.dsem() for _ in range(2)]
        sa = [k.dsem() for _ in range(2)]
        so = [k.dsem() for _ in range(2)]
        pst = [k.ptile([128, 4, 128]) for _ in range(4)]
        outb = [k.tile([128, 8, 128 * grp], nsub=2 * grp) for _ in range(2)]
        dv = dstT.rearrange("(c p) t -> p c t", p=128)
        pi = 0
        for t in range(nt):
            xb = xin[t % 2]
            k.dma(xb[:], src[t * 128:(t + 1) * 128, :], writes=[xb], sem=sx[t % 2])
            if add is not None:
                ab = ain[t % 2]
                k.dma(ab[:], add[t * 128:(t + 1) * 128, :], writes=[ab], sem=sa[t % 2])
                k.op("dve", lambda e, xb=xb, ab=ab: e.tensor_tensor(out=xb[:], in0=xb[:], in1=ab[:], op=ALU.add),
                     reads=[xb, ab], writes=[xb])
            ob = outb[(t // grp) % 2]
            tt = t % grp
            for h in range(2):
                p = pst[pi % 4]
                pi += 1
                for cc in range(4):
                    c = h * 4 + cc
                    k.op("pe", lambda e, p=p, cc=cc, c=c, xb=xb: e.transpose(
                        p[:, cc, :], xb[:, c * 128:(c + 1) * 128], self.ident[:]),
                        reads=[xb, self.ident], writes=[p], accum=True)
                dst = ob[:, h * 4:(h + 1) * 4, tt * 128:(tt + 1) * 128]
                if h == 0:
                    k.op("act", lambda e, p=p, dst=dst: e.copy(out=dst, in_=p[:]),
                         reads=[p], writes=[ob.ch(h * grp + tt)])
                else:
                    k.op("dve", lambda e, p=p, dst=dst: e.tensor_copy(out=dst, in_=p[:]),
                         reads=[p], writes=[ob.ch(h * grp + tt)])
            if tt == grp - 1:
                g = t // grp
                k.dma(dv[:, :, g * 128 * grp:(g + 1) * 128 * grp], ob[:], reads=[ob], sem=so[g % 2])
        k.end()

    def ln_pass(self, X, Tn, layer, first=False, Y=None, ymul=None, lnj=0, which=0,
                X_out=None, H_out=None, H_tm=None, X_tm=None, modj=0, do_mod=True, Y_tm=False, mod_layer=None):
        k = self.k
        if mod_layer is None:
            mod_layer = layer
        k.begin()
        TT = min(512, Tn)
        ntile = Tn // TT
        xv = X.rearrange("(c p) t -> p c t", p=128)
        xb = [k.tile([128, 8, TT], nsub=8) for _ in range(2)]
        sxs = [k.dsem() for _ in range(2)]
        if not first:
            yv = Y.rearrange("(c p) t -> p c t", p=128)
            yb = [k.tile([128, 8, TT], nsub=8) for _ in range(2)]
            sys_ = [k.dsem() for _ in range(2)]
        sq = [k.tile([128, 8, TT], nsub=8) for _ in range(2)]
        hb = [k.tile([128, 8, TT], nsub=8) for _ in range(2)] if do_mod else None
        so = [k.dsem() for _ in range(2)]
        so2 = [k.dsem() for _ in range(2)]
        st = [k.tile([128, 4, TT], nsub=3) for _ in range(2)]
        ps1 = [k.ptile([128, TT]) for _ in range(2)]
        ps2 = [k.ptile([128, TT]) for _ in range(2)]
        if X_tm is not None or H_tm is not None:
            pst = [k.ptile([128, 4, 128]) for _ in range(2)]
            tmb = [k.tile([128, D], nsub=2) for _ in range(2)]
            stm = [k.dsem() for _ in range(2)]
        self._tmi = 0

        def stats_norm(zb, sqb, sb, p1, p2, eps):
            for c in range(8):
                k.op("act", lambda e, c=c: e.activation(out=sqb[:, c, :], in_=zb[:, c, :], func=AF.Square),
                     reads=[zb.ch(c)], writes=[sqb.ch(c)])
            for c in range(8):
                k.op("pe", lambda e, c=c: e.matmul(p1[:], self.ones[:], zb[:, c, :], start=(c == 0), stop=(c == 7)),
                     reads=[self.ones, zb.ch(c)], writes=[p1], accum=True)
            for c in range(8):
                k.op("pe", lambda e, c=c: e.matmul(p2[:], self.ones[:], sqb[:, c, :], start=(c == 0), stop=(c == 7)),
                     reads=[self.ones, sqb.ch(c)], writes=[p2], accum=True)
            k.op("dve", lambda e: e.tensor_scalar(out=sb[:, 0, :], in0=p1[:], scalar1=1.0 / D, scalar2=None, op0=ALU.mult),
                 reads=[p1], writes=[sb.ch(0)])
            k.op("dve", lambda e: e.tensor_tensor(out=sb[:, 2, :], in0=sb[:, 0, :], in1=sb[:, 0, :], op=ALU.mult),
                 reads=[sb.ch(0)], writes=[sb.ch(2)])
            k.op("dve", lambda e: e.scalar_tensor_tensor(out=sb[:, 1, :], in0=p2[:], scalar=1.0 / D, in1=sb[:, 2, :],
                                                         op0=ALU.mult, op1=ALU.subtract),
                 reads=[p2, sb.ch(2)], writes=[sb.ch(1)])
            k.op("dve", lambda e: e.tensor_scalar(out=sb[:, 1, :], in0=sb[:, 1, :], scalar1=eps, scalar2=None,
                                                  op0=ALU.add),
                 reads=[sb.ch(1)], writes=[sb.ch(1)])
            k.op("act", lambda e: e.sqrt(out=sb[:, 1, :], in_=sb[:, 1, :]),
                 reads=[sb.ch(1)], writes=[sb.ch(1)])
            k.op("dve", lambda e: e.reciprocal(out=sb[:, 1, :], in_=sb[:, 1, :]),
                 reads=[sb.ch(1)], writes=[sb.ch(1)])
            for c in range(8):
                eng = "dve" if c % 2 == 0 else "pool"
                k.op(eng, lambda e, c=c: e.tensor_tensor(out=zb[:, c, :], in0=zb[:, c, :], in1=sb[:, 0, :], op=ALU.subtract),
                     reads=[zb.ch(c), sb.ch(0)], writes=[zb.ch(c)])
                k.op(eng, lambda e, c=c: e.tensor_tensor(out=zb[:, c, :], in0=zb[:, c, :], in1=sb[:, 1, :], op=ALU.mult),
                     reads=[zb.ch(c), sb.ch(1)], writes=[zb.ch(c)])

        def emit_tm(srcb, dst_tm, t0):
            for q in range(TT // 128):
                i = self._tmi
                self._tmi += 1
                tb = tmb[i % 2]
                for h in range(2):
                    p = pst[h]
                    for cc in range(4):
                        c = h * 4 + cc
                        k.op("pe", lambda e, p=p, cc=cc, c=c, q=q: e.transpose(
                            p[:, cc, :], srcb[:, c, q * 128:(q + 1) * 128], self.ident[:]),
                            reads=[srcb.ch(c), self.ident], writes=[p], accum=True)
                    if h == 0:
                        k.op("act", lambda e, p=p, tb=tb, h=h: e.copy(
                            out=tb[:, h * 512:(h + 1) * 512], in_=p[:].rearrange("p a b -> p (a b)")),
                            reads=[p], writes=[tb.ch(h)])
                    else:
                        k.op("dve", lambda e, p=p, tb=tb, h=h: e.tensor_copy(
                            out=tb[:, h * 512:(h + 1) * 512], in_=p[:].rearrange("p a b -> p (a b)")),
                            reads=[p], writes=[tb.ch(h)])
                k.dma(dst_tm[t0 + q * 128:t0 + (q + 1) * 128, :], tb[:], reads=[tb], sem=stm[i % 2])

        for t in range(ntile):
            zb = xb[t % 2]
            sl = slice(t * TT, (t + 1) * TT)
            k.dma(zb[:], xv[:, :, sl], writes=[zb], sem=sxs[t % 2])
            sqb, sb, p1, p2 = sq[t % 2], st[t % 2], ps1[t % 2], ps2[t % 2]
            if not first:
                ybb = yb[t % 2]
                if Y_tm:
                    raise NotImplementedError
                k.dma(ybb[:], yv[:, :, sl], writes=[ybb], sem=sys_[t % 2])
                for c in range(8):
                    k.op("act", lambda e, c=c: e.mul(out=zb[:, c, :], in_=zb[:, c, :], mul=float(ALPHA)),
                         reads=[zb.ch(c)], writes=[zb.ch(c)])
                    k.op("dve", lambda e, c=c: e.scalar_tensor_tensor(
                        out=zb[:, c, :], in0=ybb[:, c, :], scalar=ymul(c), in1=zb[:, c, :],
                        op0=ALU.mult, op1=ALU.add), reads=[ybb.ch(c), zb.ch(c), self.modp], writes=[zb.ch(c)])
                stats_norm(zb, sqb, sb, p1, p2, 1e-5)
                gc0, _ = PV_COLS["ln_g%d_%d" % (layer, lnj)]
                bc0, _ = PV_COLS["ln_b%d_%d" % (layer, lnj)]
                for c in range(8):
                    k.op("act", lambda e, c=c: e.activation(
                        out=zb[:, c, :], in_=zb[:, c, :], func=AF.Identity,
                        scale=self.pv[:, gc0 + c:gc0 + c + 1], bias=self.pv[:, bc0 + c:bc0 + c + 1]),
                        reads=[zb.ch(c), self.pv], writes=[zb.ch(c)])
                if X_out is not None:
                    k.dma(X_out.rearrange("(c p) t -> p c t", p=128)[:, :, sl], zb[:], reads=[zb], sem=so[t % 2])
                if X_tm is not None:
                    emit_tm(zb, X_tm, t * TT)
            if do_mod:
                h = hb[t % 2]
                for c in range(8):
                    k.op("pool", lambda e, c=c: e.tensor_copy(out=h[:, c, :], in_=zb[:, c, :]),
                         reads=[zb.ch(c)], writes=[h.ch(c)])
                stats_norm(h, sqb, sb, p1, p2, 1e-6)
                for c in range(8):
                    k.op("act", lambda e, c=c: e.activation(
                        out=h[:, c, :], in_=h[:, c, :], func=AF.Identity,
                        scale=self.mv(mod_layer, modj + 1, c, which, plus1=True),
                        bias=self.mv(mod_layer, modj, c, which)),
                        reads=[h.ch(c), self.modv, self.modp], writes=[h.ch(c)])
                if H_out is not None:
                    k.dma(H_out.rearrange("(c p) t -> p c t", p=128)[:, :, sl], h[:], reads=[h], sem=so2[t % 2])
                if H_tm is not None:
                    emit_tm(h, H_tm, t * TT)
        k.end()

    def moe(self, layer, HFM, HTM, YM, Tn):
        k, nc = self.k, self.nc
        cap = 2 * Tn // NE
        JP = min(128, cap)
        nch = cap // JP
        ntt = Tn // 128
        piece = "moe%d" % layer
        W1, W3, W2 = self.W(piece, "w1"), self.W(piece, "w3"), self.W(piece, "w2")
        idxT, gateT = self.idxT, self.gateT
        k.begin()
        rw = k.tile([128, 8, NE])
        s = k.dsem()
        rsrc = self.W("misc", "moe_router")[layer * D:(layer + 1) * D, :].rearrange("(c p) e -> p c e", p=128)
        k.dma(rw[:], rsrc, writes=[rw], sem=s)
        zt = k.tile([128, D])
        k.op("pool", lambda e: e.memset(zt[:], 0.0), writes=[zt])
        sz = k.dsem()
        for tt in range(ntt):
            k.dma(YM[tt * 128:(tt + 1) * 128, :], zt[:], reads=[zt], sem=sz)
        TT = min(512, Tn)
        hb = [k.tile([128, 8, TT]) for _ in range(2)]
        sh = [k.dsem() for _ in range(2)]
        hv = HFM.rearrange("(c p) t -> p c t", p=128)
        lg = k.ptile([128, ntt, NE])
        for t in range(Tn // TT):
            b = hb[t % 2]
            k.dma(b[:], hv[:, :, t * TT:(t + 1) * TT], writes=[b], sem=sh[t % 2])
            for q in range(TT // 128):
                tt = t * (TT // 128) + q
                for c in range(8):
                    k.op("pe", lambda e, b=b, c=c, q=q, tt=tt: e.matmul(
                        lg[:, tt, :], b[:, c, q * 128:(q + 1) * 128], rw[:, c, :], start=(c == 0), stop=(c == 7)),
                        reads=[b, rw], writes=[lg], accum=True)
        aff = k.tile([128, ntt, NE])
        mx = k.tile([128, ntt])
        k.op("dve", lambda e: e.tensor_reduce(out=mx[:], in_=lg[:], axis=AX.X, op=ALU.max), reads=[lg], writes=[mx])
        k.op("dve", lambda e: e.tensor_tensor(out=aff[:], in0=lg[:], in1=mx[:].unsqueeze(2).to_broadcast([128, ntt, NE]),
                                              op=ALU.subtract), reads=[lg, mx], writes=[aff])
        k.op("act", lambda e: e.activation(out=aff[:], in_=aff[:], func=AF.Exp), reads=[aff], writes=[aff])
        k.op("dve", lambda e: e.tensor_reduce(out=mx[:], in_=aff[:], axis=AX.X, op=ALU.add), reads=[aff], writes=[mx])
        k.op("dve", lambda e: e.reciprocal(out=mx[:], in_=mx[:]), reads=[mx], writes=[mx])
        k.op("dve", lambda e: e.tensor_tensor(out=aff[:], in0=aff[:], in1=mx[:].unsqueeze(2).to_broadcast([128, ntt, NE]),
                                              op=ALU.mult), reads=[aff, mx], writes=[aff])
        affT = k.tile([NE, Tn])
        pT = [k.ptile([NE, 4, 128]) for _ in range(2)]
        ng = (ntt + 3) // 4
        for g in range(ng):
            p = pT[g % 2]
            n = min(4, ntt - g * 4)
            for q in range(n):
                tt = g * 4 + q
                k.op("pe", lambda e, p=p, q=q, tt=tt: e.transpose(p[:, q, :], aff[:, tt, :], self.ident[:]),
                     reads=[aff, self.ident], writes=[p], accum=True)
            k.op("act", lambda e, p=p, g=g, n=n: e.copy(
                out=affT[:, g * 512:g * 512 + n * 128], in_=p[:, 0:n, :].rearrange("p a b -> p (a b)")),
                reads=[p], writes=[affT], accum=True)
        work = k.tile([NE, Tn])
        vals = k.tile([NE, cap])
        idxu = k.tile([NE, cap], U32)
        k.op("dve", lambda e: e.tensor_copy(out=work[:], in_=affT[:]), reads=[affT], writes=[work])
        for r in range(cap // 8):
            sl = slice(r * 8, (r + 1) * 8)
            k.op("dve", lambda e, sl=sl: e.max(out=vals[:, sl], in_=work[:]), reads=[work], writes=[vals], accum=True)
            k.op("dve", lambda e, sl=sl: e.max_index(out=idxu[:, sl], in_max=vals[:, sl], in_values=work[:]),
                 reads=[work, vals], writes=[idxu], accum=True)
            k.op("dve", lambda e, sl=sl: e.match_replace(out=work[:], in_to_replace=vals[:, sl], in_values=work[:],
                                                         imm_value=-1.0), reads=[vals, work], writes=[work])
        idxf = k.tile([NE, cap])
        k.op("dve", lambda e: e.tensor_copy(out=idxf[:], in_=idxu[:]), reads=[idxu], writes=[idxf])
        pI = k.ptile([128, nch, NE])
        pG = k.ptile([128, nch, NE])
        for ch in range(nch):
            k.op("pe", lambda e, ch=ch: e.transpose(pI[:JP, ch, :], idxf[:, ch * JP:(ch + 1) * JP], self.ident[:NE, :NE]),
                 reads=[idxf, self.ident], writes=[pI], accum=True)
            k.op("pe", lambda e, ch=ch: e.transpose(pG[:JP, ch, :], vals[:, ch * JP:(ch + 1) * JP], self.ident[:NE, :NE]),
                 reads=[vals, self.ident], writes=[pG], accum=True)
        k.op("dve", lambda e: e.tensor_copy(out=idxT[:JP, :nch, :], in_=pI[:JP]), reads=[pI], writes=[idxT])
        k.op("dve", lambda e: e.tensor_copy(out=gateT[:JP, :nch, :], in_=pG[:JP]), reads=[pG], writes=[gateT])
        k.end()
        k.begin()
        Xe = k.tile([128, nch, D], nsub=nch)
        XeT = k.tile([128, 8, cap], F32R, nsub=8)
        heT = k.tile([128, 16, cap], F32R, nsub=16)
        Ye = k.tile([128, nch, D], nsub=nch)
        w1s = [k.tile([128, 8, 256]) for _ in range(1)]
        w3s = [k.tile([128, 8, 256]) for _ in range(1)]
        w2s = [k.tile([128, 16, 256]) for _ in range(1)]
        w1t = [k.tile([128, 8, 256], F32R) for _ in range(2)]
        w3t = [k.tile([128, 8, 256], F32R) for _ in range(2)]
        w2t = [k.tile([128, 16, 256], F32R) for _ in range(1)]
        tmp = [k.tile([128, cap]) for _ in range(2)]
        s1 = [k.dsem() for _ in range(2)]
        s3 = [k.dsem() for _ in range(2)]
        s2 = [k.dsem() for _ in range(2)]
        sg = k.dsem()
        ss = k.dsem()
        ph1 = [k.ptile([128, cap]) for _ in range(2)]
        ph3 = [k.ptile([128, cap]) for _ in range(2)]
        py = [k.ptile([128, 512]) for _ in range(2)]
        pt = [k.ptile([128, 4, 128]) for _ in range(2)]
        i1 = i2 = ip = iy = 0
        for ex in range(NE):
            for ch in range(nch):
                k.dma(None, None, reads=[idxT], writes=[Xe.ch(ch)], sem=sg, q="pool",
                      fn=lambda e, ch=ch, ex=ex: e.indirect_dma_start(
                          out=Xe[:JP, ch, :], out_offset=None, in_=HTM[:, :],
                          in_offset=bass.IndirectOffsetOnAxis(ap=idxT[:JP, ch, ex:ex + 1], axis=0)))
            for dc in range(8):
                for ch in range(nch):
                    if ch % 4 == 0:
                        p = pt[ip % 2]
                        ip += 1
                    k.op("pe", lambda e, p=p, ch=ch, dc=dc: e.transpose(
                        p[:, ch % 4, :JP], Xe[:JP, ch, dc * 128:(dc + 1) * 128], self.ident[:JP, :JP]),
                        reads=[Xe.ch(ch), self.ident], writes=[p], accum=True)
                    if ch % 4 == 3 or ch == nch - 1:
                        c0 = (ch // 4) * 4
                        n = ch - c0 + 1
                        eng = "act" if dc % 2 == 0 else "dve"
                        if eng == "act":
                            k.op("act", lambda e, p=p, dc=dc, c0=c0, n=n: e.copy(
                                out=XeT[:, dc, c0 * JP:(c0 + n) * JP].rearrange("p (a b) -> p a b", a=n),
                                in_=p[:, 0:n, :JP]), reads=[p], writes=[XeT.ch(dc)])
                        else:
                            k.op("dve", lambda e, p=p, dc=dc, c0=c0, n=n: e.tensor_copy(
                                out=XeT[:, dc, c0 * JP:(c0 + n) * JP].rearrange("p (a b) -> p a b", a=n),
                                in_=p[:, 0:n, :JP]), reads=[p], writes=[XeT.ch(dc)])
            for g in range(8):
                a1, a3 = w1t[i1 % 2], w3t[i1 % 2]
                b1, b3 = w1s[0], w3s[0]
                r0 = ex * D
                k.dma(b1[:], W1[r0:r0 + D, g * 256:(g + 1) * 256].rearrange("(c p) n -> p c n", p=128),
                      writes=[b1], sem=s1[0], q="sp")
                k.dma(b3[:], W3[r0:r0 + D, g * 256:(g + 1) * 256].rearrange("(c p) n -> p c n", p=128),
                      writes=[b3], sem=s3[0], q="sp")
                k.rnd("pool", a1[:], b1[:], [b1], [a1])
                k.rnd("pool", a3[:], b3[:], [b3], [a3])
                i1 += 1
                for fl in range(2):
                    fc = g * 2 + fl
                    p1, p3 = ph1[fc % 2], ph3[fc % 2]
                    for dc in range(8):
                        k.op("pe", lambda e, p1=p1, a1=a1, dc=dc, fl=fl: e.matmul(
                            p1[:], a1[:, dc, fl * 128:(fl + 1) * 128], XeT[:, dc, :], start=(dc == 0), stop=(dc == 7)),
                            reads=[a1, XeT.ch(dc)], writes=[p1], accum=True)
                    for dc in range(8):
                        k.op("pe", lambda e, p3=p3, a3=a3, dc=dc, fl=fl: e.matmul(
                            p3[:], a3[:, dc, fl * 128:(fl + 1) * 128], XeT[:, dc, :], start=(dc == 0), stop=(dc == 7)),
                            reads=[a3, XeT.ch(dc)], writes=[p3], accum=True)
                    tb = tmp[fc % 2]
                    k.op("act", lambda e, tb=tb, p1=p1: e.activation(out=tb[:], in_=p1[:], func=AF.Silu),
                         reads=[p1], writes=[tb])
                    k.op("dve", lambda e, tb=tb, p3=p3, fc=fc: e.tensor_tensor(
                        out=heT[:, fc, :], in0=tb[:], in1=p3[:], op=ALU.mult),
                        reads=[tb, p3], writes=[heT.ch(fc)])
            for q in range(4):
                a2 = w2t[0]
                b2 = w2s[0]
                r0 = ex * FF
                k.dma(b2[:], W2[r0:r0 + FF, q * 256:(q + 1) * 256].rearrange("(c p) n -> p c n", p=128),
                      writes=[b2], sem=s2[0], q="sp")
                k.rnd("pool", a2[:], b2[:], [b2], [a2])
                i2 += 1
                for ch in range(nch):
                    p = py[iy % 2]
                    iy += 1
                    for fc in range(16):
                        k.op("pe", lambda e, p=p, a2=a2, fc=fc, ch=ch: e.matmul(
                            p[:JP, 0:256], heT[:, fc, ch * JP:(ch + 1) * JP], a2[:, fc, :],
                            start=(fc == 0), stop=(fc == 15)),
                            reads=[a2, heT.ch(fc)], writes=[p], accum=True)
                    k.op("act", lambda e, p=p, ch=ch, q=q, ex=ex: e.activation(
                        out=Ye[:JP, ch, q * 256:(q + 1) * 256], in_=p[:JP, 0:256], func=AF.Identity,
                        scale=gateT[:JP, ch, ex:ex + 1]),
                        reads=[p, gateT], writes=[Ye.ch(ch)], accum=True)
            for ch in range(nch):
                k._wait("pool", [(ss, k.cnt[ss])])
                k.dma(None, None, reads=[idxT, Ye.ch(ch)], sem=ss, q="pool",
                      fn=lambda e, ch=ch, ex=ex: e.indirect_dma_start(
                          out=YM[:, :], out_offset=bass.IndirectOffsetOnAxis(ap=idxT[:JP, ch, ex:ex + 1], axis=0),
                          in_=Ye[:JP, ch, :], in_offset=None, compute_op=ALU.add))
        k.end()

    def gemm(self, XT, Wap, K, N, Tn, out, bias_pv=None, act=None, out_tm=False, bias_row=None):
        k = self.k
        k.begin()
        KC = K // 128
        wt = k.tile([128, KC, N], F32R, nsub=KC)
        ws = [k.tile([128, N]) for _ in range(2)]
        sw = [k.dsem() for _ in range(2)]
        for kc in range(KC):
            w_ = ws[kc % 2]
            k.dma(w_[:], Wap[kc * 128:(kc + 1) * 128, :], writes=[w_], sem=sw[kc % 2])
            k.rnd(("pool", "act")[kc % 2], wt[:, kc, :], w_[:], [w_], [wt.ch(kc)])
        TT = 512 if Tn % 512 == 0 else 256
        xv = XT.rearrange("(c p) t -> p c t", p=128)
        xs = [k.tile([128, KC, TT]) for _ in range(2)]
        xb = [k.tile([128, KC, TT], F32R) for _ in range(2)]
        sx = [k.dsem() for _ in range(2)]
        so = [k.dsem() for _ in range(2)]
        ps = [k.ptile([128, 512]) for _ in range(4)]
        func = act if act is not None else AF.Identity
        ip = io = 0
        if not out_tm:
            ov = out.rearrange("(c p) t -> p c t", p=128)
            ob = [k.tile([128, 4, TT], nsub=4) for _ in range(2)]
            for t in range(Tn // TT):
                x_ = xs[t % 2]
                b = xb[t % 2]
                k.dma(x_[:], xv[:, :, t * TT:(t + 1) * TT], writes=[x_], sem=sx[t % 2])
                k.rnd("pool", b[:], x_[:], [x_], [b])
                for n in range(N // 128):
                    p = ps[ip % 4]
                    ip += 1
                    for kc in range(KC):
                        k.op("pe", lambda e, p=p, kc=kc, n=n, b=b: e.matmul(
                            p[:, :TT], wt[:, kc, n * 128:(n + 1) * 128], b[:, kc, :], start=(kc == 0), stop=(kc == KC - 1)),
                            reads=[wt.ch(kc), b], writes=[p], accum=True)
                    o = ob[io % 2]
                    if bias_pv is not None:
                        c0, _ = PV_COLS[bias_pv]
                        k.op("act", lambda e, p=p, o=o, n=n, c0=c0: e.activation(
                            out=o[:, n % 4, :], in_=p[:, :TT], func=func, bias=self.pv[:, c0 + n:c0 + n + 1]),
                            reads=[p, self.pv], writes=[o.ch(n % 4)])
                    else:
                        k.op("act", lambda e, p=p, o=o, n=n: e.activation(out=o[:, n % 4, :], in_=p[:, :TT], func=func),
                             reads=[p], writes=[o.ch(n % 4)])
                    if n % 4 == 3 or n == N // 128 - 1:
                        n0 = (n // 4) * 4
                        k.dma(ov[:, n0:n + 1, t * TT:(t + 1) * TT], o[:, 0:n - n0 + 1, :], reads=[o], sem=so[io % 2])
                        io += 1
        else:
            brow = None
            if bias_row is not None:
                brow = k.tile([128, N])
                sb_ = k.dsem()
                k.dma(brow[:], bias_row.partition_broadcast(128), writes=[brow], sem=sb_)
            ob = [k.tile([128, 512]) for _ in range(2)]
            for t in range(Tn // TT):
                x_ = xs[t % 2]
                b = xb[t % 2]
                k.dma(x_[:], xv[:, :, t * TT:(t + 1) * TT], writes=[x_], sem=sx[t % 2])
                k.rnd("pool", b[:], x_[:], [x_], [b])
                for q in range(TT // 128):
                    for n in range(N // 512):
                        p = ps[ip % 4]
                        ip += 1
                        for kc in range(KC):
                            k.op("pe", lambda e, p=p, kc=kc, n=n, b=b, q=q: e.matmul(
                                p[:], b[:, kc, q * 128:(q + 1) * 128], wt[:, kc, n * 512:(n + 1) * 512],
                                start=(kc == 0), stop=(kc == KC - 1)), reads=[wt.ch(kc), b], writes=[p], accum=True)
                        o = ob[io % 2]
                        if brow is not None:
                            k.op("dve", lambda e, p=p, o=o, n=n: e.tensor_tensor(
                                out=o[:], in0=p[:], in1=brow[:, n * 512:(n + 1) * 512], op=ALU.add),
                                reads=[p, brow], writes=[o])
                            if act is not None:
                                k.op("act", lambda e, o=o: e.activation(out=o[:], in_=o[:], func=act), reads=[o], writes=[o])
                        else:
                            k.op("act", lambda e, p=p, o=o: e.activation(out=o[:], in_=p[:], func=func), reads=[p], writes=[o])
                        r0 = t * TT + q * 128
                        k.dma(out[r0:r0 + 128, n * 512:(n + 1) * 512], o[:], reads=[o], sem=so[io % 2])
                        io += 1
        k.end()

    def fnet(self, HL, Y):
        k = self.k
        PQ = self.dt("fn_PQ", [2, T, D])
        MX = self.dt("fn_MX", [D, T])
        k.begin()
        cs0 = k.tile([128, 256])
        cs = k.tile([128, 256], F32R)
        s0 = k.dsem()
        k.dma(cs0[:], self.W("fnet", "fn_cs"), writes=[cs0], sem=s0)
        k.rnd("dve", cs[:], cs0[:], [cs0], [cs])
        hv = HL.rearrange("(c p) t -> p c t", p=128)
        hb0 = [k.tile([128, 8, 512]) for _ in range(2)]
        hb = [k.tile([128, 8, 512], F32R) for _ in range(2)]
        sh = [k.dsem() for _ in range(2)]
        ps = [k.ptile([128, 2, 256]) for _ in range(4)]
        pb = [k.tile([128, D], nsub=4) for _ in range(2)]
        qb = [k.tile([128, D], nsub=4) for _ in range(2)]
        so = [k.dsem() for _ in range(2)]
        ip = io = 0
        for t in range(T // 512):
            b0 = hb0[t % 2]
            b = hb[t % 2]
            k.dma(b0[:], hv[:, :, t * 512:(t + 1) * 512], writes=[b0], sem=sh[t % 2])
            k.rnd("pool", b[:], b0[:], [b0], [b])
            for q in range(4):
                po, qo = pb[io % 2], qb[io % 2]
                for g2 in range(4):
                    p = ps[ip % 4]
                    ip += 1
                    for gl in range(2):
                        g = g2 * 2 + gl
                        k.op("pe", lambda e, p=p, gl=gl, g=g, b=b, q=q: e.matmul(
                            p[:, gl, :], b[:, g, q * 128:(q + 1) * 128], cs[:], start=True, stop=True),
                            reads=[b, cs], writes=[p], accum=True)
                    if g2 % 2 == 0:
                        k.op("act", lambda e, p=p, po=po, g2=g2: e.copy(
                            out=po[:, g2 * 256:(g2 + 1) * 256].rearrange("p (a b) -> p a b", a=2), in_=p[:, :, 0:128]),
                            reads=[p], writes=[po.ch(g2)])
                        k.op("act", lambda e, p=p, qo=qo, g2=g2: e.copy(
                            out=qo[:, g2 * 256:(g2 + 1) * 256].rearrange("p (a b) -> p a b", a=2), in_=p[:, :, 128:256]),
                            reads=[p], writes=[qo.ch(g2)])
                    else:
                        k.op("dve", lambda e, p=p, po=po, g2=g2: e.tensor_copy(
                            out=po[:, g2 * 256:(g2 + 1) * 256].rearrange("p (a b) -> p a b", a=2), in_=p[:, :, 0:128]),
                            reads=[p], writes=[po.ch(g2)])
                        k.op("dve", lambda e, p=p, qo=qo, g2=g2: e.tensor_copy(
                            out=qo[:, g2 * 256:(g2 + 1) * 256].rearrange("p (a b) -> p a b", a=2), in_=p[:, :, 128:256]),
                            reads=[p], writes=[qo.ch(g2)])
                r0 = t * 512 + q * 128
                k.dma(PQ[0, r0:r0 + 128, :], po[:], reads=[po], sem=so[io % 2])
                k.dma(PQ[1, r0:r0 + 128, :], qo[:], reads=[qo], sem=so[io % 2])
                io += 1
        k.end()
        if self.test == "fnet" and self.stop_at == 1:
            return
        k.begin()
        CT, ST = self.W("fnet", "fn_nct"), self.W("fnet", "fn_nst")
        ps = [k.ptile([128, 512]) for _ in range(8)]
        pt0 = [k.tile([128, 512]) for _ in range(3)]
        qt0 = [k.tile([128, 512]) for _ in range(3)]
        ct0 = [k.tile([128, 512]) for _ in range(3)]
        st0 = [k.tile([128, 512]) for _ in range(3)]
        pt = [k.tile([128, 512], F32R) for _ in range(3)]
        qt = [k.tile([128, 512], F32R) for _ in range(3)]
        ct = [k.tile([128, 512], F32R) for _ in range(3)]
        st_ = [k.tile([128, 512], F32R) for _ in range(3)]
        sp_ = [k.dsem() for _ in range(3)]
        sq_ = [k.dsem() for _ in range(3)]
        sc_ = [k.dsem() for _ in range(3)]
        ss_ = [k.dsem() for _ in range(3)]
        ob = [k.tile([128, 4, 512], nsub=4) for _ in range(2)]
        so = [k.dsem() for _ in range(2)]
        mv_ = MX.rearrange("(c p) t -> p c t", p=128)
        it = 0
        io = 0
        for kb in range(T // 512):
            for half in range(2):
                pp = [ps[(io % 2) * 4 + j] for j in range(4)]
                for tt in range(T // 128):
                    i = it % 3
                    it += 1
                    k.dma(pt0[i][:], PQ[0, tt * 128:(tt + 1) * 128, half * 512:(half + 1) * 512], writes=[pt0[i]], sem=sp_[i], q="sp")
                    k.dma(qt0[i][:], PQ[1, tt * 128:(tt + 1) * 128, half * 512:(half + 1) * 512], writes=[qt0[i]], sem=sq_[i], q="sp")
                    k.dma(ct0[i][:], CT[tt * 128:(tt + 1) * 128, kb * 512:(kb + 1) * 512], writes=[ct0[i]], sem=sc_[i], q="sp")
                    k.dma(st0[i][:], ST[tt * 128:(tt + 1) * 128, kb * 512:(kb + 1) * 512], writes=[st0[i]], sem=ss_[i], q="sp")
                    k.rnd("pool", pt[i][:], pt0[i][:], [pt0[i]], [pt[i]])
                    k.rnd("dve", qt[i][:], qt0[i][:], [qt0[i]], [qt[i]])
                    k.rnd("pool", ct[i][:], ct0[i][:], [ct0[i]], [ct[i]])
                    k.rnd("act", st_[i][:], st0[i][:], [st0[i]], [st_[i]])
                    for j in range(4):
                        k.op("pe", lambda e, j=j, i=i, tt=tt: e.matmul(
                            pp[j][:], pt[i][:, j * 128:(j + 1) * 128], ct[i][:], start=(tt == 0), stop=False),
                            reads=[pt[i], ct[i]], writes=[pp[j]], accum=True)
                        k.op("pe", lambda e, j=j, i=i, tt=tt: e.matmul(
                            pp[j][:], qt[i][:, j * 128:(j + 1) * 128], st_[i][:], start=False, stop=(tt == T // 128 - 1)),
                            reads=[qt[i], st_[i]], writes=[pp[j]], accum=True)
                o = ob[io % 2]
                for j in range(4):
                    if j % 2 == 0:
                        k.op("act", lambda e, j=j, o=o: e.copy(out=o[:, j, :], in_=pp[j][:]), reads=[pp[j]], writes=[o.ch(j)])
                    else:
                        k.op("dve", lambda e, j=j, o=o: e.tensor_copy(out=o[:, j, :], in_=pp[j][:]), reads=[pp[j]], writes=[o.ch(j)])
                k.dma(mv_[:, half * 4:(half + 1) * 4, kb * 512:(kb + 1) * 512], o[:], reads=[o], sem=so[io % 2])
                io += 1
        k.end()
        if self.test == "fnet" and self.stop_at == 2:
            return
        self.gemm(MX, self.W("fnet", "fn_wo"), D, D, T, Y, bias_pv="fn_bo")

    def hy_tabs(self, L, names):
        k = self.k
        NH = L // 64
        N1 = 2 * NH
        shp = {"F1": (NH, 2 * N1), "c": (64, N1), "s": (64, N1), "cT": (N1, 64), "sT": (N1, 64),
               "C2": (64, 64), "S2": (64, 64), "nS2": (64, 64), "RA": (64, 128), "RB": (64, 128),
               "C1": (N1, NH), "nS1": (N1, NH)}
        out = {}
        sm = k.dsem()
        for nm in names:
            r, c = shp[nm]
            t = k.tile([max(r, 1), c])
            key = nm if nm in ("C2", "S2", "nS2", "RA", "RB") else "%s_%d" % (nm, L)
            k.dma(t[:], self.W("hy", key), writes=[t], sem=sm)
            out[nm] = t
        return out

    def hy_filters(self, j, L, HF, RN):
        k = self.k
        N = 2 * L
        k.begin()
        s0 = k.dsem()
        zT = k.tile([33, L])
        w1 = k.tile([33, 64])
        w2 = k.tile([64, 2, 64])
        w3 = k.tile([64, 4096])
        hv = k.tile([64, 4])
        k.dma(zT[:], self.W("hy", "zT_%d" % L), writes=[zT], sem=s0)
        k.dma(w1[:], self.W("hy", "f_w1")[j * 33:(j + 1) * 33, :], writes=[w1], sem=s0)
        for n in range(2):
            k.dma(w2[:, n, :], self.W("hy", "f_w2")[(j * 2 + n) * 64:(j * 2 + n + 1) * 64, :], writes=[w2], sem=s0)
        k.dma(w3[:], self.W("hy", "f_w3")[j * 64:(j + 1) * 64, :], writes=[w3], sem=s0)
        k.dma(hv[:], self.W("hy", "hyv")[j * 64:(j + 1) * 64, :], writes=[hv], sem=s0)
        a = [k.tile([64, L]) for _ in range(3)]
        ps = [k.ptile([128, 512]) for _ in range(2)]
        tmp = [k.tile([64, 512]) for _ in range(2)]
        tmpi = [k.tile([64, 512], I32) for _ in range(2)]
        tmpf = [k.tile([64, 512]) for _ in range(2)]
        CT = min(512, L)
        it = 0
        for layer in range(3):
            for ct in range(L // CT):
                p = ps[it % 2]
                tb = tmp[it % 2]
                it += 1
                sl = slice(ct * CT, (ct + 1) * CT)
                if layer == 0:
                    k.op("pe", lambda e, p=p, sl=sl: e.matmul(p[:64, :CT], w1[:, :], zT[:, sl], start=True, stop=True),
                         reads=[w1, zT], writes=[p])
                else:
                    src = a[layer - 1]
                    k.op("pe", lambda e, p=p, sl=sl, src=src, layer=layer: e.matmul(
                        p[:64, :CT], w2[:, layer - 1, :], src[:, sl], start=True, stop=True),
                        reads=[w2, src], writes=[p])
                bcol = hv[:, 0:1] if layer == 0 else hv[:, 1 + layer:2 + layer]
                k.op("dve", lambda e, p=p, tb=tb, bcol=bcol: e.tensor_scalar(
                    out=tb[:, :CT], in0=p[:64, :CT], scalar1=bcol, scalar2=hv[:, 1:2], op0=ALU.add, op1=ALU.mult),
                    reads=[p, hv], writes=[tb])
                k.op("dve", lambda e, tb=tb: e.tensor_scalar(
                    out=tb[:, :CT], in0=tb[:, :CT], scalar1=1.0 / (2 * math.pi), scalar2=16.0, op0=ALU.mult, op1=ALU.add),
                    reads=[tb], writes=[tb])
                ti, tf = tmpi[it % 2], tmpf[it % 2]
                k.op("dve", lambda e, tb=tb, ti=ti: e.tensor_copy(out=ti[:, :CT], in_=tb[:, :CT]), reads=[tb], writes=[ti])
                k.op("dve", lambda e, tf=tf, ti=ti: e.tensor_copy(out=tf[:, :CT], in_=ti[:, :CT]), reads=[ti], writes=[tf])
                k.op("dve", lambda e, tb=tb, tf=tf: e.tensor_tensor(out=tb[:, :CT], in0=tb[:, :CT], in1=tf[:, :CT], op=ALU.subtract),
                     reads=[tb, tf], writes=[tb])
                k.op("dve", lambda e, tb=tb, tf=tf: e.scalar_tensor_tensor(
                    out=tf[:, :CT], in0=tb[:, :CT], scalar=0.5, in1=tb[:, :CT], op0=ALU.is_gt, op1=ALU.subtract),
                    reads=[tb], writes=[tf])
                dst = a[layer]
                k.op("act", lambda e, tf=tf, dst=dst, sl=sl: e.activation(
                    out=dst[:, sl], in_=tf[:, :CT], func=AF.Sin, scale=-2 * math.pi),
                    reads=[tf], writes=[dst], accum=True)
        a3 = a[2]
        win = [k.tile([128, 512]) for _ in range(2)]
        sw = [k.dsem() for _ in range(2)]
        hs = [k.tile([128, 512]) for _ in range(2)]
        ha = [k.tile([128, 512]) for _ in range(2)]
        so = [k.dsem() for _ in range(2)]
        pn = k.ptile([128, 512])
        nrm = k.tile([128, 8, 512], nsub=8)
        WIN = self.W("hy", "win_%d" % L)
        LT = min(128, L)
        nlt = L // LT
        it = 0
        for ct in range(8):
            dcol = (ct % 2) * 512
            for lt in range(nlt):
                i = it % 2
                it += 1
                p = ps[i]
                k.dma(win[i][:LT, :], WIN[lt * LT:(lt + 1) * LT, dcol:dcol + 512], writes=[win[i]], sem=sw[i])
                k.op("pe", lambda e, p=p, lt=lt, ct=ct: e.matmul(
                    p[:LT, :], a3[:, lt * LT:(lt + 1) * LT], w3[:, ct * 512:(ct + 1) * 512], start=True, stop=True),
                    reads=[a3, w3], writes=[p])
                k.op("dve", lambda e, p=p, i=i: e.tensor_tensor(out=hs[i][:LT, :], in0=p[:LT, :], in1=win[i][:LT, :], op=ALU.mult),
                     reads=[p, win[i]], writes=[hs[i]])
                if ct >= 4 and lt == 0:
                    k.op("dve", lambda e, i=i: e.memset(hs[i][0:1, :], 0.0), reads=[hs[i]], writes=[hs[i]])
                k.dma(HF[lt * LT:(lt + 1) * LT, ct * 512:(ct + 1) * 512], hs[i][:LT, :], reads=[hs[i]], sem=so[i])
                k.op("act", lambda e, i=i: e.activation(out=ha[i][:LT, :], in_=hs[i][:LT, :], func=AF.Abs),
                     reads=[hs[i]], writes=[ha[i]])
                k.op("pe", lambda e, i=i, lt=lt: e.matmul(pn[:, :], self.ones[:LT, :], ha[i][:LT, :],
                                                          start=(lt == 0), stop=(lt == nlt - 1)),
                     reads=[self.ones, ha[i]], writes=[pn], accum=True)
            k.op("act", lambda e, ct=ct: e.copy(out=nrm[:, ct, :], in_=pn[:, :]), reads=[pn], writes=[nrm.ch(ct)])
        rn = k.tile([128, 8, 512], nsub=8)
        for q in range(4):
            k.op("dve", lambda e, q=q: e.tensor_tensor(out=rn[:, q, :], in0=nrm[:, q, :], in1=nrm[:, q + 4, :], op=ALU.add),
                 reads=[nrm.ch(q), nrm.ch(q + 4)], writes=[rn.ch(q)])
            k.op("dve", lambda e, q=q: e.tensor_scalar(out=rn[:, q, :], in0=rn[:, q, :], scalar1=float(N), scalar2=None, op0=ALU.mult),
                 reads=[rn.ch(q)], writes=[rn.ch(q)])
            k.op("dve", lambda e, q=q: e.reciprocal(out=rn[:, q, :], in_=rn[:, q, :]), reads=[rn.ch(q)], writes=[rn.ch(q)])
            k.op("pool", lambda e, q=q: e.tensor_copy(out=rn[:, q + 4, :], in_=rn[:, q, :]), reads=[rn.ch(q)], writes=[rn.ch(q + 4)])
        sr = k.dsem()
        k.dma(RN[0:1, :], rn[0:1, :, :].rearrange("p a b -> p (a b)"), reads=[rn], sem=sr)
        k.end()

    def _fft_fwd(self, L, tb, zs, g0, G, A, Bt, X):
        k = self.k
        NH = L // 64
        N1 = 2 * NH
        for g in range(G):
            k.op("pe", lambda e, g=g: e.matmul(A[:64, g, 0:2 * N1], zs[:NH, :, g0 + g], tb["F1"][:NH, :],
                                               start=True, stop=True),
                 reads=[zs, tb["F1"]], writes=[A], accum=True)
        Ar, Ai = A[:64, :G, 0:N1], A[:64, :G, N1:2 * N1]
        cb = tb["c"][:, :].unsqueeze(1).to_broadcast([64, G, N1])
        sb = tb["s"][:, :].unsqueeze(1).to_broadcast([64, G, N1])
        t1, t2, t3, t4, Br, Bi = Bt
        v = lambda t: t[:64, :G * N1].rearrange("p (g n) -> p g n", g=G)
        k.op("dve", lambda e: e.tensor_tensor(out=v(t1), in0=Ar, in1=cb, op=ALU.mult), reads=[A, tb["c"]], writes=[t1])
        k.op("dve", lambda e: e.tensor_tensor(out=v(t2), in0=Ai, in1=sb, op=ALU.mult), reads=[A, tb["s"]], writes=[t2])
        k.op("dve", lambda e: e.tensor_tensor(out=v(t3), in0=Ai, in1=cb, op=ALU.mult), reads=[A, tb["c"]], writes=[t3])
        k.op("dve", lambda e: e.tensor_tensor(out=v(t4), in0=Ar, in1=sb, op=ALU.mult), reads=[A, tb["s"]], writes=[t4])
        k.op("pool", lambda e: e.tensor_tensor(out=Br[:64, :G * N1], in0=t1[:64, :G * N1], in1=t2[:64, :G * N1], op=ALU.add),
             reads=[t1, t2], writes=[Br])
        k.op("pool", lambda e: e.tensor_tensor(out=Bi[:64, :G * N1], in0=t3[:64, :G * N1], in1=t4[:64, :G * N1], op=ALU.subtract),
             reads=[t3, t4], writes=[Bi])
        W_ = G * N1
        k.op("pe", lambda e: e.matmul(X["r"][:64, :W_], tb["C2"][:, :], Br[:64, :W_], start=True, stop=False),
             reads=[tb["C2"], Br], writes=[X["r"]], accum=True)
        k.op("pe", lambda e: e.matmul(X["r"][:64, :W_], tb["S2"][:, :], Bi[:64, :W_], start=False, stop=True),
             reads=[tb["S2"], Bi], writes=[X["r"]], accum=True)
        k.op("pe", lambda e: e.matmul(X["i"][:64, :W_], tb["C2"][:, :], Bi[:64, :W_], start=True, stop=False),
             reads=[tb["C2"], Bi], writes=[X["i"]], accum=True)
        k.op("pe", lambda e: e.matmul(X["i"][:64, :W_], tb["nS2"][:, :], Br[:64, :W_], start=False, stop=True),
             reads=[tb["nS2"], Br], writes=[X["i"]], accum=True)

    def hy_filter_fft(self, L, HF, RN, SPEC):
        k = self.k
        NH = L // 64
        N1 = 2 * NH
        G = 512 // N1
        k.begin()
        tb = self.hy_tabs(L, ["F1", "c", "s", "C2", "S2", "nS2"])
        rnb = k.tile([64, 4096])
        s0 = k.dsem()
        k.dma(rnb[:], RN[0:1, :].partition_broadcast(64), writes=[rnb], sem=s0)
        zs = [k.tile([64, 64, 128]) for _ in range(2)]
        sz = [k.dsem() for _ in range(2)]
        A = k.ptile([128, G, 2 * N1])
        X = {"r": k.ptile([128, 512]), "i": k.ptile([128, 512])}
        Bt = [k.tile([64, 512]) for _ in range(6)]
        xo = [[k.tile([64, 512]) for _ in range(2)] for _ in range(2)]
        so = [k.dsem() for _ in range(2)]
        hv = HF.rearrange("(a b) c -> a b c", b=64)
        io = 0
        for cb in range(32):
            z = zs[cb % 2]
            k.dma(z[:NH, :, :], hv[:, :, cb * 128:(cb + 1) * 128], writes=[z], sem=sz[cb % 2])
            for sb in range(128 // G):
                g0 = sb * G
                ch0 = cb * 128 + g0
                self._fft_fwd(L, tb, z, g0, G, A, Bt, X)
                rv = rnb[:, ch0:ch0 + G].unsqueeze(2).to_broadcast([64, G, N1])
                for ri, nm in enumerate(("r", "i")):
                    o = xo[io % 2][ri]
                    k.op("dve", lambda e, o=o, nm=nm, rv=rv: e.tensor_tensor(
                        out=o[:, :G * N1].rearrange("p (g n) -> p g n", g=G),
                        in0=X[nm][:64, :G * N1].rearrange("p (g n) -> p g n", g=G), in1=rv, op=ALU.mult),
                        reads=[X[nm], rnb], writes=[o])
                    k.dma(SPEC[ri, :, ch0:ch0 + G, :], o[:, :G * N1].rearrange("p (g n) -> p g n", g=G),
                          reads=[o], sem=so[io % 2])
                io += 1
        k.end()

    def hy_conv(self, L, Z, zc0, XG, xc0, SPEC, o, bias_ap, OUT):
        k = self.k
        NH = L // 64
        N1 = 2 * NH
        G = 4 if N1 == 128 else 8
        k.begin()
        tb = self.hy_tabs(L, ["F1", "c", "s", "C2", "S2", "nS2", "RA", "RB", "cT", "sT", "C1", "nS1"])
        brow = k.tile([64, 1024])
        s0 = k.dsem()
        k.dma(brow[:], bias_ap.partition_broadcast(64), writes=[brow], sem=s0)
        zs = [k.tile([64, 64, 128]) for _ in range(2)]
        xs = [k.tile([64, 64, 128]) for _ in range(2)]
        sz = [k.dsem() for _ in range(2)]
        sx = [k.dsem() for _ in range(2)]
        so = [k.dsem() for _ in range(2)]
        A = k.ptile([128, G, 2 * N1])
        X = {"r": k.ptile([128, 512]), "i": k.ptile([128, 512])}
        Zp = k.ptile([128, G, 128])
        yp = k.ptile([128, 512])
        Bt = [k.tile([64, 512]) for _ in range(6)]
        Kt = [[k.tile([64, 512]) for _ in range(4)] for _ in range(2)]
        sk = [k.dsem() for _ in range(2)]
        Kc = [k.tile([64, 512]) for _ in range(2)]
        Yt = [k.tile([64, 512]) for _ in range(6)]
        Zt = [k.tile([128, G * 64]) for _ in range(6)]
        et = [k.tile([64, 64, G]) for _ in range(2)]
        zv = Z.rearrange("(a b) c -> a b c", b=64)
        xv = XG.rearrange("(a b) c -> a b c", b=64)
        ov = OUT.rearrange("(a b) c -> a b c", b=64)
        W_ = G * N1
        ik = 0
        for cb in range(8):
            z, x = zs[cb % 2], xs[cb % 2]
            k.dma(z[:NH, :, :], zv[:, :, zc0 + cb * 128:zc0 + (cb + 1) * 128], writes=[z], sem=sz[cb % 2])
            k.dma(x[:NH, :, :], xv[:, :, xc0 + cb * 128:xc0 + (cb + 1) * 128], writes=[x], sem=sx[cb % 2])
            for sb in range(128 // G):
                g0 = sb * G
                d0 = cb * 128 + g0
                kt = Kt[ik % 2]
                for q, (ri, dr) in enumerate(((0, 0), (1, 0), (0, 1), (1, 1))):
                    ch0 = dr * 2048 + o * 1024 + d0
                    k.dma(kt[q][:, :W_].rearrange("p (g n) -> p g n", g=G), SPEC[ri, :, ch0:ch0 + G, :],
                          writes=[kt[q]], sem=sk[ik % 2])
                ik += 1
                self._fft_fwd(L, tb, z, g0, G, A, Bt, X)
                Kr, Ki = Kc
                k.op("pool", lambda e, kt=kt: e.tensor_tensor(out=Kr[:, :W_], in0=kt[0][:, :W_], in1=kt[2][:, :W_], op=ALU.add),
                     reads=[kt[0], kt[2]], writes=[Kr])
                k.op("pool", lambda e, kt=kt: e.tensor_tensor(out=Ki[:, :W_], in0=kt[1][:, :W_], in1=kt[3][:, :W_], op=ALU.subtract),
                     reads=[kt[1], kt[3]], writes=[Ki])
                y1, y2, y3, y4, Yr, Yi = Yt
                k.op("dve", lambda e: e.tensor_tensor(out=y1[:, :W_], in0=X["r"][:64, :W_], in1=Kr[:, :W_], op=ALU.mult), reads=[X["r"], Kr], writes=[y1])
                k.op("dve", lambda e: e.tensor_tensor(out=y2[:, :W_], in0=X["i"][:64, :W_], in1=Ki[:, :W_], op=ALU.mult), reads=[X["i"], Ki], writes=[y2])
                k.op("dve", lambda e: e.tensor_tensor(out=y3[:, :W_], in0=X["r"][:64, :W_], in1=Ki[:, :W_], op=ALU.mult), reads=[X["r"], Ki], writes=[y3])
                k.op("dve", lambda e: e.tensor_tensor(out=y4[:, :W_], in0=X["i"][:64, :W_], in1=Kr[:, :W_], op=ALU.mult), reads=[X["i"], Kr], writes=[y4])
                k.op("pool", lambda e: e.tensor_tensor(out=Yr[:, :W_], in0=y1[:, :W_], in1=y2[:, :W_], op=ALU.subtract), reads=[y1, y2], writes=[Yr])
                k.op("pool", lambda e: e.tensor_tensor(out=Yi[:, :W_], in0=y3[:, :W_], in1=y4[:, :W_], op=ALU.add), reads=[y3, y4], writes=[Yi])
                for g in range(G):
                    k.op("pe", lambda e, g=g: e.matmul(Zp[:N1, g, :], Yr[:, g * N1:(g + 1) * N1], tb["RA"][:, :], start=True, stop=False),
                         reads=[Yr, tb["RA"]], writes=[Zp], accum=True)
                    k.op("pe", lambda e, g=g: e.matmul(Zp[:N1, g, :], Yi[:, g * N1:(g + 1) * N1], tb["RB"][:, :], start=False, stop=True),
                         reads=[Yi, tb["RB"]], writes=[Zp], accum=True)
                Zr, Zi = Zp[:N1, :, 0:64], Zp[:N1, :, 64:128]
                cT = tb["cT"][:N1, :].unsqueeze(1).to_broadcast([N1, G, 64])
                sT = tb["sT"][:N1, :].unsqueeze(1).to_broadcast([N1, G, 64])
                u1, u2, u3, u4, Zr2, Zi2 = Zt
                v = lambda t: t[:N1, :].rearrange("p (g n) -> p g n", g=G)
                k.op("dve", lambda e: e.tensor_tensor(out=v(u1), in0=Zr, in1=cT, op=ALU.mult), reads=[Zp, tb["cT"]], writes=[u1])
                k.op("dve", lambda e: e.tensor_tensor(out=v(u2), in0=Zi, in1=sT, op=ALU.mult), reads=[Zp, tb["sT"]], writes=[u2])
                k.op("dve", lambda e: e.tensor_tensor(out=v(u3), in0=Zr, in1=sT, op=ALU.mult), reads=[Zp, tb["sT"]], writes=[u3])
                k.op("dve", lambda e: e.tensor_tensor(out=v(u4), in0=Zi, in1=cT, op=ALU.mult), reads=[Zp, tb["cT"]], writes=[u4])
                k.op("pool", lambda e: e.tensor_tensor(out=Zr2[:N1, :], in0=u1[:N1, :], in1=u2[:N1, :], op=ALU.subtract), reads=[u1, u2], writes=[Zr2])
                k.op("pool", lambda e: e.tensor_tensor(out=Zi2[:N1, :], in0=u3[:N1, :], in1=u4[:N1, :], op=ALU.add), reads=[u3, u4], writes=[Zi2])
                k.op("pe", lambda e: e.matmul(yp[:NH, :G * 64], tb["C1"][:N1, :NH], Zr2[:N1, :], start=True, stop=False),
                     reads=[tb["C1"], Zr2], writes=[yp], accum=True)
                k.op("pe", lambda e: e.matmul(yp[:NH, :G * 64], tb["nS1"][:N1, :NH], Zi2[:N1, :], start=False, stop=True),
                     reads=[tb["nS1"], Zi2], writes=[yp], accum=True)
                e1, e2 = et
                bv = brow[:NH, d0:d0 + G].unsqueeze(1).to_broadcast([NH, 64, G])
                k.op("pool", lambda e, z=z, g0=g0, bv=bv: e.tensor_tensor(out=e1[:NH, :, :], in0=z[:NH, :, g0:g0 + G], in1=bv, op=ALU.mult),
                     reads=[z, brow], writes=[e1])
                k.op("dve", lambda e: e.tensor_tensor(out=e2[:NH, :, :], in0=yp[:NH, :G * 64].rearrange("p (g n) -> p n g", g=G),
                                                      in1=e1[:NH, :, :], op=ALU.add), reads=[yp, e1], writes=[e2])
                k.op("pool", lambda e, x=x, g0=g0: e.tensor_tensor(out=x[:NH, :, g0:g0 + G], in0=x[:NH, :, g0:g0 + G], in1=e2[:NH, :, :], op=ALU.mult),
                     reads=[x, e2], writes=[x])
            k.dma(ov[:, :, cb * 128:(cb + 1) * 128], x[:NH, :, :], reads=[x], sem=so[cb % 2])
        k.end()

    def hy_conv3(self, U, L, j, UC):
        k = self.k
        k.begin()
        C3 = 3 * D
        wr = k.tile([128, 3, C3])
        br = k.tile([128, C3])
        s0 = k.dsem()
        for r in range(3):
            k.dma(wr[:, r, :], self.W("hy", "conv_w")[j * 3 + r:j * 3 + r + 1, :].partition_broadcast(128), writes=[wr], sem=s0)
        k.dma(br[:], self.W("hy", "conv_b")[j:j + 1, :].partition_broadcast(128), writes=[br], sem=s0)
        ub = [[k.tile([128, C3]) for _ in range(3)] for _ in range(2)]
        su = [[k.dsem() for _ in range(3)] for _ in range(2)]
        so = [k.dsem() for _ in range(2)]
        nt = L // 128
        for t in range(nt):
            u0, u1, u2 = ub[t % 2]
            s_ = su[t % 2]
            t0 = t * 128
            if t == 0:
                k.op("pool", lambda e, u0=u0: e.memset(u0[:], 0.0), writes=[u0])
                k.dma(u0[1:128, :], U[0:127, :], writes=[u0], sem=s_[0])
            else:
                k.dma(u0[:], U[t0 - 1:t0 + 127, :], writes=[u0], sem=s_[0])
            k.dma(u1[:], U[t0:t0 + 128, :], writes=[u1], sem=s_[1])
            if t == nt - 1:
                k.op("pool", lambda e, u2=u2: e.memset(u2[:], 0.0), writes=[u2])
                k.dma(u2[0:127, :], U[t0 + 1:t0 + 128, :], writes=[u2], sem=s_[2])
            else:
                k.dma(u2[:], U[t0 + 1:t0 + 129, :], writes=[u2], sem=s_[2])
            k.op("pool", lambda e, u0=u0: e.tensor_tensor(out=u0[:], in0=u0[:], in1=wr[:, 0, :], op=ALU.mult), reads=[u0, wr], writes=[u0])
            k.op("dve", lambda e, u1=u1: e.tensor_tensor(out=u1[:], in0=u1[:], in1=wr[:, 1, :], op=ALU.mult), reads=[u1, wr], writes=[u1])
            k.op("pool", lambda e, u2=u2: e.tensor_tensor(out=u2[:], in0=u2[:], in1=wr[:, 2, :], op=ALU.mult), reads=[u2, wr], writes=[u2])
            k.op("dve", lambda e, u0=u0, u1=u1: e.tensor_tensor(out=u1[:], in0=u1[:], in1=u0[:], op=ALU.add), reads=[u0, u1], writes=[u1])
            k.op("pool", lambda e, u2=u2: e.tensor_tensor(out=u2[:], in0=u2[:], in1=br[:], op=ALU.add), reads=[u2, br], writes=[u2])
            k.op("dve", lambda e, u1=u1, u2=u2: e.tensor_tensor(out=u1[:], in0=u1[:], in1=u2[:], op=ALU.add), reads=[u1, u2], writes=[u1])
            k.dma(UC[t0:t0 + 128, :], u1[:], reads=[u1], sem=so[t % 2])
        k.end()

    def hyena(self, j, HLFM, L, Y, tag):
        N1 = L // 32
        U = self.dt("hyU" + tag, [L, 3 * D])
        UC = self.dt("hyUC" + tag, [L, 3 * D])
        HF = self.dt("hyHF" + tag, [L, 4096])
        RN = self.dt("hyRN" + tag, [1, 4096])
        SPEC = self.dt("hySP" + tag, [2, 64, 4096, N1])
        Z1 = self.dt("hyZ1" + tag, [L, D])
        Z2 = self.dt("hyZ2" + tag, [L, D])
        Z2T = self.dt("hyZ2T" + tag, [D, L])
        self.hy_filters(j, L, HF, RN)
        if self.stop_at == 1:
            return
        self.hy_filter_fft(L, HF, RN, SPEC)
        if self.stop_at == 2:
            return
        for q3 in range(3):
            self.gemm(HLFM, self.W("hy", "w_in")[j * D:(j + 1) * D, q3 * D:(q3 + 1) * D], D, D, L, U[:, q3 * D:(q3 + 1) * D],
                      out_tm=True, bias_row=self.W("hy", "b_in")[j:j + 1, q3 * D:(q3 + 1) * D])
        self.hy_conv3(U, L, j, UC)
        if self.stop_at == 3:
            return
        fb = self.W("hy", "f_bias")
        self.hy_conv(L, UC, 0, UC, D, SPEC, 0, fb[j * 2:j * 2 + 1, :], Z1)
        if self.stop_at == 4:
            return
        self.hy_conv(L, Z1, 0, UC, 2 * D, SPEC, 1, fb[j * 2 + 1:j * 2 + 2, :], Z2)
        self.transpose_in(Z2, Z2T, L)
        self.gemm(Z2T, self.W("hy", "w_out")[j * D:(j + 1) * D, :], D, D, L, Y, bias_pv="hy_b_out%d" % j)

    def rwkv(self, HC, HL, Y):
        k = self.k
        S = TC + T
        NT = S // 128
        NCH = S // 64
        H = 16
        Wr = lambda nm: self.W("rw", nm)
        rows = Wr("rows")
        XN = self.dt("rwXN", [6, D, S])
        k.begin()
        hh = [k.tile([128, T + 2]) for _ in range(2)]
        sh = [k.dsem() for _ in range(2)]
        xx = k.tile([128, T])
        xo = [k.tile([128, T]) for _ in range(2)]
        so = [k.dsem() for _ in range(2)]
        it = io = 0
        for (src, Tn, c0) in ((HC, TC, 0), (HL, T, TC)):
            for c in range(8):
                hb = hh[it % 2]
                it += 1
                k.op("pool", lambda e, hb=hb: e.memset(hb[:], 0.0), writes=[hb])
                k.dma(hb[:, 1:Tn + 1], src[c * 128:(c + 1) * 128, :], writes=[hb], sem=sh[it % 2])
                k.op("dve", lambda e, hb=hb, Tn=Tn: e.tensor_tensor(out=xx[:, :Tn], in0=hb[:, 0:Tn], in1=hb[:, 2:Tn + 2], op=ALU.add),
                     reads=[hb], writes=[xx])
                k.op("dve", lambda e, hb=hb, Tn=Tn: e.scalar_tensor_tensor(out=xx[:, :Tn], in0=xx[:, :Tn], scalar=0.5, in1=hb[:, 1:Tn + 1],
                                                                       op0=ALU.mult, op1=ALU.subtract), reads=[hb, xx], writes=[xx])
                for n in range(6):
                    o = xo[io % 2]
                    mu = self.pvc("rw_mu%d" % n, c)
                    eng = "dve"
                    k.op(eng, lambda e, o=o, hb=hb, Tn=Tn, mu=mu: e.scalar_tensor_tensor(
                        out=o[:, :Tn], in0=xx[:, :Tn], scalar=mu, in1=hb[:, 1:Tn + 1], op0=ALU.mult, op1=ALU.add),
                        reads=[xx, hb, self.pv], writes=[o])
                    k.dma(XN[n, c * 128:(c + 1) * 128, c0:c0 + Tn], o[:, :Tn], reads=[o], sem=so[io % 2])
                    io += 1
        k.end()
        Rm = self.dt("rwR", [S, D]); Km = self.dt("rwK", [S, D]); Vm = self.dt("rwV", [S, D])
        SW = [self.dt("rwSW%d" % n, [S, D]) for n in range(2)]
        Am = [self.dt("rwA%d" % n, [S, D]) for n in range(2)]
        Gm = self.dt("rwG", [S, D])
        HW = self.dt("rwHW", [128, S]); HA = self.dt("rwHA", [128, S]); HG = self.dt("rwHG", [256, S])
        self.gemm(XN[0], Wr("wr"), D, D, S, Rm, out_tm=True)
        self.gemm(XN[2], Wr("wk"), D, D, S, Km, out_tm=True)
        self.gemm(XN[3], Wr("wv"), D, D, S, Vm, out_tm=True)
        self.gemm(XN[1], Wr("w1cat"), D, 128, S, HW, act=AF.Tanh)
        self.gemm(XN[4], Wr("a1cat"), D, 128, S, HA)
        self.gemm(XN[5], Wr("g1pad"), D, 256, S, HG, act=AF.Sigmoid)
        for n in range(2):
            self.gemm(HW, Wr("w2pad%d" % n), 128, D, S, SW[n], out_tm=True, bias_row=rows[n:n + 1, :], act=AF.Sigmoid)
            self.gemm(HA, Wr("a2pad%d" % n), 128, D, S, Am[n], out_tm=True, bias_row=rows[2 + n:3 + n, :], act=AF.Sigmoid)
        self.gemm(HG, Wr("g2pad"), 256, D, S, Gm, out_tm=True)
        XT = self.dt("rwXT", [2, NT, 64, H, 4, 128])
        KD = self.dt("rwKD", [2, S, D]); NAD = self.dt("rwNAD", [2, S, D])
        PCf = self.dt("rwPC", [2, NCH, 64, H])
        BON = self.dt("rwBON", [S, D])
        k.begin()
        s0 = k.dsem()
        rw = k.tile([128, 3, D])
        for i_, r_ in enumerate((4, 5, 6)):
            k.dma(rw[:, i_, :], rows[r_:r_ + 1, :].partition_broadcast(128), writes=[rw], sem=s0)
        tri = [k.tile([128, 128]) for _ in range(2)]
        for n in range(2):
            k.dma(tri[n][:], Wr("TRI%d" % n), writes=[tri[n]], sem=s0)
        chk = k.tile([128, 2])
        k.dma(chk[:], Wr("CHK"), writes=[chk], sem=s0)
        inp = {nm: k.tile([128, D]) for nm in ("r", "k", "v", "sw0", "sw1", "a0", "a1")}
        sin = {nm: k.dsem() for nm in inp}
        srcs = {"r": Rm, "k": Km, "v": Vm, "sw0": SW[0], "sw1": SW[1], "a0": Am[0], "a1": Am[1]}
        wk_ = {nm: k.tile([128, D]) for nm in ("kkq", "kk", "t1", "t2", "lw", "kdir", "kd", "nad", "kt", "rt", "ksum")}
        sm = k.tile([128, H])
        pl = k.ptile([128, 2, 512])
        ptp = [k.ptile([64, 4, 128]) for _ in range(2)]
        ppc = k.ptile([64, H, 2])
        xts = k.tile([64, H, 4, 128])
        pcs = k.tile([64, 2, H])
        sxo = k.dsem(); sko = k.dsem(); sno = k.dsem(); spo = k.dsem(); sbo = k.dsem()
        v3 = lambda t: t[:].rearrange("p (h c) -> p h c", h=H)
        itp = 0
        for tt in range(NT):
            r0 = tt * 128
            for nm in inp:
                k.dma(inp[nm][:], srcs[nm][r0:r0 + 128, :], writes=[inp[nm]], sem=sin[nm])
            R_, K_, V_ = inp["r"], inp["k"], inp["v"]
            kkq, kk, t1, t2, lw, kdir, kd, nad, kt, rt, ksum = (wk_[n_] for n_ in ("kkq", "kk", "t1", "t2", "lw", "kdir", "kd", "nad", "kt", "rt", "ksum"))
            k.op("dve", lambda e: e.tensor_tensor(out=kkq[:], in0=K_[:], in1=rw[:, 0, :], op=ALU.mult), reads=[K_, rw], writes=[kkq])
            k.op("pool", lambda e: e.tensor_tensor(out=t1[:], in0=kkq[:], in1=kkq[:], op=ALU.mult), reads=[kkq], writes=[t1])
            k.op("dve", lambda e: e.tensor_reduce(out=sm[:], in_=v3(t1), axis=AX.X, op=ALU.add), reads=[t1], writes=[sm])
            k.op("dve", lambda e: e.tensor_scalar(out=sm[:], in0=sm[:], scalar1=1e-24, scalar2=None, op0=ALU.max), reads=[sm], writes=[sm])
            k.op("act", lambda e: e.sqrt(out=sm[:], in_=sm[:]), reads=[sm], writes=[sm])
            k.op("dve", lambda e: e.reciprocal(out=sm[:], in_=sm[:]), reads=[sm], writes=[sm])
            k.op("dve", lambda e: e.tensor_tensor(out=v3(kk), in0=v3(kkq), in1=sm[:].unsqueeze(2).to_broadcast([128, H, 64]), op=ALU.mult),
                 reads=[kkq, sm], writes=[kk])
            for n in range(2):
                SWt, At = inp["sw%d" % n], inp["a%d" % n]
                k.op("act", lambda e, SWt=SWt: e.mul(out=lw[:], in_=SWt[:], mul=-0.6065306597126334), reads=[SWt], writes=[lw])
                k.op("dve", lambda e, At=At: e.scalar_tensor_tensor(out=t1[:], in0=At[:], scalar=-1.0, in1=rw[:, 1, :], op0=ALU.add, op1=ALU.mult),
                     reads=[At, rw], writes=[t1])
                k.op("dve", lambda e: e.scalar_tensor_tensor(out=kdir[:], in0=t1[:], scalar=1.0, in1=K_[:], op0=ALU.add, op1=ALU.mult),
                     reads=[t1, K_], writes=[kdir])
                if n == 0:
                    k.op("pool", lambda e: e.tensor_copy(out=ksum[:], in_=kdir[:]), reads=[kdir], writes=[ksum])
                else:
                    k.op("pool", lambda e: e.tensor_tensor(out=ksum[:], in0=ksum[:], in1=kdir[:], op=ALU.add), reads=[kdir, ksum], writes=[ksum])
                k.op("pool", lambda e, At=At: e.tensor_tensor(out=nad[:], in0=kk[:], in1=At[:], op=ALU.mult), reads=[kk, At], writes=[nad])
                for hf in range(2):
                    k.op("pe", lambda e, hf=hf, n=n: e.matmul(pl[:, hf, :], tri[n][:], lw[:, hf * 512:(hf + 1) * 512], start=True, stop=True),
                         reads=[tri[n], lw], writes=[pl], accum=True)
                plv = pl[:].rearrange("p a b -> p (a b)")
                k.op("dve", lambda e: e.tensor_tensor(out=t2[:], in0=plv, in1=lw[:], op=ALU.subtract), reads=[pl, lw], writes=[t2])
                k.op("act", lambda e: e.activation(out=t2[:], in_=t2[:], func=AF.Exp), reads=[t2], writes=[t2])
                k.op("pool", lambda e: e.tensor_tensor(out=kt[:], in0=kk[:], in1=t2[:], op=ALU.mult), reads=[kk, t2], writes=[kt])
                k.op("act", lambda e: e.activation(out=t1[:], in_=plv, func=AF.Exp), reads=[pl], writes=[t1])
                k.op("dve", lambda e: e.tensor_tensor(out=rt[:], in0=R_[:], in1=t1[:], op=ALU.mult), reads=[R_, t1], writes=[rt])
                k.op("act", lambda e: e.activation(out=t2[:], in_=plv, func=AF.Exp, scale=-1.0), reads=[pl], writes=[t2])
                k.op("dve", lambda e: e.tensor_tensor(out=kd[:], in0=kdir[:], in1=t2[:], op=ALU.mult), reads=[kdir, t2], writes=[kd])
                k.op("dve", lambda e: e.scalar_tensor_tensor(out=nad[:], in0=nad[:], scalar=-1.0, in1=t2[:], op0=ALU.mult, op1=ALU.mult),
                     reads=[nad, t2], writes=[nad])
                k.dma(KD[n, r0:r0 + 128, :], kd[:], reads=[kd], sem=sko)
                k.dma(NAD[n, r0:r0 + 128, :], nad[:], reads=[nad], sem=sno)
                for h in range(H):
                    k.op("pe", lambda e, h=h: e.matmul(ppc[:, h, :], lw[:, h * 64:(h + 1) * 64], chk[:], start=True, stop=True),
                         reads=[lw, chk], writes=[ppc], accum=True)
                k.op("act", lambda e: e.activation(out=pcs[:].rearrange("p c h -> p h c"), in_=ppc[:], func=AF.Exp), reads=[ppc], writes=[pcs])
                for cc in range(2):
                    k.dma(PCf[n, tt * 2 + cc, :, :], pcs[:, cc, :], reads=[pcs], sem=spo)
                for q, srct in enumerate((kt, rt, kd, nad)):
                    for hg in range(4):
                        p = ptp[itp % 2]
                        itp += 1
                        for hl in range(4):
                            h = hg * 4 + hl
                            k.op("pe", lambda e, p=p, hl=hl, h=h, srct=srct: e.transpose(p[:, hl, :], srct[:, h * 64:(h + 1) * 64], self.ident[:]),
                                 reads=[srct, self.ident], writes=[p], accum=True)
                        if itp % 2 == 0:
                            k.op("act", lambda e, p=p, hg=hg, q=q: e.copy(out=xts[:, hg * 4:(hg + 1) * 4, q, :], in_=p[:]), reads=[p], writes=[xts], accum=True)
                        else:
                            k.op("dve", lambda e, p=p, hg=hg, q=q: e.tensor_copy(out=xts[:, hg * 4:(hg + 1) * 4, q, :], in_=p[:]), reads=[p], writes=[xts], accum=True)
                k._wait("sp", [(k.esem["act"], k.cnt[k.esem["act"]]), (k.esem["dve"], k.cnt[k.esem["dve"]])])
                k.dma(XT[n, tt], xts[:], reads=[xts], sem=sxo, q="sp")
            k.op("dve", lambda e: e.tensor_tensor(out=t1[:], in0=ksum[:], in1=rw[:, 2, :], op=ALU.mult), reads=[ksum, rw], writes=[t1])
            k.op("dve", lambda e: e.tensor_tensor(out=t1[:], in0=t1[:], in1=R_[:], op=ALU.mult), reads=[t1, R_], writes=[t1])
            k.op("dve", lambda e: e.tensor_reduce(out=sm[:], in_=v3(t1), axis=AX.X, op=ALU.add), reads=[t1], writes=[sm])
            k.op("dve", lambda e: e.tensor_tensor(out=v3(t2), in0=v3(V_), in1=sm[:].unsqueeze(2).to_broadcast([128, H, 64]), op=ALU.mult),
                 reads=[V_, sm], writes=[t2])
            k.dma(BON[r0:r0 + 128, :], t2[:], reads=[t2], sem=sbo)
        k.end()
        if self.stop_at == 3:
            return
        CH = self.dt("rwCH", [2, NCH, 64, H, 256])
        k.begin()
        s0 = k.dsem()
        msk = [k.tile([64, 512]) for _ in range(2)]
        for n in range(2):
            k.dma(msk[n][:], Wr("MASK%d" % n), writes=[msk[n]], sem=s0)
        idn = k.tile([64, 64])
        k.dma(idn[:], Wr("IDN"), writes=[idn], sem=s0)
        xt = [k.tile([64, H, 4, 128]) for _ in range(2)]
        sx = [k.dsem() for _ in range(2)]
        sc = [k.tile([64, H, 320]) for _ in range(2)]
        psc = k.ptile([64, 2, 512])
        pN = k.ptile([64, H, 64]); pL = k.ptile([64, H, 64]); pX = k.ptile([64, H, 64])
        Nb = [k.tile([64, H, 64]) for _ in range(2)]
        Lb = [k.tile([64, H, 64]) for _ in range(2)]
        ILb = k.tile([64, H, 64])
        Xb = [k.tile([64, H, 64]) for _ in range(2)]
        so1 = [k.dsem() for _ in range(2)]
        so2 = [k.dsem() for _ in range(2)]
        idb = idn[:].unsqueeze(1).to_broadcast([64, H, 64])
        ci = 0
        for tt in range(NT):
            for n in range(2):
                x = xt[(tt * 2 + n) % 2]
                k.dma(x[:], XT[n, tt], writes=[x], sem=sx[(tt * 2 + n) % 2])
                for cc in range(2):
                    cs = slice(cc * 64, (cc + 1) * 64)
                    s_ = sc[ci % 2]
                    for hp in range(8):
                        for hl in range(2):
                            h = hp * 2 + hl
                            k.op("pe", lambda e, hl=hl, h=h, x=x, cs=cs: e.matmul(
                                psc[:, hl, 0:128].rearrange("p (a b) -> p a b", a=2), x[:, h, 3, cs], x[:, h, 0:2, cs], start=True, stop=True),
                                reads=[x], writes=[psc], accum=True)
                            k.op("pe", lambda e, hl=hl, h=h, x=x, cs=cs: e.matmul(
                                psc[:, hl, 128:256].rearrange("p (a b) -> p a b", a=2), x[:, h, 2, cs], x[:, h, 0:2, cs], start=True, stop=True),
                                reads=[x], writes=[psc], accum=True)
                            k.op("pe", lambda e, hl=hl, h=h, x=x, cs=cs: e.matmul(
                                psc[:, hl, 256:320], x[:, h, 0, cs], x[:, h, 3, cs], start=True, stop=True),
                                reads=[x], writes=[psc], accum=True)
                        k.op("dve", lambda e, hp=hp, s_=s_, n=n: e.tensor_tensor(
                            out=s_[:, hp * 2:hp * 2 + 2, :], in0=psc[:, :, 0:320],
                            in1=msk[n][:, 0:320].unsqueeze(1).to_broadcast([64, 2, 320]), op=ALU.mult),
                            reads=[psc, msk[n]], writes=[s_], accum=True)
                    N2, L2 = s_[:, :, 0:64], s_[:, :, 256:320]
                    Xc = Xb[0]
                    k.op("pool", lambda e, Xc=Xc, N2=N2: e.tensor_tensor(out=Xc[:], in0=idb, in1=N2, op=ALU.subtract), reads=[idn, s_], writes=[Xc])
                    Ncur, Lcur, Nbuf, Lbuf = N2, L2, s_, s_
                    for j in range(1, 6):
                        Ln = Lb[j % 2]
                        for h in range(H):
                            k.op("pe", lambda e, h=h, Ncur=Ncur, Lcur=Lcur: e.matmul(pL[:, h, :], Ncur[:, h, :], Lcur[:, h, :], start=True, stop=True),
                                 reads=[Nbuf, Lbuf], writes=[pL], accum=True)
                        if j < 5:
                            Nn = Nb[j % 2]
                            for h in range(H):
                                k.op("pe", lambda e, h=h, Ncur=Ncur, Lcur=Lcur: e.matmul(pN[:, h, :], Lcur[:, h, :], Ncur[:, h, :], start=True, stop=True),
                                     reads=[Nbuf, Lbuf], writes=[pN], accum=True)
                            k.op("act", lambda e, Nn=Nn: e.copy(out=Nn[:], in_=pN[:]), reads=[pN], writes=[Nn])
                        k.op("dve", lambda e: e.tensor_tensor(out=ILb[:], in0=pL[:], in1=idb, op=ALU.add), reads=[pL, idn], writes=[ILb])
                        if j < 5:
                            k.op("dve", lambda e, Ln=Ln: e.tensor_copy(out=Ln[:], in_=pL[:]), reads=[pL], writes=[Ln])
                        Xn = Xb[j % 2]
                        for h in range(H):
                            k.op("pe", lambda e, h=h, Xc=Xc: e.matmul(pX[:, h, :], ILb[:, h, :], Xc[:, h, :], start=True, stop=True),
                                 reads=[ILb, Xc], writes=[pX], accum=True)
                        k.op("act", lambda e, Xn=Xn: e.copy(out=Xn[:], in_=pX[:]), reads=[pX], writes=[Xn])
                        Xc = Xn
                        if j < 5:
                            Ncur, Lcur, Nbuf, Lbuf = Nn[:], Ln[:], Nn, Ln
                    chn = tt * 2 + cc
                    k.dma(CH[n, chn, :, :, 0:64], Xc[:], reads=[Xc], sem=so1[ci % 2])
                    k.dma(CH[n, chn, :, :, 64:256], s_[:, :, 64:256], reads=[s_], sem=so2[ci % 2])
                    ci += 1
        k.end()
        if self.stop_at == 4:
            return
        YW = self.dt("rwYW", [2, S, D])
        k.begin()
        M = [k.tile([64, H, 64]) for _ in range(2)]
        for n in range(2):
            k.op("pool", lambda e, n=n: e.memset(M[n][:], 0.0), writes=[M[n]])
        ktrt = [k.tile([64, H, 2, 64]) for _ in range(2)]
        cht = [k.tile([64, H, 256]) for _ in range(2)]
        kdt = [k.tile([64, D]) for _ in range(2)]
        nadt = [k.tile([64, D]) for _ in range(2)]
        vt = [k.tile([64, D]) for _ in range(2)]
        pct = [k.tile([64, H]) for _ in range(2)]
        sl_ = [[k.dsem() for _ in range(6)] for _ in range(2)]
        W0s = k.tile([64, H, 64]); Us = k.tile([64, H, 64])
        Ys = [k.tile([64, H, 64]) for _ in range(2)]
        sy = [k.dsem() for _ in range(2)]
        pW = k.ptile([64, H, 64]); pU = k.ptile([64, H, 64]); pY = k.ptile([64, H, 64]); pM = k.ptile([64, H, 64])
        order = {0: list(range(NCH)), 1: list(range(TC // 64 - 1, -1, -1)) + list(range(NCH - 1, TC // 64 - 1, -1))}
        for step in range(NCH):
            for n in range(2):
                chn = order[n][step]
                tt, cc = chn // 2, chn % 2
                cs = slice(cc * 64, (cc + 1) * 64)
                r0 = chn * 64
                b = n
                sl = sl_[b]
                k.dma(ktrt[b][:], XT[n, tt, :, :, 0:2, cs], writes=[ktrt[b]], sem=sl[0], q="sp")
                k.dma(cht[b][:], CH[n, chn], writes=[cht[b]], sem=sl[1], q="act")
                k.dma(kdt[b][:], KD[n, r0:r0 + 64, :], writes=[kdt[b]], sem=sl[2], q="sp")
                k.dma(nadt[b][:], NAD[n, r0:r0 + 64, :], writes=[nadt[b]], sem=sl[3], q="act")
                k.dma(vt[b][:], Vm[r0:r0 + 64, :], writes=[vt[b]], sem=sl[4], q="sp")
                k.dma(pct[b][:], PCf[n, chn], writes=[pct[b]], sem=sl[5], q="act")
                kr, ch_, kd_, nad_, v_, pc_, Mn = ktrt[b], cht[b], kdt[b], nadt[b], vt[b], pct[b], M[n]
                for h in range(H):
                    hs = slice(h * 64, (h + 1) * 64)
                    k.op("pe", lambda e, h=h: e.matmul(pW[:, h, :], kr[:, h, 0, :], Mn[:, h, :], start=True, stop=False),
                         reads=[kr, Mn], writes=[pW], accum=True)
                    k.op("pe", lambda e, h=h, hs=hs: e.matmul(pW[:, h, :], ch_[:, h, 128:192], v_[:, hs], start=False, stop=True),
                         reads=[ch_, v_], writes=[pW], accum=True)
                k.op("act", lambda e: e.copy(out=W0s[:], in_=pW[:]), reads=[pW], writes=[W0s])
                for h in range(H):
                    k.op("pe", lambda e, h=h: e.matmul(pU[:, h, :], ch_[:, h, 0:64], W0s[:, h, :], start=True, stop=True),
                         reads=[ch_, W0s], writes=[pU], accum=True)
                k.op("dve", lambda e: e.tensor_copy(out=Us[:], in_=pU[:]), reads=[pU], writes=[Us])
                if chn >= TC // 64:
                    ys = Ys[n]
                    for h in range(H):
                        hs = slice(h * 64, (h + 1) * 64)
                        k.op("pe", lambda e, h=h: e.matmul(pY[:, h, :], kr[:, h, 1, :], Mn[:, h, :], start=True, stop=False),
                             reads=[kr, Mn], writes=[pY], accum=True)
                        k.op("pe", lambda e, h=h, hs=hs: e.matmul(pY[:, h, :], ch_[:, h, 192:256], v_[:, hs], start=False, stop=False),
                             reads=[ch_, v_], writes=[pY], accum=True)
                        k.op("pe", lambda e, h=h: e.matmul(pY[:, h, :], ch_[:, h, 64:128], Us[:, h, :], start=False, stop=True),
                             reads=[ch_, Us], writes=[pY], accum=True)
                    k.op("act", lambda e, ys=ys: e.copy(out=ys[:], in_=pY[:]), reads=[pY], writes=[ys])
                    k.dma(YW[n, r0:r0 + 64, :], ys[:].rearrange("p h c -> p (h c)"), reads=[ys], sem=sy[n])
                for h in range(H):
                    hs = slice(h * 64, (h + 1) * 64)
                    k.op("pe", lambda e, h=h, hs=hs: e.matmul(pM[:, h, :], kd_[:, hs], v_[:, hs], start=True, stop=False),
                         reads=[kd_, v_], writes=[pM], accum=True)
                    k.op("pe", lambda e, h=h, hs=hs: e.matmul(pM[:, h, :], nad_[:, hs], Us[:, h, :], start=False, stop=True),
                         reads=[nad_, Us], writes=[pM], accum=True)
                k.op("dve", lambda e, Mn=Mn: e.tensor_tensor(out=Mn[:], in0=Mn[:], in1=pM[:], op=ALU.add), reads=[Mn, pM], writes=[Mn])
                k.op("dve", lambda e, Mn=Mn, pc_=pc_: e.tensor_tensor(out=Mn[:], in0=Mn[:], in1=pc_[:].unsqueeze(2).to_broadcast([64, H, 64]), op=ALU.mult),
                     reads=[Mn, pc_], writes=[Mn])
        k.end()
        if self.stop_at == 5:
            return
        O = self.dt("rwO", [T, D])
        k.begin()
        s0 = k.dsem()
        gw = k.tile([128, 2, D])
        for i_, r_ in enumerate((7, 8)):
            k.dma(gw[:, i_, :], rows[r_:r_ + 1, :].partition_broadcast(128), writes=[gw], sem=s0)
        ya = [k.tile([128, D]) for _ in range(2)]
        yb_ = [k.tile([128, D]) for _ in range(2)]
        bo = [k.tile([128, D]) for _ in range(2)]
        gt = [k.tile([128, D]) for _ in range(2)]
        ss = [[k.dsem() for _ in range(4)] for _ in range(2)]
        sq_ = k.tile([128, D])
        st1 = k.tile([128, H]); st2 = k.tile([128, H])
        so = [k.dsem() for _ in range(2)]
        for t in range(T // 128):
            b = t % 2
            r0 = TC + t * 128
            k.dma(ya[b][:], YW[0, r0:r0 + 128, :], writes=[ya[b]], sem=ss[b][0])
            k.dma(yb_[b][:], YW[1, r0:r0 + 128, :], writes=[yb_[b]], sem=ss[b][1])
            k.dma(bo[b][:], BON[r0:r0 + 128, :], writes=[bo[b]], sem=ss[b][2])
            k.dma(gt[b][:], Gm[r0:r0 + 128, :], writes=[gt[b]], sem=ss[b][3])
            w = ya[b]
            w3_ = w[:].rearrange("p (h c) -> p h c", h=H)
            k.op("dve", lambda e, w=w, b=b: e.tensor_tensor(out=w[:], in0=w[:], in1=yb_[b][:], op=ALU.add), reads=[w, yb_[b]], writes=[w])
            k.op("dve", lambda e, w3_=w3_: e.tensor_reduce(out=st1[:], in_=w3_, axis=AX.X, op=ALU.add), reads=[w], writes=[st1])
            k.op("dve", lambda e: e.tensor_scalar(out=st1[:], in0=st1[:], scalar1=1.0 / 64, scalar2=None, op0=ALU.mult), reads=[st1], writes=[st1])
            k.op("dve", lambda e, w3_=w3_: e.tensor_tensor(out=w3_, in0=w3_, in1=st1[:].unsqueeze(2).to_broadcast([128, H, 64]), op=ALU.subtract),
                 reads=[w, st1], writes=[w])
            k.op("pool", lambda e, w=w: e.tensor_tensor(out=sq_[:], in0=w[:], in1=w[:], op=ALU.mult), reads=[w], writes=[sq_])
            k.op("dve", lambda e: e.tensor_reduce(out=st2[:], in_=sq_[:].rearrange("p (h c) -> p h c", h=H), axis=AX.X, op=ALU.add), reads=[sq_], writes=[st2])
            k.op("dve", lambda e: e.tensor_scalar(out=st2[:], in0=st2[:], scalar1=1.0 / 64, scalar2=64e-5, op0=ALU.mult, op1=ALU.add), reads=[st2], writes=[st2])
            k.op("act", lambda e: e.sqrt(out=st2[:], in_=st2[:]), reads=[st2], writes=[st2])
            k.op("dve", lambda e: e.reciprocal(out=st2[:], in_=st2[:]), reads=[st2], writes=[st2])
            k.op("dve", lambda e, w3_=w3_: e.tensor_tensor(out=w3_, in0=w3_, in1=st2[:].unsqueeze(2).to_broadcast([128, H, 64]), op=ALU.mult),
                 reads=[w, st2], writes=[w])
            k.op("pool", lambda e, w=w: e.tensor_tensor(out=w[:], in0=w[:], in1=gw[:, 0, :], op=ALU.mult), reads=[w, gw], writes=[w])
            k.op("pool", lambda e, w=w: e.tensor_tensor(out=w[:], in0=w[:], in1=gw[:, 1, :], op=ALU.add), reads=[w, gw], writes=[w])
            k.op("dve", lambda e, w=w, b=b: e.tensor_tensor(out=w[:], in0=w[:], in1=bo[b][:], op=ALU.add), reads=[w, bo[b]], writes=[w])
            k.op("dve", lambda e, w=w, b=b: e.tensor_tensor(out=w[:], in0=w[:], in1=gt[b][:], op=ALU.mult), reads=[w, gt[b]], writes=[w])
            k.dma(O[t * 128:(t + 1) * 128, :], w[:], reads=[w], sem=so[b])
        k.end()
        OT = self.dt("rwOT", [D, T])
        self.transpose_in(O, OT, T)
        self.gemm(OT, Wr("wo"), D, D, T, Y)

    def final_dbg(self):
        k = self.k
        k.begin()
        s = k.dsem()
        for name in self.dbg:
            if name == "modv":
                o = self.dt("dbg_modv", [128, DEPTH * 48 * 2], kind="ExternalOutput")
                k.dma(o, self.modv[:].rearrange("p a b c -> p (a b c)"), reads=[self.modv], sem=s)
                continue
            src = self.dram[name]
            o = self.dt("dbg_" + name, list(src.shape), kind="ExternalOutput")
            k.dma(o, src.ap(), sem=s, q="sp")
        k.end()


_CACHE = {}


def run(inputs, layers=(0, 1, 2, 3), dbg=(), cores=NCORES, test=None, extra=None):
    key = (tuple(layers), tuple(dbg), test)
    if key not in _CACHE:
        _CACHE[key] = Prog(list(layers), dbg, test)
    prog = _CACHE[key]
    blobs, lay = pack_host(inputs, list(layers))
    in_maps = []
    for c in range(cores):
        m = {"x": np.ascontiguousarray(inputs["x"][c]),
             "c": np.ascontiguousarray(inputs["c"][c:c + 1]),
             "ctx": np.ascontiguousarray(inputs["ctx"][c]),
             "c_ctx": np.ascontiguousarray(inputs["c_ctx"][None, :])}
        for piece, flat in blobs.items():
            m["blob_" + piece] = flat
        if extra:
            m.update(extra)
        in_maps.append(m)
    res = run_bass_kernel_spmd(prog.nc, in_maps, core_ids=list(range(cores)))
    return res.results


def kernel(**inputs):
    res = run(inputs)
    return np.stack([r["out"] for r in res], 0).astype(np.float32)
```

```python
import math
import numpy as np
from contextlib import ExitStack
import concourse.bass as bass
import concourse.mybir as mybir
from concourse.bass_utils import run_bass_kernel_spmd

F32 = mybir.dt.float32
F32R = mybir.dt.float32r
I32 = mybir.dt.int32
U32 = mybir.dt.uint32
AF = mybir.ActivationFunctionType
ALU = mybir.AluOpType
AX = mybir.AxisListType

NCORES = 8
D = 1024
T = 4096
TC = 256
DEPTH = 4
NE = 16
FF = 2048
ALPHA = (2 * DEPTH) ** 0.25
BW = 1024


class Buf:
    def __init__(self, name, ap=None, nsub=0):
        self.name = name
        self.t = ap
        self.w = []
        self.r = []
        self.kids = [Buf(name + str(i), ap) for i in range(nsub)]

    def __getitem__(self, idx):
        return self.t[idx]

    def ch(self, i):
        return self.kids[i]

    def leaves(self):
        if not self.kids:
            return [self]
        out = []
        for c in self.kids:
            out += c.leaves()
        return out


def _lv(bufs):
    out = []
    for b in bufs:
        out += b.leaves()
    return out


class KB:
    def __init__(self, nc, n_dma_sems=88):
        self.nc = nc
        self.eng = {"pe": nc.tensor, "dve": nc.vector, "act": nc.scalar,
                    "pool": nc.gpsimd, "sp": nc.sync}
        self.esem = {}
        self.cnt = {}
        self.semh = {}
        self.seen = {k: {} for k in self.eng}
        for k in self.eng:
            nm = "e_" + k
            self.semh[nm] = nc.alloc_semaphore(name=nm)
            self.esem[k] = nm
            self.cnt[nm] = 0
        self.free_dma = []
        for i in range(n_dma_sems):
            nm = "d%d" % i
            self.semh[nm] = nc.alloc_semaphore(name=nm)
            self.cnt[nm] = 0
            self.free_dma.append(nm)
        self.phase_sems = []
        self.stack = None
        self.rr = 0
        self.pstack = ExitStack()
        self.semreg = {}

    def begin(self):
        self.stack = ExitStack()
        self.phase_sems = []

    def end(self):
        self.barrier()
        self.stack.close()
        self.stack = None
        for nm in self.phase_sems:
            self.semreg.pop(nm, None)
        self.free_dma = self.phase_sems + self.free_dma
        self.phase_sems = []

    def dsem(self, persistent=False):
        nm = self.free_dma.pop()
        if not persistent:
            self.phase_sems.append(nm)
        return nm

    def tile(self, shape, dtype=F32, name=None, persistent=False, nsub=0):
        st = self.pstack if persistent else self.stack
        t = st.enter_context(self.nc.sbuf_tensor(list(shape), dtype))
        return Buf(name or "t", t, nsub)

    def ptile(self, shape, dtype=F32, name=None, nsub=0):
        t = self.stack.enter_context(self.nc.psum_tensor(list(shape), dtype))
        return Buf(name or "p", t, nsub)

    def _wait(self, ek, toks):
        need = {}
        for (s, v) in toks:
            if v > need.get(s, 0):
                need[s] = v
        for s, v in need.items():
            if self.seen[ek].get(s, 0) >= v:
                continue
            self.eng[ek].wait_ge(self.semh[s], v)
            self.seen[ek][s] = v

    def rnd(self, ek, dst_ap, src_ap, reads, writes):
        if ek == "act":
            return self.op("act", lambda e: e.copy(out=dst_ap, in_=src_ap), reads=reads, writes=writes)
        return self.op(ek, lambda e: e.tensor_copy(out=dst_ap, in_=src_ap), reads=reads, writes=writes)

    def barrier(self):
        toks = [(s, c) for s, c in self.cnt.items() if c > 0]
        for ek in self.eng:
            self._wait(ek, toks)

    def op(self, ek, fn, reads=(), writes=(), accum=False):
        reads = _lv(reads)
        writes = _lv(writes)
        toks = []
        es = self.esem[ek]
        for b in reads:
            toks += b.w
        for b in writes:
            if accum:
                toks += [t for t in b.w if t[0] != es]
            else:
                toks += b.w
            toks += b.r
        self._wait(ek, toks)
        ins = fn(self.eng[ek])
        self.cnt[es] += 1
        tok = (es, self.cnt[es])
        ins.then_inc(self.semh[es], 1)
        for b in reads:
            b.r.append(tok)
        for b in writes:
            b.w = [tok]
            b.r = []
        return ins

    def dma(self, out_ap, in_ap, reads=(), writes=(), sem=None, q=None, fn=None, **kw):
        reads = _lv(reads)
        writes = _lv(writes)
        if q is None:
            q = ("sp", "act")[self.rr % 2]
            self.rr += 1
        toks = []
        for b in reads:
            toks += b.w
        for b in writes:
            toks += [t for t in b.w if t[0] != sem]
            toks += b.r
        self._wait(q, toks)
        if fn is not None:
            ins = fn(self.eng[q])
        else:
            ins = self.eng[q].dma_start(out=out_ap, in_=in_ap, **kw)
        self.cnt[sem] += 16
        tok = (sem, self.cnt[sem])
        ins.then_inc(self.semh[sem], 16)
        reg = self.semreg.setdefault(sem, {})
        for b in reg.values():
            b.w = [tok if t[0] == sem else t for t in b.w]
            b.r = [tok if t[0] == sem else t for t in b.r]
        for b in reads:
            b.r.append(tok)
            reg[id(b)] = b
        for b in writes:
            b.w = [tok]
            b.r = []
            reg[id(b)] = b
        return ins


def _pv_cols():
    cols = {}
    off = 0

    def add(name, n):
        nonlocal off
        cols[name] = (off, n // 128)
        off += n // 128
    for i in range(DEPTH):
        add("mod_b%d" % i, 6 * D)
        for j in range(2):
            add("ln_g%d_%d" % (i, j), D)
            add("ln_b%d_%d" % (i, j), D)
    for j in range(2):
        add("hy_b_out%d" % j, D)
    for n in range(6):
        add("rw_mu%d" % n, D)
    add("fn_bo", D)
    return cols, off


PV_COLS, PV_N = _pv_cols()


def blob_spec(layers):
    sp = {"misc": []}
    m = sp["misc"]
    m.append(("ident", (128, 128)))
    m.append(("pv", (128, 1024)))
    m.append(("pos", (T, D)))
    m.append(("mod_w", (DEPTH * D, 6 * D)))
    m.append(("moe_router", (DEPTH * D, NE)))
    for i in layers:
        sp["moe%d" % i] = [("w1", (NE * D, FF)), ("w3", (NE * D, FF)), ("w2", (NE * FF, D))]
    if 0 in layers or 3 in layers:
        h = [("w_in", (2 * D, 3 * D)), ("b_in", (2, 3 * D)), ("conv_w", (6, 3 * D)), ("conv_b", (2, 3 * D)),
             ("f_w1", (66, 64)), ("hyv", (128, 4)), ("f_w2", (256, 64)), ("f_w3", (128, 4096)),
             ("f_bias", (4, D)), ("w_out", (2 * D, D)),
             ("C2", (64, 64)), ("S2", (64, 64)), ("nS2", (64, 64)), ("RA", (64, 128)), ("RB", (64, 128))]
        for L in (T, TC):
            NH = L // 64
            N1 = 2 * NH
            h += [("zT_%d" % L, (33, L)), ("win_%d" % L, (L, D)), ("F1_%d" % L, (NH, 2 * N1)),
                  ("c_%d" % L, (64, N1)), ("s_%d" % L, (64, N1)), ("cT_%d" % L, (N1, 64)), ("sT_%d" % L, (N1, 64)),
                  ("C1_%d" % L, (N1, NH)), ("nS1_%d" % L, (N1, NH))]
        sp["hy"] = h
    if 1 in layers:
        sp["rw"] = [("wr", (D, D)), ("wk", (D, D)), ("wv", (D, D)), ("wo", (D, D)),
                    ("w1cat", (D, 128)), ("a1cat", (D, 128)), ("g1pad", (D, 256)),
                    ("w2pad0", (128, D)), ("w2pad1", (128, D)), ("a2pad0", (128, D)), ("a2pad1", (128, D)),
                    ("g2pad", (256, D)), ("rows", (9, D)),
                    ("TRI0", (128, 128)), ("TRI1", (128, 128)), ("CHK", (128, 2)),
                    ("MASK0", (64, 512)), ("MASK1", (64, 512)), ("IDN", (64, 64))]
    if 2 in layers:
        sp["fnet"] = [("fn_cs", (128, 256)), ("fn_wo", (D, D)), ("fn_nct", (T, T)), ("fn_nst", (T, T))]
    return sp


def blob_layout(spec):
    lay = {}
    for piece, items in spec.items():
        off = 0
        ent = {}
        for name, (R, C) in items:
            n = R * C
            rows = (n + BW - 1) // BW
            ent[name] = (off, rows, R, C)
            off += rows
        tot = (off + 8 * 16 - 1) // (8 * 16) * (8 * 16)
        lay[piece] = (tot, ent)
    return lay


def grid_pos_embed():
    rows = T // 64
    r_idx = np.repeat(np.arange(rows, dtype=np.float32), 64)
    c_idx = np.tile(np.arange(64, dtype=np.float32), rows)
    quarter = D // 4
    omega = (1.0 / (10000.0 ** (np.arange(quarter, dtype=np.float32) / np.float32(quarter)))).astype(np.float32)

    def emb(p):
        a = (p[:, None] * omega[None, :]).astype(np.float32)
        return np.concatenate([np.sin(a), np.cos(a)], -1)
    return np.concatenate([emb(r_idx), emb(c_idx)], -1).astype(np.float32)


def hy_const_tables():
    out = {}
    n2 = np.arange(64)
    a2 = 2 * np.pi * (np.outer(n2, n2) % 64) / 64.0
    C2, S2 = np.cos(a2), np.sin(a2)
    out["C2"], out["S2"], out["nS2"] = C2, S2, -S2
    out["RA"] = np.concatenate([C2, S2], 1)
    out["RB"] = np.concatenate([-S2, C2], 1)
    for L in (T, TC):
        NH = L // 64
        N1 = 2 * NH
        N = 64 * N1
        a1 = 2 * np.pi * (np.outer(np.arange(NH), np.arange(N1)) % N1) / N1
        out["F1_%d" % L] = np.concatenate([np.cos(a1), -np.sin(a1)], 1)
        at = 2 * np.pi * (np.outer(n2, np.arange(N1)) % N) / N
        out["c_%d" % L], out["s_%d" % L] = np.cos(at), np.sin(at)
        out["cT_%d" % L], out["sT_%d" % L] = np.cos(at).T, np.sin(at).T
        ai = 2 * np.pi * (np.outer(np.arange(N1), np.arange(NH)) % N1) / N1
        out["C1_%d" % L], out["nS1_%d" % L] = np.cos(ai), -np.sin(ai)
        pos = np.arange(L, dtype=np.float32)
        t01 = (pos / np.float32(max(L - 1, 1))).astype(np.float32)
        f = np.linspace(1e-4, 15, 16, dtype=np.float32)
        ang = (f[None, :] * (np.float32(2.0 * math.pi) * pos / np.float32(L))[:, None]).astype(np.float32)
        z = np.concatenate([t01[:, None], np.cos(ang), -np.sin(ang)], -1).astype(np.float32)
        out["zT_%d" % L] = z.T
        deltas = np.linspace(math.log(1e-2) / 1.5, math.log(1e-2) / 0.3, D, dtype=np.float32)
        out["win_%d" % L] = np.exp(-t01[:, None] * np.abs(deltas)[None, :]).astype(np.float32)
    return {k_: np.ascontiguousarray(v, dtype=np.float32) for k_, v in out.items()}


def rw_const_tables():
    out = {}
    i = np.arange(128)
    same = (i[:, None] // 64) == (i[None, :] // 64)
    out["TRI0"] = (same & (i[:, None] <= i[None, :])).astype(np.float32)
    out["TRI1"] = (same & (i[:, None] >= i[None, :])).astype(np.float32)
    out["CHK"] = np.stack([(i < 64), (i >= 64)], 1).astype(np.float32)
    a = np.arange(64)
    for n in range(2):
        before = (a[:, None] < a[None, :]) if n == 0 else (a[:, None] > a[None, :])
        ateq = before | (a[:, None] == a[None, :])
        m = np.concatenate([-before.astype(np.float32), ateq.astype(np.float32), before.astype(np.float32),
                            ateq.astype(np.float32), -before.T.astype(np.float32), np.zeros((64, 192), np.float32)], 1)
        out["MASK%d" % n] = m
    out["IDN"] = np.eye(64, dtype=np.float32)
    return out


def pack_host(inputs, layers):
    spec = blob_spec(layers)
    lay = blob_layout(spec)
    src = {}
    src["ident"] = np.eye(128, dtype=np.float32)
    pv = np.zeros((128, 1024), np.float32)

    def putv(name, v):
        c0, n = PV_COLS[name]
        pv[:, c0:c0 + n] = np.asarray(v, np.float32).reshape(n, 128).T
    for i in range(DEPTH):
        putv("mod_b%d" % i, inputs["mod_b"][i])
        for j in range(2):
            putv("ln_g%d_%d" % (i, j), inputs["ln_g"][i, j])
            putv("ln_b%d_%d" % (i, j), inputs["ln_b"][i, j])
    for j in range(2):
        putv("hy_b_out%d" % j, inputs["hy_b_out"][j])
    for n in range(6):
        putv("rw_mu%d" % n, inputs["rw_mu"][0, n])
    putv("fn_bo", inputs["fn_bo"][0])
    src["pv"] = pv
    src["pos"] = grid_pos_embed()
    src["mod_w"] = inputs["mod_w"].reshape(DEPTH * D, 6 * D)
    src["moe_router"] = inputs["moe_router"].reshape(DEPTH * D, NE)
    if 0 in layers or 3 in layers:
        src["w_in"] = inputs["hy_w_in"].reshape(2 * D, 3 * D)
        src["b_in"] = inputs["hy_b_in"]
        src["conv_w"] = inputs["hy_conv_w"].reshape(6, 3 * D)
        src["conv_b"] = inputs["hy_conv_b"]
        src["f_w1"] = inputs["hy_f_w1"].reshape(66, 64)
        src["hyv"] = np.concatenate([np.stack([inputs["hy_f_b1"][j], inputs["hy_f_freq"][j],
                                               inputs["hy_f_b2"][j, 0], inputs["hy_f_b2"][j, 1]], 1) for j in range(2)], 0)
        src["f_w2"] = inputs["hy_f_w2"].reshape(256, 64)
        src["f_w3"] = inputs["hy_f_w3"].reshape(128, 4096)
        src["f_bias"] = inputs["hy_f_bias"].reshape(4, D)
        src["w_out"] = inputs["hy_w_out"].reshape(2 * D, D)
        src.update(hy_const_tables())
    if 1 in layers:
        src["wr"], src["wk"], src["wv"], src["wo"] = inputs["rw_wr"][0], inputs["rw_wk"][0], inputs["rw_wv"][0], inputs["rw_wo"][0]
        src["w1cat"] = np.concatenate([inputs["rw_w1"][0, 0], inputs["rw_w1"][0, 1]], 1)
        src["a1cat"] = np.concatenate([inputs["rw_a1"][0, 0], inputs["rw_a1"][0, 1]], 1)
        g1 = np.zeros((D, 256), np.float32); g1[:, :160] = inputs["rw_g1"][0]
        g2 = np.zeros((256, D), np.float32); g2[:160] = inputs["rw_g2"][0]
        src["g1pad"], src["g2pad"] = g1, g2
        for n in range(2):
            w2 = np.zeros((128, D), np.float32); w2[n * 64:(n + 1) * 64] = inputs["rw_w2"][0, n]
            a2 = np.zeros((128, D), np.float32); a2[n * 64:(n + 1) * 64] = inputs["rw_a2"][0, n]
            src["w2pad%d" % n], src["a2pad%d" % n] = w2, a2
        src["rows"] = np.stack([inputs["rw_w0"][0, 0], inputs["rw_w0"][0, 1], inputs["rw_a0"][0, 0], inputs["rw_a0"][0, 1],
                                inputs["rw_kk"][0], inputs["rw_ka"][0], inputs["rw_rk"][0], inputs["rw_gn_g"][0], inputs["rw_gn_b"][0]], 0)
        src.update(rw_const_tables())
    if 2 in layers:
        dk = np.outer(np.arange(128), np.arange(128)) % 128
        ang = 2.0 * np.pi * dk / 128.0
        nrm = 1.0 / math.sqrt(T * 128.0)
        src["fn_cs"] = np.concatenate([-np.cos(ang) * nrm, np.sin(ang) * nrm], 1).astype(np.float32)
        tk = (np.outer(np.arange(T, dtype=np.int64), np.arange(T, dtype=np.int64)) % T).astype(np.float64)
        src["fn_nct"] = (-np.cos(2.0 * np.pi * tk / T)).astype(np.float32)
        src["fn_nst"] = (-np.sin(2.0 * np.pi * tk / T)).astype(np.float32)
        del tk
        src["fn_wo"] = inputs["fn_wo"][0]
    blobs = {}
    for piece, (tot, ent) in lay.items():
        flat = np.zeros((tot, BW), np.float32)
        for name, (off, rows, R, C) in ent.items():
            if piece.startswith("moe"):
                i = int(piece[3:])
                a = {"w1": inputs["moe_w1"][i], "w3": inputs["moe_w3"][i], "w2": inputs["moe_w2"][i]}[name]
            else:
                a = src[name]
            a = np.ascontiguousarray(a, dtype=np.float32).reshape(-1)
            flat[off:off + rows].reshape(-1)[:a.size] = a
        blobs[piece] = flat
    return blobs, lay


class Prog:
    def __init__(self, layers, dbg=(), test=None):
        self.test = test
        import os
        self.stop_at = int(os.environ.get("STOP_AT", "0"))
        self.layers = layers
        self.dbg = list(dbg)
        nc = bass.Bass("TRN2", target_bir_lowering=False)
        self.nc = nc
        self.k = KB(nc)
        self.lay = blob_layout(blob_spec(layers))
        self.dram = {}
        self.build()

    def dt(self, name, shape, kind="Internal", dtype=F32):
        t = self.nc.dram_tensor(name, list(shape), dtype, kind=kind)
        self.dram[name] = t
        return t.ap()

    def W(self, piece, name):
        tot, ent = self.lay[piece]
        off, rows, R, C = ent[name]
        g = self.gath[piece]
        v = g[off:off + rows, :]
        if C == BW:
            return v[:R, :]
        if C < BW:
            return v.rearrange("r (a c) -> (r a) c", c=C)[:R, :]
        return v.rearrange("(r a) c -> r (a c)", a=C // BW)[:R, :]

    def build(self):
        nc, k = self.nc, self.k
        self.x_in = self.dt("x", [T, D], kind="ExternalInput")
        self.c_in = self.dt("c", [1, D], kind="ExternalInput")
        self.ctx_in = self.dt("ctx", [TC, D], kind="ExternalInput")
        self.cctx_in = self.dt("c_ctx", [1, D], kind="ExternalInput")
        self.shard = {}
        self.gath = {}
        self.bounce = {}
        for piece, (tot, ent) in self.lay.items():
            self.gath[piece] = self.dt("blob_" + piece, [tot, BW], kind="ExternalInput")
        self.out = self.dt("out", [T, D], kind="ExternalOutput")

        self.ident = k.tile([128, 128], persistent=True)
        self.pv = k.tile([128, 1024], persistent=True)
        self.ones = k.tile([128, 128], persistent=True)
        self.modv = k.tile([128, DEPTH, 48, 2], persistent=True)
        self.modp = k.tile([128, DEPTH, 48, 2], persistent=True)
        self.idxT = k.tile([128, 4, NE], I32, persistent=True)
        self.gateT = k.tile([128, 4, NE], persistent=True)
        self.phase_consts()
        if self.test == "rw":
            injc = self.dt("injc", [TC, D], kind="ExternalInput")
            injl = self.dt("inj", [T, D], kind="ExternalInput")
            HCF = self.dt("HCF", [D, TC]); HLF = self.dt("HLF", [D, T])
            Y = self.dt("YR", [D, T])
            self.transpose_in(injc, HCF, TC)
            self.transpose_in(injl, HLF, T)
            self.rwkv(HCF, HLF, Y)
            self.final_dbg()
            return
        if self.test in ("hy", "hyc"):
            L = T if self.test == "hy" else TC
            inj = self.dt("inj", [L, D], kind="ExternalInput")
            HFM = self.dt("HFM", [D, L])
            Y = self.dt("YH", [D, L])
            self.transpose_in(inj, HFM, L)
            self.hyena(0, HFM, L, Y, "t")
            self.final_dbg()
            return
        if self.test == "fnet":
            inj = self.dt("inj", [T, D], kind="ExternalInput")
            HFM = self.dt("HFM", [D, T])
            Y = self.dt("YF", [D, T])
            self.transpose_in(inj, HFM, T)
            self.fnet(HFM, Y)
            self.final_dbg()
            return
        if self.test == "moe":
            inj = self.dt("inj", [T, D], kind="ExternalInput")
            HFM = self.dt("HFM", [D, T])
            YM = self.dt("YM", [T, D])
            self.transpose_in(inj, HFM, T)
            self.moe(0, HFM, inj, YM, T)
            self.final_dbg()
            return
        self.phase_modvec()
        self.XT = self.dt("XT", [D, T])
        self.XCT = self.dt("XCT", [D, TC])
        XT, XCT = self.XT, self.XCT
        self.transpose_in(self.x_in, XT, T, add=self.W("misc", "pos"))
        self.transpose_in(self.ctx_in, XCT, TC)
        HL = self.dt("HL", [D, T]); HC = self.dt("HC", [D, TC])
        YL = self.dt("YL", [D, T]); YC = self.dt("YC", [D, TC])
        HL2 = self.dt("HL2", [D, T]); HL2TM = self.dt("HL2TM", [T, D])
        HC2 = self.dt("HC2", [D, TC]); HC2TM = self.dt("HC2TM", [TC, D])
        YM = self.dt("YM", [T, D]); YMT = self.dt("YMT", [D, T])
        YMC = self.dt("YMC", [TC, D]); YMCT = self.dt("YMCT", [D, TC])
        self.ln_pass(XT, T, layer=0, first=True, H_out=HL, modj=0, which=0)
        self.ln_pass(XCT, TC, layer=0, first=True, H_out=HC, modj=0, which=1)
        for i in range(DEPTH):
            if i not in self.layers:
                break
            kind = i % 3
            ctx_full = (i == 0)
            if kind == 0:
                self.hyena(i // 3, HL, T, YL, "l%d" % i)
                if ctx_full:
                    self.hyena(i // 3, HC, TC, YC, "c%d" % i)
            elif kind == 1:
                self.rwkv(HC, HL, YL)
            else:
                self.fnet(HL, YL)
            self.ln_pass(XT, T, layer=i, Y=YL, ymul=lambda c, i=i: self.mv(i, 2, c, 0, plus1=True), lnj=0,
                         X_out=XT, H_out=HL2, H_tm=HL2TM, modj=3, which=0)
            self.moe(i, HL2, HL2TM, YM, T)
            self.transpose_in(YM, YMT, T)
            last = (i == DEPTH - 1)
            self.ln_pass(XT, T, layer=i, Y=YMT, ymul=lambda c, i=i: self.mv(i, 5, c, 0, plus1=True), lnj=1,
                         X_out=None if last else XT, X_tm=self.out if last else None,
                         H_out=None if last else HL, do_mod=not last, modj=0, which=0, mod_layer=i + 1)
            if ctx_full:
                self.ln_pass(XCT, TC, layer=i, Y=YC, ymul=lambda c, i=i: self.mv(i, 2, c, 1, plus1=True), lnj=0,
                             X_out=XCT, H_out=HC2, H_tm=HC2TM, modj=3, which=1)
                self.moe(i, HC2, HC2TM, YMC, TC)
                self.transpose_in(YMC, YMCT, TC)
                self.ln_pass(XCT, TC, layer=i, Y=YMCT, ymul=lambda c, i=i: self.mv(i, 5, c, 1, plus1=True), lnj=1,
                             X_out=XCT, H_out=HC, modj=0, which=1, mod_layer=i + 1)
        self.final_dbg()

    def phase_consts(self):
        k = self.k
        k.begin()
        s = k.dsem()
        k.dma(self.ident[:], self.W("misc", "ident"), writes=[self.ident], sem=s)
        k.dma(self.pv[:], self.W("misc", "pv"), writes=[self.pv], sem=s)
        k.op("dve", lambda e: e.memset(self.ones[:], 1.0), writes=[self.ones])
        k.end()

    def pvc(self, name, c=None):
        c0, n = PV_COLS[name]
        if c is None:
            return self.pv[:, c0:c0 + n]
        return self.pv[:, c0 + c:c0 + c + 1]

    def phase_modvec(self):
        k = self.k
        k.begin()
        sc = k.tile([128, 8, 2])
        s = k.dsem()
        if True:
            k.dma(sc[:, :, 0], self.c_in.rearrange("o (c p) -> p (o c)", p=128), writes=[sc], sem=s, allow_slow_non_contiguous=True)
            k.dma(sc[:, :, 1], self.cctx_in.rearrange("o (c p) -> p (o c)", p=128), writes=[sc], sem=s, allow_slow_non_contiguous=True)
        k.op("act", lambda e: e.activation(out=sc[:], in_=sc[:], func=AF.Silu), reads=[sc], writes=[sc])
        mw = self.W("misc", "mod_w")
        wb = [k.tile([128, 8, 1536]) for _ in range(2)]
        ws = [k.dsem() for _ in range(2)]
        ps = [k.ptile([128, 12, 2]) for _ in range(2)]
        it = 0
        for i in range(DEPTH):
            for g in range(4):
                b = wb[it % 2]
                src = mw[i * D:(i + 1) * D, g * 1536:(g + 1) * 1536].rearrange("(c p) n -> p c n", p=128)
                k.dma(b[:], src, writes=[b], sem=ws[it % 2])
                p = ps[it % 2]
                for j in range(12):
                    for c in range(8):
                        k.op("pe", lambda e, j=j, c=c, b=b, p=p: e.matmul(
                            p[:, j, :], b[:, c, j * 128:(j + 1) * 128], sc[:, c, :],
                            start=(c == 0), stop=(c == 7)),
                            reads=[b, sc], writes=[p], accum=True)
                c0, _ = PV_COLS["mod_b%d" % i]
                bias = self.pv[:, c0 + g * 12:c0 + (g + 1) * 12]
                k.op("dve", lambda e, p=p, i=i, g=g, bias=bias: e.tensor_tensor(
                    out=self.modv[:, i, g * 12:(g + 1) * 12, :], in0=p[:],
                    in1=bias.unsqueeze(2).to_broadcast([128, 12, 2]), op=ALU.add),
                    reads=[p, self.pv], writes=[self.modv])
                it += 1
        k.op("dve", lambda e: e.tensor_scalar(out=self.modp[:], in0=self.modv[:], scalar1=1.0,
                                              scalar2=None, op0=ALU.add),
             reads=[self.modv], writes=[self.modp])
        k.end()

    def mv(self, layer, j, c, which=0, plus1=False):
        t = self.modp if plus1 else self.modv
        return t[:, layer, j * 8 + c, which:which + 1]

    def transpose_in(self, src, dstT, Tn, add=None):
        k = self.k
        k.begin()
        nt = Tn // 128
        grp = min(4, nt)
        xin = [k.tile([128, D]) for _ in range(2)]
        ain = [k.tile([128, D]) for _ in range(2)] if add is not None else None
        sx = [k.dsem() for _ in range(2)]
        sa = [k.dsem() for _ in range(2)]
        so = [k.dsem() for _ in range(2)]
        pst = [k.ptile([128, 4, 128]) for _ in range(4)]
        outb = [k.tile([128, 8, 128 * grp], nsub=2 * grp) for _ in range(2)]
        dv = dstT.rearrange("(c p) t -> p c t", p=128)
        pi = 0
        for t in range(nt):
            xb = xin[t % 2]
            k.dma(xb[:], src[t * 128:(t + 1) * 128, :], writes=[xb], sem=sx[t % 2])
            if add is not None:
                ab = ain[t % 2]
                k.dma(ab[:], add[t * 128:(t + 1) * 128, :], writes=[ab], sem=sa[t % 2])
                k.op("dve", lambda e, xb=xb, ab=ab: e.tensor_tensor(out=xb[:], in0=xb[:], in1=ab[:], op=ALU.add),
                     reads=[xb, ab], writes=[xb])
            ob = outb[(t // grp) % 2]
            tt = t % grp
            for h in range(2):
                p = pst[pi % 4]
                pi += 1
                for cc in range(4):
                    c = h * 4 + cc
                    k.op("pe", lambda e, p=p, cc=cc, c=c, xb=xb: e.transpose(
                        p[:, cc, :], xb[:, c * 128:(c + 1) * 128], self.ident[:]),
                        reads=[xb, self.ident], writes=[p], accum=True)
                dst = ob[:, h * 4:(h + 1) * 4, tt * 128:(tt + 1) * 128]
                if h == 0:
                    k.op("act", lambda e, p=p, dst=dst: e.copy(out=dst, in_=p[:]),
                         reads=[p], writes=[ob.ch(h * grp + tt)])
                else:
                    k.op("dve", lambda e, p=p, dst=dst: e.tensor_copy(out=dst, in_=p[:]),
                         reads=[p], writes=[ob.ch(h * grp + tt)])
            if tt == grp - 1:
                g = t // grp
                k.dma(dv[:, :, g * 128 * grp:(g + 1) * 128 * grp], ob[:], reads=[ob], sem=so[g % 2])
        k.end()

    def ln_pass(self, X, Tn, layer, first=False, Y=None, ymul=None, lnj=0, which=0,
                X_out=None, H_out=None, H_tm=None, X_tm=None, modj=0, do_mod=True, Y_tm=False, mod_layer=None):
        k = self.k
        if mod_layer is None:
            mod_layer = layer
        k.begin()
        TT = min(512, Tn)
        ntile = Tn // TT
        xv = X.rearrange("(c p) t -> p c t", p=128)
        xb = [k.tile([128, 8, TT], nsub=8) for _ in range(2)]
        sxs = [k.dsem() for _ in range(2)]
        if not first:
            yv = Y.rearrange("(c p) t -> p c t", p=128)
            yb = [k.tile([128, 8, TT], nsub=8) for _ in range(2)]
            sys_ = [k.dsem() for _ in range(2)]
        sq = [k.tile([128, 8, TT], nsub=8) for _ in range(2)]
        hb = [k.tile([128, 8, TT], nsub=8) for _ in range(2)] if do_mod else None
        so = [k.dsem() for _ in range(2)]
        so2 = [k.dsem() for _ in range(2)]
        st = [k.tile([128, 4, TT], nsub=3) for _ in range(2)]
        ps1 = [k.ptile([128, TT]) for _ in range(2)]
        ps2 = [k.ptile([128, TT]) for _ in range(2)]
        if X_tm is not None or H_tm is not None:
            pst = [k.ptile([128, 4, 128]) for _ in range(2)]
            tmb = [k.tile([128, D], nsub=2) for _ in range(2)]
            stm = [k.dsem() for _ in range(2)]
        self._tmi = 0

        def stats_norm(zb, sqb, sb, p1, p2, eps):
            for c in range(8):
                k.op("act", lambda e, c=c: e.activation(out=sqb[:, c, :], in_=zb[:, c, :], func=AF.Square),
                     reads=[zb.ch(c)], writes=[sqb.ch(c)])
            for c in range(8):
                k.op("pe", lambda e, c=c: e.matmul(p1[:], self.ones[:], zb[:, c, :], start=(c == 0), stop=(c == 7)),
                     reads=[self.ones, zb.ch(c)], writes=[p1], accum=True)
            for c in range(8):
                k.op("pe", lambda e, c=c: e.matmul(p2[:], self.ones[:], sqb[:, c, :], start=(c == 0), stop=(c == 7)),
                     reads=[self.ones, sqb.ch(c)], writes=[p2], accum=True)
            k.op("dve", lambda e: e.tensor_scalar(out=sb[:, 0, :], in0=p1[:], scalar1=1.0 / D, scalar2=None, op0=ALU.mult),
                 reads=[p1], writes=[sb.ch(0)])
            k.op("dve", lambda e: e.tensor_tensor(out=sb[:, 2, :], in0=sb[:, 0, :], in1=sb[:, 0, :], op=ALU.mult),
                 reads=[sb.ch(0)], writes=[sb.ch(2)])
            k.op("dve", lambda e: e.scalar_tensor_tensor(out=sb[:, 1, :], in0=p2[:], scalar=1.0 / D, in1=sb[:, 2, :],
                                                         op0=ALU.mult, op1=ALU.subtract),
                 reads=[p2, sb.ch(2)], writes=[sb.ch(1)])
            k.op("dve", lambda e: e.tensor_scalar(out=sb[:, 1, :], in0=sb[:, 1, :], scalar1=eps, scalar2=None,
                                                  op0=ALU.add),
                 reads=[sb.ch(1)], writes=[sb.ch(1)])
            k.op("act", lambda e: e.sqrt(out=sb[:, 1, :], in_=sb[:, 1, :]),
                 reads=[sb.ch(1)], writes=[sb.ch(1)])
            k.op("dve", lambda e: e.reciprocal(out=sb[:, 1, :], in_=sb[:, 1, :]),
                 reads=[sb.ch(1)], writes=[sb.ch(1)])
            for c in range(8):
                eng = "dve" if c % 2 == 0 else "pool"
                k.op(eng, lambda e, c=c: e.tensor_tensor(out=zb[:, c, :], in0=zb[:, c, :], in1=sb[:, 0, :], op=ALU.subtract),
                     reads=[zb.ch(c), sb.ch(0)], writes=[zb.ch(c)])
                k.op(eng, lambda e, c=c: e.tensor_tensor(out=zb[:, c, :], in0=zb[:, c, :], in1=sb[:, 1, :], op=ALU.mult),
                     reads=[zb.ch(c), sb.ch(1)], writes=[zb.ch(c)])

        def emit_tm(srcb, dst_tm, t0):
            for q in range(TT // 128):
                i = self._tmi
                self._tmi += 1
                tb = tmb[i % 2]
                for h in range(2):
                    p = pst[h]
                    for cc in range(4):
                        c = h * 4 + cc
                        k.op("pe", lambda e, p=p, cc=cc, c=c, q=q: e.transpose(
                            p[:, cc, :], srcb[:, c, q * 128:(q + 1) * 128], self.ident[:]),
                            reads=[srcb.ch(c), self.ident], writes=[p], accum=True)
                    if h == 0:
                        k.op("act", lambda e, p=p, tb=tb, h=h: e.copy(
                            out=tb[:, h * 512:(h + 1) * 512], in_=p[:].rearrange("p a b -> p (a b)")),
                            reads=[p], writes=[tb.ch(h)])
                    else:
                        k.op("dve", lambda e, p=p, tb=tb, h=h: e.tensor_copy(
                            out=tb[:, h * 512:(h + 1) * 512], in_=p[:].rearrange("p a b -> p (a b)")),
                            reads=[p], writes=[tb.ch(h)])
                k.dma(dst_tm[t0 + q * 128:t0 + (q + 1) * 128, :], tb[:], reads=[tb], sem=stm[i % 2])

        for t in range(ntile):
            zb = xb[t % 2]
            sl = slice(t * TT, (t + 1) * TT)
            k.dma(zb[:], xv[:, :, sl], writes=[zb], sem=sxs[t % 2])
            sqb, sb, p1, p2 = sq[t % 2], st[t % 2], ps1[t % 2], ps2[t % 2]
            if not first:
                ybb = yb[t % 2]
                if Y_tm:
                    raise NotImplementedError
                k.dma(ybb[:], yv[:, :, sl], writes=[ybb], sem=sys_[t % 2])
                for c in range(8):
                    k.op("act", lambda e, c=c: e.mul(out=zb[:, c, :], in_=zb[:, c, :], mul=float(ALPHA)),
                         reads=[zb.ch(c)], writes=[zb.ch(c)])
                    k.op("dve", lambda e, c=c: e.scalar_tensor_tensor(
                        out=zb[:, c, :], in0=ybb[:, c, :], scalar=ymul(c), in1=zb[:, c, :],
                        op0=ALU.mult, op1=ALU.add), reads=[ybb.ch(c), zb.ch(c), self.modp], writes=[zb.ch(c)])
                stats_norm(zb, sqb, sb, p1, p2, 1e-5)
                gc0, _ = PV_COLS["ln_g%d_%d" % (layer, lnj)]
                bc0, _ = PV_COLS["ln_b%d_%d" % (layer, lnj)]
                for c in range(8):
                    k.op("act", lambda e, c=c: e.activation(
                        out=zb[:, c, :], in_=zb[:, c, :], func=AF.Identity,
                        scale=self.pv[:, gc0 + c:gc0 + c + 1], bias=self.pv[:, bc0 + c:bc0 + c + 1]),
                        reads=[zb.ch(c), self.pv], writes=[zb.ch(c)])
                if X_out is not None:
                    k.dma(X_out.rearrange("(c p) t -> p c t", p=128)[:, :, sl], zb[:], reads=[zb], sem=so[t % 2])
                if X_tm is not None:
                    emit_tm(zb, X_tm, t * TT)
            if do_mod:
                h = hb[t % 2]
                for c in range(8):
                    k.op("pool", lambda e, c=c: e.tensor_copy(out=h[:, c, :], in_=zb[:, c, :]),
                         reads=[zb.ch(c)], writes=[h.ch(c)])
                stats_norm(h, sqb, sb, p1, p2, 1e-6)
                for c in range(8):
                    k.op("act", lambda e, c=c: e.activation(
                        out=h[:, c, :], in_=h[:, c, :], func=AF.Identity,
                        scale=self.mv(mod_layer, modj + 1, c, which, plus1=True),
                        bias=self.mv(mod_layer, modj, c, which)),
                        reads=[h.ch(c), self.modv, self.modp], writes=[h.ch(c)])
                if H_out is not None:
                    k.dma(H_out.rearrange("(c p) t -> p c t", p=128)[:, :, sl], h[:], reads=[h], sem=so2[t % 2])
                if H_tm is not None:
                    emit_tm(h, H_tm, t * TT)
        k.end()

    def moe(self, layer, HFM, HTM, YM, Tn):
        k, nc = self.k, self.nc
        cap = 2 * Tn // NE
        JP = min(128, cap)
        nch = cap // JP
        ntt = Tn // 128
        piece = "moe%d" % layer
        W1, W3, W2 = self.W(piece, "w1"), self.W(piece, "w3"), self.W(piece, "w2")
        idxT, gateT = self.idxT, self.gateT
        k.begin()
        rw = k.tile([128, 8, NE])
        s = k.dsem()
        rsrc = self.W("misc", "moe_router")[layer * D:(layer + 1) * D, :].rearrange("(c p) e -> p c e", p=128)
        k.dma(rw[:], rsrc, writes=[rw], sem=s)
        zt = k.tile([128, D])
        k.op("pool", lambda e: e.memset(zt[:], 0.0), writes=[zt])
        sz = k.dsem()
        for tt in range(ntt):
            k.dma(YM[tt * 128:(tt + 1) * 128, :], zt[:], reads=[zt], sem=sz)
        TT = min(512, Tn)
        hb = [k.tile([128, 8, TT]) for _ in range(2)]
        sh = [k.dsem() for _ in range(2)]
        hv = HFM.rearrange("(c p) t -> p c t", p=128)
        lg = k.ptile([128, ntt, NE])
        for t in range(Tn // TT):
            b = hb[t % 2]
            k.dma(b[:], hv[:, :, t * TT:(t + 1) * TT], writes=[b], sem=sh[t % 2])
            for q in range(TT // 128):
                tt = t * (TT // 128) + q
                for c in range(8):
                    k.op("pe", lambda e, b=b, c=c, q=q, tt=tt: e.matmul(
                        lg[:, tt, :], b[:, c, q * 128:(q + 1) * 128], rw[:, c, :], start=(c == 0), stop=(c == 7)),
                        reads=[b, rw], writes=[lg], accum=True)
        aff = k.tile([128, ntt, NE])
        mx = k.tile([128, ntt])
        k.op("dve", lambda e: e.tensor_reduce(out=mx[:], in_=lg[:], axis=AX.X, op=ALU.max), reads=[lg], writes=[mx])
        k.op("dve", lambda e: e.tensor_tensor(out=aff[:], in0=lg[:], in1=mx[:].unsqueeze(2).to_broadcast([128, ntt, NE]),
                                              op=ALU.subtract), reads=[lg, mx], writes=[aff])
        k.op("act", lambda e: e.activation(out=aff[:], in_=aff[:], func=AF.Exp), reads=[aff], writes=[aff])
        k.op("dve", lambda e: e.tensor_reduce(out=mx[:], in_=aff[:], axis=AX.X, op=ALU.add), reads=[aff], writes=[mx])
        k.op("dve", lambda e: e.reciprocal(out=mx[:], in_=mx[:]), reads=[mx], writes=[mx])
        k.op("dve", lambda e: e.tensor_tensor(out=aff[:], in0=aff[:], in1=mx[:].unsqueeze(2).to_broadcast([128, ntt, NE]),
                                              op=ALU.mult), reads=[aff, mx], writes=[aff])
        affT = k.tile([NE, Tn])
        pT = [k.ptile([NE, 4, 128]) for _ in range(2)]
        ng = (ntt + 3) // 4
        for g in range(ng):
            p = pT[g % 2]
            n = min(4, ntt - g * 4)
            for q in range(n):
                tt = g * 4 + q
                k.op("pe", lambda e, p=p, q=q, tt=tt: e.transpose(p[:, q, :], aff[:, tt, :], self.ident[:]),
                     reads=[aff, self.ident], writes=[p], accum=True)
            k.op("act", lambda e, p=p, g=g, n=n: e.copy(
                out=affT[:, g * 512:g * 512 + n * 128], in_=p[:, 0:n, :].rearrange("p a b -> p (a b)")),
                reads=[p], writes=[affT], accum=True)
        work = k.tile([NE, Tn])
        vals = k.tile([NE, cap])
        idxu = k.tile([NE, cap], U32)
        k.op("dve", lambda e: e.tensor_copy(out=work[:], in_=affT[:]), reads=[affT], writes=[work])
        for r in range(cap // 8):
            sl = slice(r * 8, (r + 1) * 8)
            k.op("dve", lambda e, sl=sl: e.max(out=vals[:, sl], in_=work[:]), reads=[work], writes=[vals], accum=True)
            k.op("dve", lambda e, sl=sl: e.max_index(out=idxu[:, sl], in_max=vals[:, sl], in_values=work[:]),
                 reads=[work, vals], writes=[idxu], accum=True)
            k.op("dve", lambda e, sl=sl: e.match_replace(out=work[:], in_to_replace=vals[:, sl], in_values=work[:],
                                                         imm_value=-1.0), reads=[vals, work], writes=[work])
        idxf = k.tile([NE, cap])
        k.op("dve", lambda e: e.tensor_copy(out=idxf[:], in_=idxu[:]), reads=[idxu], writes=[idxf])
        pI = k.ptile([128, nch, NE])
        pG = k.ptile([128, nch, NE])
        for ch in range(nch):
            k.op("pe", lambda e, ch=ch: e.transpose(pI[:JP, ch, :], idxf[:, ch * JP:(ch + 1) * JP], self.ident[:NE, :NE]),
                 reads=[idxf, self.ident], writes=[pI], accum=True)
            k.op("pe", lambda e, ch=ch: e.transpose(pG[:JP, ch, :], vals[:, ch * JP:(ch + 1) * JP], self.ident[:NE, :NE]),
                 reads=[vals, self.ident], writes=[pG], accum=True)
        k.op("dve", lambda e: e.tensor_copy(out=idxT[:JP, :nch, :], in_=pI[:JP]), reads=[pI], writes=[idxT])
        k.op("dve", lambda e: e.tensor_copy(out=gateT[:JP, :nch, :], in_=pG[:JP]), reads=[pG], writes=[gateT])
        k.end()
        k.begin()
        Xe = k.tile([128, nch, D], nsub=nch)
        XeT = k.tile([128, 8, cap], F32R, nsub=8)
        heT = k.tile([128, 16, cap], F32R, nsub=16)
        Ye = k.tile([128, nch, D], nsub=nch)
        w1s = [k.tile([128, 8, 256]) for _ in range(2)]
        w3s = [k.tile([128, 8, 256]) for _ in range(2)]
        w2s = [k.tile([128, 8, 256]) for _ in range(2)]
        w1t = [k.tile([128, 8, 256], F32R) for _ in range(2)]
        w3t = [k.tile([128, 8, 256], F32R) for _ in range(2)]
        w2t = [k.tile([128, 16, 256], F32R, nsub=2) for _ in range(1)]
        tmp = [k.tile([128, cap]) for _ in range(2)]
        s1 = [k.dsem() for _ in range(2)]
        s3 = [k.dsem() for _ in range(2)]
        s2 = [k.dsem() for _ in range(2)]
        sg = k.dsem()
        ss = k.dsem()
        ph1 = [k.ptile([128, cap]) for _ in range(2)]
        ph3 = [k.ptile([128, cap]) for _ in range(2)]
        py = [k.ptile([128, 512]) for _ in range(2)]
        pt = [k.ptile([128, 4, 128]) for _ in range(2)]
        i1 = i2 = ip = iy = 0
        for ex in range(NE):
            for ch in range(nch):
                k.dma(None, None, reads=[idxT], writes=[Xe.ch(ch)], sem=sg, q="pool",
                      fn=lambda e, ch=ch, ex=ex: e.indirect_dma_start(
                          out=Xe[:JP, ch, :], out_offset=None, in_=HTM[:, :],
                          in_offset=bass.IndirectOffsetOnAxis(ap=idxT[:JP, ch, ex:ex + 1], axis=0)))
            for dc in range(8):
                for ch in range(nch):
                    if ch % 4 == 0:
                        p = pt[ip % 2]
                        ip += 1
                    k.op("pe", lambda e, p=p, ch=ch, dc=dc: e.transpose(
                        p[:, ch % 4, :JP], Xe[:JP, ch, dc * 128:(dc + 1) * 128], self.ident[:JP, :JP]),
                        reads=[Xe.ch(ch), self.ident], writes=[p], accum=True)
                    if ch % 4 == 3 or ch == nch - 1:
                        c0 = (ch // 4) * 4
                        n = ch - c0 + 1
                        eng = "act" if dc % 2 == 0 else "dve"
                        if eng == "act":
                            k.op("act", lambda e, p=p, dc=dc, c0=c0, n=n: e.copy(
                                out=XeT[:, dc, c0 * JP:(c0 + n) * JP].rearrange("p (a b) -> p a b", a=n),
                                in_=p[:, 0:n, :JP]), reads=[p], writes=[XeT.ch(dc)])
                        else:
                            k.op("dve", lambda e, p=p, dc=dc, c0=c0, n=n: e.tensor_copy(
                                out=XeT[:, dc, c0 * JP:(c0 + n) * JP].rearrange("p (a b) -> p a b", a=n),
                                in_=p[:, 0:n, :JP]), reads=[p], writes=[XeT.ch(dc)])
            for g in range(8):
                a1, a3 = w1t[i1 % 2], w3t[i1 % 2]
                b1, b3 = w1s[i1 % 2], w3s[i1 % 2]
                r0 = ex * D
                k.dma(b1[:], W1[r0:r0 + D, g * 256:(g + 1) * 256].rearrange("(c p) n -> p c n", p=128),
                      writes=[b1], sem=s1[i1 % 2], q="sp")
                k.dma(b3[:], W3[r0:r0 + D, g * 256:(g + 1) * 256].rearrange("(c p) n -> p c n", p=128),
                      writes=[b3], sem=s3[i1 % 2], q="sp")
                k.rnd("act", a1[:], b1[:], [b1], [a1])
                k.rnd("dve", a3[:], b3[:], [b3], [a3])
                i1 += 1
                for fl in range(2):
                    fc = g * 2 + fl
                    p1, p3 = ph1[fc % 2], ph3[fc % 2]
                    for dc in range(8):
                        k.op("pe", lambda e, p1=p1, a1=a1, dc=dc, fl=fl: e.matmul(
                            p1[:], a1[:, dc, fl * 128:(fl + 1) * 128], XeT[:, dc, :], start=(dc == 0), stop=(dc == 7)),
                            reads=[a1, XeT.ch(dc)], writes=[p1], accum=True)
                    for dc in range(8):
                        k.op("pe", lambda e, p3=p3, a3=a3, dc=dc, fl=fl: e.matmul(
                            p3[:], a3[:, dc, fl * 128:(fl + 1) * 128], XeT[:, dc, :], start=(dc == 0), stop=(dc == 7)),
                            reads=[a3, XeT.ch(dc)], writes=[p3], accum=True)
                    tb = tmp[fc % 2]
                    k.op("act", lambda e, tb=tb, p1=p1: e.activation(out=tb[:], in_=p1[:], func=AF.Silu),
                         reads=[p1], writes=[tb])
                    k.op("dve", lambda e, tb=tb, p3=p3, fc=fc: e.tensor_tensor(
                        out=heT[:, fc, :], in0=tb[:], in1=p3[:], op=ALU.mult),
                        reads=[tb, p3], writes=[heT.ch(fc)])
            for q in range(4):
                a2 = w2t[0]
                r0 = ex * FF
                for hf in range(2):
                    b2 = w2s[i2 % 2]
                    k.dma(b2[:], W2[r0 + hf * 1024:r0 + (hf + 1) * 1024, q * 256:(q + 1) * 256].rearrange("(c p) n -> p c n", p=128),
                          writes=[b2], sem=s2[i2 % 2], q="sp")
                    k.rnd("pool", a2[:, hf * 8:(hf + 1) * 8, :], b2[:], [b2], [a2.ch(hf)])
                    i2 += 1
                for ch in range(nch):
                    p = py[iy % 2]
                    iy += 1
                    for fc in range(16):
                        k.op("pe", lambda e, p=p, a2=a2, fc=fc, ch=ch: e.matmul(
                            p[:JP, 0:256], heT[:, fc, ch * JP:(ch + 1) * JP], a2[:, fc, :],
                            start=(fc == 0), stop=(fc == 15)),
                            reads=[a2, heT.ch(fc)], writes=[p], accum=True)
                    k.op("act", lambda e, p=p, ch=ch, q=q, ex=ex: e.activation(
                        out=Ye[:JP, ch, q * 256:(q + 1) * 256], in_=p[:JP, 0:256], func=AF.Identity,
                        scale=gateT[:JP, ch, ex:ex + 1]),
                        reads=[p, gateT], writes=[Ye.ch(ch)], accum=True)
            for ch in range(nch):
                k._wait("pool", [(ss, k.cnt[ss])])
                k.dma(None, None, reads=[idxT, Ye.ch(ch)], sem=ss, q="pool",
                      fn=lambda e, ch=ch, ex=ex: e.indirect_dma_start(
                          out=YM[:, :], out_offset=bass.IndirectOffsetOnAxis(ap=idxT[:JP, ch, ex:ex + 1], axis=0),
                          in_=Ye[:JP, ch, :], in_offset=None, compute_op=ALU.add))
        k.end()

    def gemm(self, XT, Wap, K, N, Tn, out, bias_pv=None, act=None, out_tm=False, bias_row=None):
        k = self.k
        k.begin()
        KC = K // 128
        wt = k.tile([128, KC, N], F32R, nsub=KC)
        ws = [k.tile([128, N]) for _ in range(2)]
        sw = [k.dsem() for _ in range(2)]
        for kc in range(KC):
            w_ = ws[kc % 2]
            k.dma(w_[:], Wap[kc * 128:(kc + 1) * 128, :], writes=[w_], sem=sw[kc % 2])
            k.rnd(("pool", "act")[kc % 2], wt[:, kc, :], w_[:], [w_], [wt.ch(kc)])
        TT = 512 if Tn % 512 == 0 else 256
        xv = XT.rearrange("(c p) t -> p c t", p=128)
        xs = [k.tile([128, KC, TT]) for _ in range(2)]
        xb = [k.tile([128, KC, TT], F32R) for _ in range(2)]
        sx = [k.dsem() for _ in range(2)]
        so = [k.dsem() for _ in range(2)]
        ps = [k.ptile([128, 512]) for _ in range(4)]
        func = act if act is not None else AF.Identity
        ip = io = 0
        if not out_tm:
            ov = out.rearrange("(c p) t -> p c t", p=128)
            ob = [k.tile([128, 4, TT], nsub=4) for _ in range(2)]
            for t in range(Tn // TT):
                x_ = xs[t % 2]
                b = xb[t % 2]
                k.dma(x_[:], xv[:, :, t * TT:(t + 1) * TT], writes=[x_], sem=sx[t % 2])
                k.rnd("pool", b[:], x_[:], [x_], [b])
                for n in range(N // 128):
                    p = ps[ip % 4]
                    ip += 1
                    for kc in range(KC):
                        k.op("pe", lambda e, p=p, kc=kc, n=n, b=b: e.matmul(
                            p[:, :TT], wt[:, kc, n * 128:(n + 1) * 128], b[:, kc, :], start=(kc == 0), stop=(kc == KC - 1)),
                            reads=[wt.ch(kc), b], writes=[p], accum=True)
                    o = ob[io % 2]
                    if bias_pv is not None:
                        c0, _ = PV_COLS[bias_pv]
                        k.op("act", lambda e, p=p, o=o, n=n, c0=c0: e.activation(
                            out=o[:, n % 4, :], in_=p[:, :TT], func=func, bias=self.pv[:, c0 + n:c0 + n + 1]),
                            reads=[p, self.pv], writes=[o.ch(n % 4)])
                    else:
                        k.op("act", lambda e, p=p, o=o, n=n: e.activation(out=o[:, n % 4, :], in_=p[:, :TT], func=func),
                             reads=[p], writes=[o.ch(n % 4)])
                    if n % 4 == 3 or n == N // 128 - 1:
                        n0 = (n // 4) * 4
                        k.dma(ov[:, n0:n + 1, t * TT:(t + 1) * TT], o[:, 0:n - n0 + 1, :], reads=[o], sem=so[io % 2])
                        io += 1
        else:
            brow = None
            if bias_row is not None:
                brow = k.tile([128, N])
                sb_ = k.dsem()
                k.dma(brow[:], bias_row.partition_broadcast(128), writes=[brow], sem=sb_)
            ob = [k.tile([128, 512]) for _ in range(2)]
            for t in range(Tn // TT):
                x_ = xs[t % 2]
                b = xb[t % 2]
                k.dma(x_[:], xv[:, :, t * TT:(t + 1) * TT], writes=[x_], sem=sx[t % 2])
                k.rnd("pool", b[:], x_[:], [x_], [b])
                for q in range(TT // 128):
                    for n in range(N // 512):
                        p = ps[ip % 4]
                        ip += 1
                        for kc in range(KC):
                            k.op("pe", lambda e, p=p, kc=kc, n=n, b=b, q=q: e.matmul(
                                p[:], b[:, kc, q * 128:(q + 1) * 128], wt[:, kc, n * 512:(n + 1) * 512],
                                start=(kc == 0), stop=(kc == KC - 1)), reads=[wt.ch(kc), b], writes=[p], accum=True)
                        o = ob[io % 2]
                        if brow is not None:
                            k.op("dve", lambda e, p=p, o=o, n=n: e.tensor_tensor(
                                out=o[:], in0=p[:], in1=brow[:, n * 512:(n + 1) * 512], op=ALU.add),
                                reads=[p, brow], writes=[o])
                            if act is not None:
                                k.op("act", lambda e, o=o: e.activation(out=o[:], in_=o[:], func=act), reads=[o], writes=[o])
                        else:
                            k.op("act", lambda e, p=p, o=o: e.activation(out=o[:], in_=p[:], func=func), reads=[p], writes=[o])
                        r0 = t * TT + q * 128
                        k.dma(out[r0:r0 + 128, n * 512:(n + 1) * 512], o[:], reads=[o], sem=so[io % 2])
                        io += 1
        k.end()

    def fnet(self, HL, Y):
        k = self.k
        PQ = self.dt("fn_PQ", [2, T, D])
        MX = self.dt("fn_MX", [D, T])
        k.begin()
        cs0 = k.tile([128, 256])
        cs = k.tile([128, 256], F32R)
        s0 = k.dsem()
        k.dma(cs0[:], self.W("fnet", "fn_cs"), writes=[cs0], sem=s0)
        k.rnd("dve", cs[:], cs0[:], [cs0], [cs])
        hv = HL.rearrange("(c p) t -> p c t", p=128)
        hb0 = [k.tile([128, 8, 512]) for _ in range(2)]
        hb = [k.tile([128, 8, 512], F32R) for _ in range(2)]
        sh = [k.dsem() for _ in range(2)]
        ps = [k.ptile([128, 2, 256]) for _ in range(4)]
        pb = [k.tile([128, D], nsub=4) for _ in range(2)]
        qb = [k.tile([128, D], nsub=4) for _ in range(2)]
        so = [k.dsem() for _ in range(2)]
        ip = io = 0
        for t in range(T // 512):
            b0 = hb0[t % 2]
            b = hb[t % 2]
            k.dma(b0[:], hv[:, :, t * 512:(t + 1) * 512], writes=[b0], sem=sh[t % 2])
            k.rnd("pool", b[:], b0[:], [b0], [b])
            for q in range(4):
                po, qo = pb[io % 2], qb[io % 2]
                for g2 in range(4):
                    p = ps[ip % 4]
                    ip += 1
                    for gl in range(2):
                        g = g2 * 2 + gl
                        k.op("pe", lambda e, p=p, gl=gl, g=g, b=b, q=q: e.matmul(
                            p[:, gl, :], b[:, g, q * 128:(q + 1) * 128], cs[:], start=True, stop=True),
                            reads=[b, cs], writes=[p], accum=True)
                    if g2 % 2 == 0:
                        k.op("act", lambda e, p=p, po=po, g2=g2: e.copy(
                            out=po[:, g2 * 256:(g2 + 1) * 256].rearrange("p (a b) -> p a b", a=2), in_=p[:, :, 0:128]),
                            reads=[p], writes=[po.ch(g2)])
                        k.op("act", lambda e, p=p, qo=qo, g2=g2: e.copy(
                            out=qo[:, g2 * 256:(g2 + 1) * 256].rearrange("p (a b) -> p a b", a=2), in_=p[:, :, 128:256]),
                            reads=[p], writes=[qo.ch(g2)])
                    else:
                        k.op("dve", lambda e, p=p, po=po, g2=g2: e.tensor_copy(
                            out=po[:, g2 * 256:(g2 + 1) * 256].rearrange("p (a b) -> p a b", a=2), in_=p[:, :, 0:128]),
                            reads=[p], writes=[po.ch(g2)])
                        k.op("dve", lambda e, p=p, qo=qo, g2=g2: e.tensor_copy(
                            out=qo[:, g2 * 256:(g2 + 1) * 256].rearrange("p (a b) -> p a b", a=2), in_=p[:, :, 128:256]),
                            reads=[p], writes=[qo.ch(g2)])
                r0 = t * 512 + q * 128
                k.dma(PQ[0, r0:r0 + 128, :], po[:], reads=[po], sem=so[io % 2])
                k.dma(PQ[1, r0:r0 + 128, :], qo[:], reads=[qo], sem=so[io % 2])
                io += 1
        k.end()
        if self.test == "fnet" and self.stop_at == 1:
            return
        k.begin()
        CT, ST = self.W("fnet", "fn_nct"), self.W("fnet", "fn_nst")
        ps = [k.ptile([128, 512]) for _ in range(8)]
        pt0 = [k.tile([128, 512]) for _ in range(3)]
        qt0 = [k.tile([128, 512]) for _ in range(3)]
        ct0 = [k.tile([128, 512]) for _ in range(3)]
        st0 = [k.tile([128, 512]) for _ in range(3)]
        pt = [k.tile([128, 512], F32R) for _ in range(3)]
        qt = [k.tile([128, 512], F32R) for _ in range(3)]
        ct = [k.tile([128, 512], F32R) for _ in range(3)]
        st_ = [k.tile([128, 512], F32R) for _ in range(3)]
        sp_ = [k.dsem() for _ in range(3)]
        sq_ = [k.dsem() for _ in range(3)]
        sc_ = [k.dsem() for _ in range(3)]
        ss_ = [k.dsem() for _ in range(3)]
        ob = [k.tile([128, 4, 512], nsub=4) for _ in range(2)]
        so = [k.dsem() for _ in range(2)]
        mv_ = MX.rearrange("(c p) t -> p c t", p=128)
        it = 0
        io = 0
        for kb in range(T // 512):
            for half in range(2):
                pp = [ps[(io % 2) * 4 + j] for j in range(4)]
                for tt in range(T // 128):
                    i = it % 3
                    it += 1
                    k.dma(pt0[i][:], PQ[0, tt * 128:(tt + 1) * 128, half * 512:(half + 1) * 512], writes=[pt0[i]], sem=sp_[i], q="sp")
                    k.dma(qt0[i][:], PQ[1, tt * 128:(tt + 1) * 128, half * 512:(half + 1) * 512], writes=[qt0[i]], sem=sq_[i], q="sp")
                    k.dma(ct0[i][:], CT[tt * 128:(tt + 1) * 128, kb * 512:(kb + 1) * 512], writes=[ct0[i]], sem=sc_[i], q="sp")
                    k.dma(st0[i][:], ST[tt * 128:(tt + 1) * 128, kb * 512:(kb + 1) * 512], writes=[st0[i]], sem=ss_[i], q="sp")
                    k.rnd("pool", pt[i][:], pt0[i][:], [pt0[i]], [pt[i]])
                    k.rnd("dve", qt[i][:], qt0[i][:], [qt0[i]], [qt[i]])
                    k.rnd("pool", ct[i][:], ct0[i][:], [ct0[i]], [ct[i]])
                    k.rnd("act", st_[i][:], st0[i][:], [st0[i]], [st_[i]])
                    for j in range(4):
                        k.op("pe", lambda e, j=j, i=i, tt=tt: e.matmul(
                            pp[j][:], pt[i][:, j * 128:(j + 1) * 128], ct[i][:], start=(tt == 0), stop=False),
                            reads=[pt[i], ct[i]], writes=[pp[j]], accum=True)
                        k.op("pe", lambda e, j=j, i=i, tt=tt: e.matmul(
                            pp[j][:], qt[i][:, j * 128:(j + 1) * 128], st_[i][:], start=False, stop=(tt == T // 128 - 1)),
                            reads=[qt[i], st_[i]], writes=[pp[j]], accum=True)
                o = ob[io % 2]
                for j in range(4):
                    if j % 2 == 0:
                        k.op("act", lambda e, j=j, o=o: e.copy(out=o[:, j, :], in_=pp[j][:]), reads=[pp[j]], writes=[o.ch(j)])
                    else:
                        k.op("dve", lambda e, j=j, o=o: e.tensor_copy(out=o[:, j, :], in_=pp[j][:]), reads=[pp[j]], writes=[o.ch(j)])
                k.dma(mv_[:, half * 4:(half + 1) * 4, kb * 512:(kb + 1) * 512], o[:], reads=[o], sem=so[io % 2])
                io += 1
        k.end()
        if self.test == "fnet" and self.stop_at == 2:
            return
        self.gemm(MX, self.W("fnet", "fn_wo"), D, D, T, Y, bias_pv="fn_bo")

    def hy_tabs(self, L, names):
        k = self.k
        NH = L // 64
        N1 = 2 * NH
        shp = {"F1": (NH, 2 * N1), "c": (64, N1), "s": (64, N1), "cT": (N1, 64), "sT": (N1, 64),
               "C2": (64, 64), "S2": (64, 64), "nS2": (64, 64), "RA": (64, 128), "RB": (64, 128),
               "C1": (N1, NH), "nS1": (N1, NH)}
        out = {}
        sm = k.dsem()
        for nm in names:
            r, c = shp[nm]
            t = k.tile([max(r, 1), c])
            key = nm if nm in ("C2", "S2", "nS2", "RA", "RB") else "%s_%d" % (nm, L)
            k.dma(t[:], self.W("hy", key), writes=[t], sem=sm)
            out[nm] = t
        return out

    def hy_filters(self, j, L, HF, RN):
        k = self.k
        N = 2 * L
        k.begin()
        s0 = k.dsem()
        zT = k.tile([33, L])
        w1 = k.tile([33, 64])
        w2 = k.tile([64, 2, 64])
        w3 = k.tile([64, 4096])
        hv = k.tile([64, 4])
        k.dma(zT[:], self.W("hy", "zT_%d" % L), writes=[zT], sem=s0)
        k.dma(w1[:], self.W("hy", "f_w1")[j * 33:(j + 1) * 33, :], writes=[w1], sem=s0)
        for n in range(2):
            k.dma(w2[:, n, :], self.W("hy", "f_w2")[(j * 2 + n) * 64:(j * 2 + n + 1) * 64, :], writes=[w2], sem=s0)
        k.dma(w3[:], self.W("hy", "f_w3")[j * 64:(j + 1) * 64, :], writes=[w3], sem=s0)
        k.dma(hv[:], self.W("hy", "hyv")[j * 64:(j + 1) * 64, :], writes=[hv], sem=s0)
        a = [k.tile([64, L]) for _ in range(3)]
        ps = [k.ptile([128, 512]) for _ in range(2)]
        tmp = [k.tile([64, 512]) for _ in range(2)]
        tmpi = [k.tile([64, 512], I32) for _ in range(2)]
        tmpf = [k.tile([64, 512]) for _ in range(2)]
        CT = min(512, L)
        it = 0
        for layer in range(3):
            for ct in range(L // CT):
                p = ps[it % 2]
                tb = tmp[it % 2]
                it += 1
                sl = slice(ct * CT, (ct + 1) * CT)
                if layer == 0:
                    k.op("pe", lambda e, p=p, sl=sl: e.matmul(p[:64, :CT], w1[:, :], zT[:, sl], start=True, stop=True),
                         reads=[w1, zT], writes=[p])
                else:
                    src = a[layer - 1]
                    k.op("pe", lambda e, p=p, sl=sl, src=src, layer=layer: e.matmul(
                        p[:64, :CT], w2[:, layer - 1, :], src[:, sl], start=True, stop=True),
                        reads=[w2, src], writes=[p])
                bcol = hv[:, 0:1] if layer == 0 else hv[:, 1 + layer:2 + layer]
                k.op("dve", lambda e, p=p, tb=tb, bcol=bcol: e.tensor_scalar(
                    out=tb[:, :CT], in0=p[:64, :CT], scalar1=bcol, scalar2=hv[:, 1:2], op0=ALU.add, op1=ALU.mult),
                    reads=[p, hv], writes=[tb])
                k.op("dve", lambda e, tb=tb: e.tensor_scalar(
                    out=tb[:, :CT], in0=tb[:, :CT], scalar1=1.0 / (2 * math.pi), scalar2=16.0, op0=ALU.mult, op1=ALU.add),
                    reads=[tb], writes=[tb])
                ti, tf = tmpi[it % 2], tmpf[it % 2]
                k.op("dve", lambda e, tb=tb, ti=ti: e.tensor_copy(out=ti[:, :CT], in_=tb[:, :CT]), reads=[tb], writes=[ti])
                k.op("dve", lambda e, tf=tf, ti=ti: e.tensor_copy(out=tf[:, :CT], in_=ti[:, :CT]), reads=[ti], writes=[tf])
                k.op("dve", lambda e, tb=tb, tf=tf: e.tensor_tensor(out=tb[:, :CT], in0=tb[:, :CT], in1=tf[:, :CT], op=ALU.subtract),
                     reads=[tb, tf], writes=[tb])
                k.op("dve", lambda e, tb=tb, tf=tf: e.scalar_tensor_tensor(
                    out=tf[:, :CT], in0=tb[:, :CT], scalar=0.5, in1=tb[:, :CT], op0=ALU.is_gt, op1=ALU.subtract),
                    reads=[tb], writes=[tf])
                dst = a[layer]
                k.op("act", lambda e, tf=tf, dst=dst, sl=sl: e.activation(
                    out=dst[:, sl], in_=tf[:, :CT], func=AF.Sin, scale=-2 * math.pi),
                    reads=[tf], writes=[dst], accum=True)
        a3 = a[2]
        win = [k.tile([128, 512]) for _ in range(2)]
        sw = [k.dsem() for _ in range(2)]
        hs = [k.tile([128, 512]) for _ in range(2)]
        ha = [k.tile([128, 512]) for _ in range(2)]
        so = [k.dsem() for _ in range(2)]
        pn = k.ptile([128, 512])
        nrm = k.tile([128, 8, 512], nsub=8)
        WIN = self.W("hy", "win_%d" % L)
        LT = min(128, L)
        nlt = L // LT
        it = 0
        for ct in range(8):
            dcol = (ct % 2) * 512
            for lt in range(nlt):
                i = it % 2
                it += 1
                p = ps[i]
                k.dma(win[i][:LT, :], WIN[lt * LT:(lt + 1) * LT, dcol:dcol + 512], writes=[win[i]], sem=sw[i])
                k.op("pe", lambda e, p=p, lt=lt, ct=ct: e.matmul(
                    p[:LT, :], a3[:, lt * LT:(lt + 1) * LT], w3[:, ct * 512:(ct + 1) * 512], start=True, stop=True),
                    reads=[a3, w3], writes=[p])
                k.op("dve", lambda e, p=p, i=i: e.tensor_tensor(out=hs[i][:LT, :], in0=p[:LT, :], in1=win[i][:LT, :], op=ALU.mult),
                     reads=[p, win[i]], writes=[hs[i]])
                if ct >= 4 and lt == 0:
                    k.op("dve", lambda e, i=i: e.memset(hs[i][0:1, :], 0.0), reads=[hs[i]], writes=[hs[i]])
                k.dma(HF[lt * LT:(lt + 1) * LT, ct * 512:(ct + 1) * 512], hs[i][:LT, :], reads=[hs[i]], sem=so[i])
                k.op("act", lambda e, i=i: e.activation(out=ha[i][:LT, :], in_=hs[i][:LT, :], func=AF.Abs),
                     reads=[hs[i]], writes=[ha[i]])
                k.op("pe", lambda e, i=i, lt=lt: e.matmul(pn[:, :], self.ones[:LT, :], ha[i][:LT, :],
                                                          start=(lt == 0), stop=(lt == nlt - 1)),
                     reads=[self.ones, ha[i]], writes=[pn], accum=True)
            k.op("act", lambda e, ct=ct: e.copy(out=nrm[:, ct, :], in_=pn[:, :]), reads=[pn], writes=[nrm.ch(ct)])
        rn = k.tile([128, 8, 512], nsub=8)
        for q in range(4):
            k.op("dve", lambda e, q=q: e.tensor_tensor(out=rn[:, q, :], in0=nrm[:, q, :], in1=nrm[:, q + 4, :], op=ALU.add),
                 reads=[nrm.ch(q), nrm.ch(q + 4)], writes=[rn.ch(q)])
            k.op("dve", lambda e, q=q: e.tensor_scalar(out=rn[:, q, :], in0=rn[:, q, :], scalar1=float(N), scalar2=None, op0=ALU.mult),
                 reads=[rn.ch(q)], writes=[rn.ch(q)])
            k.op("dve", lambda e, q=q: e.reciprocal(out=rn[:, q, :], in_=rn[:, q, :]), reads=[rn.ch(q)], writes=[rn.ch(q)])
            k.op("pool", lambda e, q=q: e.tensor_copy(out=rn[:, q + 4, :], in_=rn[:, q, :]), reads=[rn.ch(q)], writes=[rn.ch(q + 4)])
        sr = k.dsem()
        k.dma(RN[0:1, :], rn[0:1, :, :].rearrange("p a b -> p (a b)"), reads=[rn], sem=sr)
        k.end()

    def _fft_fwd(self, L, tb, zs, g0, G, A, Bt, X):
        k = self.k
        NH = L // 64
        N1 = 2 * NH
        for g in range(G):
            k.op("pe", lambda e, g=g: e.matmul(A[:64, g, 0:2 * N1], zs[:NH, :, g0 + g], tb["F1"][:NH, :],
                                               start=True, stop=True),
                 reads=[zs, tb["F1"]], writes=[A], accum=True)
        Ar, Ai = A[:64, :G, 0:N1], A[:64, :G, N1:2 * N1]
        cb = tb["c"][:, :].unsqueeze(1).to_broadcast([64, G, N1])
        sb = tb["s"][:, :].unsqueeze(1).to_broadcast([64, G, N1])
        t1, t2, t3, t4, Br, Bi = Bt
        v = lambda t: t[:64, :G * N1].rearrange("p (g n) -> p g n", g=G)
        k.op("dve", lambda e: e.tensor_tensor(out=v(t1), in0=Ar, in1=cb, op=ALU.mult), reads=[A, tb["c"]], writes=[t1])
        k.op("dve", lambda e: e.tensor_tensor(out=v(t2), in0=Ai, in1=sb, op=ALU.mult), reads=[A, tb["s"]], writes=[t2])
        k.op("dve", lambda e: e.tensor_tensor(out=v(t3), in0=Ai, in1=cb, op=ALU.mult), reads=[A, tb["c"]], writes=[t3])
        k.op("dve", lambda e: e.tensor_tensor(out=v(t4), in0=Ar, in1=sb, op=ALU.mult), reads=[A, tb["s"]], writes=[t4])
        k.op("pool", lambda e: e.tensor_tensor(out=Br[:64, :G * N1], in0=t1[:64, :G * N1], in1=t2[:64, :G * N1], op=ALU.add),
             reads=[t1, t2], writes=[Br])
        k.op("pool", lambda e: e.tensor_tensor(out=Bi[:64, :G * N1], in0=t3[:64, :G * N1], in1=t4[:64, :G * N1], op=ALU.subtract),
             reads=[t3, t4], writes=[Bi])
        W_ = G * N1
        k.op("pe", lambda e: e.matmul(X["r"][:64, :W_], tb["C2"][:, :], Br[:64, :W_], start=True, stop=False),
             reads=[tb["C2"], Br], writes=[X["r"]], accum=True)
        k.op("pe", lambda e: e.matmul(X["r"][:64, :W_], tb["S2"][:, :], Bi[:64, :W_], start=False, stop=True),
             reads=[tb["S2"], Bi], writes=[X["r"]], accum=True)
        k.op("pe", lambda e: e.matmul(X["i"][:64, :W_], tb["C2"][:, :], Bi[:64, :W_], start=True, stop=False),
             reads=[tb["C2"], Bi], writes=[X["i"]], accum=True)
        k.op("pe", lambda e: e.matmul(X["i"][:64, :W_], tb["nS2"][:, :], Br[:64, :W_], start=False, stop=True),
             reads=[tb["nS2"], Br], writes=[X["i"]], accum=True)

    def hy_filter_fft(self, L, HF, RN, SPEC):
        k = self.k
        NH = L // 64
        N1 = 2 * NH
        G = 512 // N1
        k.begin()
        tb = self.hy_tabs(L, ["F1", "c", "s", "C2", "S2", "nS2"])
        rnb = k.tile([64, 4096])
        s0 = k.dsem()
        k.dma(rnb[:], RN[0:1, :].partition_broadcast(64), writes=[rnb], sem=s0)
        zs = [k.tile([64, 64, 128]) for _ in range(2)]
        sz = [k.dsem() for _ in range(2)]
        A = k.ptile([128, G, 2 * N1])
        X = {"r": k.ptile([128, 512]), "i": k.ptile([128, 512])}
        Bt = [k.tile([64, 512]) for _ in range(6)]
        xo = [[k.tile([64, 512]) for _ in range(2)] for _ in range(2)]
        so = [k.dsem() for _ in range(2)]
        hv = HF.rearrange("(a b) c -> a b c", b=64)
        io = 0
        for cb in range(32):
            z = zs[cb % 2]
            k.dma(z[:NH, :, :], hv[:, :, cb * 128:(cb + 1) * 128], writes=[z], sem=sz[cb % 2])
            for sb in range(128 // G):
                g0 = sb * G
                ch0 = cb * 128 + g0
                self._fft_fwd(L, tb, z, g0, G, A, Bt, X)
                rv = rnb[:, ch0:ch0 + G].unsqueeze(2).to_broadcast([64, G, N1])
                for ri, nm in enumerate(("r", "i")):
                    o = xo[io % 2][ri]
                    k.op("dve", lambda e, o=o, nm=nm, rv=rv: e.tensor_tensor(
                        out=o[:, :G * N1].rearrange("p (g n) -> p g n", g=G),
                        in0=X[nm][:64, :G * N1].rearrange("p (g n) -> p g n", g=G), in1=rv, op=ALU.mult),
                        reads=[X[nm], rnb], writes=[o])
                    k.dma(SPEC[ri, :, ch0:ch0 + G, :], o[:, :G * N1].rearrange("p (g n) -> p g n", g=G),
                          reads=[o], sem=so[io % 2])
                io += 1
        k.end()

    def hy_conv(self, L, Z, zc0, XG, xc0, SPEC, o, bias_ap, OUT):
        k = self.k
        NH = L // 64
        N1 = 2 * NH
        G = 4 if N1 == 128 else 8
        k.begin()
        tb = self.hy_tabs(L, ["F1", "c", "s", "C2", "S2", "nS2", "RA", "RB", "cT", "sT", "C1", "nS1"])
        brow = k.tile([64, 1024])
        s0 = k.dsem()
        k.dma(brow[:], bias_ap.partition_broadcast(64), writes=[brow], sem=s0)
        zs = [k.tile([64, 64, 128]) for _ in range(2)]
        xs = [k.tile([64, 64, 128]) for _ in range(2)]
        sz = [k.dsem() for _ in range(2)]
        sx = [k.dsem() for _ in range(2)]
        so = [k.dsem() for _ in range(2)]
        A = k.ptile([128, G, 2 * N1])
        X = {"r": k.ptile([128, 512]), "i": k.ptile([128, 512])}
        Zp = k.ptile([128, G, 128])
        yp = k.ptile([128, 512])
        Bt = [k.tile([64, 512]) for _ in range(6)]
        Kt = [[k.tile([64, 512]) for _ in range(4)] for _ in range(2)]
        sk = [k.dsem() for _ in range(2)]
        Kc = [k.tile([64, 512]) for _ in range(2)]
        Yt = [k.tile([64, 512]) for _ in range(6)]
        Zt = [k.tile([128, G * 64]) for _ in range(6)]
        et = [k.tile([64, 64, G]) for _ in range(2)]
        zv = Z.rearrange("(a b) c -> a b c", b=64)
        xv = XG.rearrange("(a b) c -> a b c", b=64)
        ov = OUT.rearrange("(a b) c -> a b c", b=64)
        W_ = G * N1
        ik = 0
        for cb in range(8):
            z, x = zs[cb % 2], xs[cb % 2]
            k.dma(z[:NH, :, :], zv[:, :, zc0 + cb * 128:zc0 + (cb + 1) * 128], writes=[z], sem=sz[cb % 2])
            k.dma(x[:NH, :, :], xv[:, :, xc0 + cb * 128:xc0 + (cb + 1) * 128], writes=[x], sem=sx[cb % 2])
            for sb in range(128 // G):
                g0 = sb * G
                d0 = cb * 128 + g0
                kt = Kt[ik % 2]
                for q, (ri, dr) in enumerate(((0, 0), (1, 0), (0, 1), (1, 1))):
                    ch0 = dr * 2048 + o * 1024 + d0
                    k.dma(kt[q][:, :W_].rearrange("p (g n) -> p g n", g=G), SPEC[ri, :, ch0:ch0 + G, :],
                          writes=[kt[q]], sem=sk[ik % 2])
                ik += 1
                self._fft_fwd(L, tb, z, g0, G, A, Bt, X)
                Kr, Ki = Kc
                k.op("pool", lambda e, kt=kt: e.tensor_tensor(out=Kr[:, :W_], in0=kt[0][:, :W_], in1=kt[2][:, :W_], op=ALU.add),
                     reads=[kt[0], kt[2]], writes=[Kr])
                k.op("pool", lambda e, kt=kt: e.tensor_tensor(out=Ki[:, :W_], in0=kt[1][:, :W_], in1=kt[3][:, :W_], op=ALU.subtract),
                     reads=[kt[1], kt[3]], writes=[Ki])
                y1, y2, y3, y4, Yr, Yi = Yt
                k.op("dve", lambda e: e.tensor_tensor(out=y1[:, :W_], in0=X["r"][:64, :W_], in1=Kr[:, :W_], op=ALU.mult), reads=[X["r"], Kr], writes=[y1])
                k.op("dve", lambda e: e.tensor_tensor(out=y2[:, :W_], in0=X["i"][:64, :W_], in1=Ki[:, :W_], op=ALU.mult), reads=[X["i"], Ki], writes=[y2])
                k.op("dve", lambda e: e.tensor_tensor(out=y3[:, :W_], in0=X["r"][:64, :W_], in1=Ki[:, :W_], op=ALU.mult), reads=[X["r"], Ki], writes=[y3])
                k.op("dve", lambda e: e.tensor_tensor(out=y4[:, :W_], in0=X["i"][:64, :W_], in1=Kr[:, :W_], op=ALU.mult), reads=[X["i"], Kr], writes=[y4])
                k.op("pool", lambda e: e.tensor_tensor(out=Yr[:, :W_], in0=y1[:, :W_], in1=y2[:, :W_], op=ALU.subtract), reads=[y1, y2], writes=[Yr])
                k.op("pool", lambda e: e.tensor_tensor(out=Yi[:, :W_], in0=y3[:, :W_], in1=y4[:, :W_], op=ALU.add), reads=[y3, y4], writes=[Yi])
                for g in range(G):
                    k.op("pe", lambda e, g=g: e.matmul(Zp[:N1, g, :], Yr[:, g * N1:(g + 1) * N1], tb["RA"][:, :], start=True, stop=False),
                         reads=[Yr, tb["RA"]], writes=[Zp], accum=True)
                    k.op("pe", lambda e, g=g: e.matmul(Zp[:N1, g, :], Yi[:, g * N1:(g + 1) * N1], tb["RB"][:, :], start=False, stop=True),
                         reads=[Yi, tb["RB"]], writes=[Zp], accum=True)
                Zr, Zi = Zp[:N1, :, 0:64], Zp[:N1, :, 64:128]
                cT = tb["cT"][:N1, :].unsqueeze(1).to_broadcast([N1, G, 64])
                sT = tb["sT"][:N1, :].unsqueeze(1).to_broadcast([N1, G, 64])
                u1, u2, u3, u4, Zr2, Zi2 = Zt
                v = lambda t: t[:N1, :].rearrange("p (g n) -> p g n", g=G)
                k.op("dve", lambda e: e.tensor_tensor(out=v(u1), in0=Zr, in1=cT, op=ALU.mult), reads=[Zp, tb["cT"]], writes=[u1])
                k.op("dve", lambda e: e.tensor_tensor(out=v(u2), in0=Zi, in1=sT, op=ALU.mult), reads=[Zp, tb["sT"]], writes=[u2])
                k.op("dve", lambda e: e.tensor_tensor(out=v(u3), in0=Zr, in1=sT, op=ALU.mult), reads=[Zp, tb["sT"]], writes=[u3])
                k.op("dve", lambda e: e.tensor_tensor(out=v(u4), in0=Zi, in1=cT, op=ALU.mult), reads=[Zp, tb["cT"]], writes=[u4])
                k.op("pool", lambda e: e.tensor_tensor(out=Zr2[:N1, :], in0=u1[:N1, :], in1=u2[:N1, :], op=ALU.subtract), reads=[u1, u2], writes=[Zr2])
                k.op("pool", lambda e: e.tensor_tensor(out=Zi2[:N1, :], in0=u3[:N1, :], in1=u4[:N1, :], op=ALU.add), reads=[u3, u4], writes=[Zi2])
                k.op("pe", lambda e: e.matmul(yp[:NH, :G * 64], tb["C1"][:N1, :NH], Zr2[:N1, :], start=True, stop=False),
                     reads=[tb["C1"], Zr2], writes=[yp], accum=True)
                k.op("pe", lambda e: e.matmul(yp[:NH, :G * 64], tb["nS1"][:N1, :NH], Zi2[:N1, :], start=False, stop=True),
                     reads=[tb["nS1"], Zi2], writes=[yp], accum=True)
                e1, e2 = et
                bv = brow[:NH, d0:d0 + G].unsqueeze(1).to_broadcast([NH, 64, G])
                k.op("pool", lambda e, z=z, g0=g0, bv=bv: e.tensor_tensor(out=e1[:NH, :, :], in0=z[:NH, :, g0:g0 + G], in1=bv, op=ALU.mult),
                     reads=[z, brow], writes=[e1])
                k.op("dve", lambda e: e.tensor_tensor(out=e2[:NH, :, :], in0=yp[:NH, :G * 64].rearrange("p (g n) -> p n g", g=G),
                                                      in1=e1[:NH, :, :], op=ALU.add), reads=[yp, e1], writes=[e2])
                k.op("pool", lambda e, x=x, g0=g0: e.tensor_tensor(out=x[:NH, :, g0:g0 + G], in0=x[:NH, :, g0:g0 + G], in1=e2[:NH, :, :], op=ALU.mult),
                     reads=[x, e2], writes=[x])
            k.dma(ov[:, :, cb * 128:(cb + 1) * 128], x[:NH, :, :], reads=[x], sem=so[cb % 2])
        k.end()

    def hy_conv3(self, U, L, j, UC):
        k = self.k
        k.begin()
        C3 = 3 * D
        wr = k.tile([128, 3, C3])
        br = k.tile([128, C3])
        s0 = k.dsem()
        for r in range(3):
            k.dma(wr[:, r, :], self.W("hy", "conv_w")[j * 3 + r:j * 3 + r + 1, :].partition_broadcast(128), writes=[wr], sem=s0)
        k.dma(br[:], self.W("hy", "conv_b")[j:j + 1, :].partition_broadcast(128), writes=[br], sem=s0)
        ub = [[k.tile([128, C3]) for _ in range(3)] for _ in range(2)]
        su = [[k.dsem() for _ in range(3)] for _ in range(2)]
        so = [k.dsem() for _ in range(2)]
        nt = L // 128
        for t in range(nt):
            u0, u1, u2 = ub[t % 2]
            s_ = su[t % 2]
            t0 = t * 128
            if t == 0:
                k.op("pool", lambda e, u0=u0: e.memset(u0[:], 0.0), writes=[u0])
                k.dma(u0[1:128, :], U[0:127, :], writes=[u0], sem=s_[0])
            else:
                k.dma(u0[:], U[t0 - 1:t0 + 127, :], writes=[u0], sem=s_[0])
            k.dma(u1[:], U[t0:t0 + 128, :], writes=[u1], sem=s_[1])
            if t == nt - 1:
                k.op("pool", lambda e, u2=u2: e.memset(u2[:], 0.0), writes=[u2])
                k.dma(u2[0:127, :], U[t0 + 1:t0 + 128, :], writes=[u2], sem=s_[2])
            else:
                k.dma(u2[:], U[t0 + 1:t0 + 129, :], writes=[u2], sem=s_[2])
            k.op("pool", lambda e, u0=u0: e.tensor_tensor(out=u0[:], in0=u0[:], in1=wr[:, 0, :], op=ALU.mult), reads=[u0, wr], writes=[u0])
            k.op("dve", lambda e, u1=u1: e.tensor_tensor(out=u1[:], in0=u1[:], in1=wr[:, 1, :], op=ALU.mult), reads=[u1, wr], writes=[u1])
            k.op("pool", lambda e, u2=u2: e.tensor_tensor(out=u2[:], in0=u2[:], in1=wr[:, 2, :], op=ALU.mult), reads=[u2, wr], writes=[u2])
            k.op("dve", lambda e, u0=u0, u1=u1: e.tensor_tensor(out=u1[:], in0=u1[:], in1=u0[:], op=ALU.add), reads=[u0, u1], writes=[u1])
            k.op("pool", lambda e, u2=u2: e.tensor_tensor(out=u2[:], in0=u2[:], in1=br[:], op=ALU.add), reads=[u2, br], writes=[u2])
            k.op("dve", lambda e, u1=u1, u2=u2: e.tensor_tensor(out=u1[:], in0=u1[:], in1=u2[:], op=ALU.add), reads=[u1, u2], writes=[u1])
            k.dma(UC[t0:t0 + 128, :], u1[:], reads=[u1], sem=so[t % 2])
        k.end()

    def hyena(self, j, HLFM, L, Y, tag):
        N1 = L // 32
        U = self.dt("hyU" + tag, [L, 3 * D])
        UC = self.dt("hyUC" + tag, [L, 3 * D])
        HF = self.dt("hyHF" + tag, [L, 4096])
        RN = self.dt("hyRN" + tag, [1, 4096])
        SPEC = self.dt("hySP" + tag, [2, 64, 4096, N1])
        Z1 = self.dt("hyZ1" + tag, [L, D])
        Z2 = self.dt("hyZ2" + tag, [L, D])
        Z2T = self.dt("hyZ2T" + tag, [D, L])
        self.hy_filters(j, L, HF, RN)
        if self.stop_at == 1:
            return
        self.hy_filter_fft(L, HF, RN, SPEC)
        if self.stop_at == 2:
            return
        for q3 in range(3):
            self.gemm(HLFM, self.W("hy", "w_in")[j * D:(j + 1) * D, q3 * D:(q3 + 1) * D], D, D, L, U[:, q3 * D:(q3 + 1) * D],
                      out_tm=True, bias_row=self.W("hy", "b_in")[j:j + 1, q3 * D:(q3 + 1) * D])
        self.hy_conv3(U, L, j, UC)
        if self.stop_at == 3:
            return
        fb = self.W("hy", "f_bias")
        self.hy_conv(L, UC, 0, UC, D, SPEC, 0, fb[j * 2:j * 2 + 1, :], Z1)
        if self.stop_at == 4:
            return
        self.hy_conv(L, Z1, 0, UC, 2 * D, SPEC, 1, fb[j * 2 + 1:j * 2 + 2, :], Z2)
        self.transpose_in(Z2, Z2T, L)
        self.gemm(Z2T, self.W("hy", "w_out")[j * D:(j + 1) * D, :], D, D, L, Y, bias_pv="hy_b_out%d" % j)

    def rwkv(self, HC, HL, Y):
        k = self.k
        S = TC + T
        NT = S // 128
        NCH = S // 64
        H = 16
        Wr = lambda nm: self.W("rw", nm)
        rows = Wr("rows")
        XN = self.dt("rwXN", [6, D, S])
        k.begin()
        hh = [k.tile([128, T + 2]) for _ in range(2)]
        sh = [k.dsem() for _ in range(2)]
        xx = k.tile([128, T])
        xo = [k.tile([128, T]) for _ in range(2)]
        so = [k.dsem() for _ in range(2)]
        it = io = 0
        for (src, Tn, c0) in ((HC, TC, 0), (HL, T, TC)):
            for c in range(8):
                hb = hh[it % 2]
                it += 1
                k.op("pool", lambda e, hb=hb: e.memset(hb[:], 0.0), writes=[hb])
                k.dma(hb[:, 1:Tn + 1], src[c * 128:(c + 1) * 128, :], writes=[hb], sem=sh[it % 2])
                k.op("dve", lambda e, hb=hb, Tn=Tn: e.tensor_tensor(out=xx[:, :Tn], in0=hb[:, 0:Tn], in1=hb[:, 2:Tn + 2], op=ALU.add),
                     reads=[hb], writes=[xx])
                k.op("dve", lambda e, hb=hb, Tn=Tn: e.scalar_tensor_tensor(out=xx[:, :Tn], in0=xx[:, :Tn], scalar=0.5, in1=hb[:, 1:Tn + 1],
                                                                       op0=ALU.mult, op1=ALU.subtract), reads=[hb, xx], writes=[xx])
                for n in range(6):
                    o = xo[io % 2]
                    mu = self.pvc("rw_mu%d" % n, c)
                    eng = "dve"
                    k.op(eng, lambda e, o=o, hb=hb, Tn=Tn, mu=mu: e.scalar_tensor_tensor(
                        out=o[:, :Tn], in0=xx[:, :Tn], scalar=mu, in1=hb[:, 1:Tn + 1], op0=ALU.mult, op1=ALU.add),
                        reads=[xx, hb, self.pv], writes=[o])
                    k.dma(XN[n, c * 128:(c + 1) * 128, c0:c0 + Tn], o[:, :Tn], reads=[o], sem=so[io % 2])
                    io += 1
        k.end()
        Rm = self.dt("rwR", [S, D]); Km = self.dt("rwK", [S, D]); Vm = self.dt("rwV", [S, D])
        SW = [self.dt("rwSW%d" % n, [S, D]) for n in range(2)]
        Am = [self.dt("rwA%d" % n, [S, D]) for n in range(2)]
        Gm = self.dt("rwG", [S, D])
        HW = self.dt("rwHW", [128, S]); HA = self.dt("rwHA", [128, S]); HG = self.dt("rwHG", [256, S])
        self.gemm(XN[0], Wr("wr"), D, D, S, Rm, out_tm=True)
        self.gemm(XN[2], Wr("wk"), D, D, S, Km, out_tm=True)
        self.gemm(XN[3], Wr("wv"), D, D, S, Vm, out_tm=True)
        self.gemm(XN[1], Wr("w1cat"), D, 128, S, HW, act=AF.Tanh)
        self.gemm(XN[4], Wr("a1cat"), D, 128, S, HA)
        self.gemm(XN[5], Wr("g1pad"), D, 256, S, HG, act=AF.Sigmoid)
        for n in range(2):
            self.gemm(HW, Wr("w2pad%d" % n), 128, D, S, SW[n], out_tm=True, bias_row=rows[n:n + 1, :], act=AF.Sigmoid)
            self.gemm(HA, Wr("a2pad%d" % n), 128, D, S, Am[n], out_tm=True, bias_row=rows[2 + n:3 + n, :], act=AF.Sigmoid)
        self.gemm(HG, Wr("g2pad"), 256, D, S, Gm, out_tm=True)
        XT = self.dt("rwXT", [2, NT, 64, H, 4, 128])
        KD = self.dt("rwKD", [2, S, D]); NAD = self.dt("rwNAD", [2, S, D])
        PCf = self.dt("rwPC", [2, NCH, 64, H])
        BON = self.dt("rwBON", [S, D])
        k.begin()
        s0 = k.dsem()
        rw = k.tile([128, 3, D])
        for i_, r_ in enumerate((4, 5, 6)):
            k.dma(rw[:, i_, :], rows[r_:r_ + 1, :].partition_broadcast(128), writes=[rw], sem=s0)
        tri = [k.tile([128, 128]) for _ in range(2)]
        for n in range(2):
            k.dma(tri[n][:], Wr("TRI%d" % n), writes=[tri[n]], sem=s0)
        chk = k.tile([128, 2])
        k.dma(chk[:], Wr("CHK"), writes=[chk], sem=s0)
        inp = {nm: k.tile([128, D]) for nm in ("r", "k", "v", "sw0", "sw1", "a0", "a1")}
        sin = {nm: k.dsem() for nm in inp}
        srcs = {"r": Rm, "k": Km, "v": Vm, "sw0": SW[0], "sw1": SW[1], "a0": Am[0], "a1": Am[1]}
        wk_ = {nm: k.tile([128, D]) for nm in ("kkq", "kk", "t1", "t2", "lw", "kdir", "kd", "nad", "kt", "rt", "ksum")}
        sm = k.tile([128, H])
        pl = k.ptile([128, 2, 512])
        ptp = [k.ptile([64, 4, 128]) for _ in range(2)]
        ppc = k.ptile([64, H, 2])
        xts = k.tile([64, H, 4, 128])
        pcs = k.tile([64, 2, H])
        sxo = k.dsem(); sko = k.dsem(); sno = k.dsem(); spo = k.dsem(); sbo = k.dsem()
        v3 = lambda t: t[:].rearrange("p (h c) -> p h c", h=H)
        itp = 0
        for tt in range(NT):
            r0 = tt * 128
            for nm in inp:
                k.dma(inp[nm][:], srcs[nm][r0:r0 + 128, :], writes=[inp[nm]], sem=sin[nm])
            R_, K_, V_ = inp["r"], inp["k"], inp["v"]
            kkq, kk, t1, t2, lw, kdir, kd, nad, kt, rt, ksum = (wk_[n_] for n_ in ("kkq", "kk", "t1", "t2", "lw", "kdir", "kd", "nad", "kt", "rt", "ksum"))
            k.op("dve", lambda e: e.tensor_tensor(out=kkq[:], in0=K_[:], in1=rw[:, 0, :], op=ALU.mult), reads=[K_, rw], writes=[kkq])
            k.op("pool", lambda e: e.tensor_tensor(out=t1[:], in0=kkq[:], in1=kkq[:], op=ALU.mult), reads=[kkq], writes=[t1])
            k.op("dve", lambda e: e.tensor_reduce(out=sm[:], in_=v3(t1), axis=AX.X, op=ALU.add), reads=[t1], writes=[sm])
            k.op("dve", lambda e: e.tensor_scalar(out=sm[:], in0=sm[:], scalar1=1e-24, scalar2=None, op0=ALU.max), reads=[sm], writes=[sm])
            k.op("act", lambda e: e.sqrt(out=sm[:], in_=sm[:]), reads=[sm], writes=[sm])
            k.op("dve", lambda e: e.reciprocal(out=sm[:], in_=sm[:]), reads=[sm], writes=[sm])
            k.op("dve", lambda e: e.tensor_tensor(out=v3(kk), in0=v3(kkq), in1=sm[:].unsqueeze(2).to_broadcast([128, H, 64]), op=ALU.mult),
                 reads=[kkq, sm], writes=[kk])
            for n in range(2):
                SWt, At = inp["sw%d" % n], inp["a%d" % n]
                k.op("act", lambda e, SWt=SWt: e.mul(out=lw[:], in_=SWt[:], mul=-0.6065306597126334), reads=[SWt], writes=[lw])
                k.op("dve", lambda e, At=At: e.scalar_tensor_tensor(out=t1[:], in0=At[:], scalar=-1.0, in1=rw[:, 1, :], op0=ALU.add, op1=ALU.mult),
                     reads=[At, rw], writes=[t1])
                k.op("dve", lambda e: e.scalar_tensor_tensor(out=kdir[:], in0=t1[:], scalar=1.0, in1=K_[:], op0=ALU.add, op1=ALU.mult),
                     reads=[t1, K_], writes=[kdir])
                if n == 0:
                    k.op("pool", lambda e: e.tensor_copy(out=ksum[:], in_=kdir[:]), reads=[kdir], writes=[ksum])
                else:
                    k.op("pool", lambda e: e.tensor_tensor(out=ksum[:], in0=ksum[:], in1=kdir[:], op=ALU.add), reads=[kdir, ksum], writes=[ksum])
                k.op("pool", lambda e, At=At: e.tensor_tensor(out=nad[:], in0=kk[:], in1=At[:], op=ALU.mult), reads=[kk, At], writes=[nad])
                for hf in range(2):
                    k.op("pe", lambda e, hf=hf, n=n: e.matmul(pl[:, hf, :], tri[n][:], lw[:, hf * 512:(hf + 1) * 512], start=True, stop=True),
                         reads=[tri[n], lw], writes=[pl], accum=True)
                plv = pl[:].rearrange("p a b -> p (a b)")
                k.op("dve", lambda e: e.tensor_tensor(out=t2[:], in0=plv, in1=lw[:], op=ALU.subtract), reads=[pl, lw], writes=[t2])
                k.op("act", lambda e: e.activation(out=t2[:], in_=t2[:], func=AF.Exp), reads=[t2], writes=[t2])
                k.op("pool", lambda e: e.tensor_tensor(out=kt[:], in0=kk[:], in1=t2[:], op=ALU.mult), reads=[kk, t2], writes=[kt])
                k.op("act", lambda e: e.activation(out=t1[:], in_=plv, func=AF.Exp), reads=[pl], writes=[t1])
                k.op("dve", lambda e: e.tensor_tensor(out=rt[:], in0=R_[:], in1=t1[:], op=ALU.mult), reads=[R_, t1], writes=[rt])
                k.op("act", lambda e: e.activation(out=t2[:], in_=plv, func=AF.Exp, scale=-1.0), reads=[pl], writes=[t2])
                k.op("dve", lambda e: e.tensor_tensor(out=kd[:], in0=kdir[:], in1=t2[:], op=ALU.mult), reads=[kdir, t2], writes=[kd])
                k.op("dve", lambda e: e.scalar_tensor_tensor(out=nad[:], in0=nad[:], scalar=-1.0, in1=t2[:], op0=ALU.mult, op1=ALU.mult),
                     reads=[nad, t2], writes=[nad])
                k.dma(KD[n, r0:r0 + 128, :], kd[:], reads=[kd], sem=sko)
                k.dma(NAD[n, r0:r0 + 128, :], nad[:], reads=[nad], sem=sno)
                for h in range(H):
                    k.op("pe", lambda e, h=h: e.matmul(ppc[:, h, :], lw[:, h * 64:(h + 1) * 64], chk[:], start=True, stop=True),
                         reads=[lw, chk], writes=[ppc], accum=True)
                k.op("act", lambda e: e.activation(out=pcs[:].rearrange("p c h -> p h c"), in_=ppc[:], func=AF.Exp), reads=[ppc], writes=[pcs])
                for cc in range(2):
                    k.dma(PCf[n, tt * 2 + cc, :, :], pcs[:, cc, :], reads=[pcs], sem=spo)
                for q, srct in enumerate((kt, rt, kd, nad)):
                    for hg in range(4):
                        p = ptp[itp % 2]
                        itp += 1
                        for hl in range(4):
                            h = hg * 4 + hl
                            k.op("pe", lambda e, p=p, hl=hl, h=h, srct=srct: e.transpose(p[:, hl, :], srct[:, h * 64:(h + 1) * 64], self.ident[:]),
                                 reads=[srct, self.ident], writes=[p], accum=True)
                        if itp % 2 == 0:
                            k.op("act", lambda e, p=p, hg=hg, q=q: e.copy(out=xts[:, hg * 4:(hg + 1) * 4, q, :], in_=p[:]), reads=[p], writes=[xts], accum=True)
                        else:
                            k.op("dve", lambda e, p=p, hg=hg, q=q: e.tensor_copy(out=xts[:, hg * 4:(hg + 1) * 4, q, :], in_=p[:]), reads=[p], writes=[xts], accum=True)
                k._wait("sp", [(k.esem["act"], k.cnt[k.esem["act"]]), (k.esem["dve"], k.cnt[k.esem["dve"]])])
                k.dma(XT[n, tt], xts[:], reads=[xts], sem=sxo, q="sp")
            k.op("dve", lambda e: e.tensor_tensor(out=t1[:], in0=ksum[:], in1=rw[:, 2, :], op=ALU.mult), reads=[ksum, rw], writes=[t1])
            k.op("dve", lambda e: e.tensor_tensor(out=t1[:], in0=t1[:], in1=R_[:], op=ALU.mult), reads=[t1, R_], writes=[t1])
            k.op("dve", lambda e: e.tensor_reduce(out=sm[:], in_=v3(t1), axis=AX.X, op=ALU.add), reads=[t1], writes=[sm])
            k.op("dve", lambda e: e.tensor_tensor(out=v3(t2), in0=v3(V_), in1=sm[:].unsqueeze(2).to_broadcast([128, H, 64]), op=ALU.mult),
                 reads=[V_, sm], writes=[t2])
            k.dma(BON[r0:r0 + 128, :], t2[:], reads=[t2], sem=sbo)
        k.end()
        if self.stop_at == 3:
            return
        CH = self.dt("rwCH", [2, NCH, 64, H, 256])
        k.begin()
        s0 = k.dsem()
        msk = [k.tile([64, 512]) for _ in range(2)]
        for n in range(2):
            k.dma(msk[n][:], Wr("MASK%d" % n), writes=[msk[n]], sem=s0)
        idn = k.tile([64, 64])
        k.dma(idn[:], Wr("IDN"), writes=[idn], sem=s0)
        xt = [k.tile([64, H, 4, 128]) for _ in range(2)]
        sx = [k.dsem() for _ in range(2)]
        sc = [k.tile([64, H, 320]) for _ in range(2)]
        psc = k.ptile([64, 2, 512])
        pN = k.ptile([64, H, 64]); pL = k.ptile([64, H, 64]); pX = k.ptile([64, H, 64])
        Nb = [k.tile([64, H, 64]) for _ in range(2)]
        Lb = [k.tile([64, H, 64]) for _ in range(2)]
        ILb = k.tile([64, H, 64])
        Xb = [k.tile([64, H, 64]) for _ in range(2)]
        so1 = [k.dsem() for _ in range(2)]
        so2 = [k.dsem() for _ in range(2)]
        idb = idn[:].unsqueeze(1).to_broadcast([64, H, 64])
        ci = 0
        for tt in range(NT):
            for n in range(2):
                x = xt[(tt * 2 + n) % 2]
                k.dma(x[:], XT[n, tt], writes=[x], sem=sx[(tt * 2 + n) % 2])
                for cc in range(2):
                    cs = slice(cc * 64, (cc + 1) * 64)
                    s_ = sc[ci % 2]
                    for hp in range(8):
                        for hl in range(2):
                            h = hp * 2 + hl
                            k.op("pe", lambda e, hl=hl, h=h, x=x, cs=cs: e.matmul(
                                psc[:, hl, 0:128].rearrange("p (a b) -> p a b", a=2), x[:, h, 3, cs], x[:, h, 0:2, cs], start=True, stop=True),
                                reads=[x], writes=[psc], accum=True)
                            k.op("pe", lambda e, hl=hl, h=h, x=x, cs=cs: e.matmul(
                                psc[:, hl, 128:256].rearrange("p (a b) -> p a b", a=2), x[:, h, 2, cs], x[:, h, 0:2, cs], start=True, stop=True),
                                reads=[x], writes=[psc], accum=True)
                            k.op("pe", lambda e, hl=hl, h=h, x=x, cs=cs: e.matmul(
                                psc[:, hl, 256:320], x[:, h, 0, cs], x[:, h, 3, cs], start=True, stop=True),
                                reads=[x], writes=[psc], accum=True)
                        k.op("dve", lambda e, hp=hp, s_=s_, n=n: e.tensor_tensor(
                            out=s_[:, hp * 2:hp * 2 + 2, :], in0=psc[:, :, 0:320],
                            in1=msk[n][:, 0:320].unsqueeze(1).to_broadcast([64, 2, 320]), op=ALU.mult),
                            reads=[psc, msk[n]], writes=[s_], accum=True)
                    N2, L2 = s_[:, :, 0:64], s_[:, :, 256:320]
                    Xc = Xb[0]
                    k.op("pool", lambda e, Xc=Xc, N2=N2: e.tensor_tensor(out=Xc[:], in0=idb, in1=N2, op=ALU.subtract), reads=[idn, s_], writes=[Xc])
                    Ncur, Lcur, Nbuf, Lbuf = N2, L2, s_, s_
                    for j in range(1, 6):
                        Ln = Lb[j % 2]
                        for h in range(H):
                            k.op("pe", lambda e, h=h, Ncur=Ncur, Lcur=Lcur: e.matmul(pL[:, h, :], Ncur[:, h, :], Lcur[:, h, :], start=True, stop=True),
                                 reads=[Nbuf, Lbuf], writes=[pL], accum=True)
                        if j < 5:
                            Nn = Nb[j % 2]
                            for h in range(H):
                                k.op("pe", lambda e, h=h, Ncur=Ncur, Lcur=Lcur: e.matmul(pN[:, h, :], Lcur[:, h, :], Ncur[:, h, :], start=True, stop=True),
                                     reads=[Nbuf, Lbuf], writes=[pN], accum=True)
                            k.op("act", lambda e, Nn=Nn: e.copy(out=Nn[:], in_=pN[:]), reads=[pN], writes=[Nn])
                        k.op("dve", lambda e: e.tensor_tensor(out=ILb[:], in0=pL[:], in1=idb, op=ALU.add), reads=[pL, idn], writes=[ILb])
                        if j < 5:
                            k.op("dve", lambda e, Ln=Ln: e.tensor_copy(out=Ln[:], in_=pL[:]), reads=[pL], writes=[Ln])
                        Xn = Xb[j % 2]
                        for h in range(H):
                            k.op("pe", lambda e, h=h, Xc=Xc: e.matmul(pX[:, h, :], ILb[:, h, :], Xc[:, h, :], start=True, stop=True),
                                 reads=[ILb, Xc], writes=[pX], accum=True)
                        k.op("act", lambda e, Xn=Xn: e.copy(out=Xn[:], in_=pX[:]), reads=[pX], writes=[Xn])
                        Xc = Xn
                        if j < 5:
                            Ncur, Lcur, Nbuf, Lbuf = Nn[:], Ln[:], Nn, Ln
                    chn = tt * 2 + cc
                    k.dma(CH[n, chn, :, :, 0:64], Xc[:], reads=[Xc], sem=so1[ci % 2])
                    k.dma(CH[n, chn, :, :, 64:256], s_[:, :, 64:256], reads=[s_], sem=so2[ci % 2])
                    ci += 1
        k.end()
        if self.stop_at == 4:
            return
        YW = self.dt("rwYW", [2, S, D])
        k.begin()
        M = [k.tile([64, H, 64]) for _ in range(2)]
        for n in range(2):
            k.op("pool", lambda e, n=n: e.memset(M[n][:], 0.0), writes=[M[n]])
        ktrt = [k.tile([64, H, 2, 64]) for _ in range(2)]
        cht = [k.tile([64, H, 256]) for _ in range(2)]
        kdt = [k.tile([64, D]) for _ in range(2)]
        nadt = [k.tile([64, D]) for _ in range(2)]
        vt = [k.tile([64, D]) for _ in range(2)]
        pct = [k.tile([64, H]) for _ in range(2)]
        sl_ = [[k.dsem() for _ in range(6)] for _ in range(2)]
        W0s = k.tile([64, H, 64]); Us = k.tile([64, H, 64])
        Ys = [k.tile([64, H, 64]) for _ in range(2)]
        sy = [k.dsem() for _ in range(2)]
        pW = k.ptile([64, H, 64]); pU = k.ptile([64, H, 64]); pY = k.ptile([64, H, 64]); pM = k.ptile([64, H, 64])
        order = {0: list(range(NCH)), 1: list(range(TC // 64 - 1, -1, -1)) + list(range(NCH - 1, TC // 64 - 1, -1))}
        for step in range(NCH):
            for n in range(2):
                chn = order[n][step]
                tt, cc = chn // 2, chn % 2
                cs = slice(cc * 64, (cc + 1) * 64)
                r0 = chn * 64
                b = n
                sl = sl_[b]
                k.dma(ktrt[b][:], XT[n, tt, :, :, 0:2, cs], writes=[ktrt[b]], sem=sl[0], q="sp")
                k.dma(cht[b][:], CH[n, chn], writes=[cht[b]], sem=sl[1], q="act")
                k.dma(kdt[b][:], KD[n, r0:r0 + 64, :], writes=[kdt[b]], sem=sl[2], q="sp")
                k.dma(nadt[b][:], NAD[n, r0:r0 + 64, :], writes=[nadt[b]], sem=sl[3], q="act")
                k.dma(vt[b][:], Vm[r0:r0 + 64, :], writes=[vt[b]], sem=sl[4], q="sp")
                k.dma(pct[b][:], PCf[n, chn], writes=[pct[b]], sem=sl[5], q="act")
                kr, ch_, kd_, nad_, v_, pc_, Mn = ktrt[b], cht[b], kdt[b], nadt[b], vt[b], pct[b], M[n]
                for h in range(H):
                    hs = slice(h * 64, (h + 1) * 64)
                    k.op("pe", lambda e, h=h: e.matmul(pW[:, h, :], kr[:, h, 0, :], Mn[:, h, :], start=True, stop=False),
                         reads=[kr, Mn], writes=[pW], accum=True)
                    k.op("pe", lambda e, h=h, hs=hs: e.matmul(pW[:, h, :], ch_[:, h, 128:192], v_[:, hs], start=False, stop=True),
                         reads=[ch_, v_], writes=[pW], accum=True)
                k.op("act", lambda e: e.copy(out=W0s[:], in_=pW[:]), reads=[pW], writes=[W0s])
                for h in range(H):
                    k.op("pe", lambda e, h=h: e.matmul(pU[:, h, :], ch_[:, h, 0:64], W0s[:, h, :], start=True, stop=True),
                         reads=[ch_, W0s], writes=[pU], accum=True)
                k.op("dve", lambda e: e.tensor_copy(out=Us[:], in_=pU[:]), reads=[pU], writes=[Us])
                if chn >= TC // 64:
                    ys = Ys[n]
                    for h in range(H):
                        hs = slice(h * 64, (h + 1) * 64)
                        k.op("pe", lambda e, h=h: e.matmul(pY[:, h, :], kr[:, h, 1, :], Mn[:, h, :], start=True, stop=False),
                             reads=[kr, Mn], writes=[pY], accum=True)
                        k.op("pe", lambda e, h=h, hs=hs: e.matmul(pY[:, h, :], ch_[:, h, 192:256], v_[:, hs], start=False, stop=False),
                             reads=[ch_, v_], writes=[pY], accum=True)
                        k.op("pe", lambda e, h=h: e.matmul(pY[:, h, :], ch_[:, h, 64:128], Us[:, h, :], start=False, stop=True),
                             reads=[ch_, Us], writes=[pY], accum=True)
                    k.op("act", lambda e, ys=ys: e.copy(out=ys[:], in_=pY[:]), reads=[pY], writes=[ys])
                    k.dma(YW[n, r0:r0 + 64, :], ys[:].rearrange("p h c -> p (h c)"), reads=[ys], sem=sy[n])
                for h in range(H):
                    hs = slice(h * 64, (h + 1) * 64)
                    k.op("pe", lambda e, h=h, hs=hs: e.matmul(pM[:, h, :], kd_[:, hs], v_[:, hs], start=True, stop=False),
                         reads=[kd_, v_], writes=[pM], accum=True)
                    k.op("pe", lambda e, h=h, hs=hs: e.matmul(pM[:, h, :], nad_[:, hs], Us[:, h, :], start=False, stop=True),
                         reads=[nad_, Us], writes=[pM], accum=True)
                k.op("dve", lambda e, Mn=Mn: e.tensor_tensor(out=Mn[:], in0=Mn[:], in1=pM[:], op=ALU.add), reads=[Mn, pM], writes=[Mn])
                k.op("dve", lambda e, Mn=Mn, pc_=pc_: e.tensor_tensor(out=Mn[:], in0=Mn[:], in1=pc_[:].unsqueeze(2).to_broadcast([64, H, 64]), op=ALU.mult),
                     reads=[Mn, pc_], writes=[Mn])
        k.end()
        if self.stop_at == 5:
            return
        O = self.dt("rwO", [T, D])
        k.begin()
        s0 = k.dsem()
        gw = k.tile([128, 2, D])
        for i_, r_ in enumerate((7, 8)):
            k.dma(gw[:, i_, :], rows[r_:r_ + 1, :].partition_broadcast(128), writes=[gw], sem=s0)
        ya = [k.tile([128, D]) for _ in range(2)]
        yb_ = [k.tile([128, D]) for _ in range(2)]
        bo = [k.tile([128, D]) for _ in range(2)]
        gt = [k.tile([128, D]) for _ in range(2)]
        ss = [[k.dsem() for _ in range(4)] for _ in range(2)]
        sq_ = k.tile([128, D])
        st1 = k.tile([128, H]); st2 = k.tile([128, H])
        so = [k.dsem() for _ in range(2)]
        for t in range(T // 128):
            b = t % 2
            r0 = TC + t * 128
            k.dma(ya[b][:], YW[0, r0:r0 + 128, :], writes=[ya[b]], sem=ss[b][0])
            k.dma(yb_[b][:], YW[1, r0:r0 + 128, :], writes=[yb_[b]], sem=ss[b][1])
            k.dma(bo[b][:], BON[r0:r0 + 128, :], writes=[bo[b]], sem=ss[b][2])
            k.dma(gt[b][:], Gm[r0:r0 + 128, :], writes=[gt[b]], sem=ss[b][3])
            w = ya[b]
            w3_ = w[:].rearrange("p (h c) -> p h c", h=H)
            k.op("dve", lambda e, w=w, b=b: e.tensor_tensor(out=w[:], in0=w[:], in1=yb_[b][:], op=ALU.add), reads=[w, yb_[b]], writes=[w])
            k.op("dve", lambda e, w3_=w3_: e.tensor_reduce(out=st1[:], in_=w3_, axis=AX.X, op=ALU.add), reads=[w], writes=[st1])
            k.op("dve", lambda e: e.tensor_scalar(out=st1[:], in0=st1[:], scalar1=1.0 / 64, scalar2=None, op0=ALU.mult), reads=[st1], writes=[st1])
            k.op("dve", lambda e, w3_=w3_: e.tensor_tensor(out=w3_, in0=w3_, in1=st1[:].unsqueeze(2).to_broadcast([128, H, 64]), op=ALU.subtract),
                 reads=[w, st1], writes=[w])
            k.op("pool", lambda e, w=w: e.tensor_tensor(out=sq_[:], in0=w[:], in1=w[:], op=ALU.mult), reads=[w], writes=[sq_])
            k.op("dve", lambda e: e.tensor_reduce(out=st2[:], in_=sq_[:].rearrange("p (h c) -> p h c", h=H), axis=AX.X, op=ALU.add), reads=[sq_], writes=[st2])
            k.op("dve", lambda e: e.tensor_scalar(out=st2[:], in0=st2[:], scalar1=1.0 / 64, scalar2=64e-5, op0=ALU.mult, op1=ALU.add), reads=[st2], writes=[st2])
            k.op("act", lambda e: e.sqrt(out=st2[:], in_=st2[:]), reads=[st2], writes=[st2])
            k.op("dve", lambda e: e.reciprocal(out=st2[:], in_=st2[:]), reads=[st2], writes=[st2])
            k.op("dve", lambda e, w3_=w3_: e.tensor_tensor(out=w3_, in0=w3_, in1=st2[:].unsqueeze(2).to_broadcast([128, H, 64]), op=ALU.mult),
                 reads=[w, st2], writes=[w])
            k.op("pool", lambda e, w=w: e.tensor_tensor(out=w[:], in0=w[:], in1=gw[:, 0, :], op=ALU.mult), reads=[w, gw], writes=[w])
            k.op("pool", lambda e, w=w: e.tensor_tensor(out=w[:], in0=w[:], in1=gw[:, 1, :], op=ALU.add), reads=[w, gw], writes=[w])
            k.op("dve", lambda e, w=w, b=b: e.tensor_tensor(out=w[:], in0=w[:], in1=bo[b][:], op=ALU.add), reads=[w, bo[b]], writes=[w])
            k.op("dve", lambda e, w=w, b=b: e.tensor_tensor(out=w[:], in0=w[:], in1=gt[b][:], op=ALU.mult), reads=[w, gt[b]], writes=[w])
            k.dma(O[t * 128:(t + 1) * 128, :], w[:], reads=[w], sem=so[b])
        k.end()
        OT = self.dt("rwOT", [D, T])
        self.transpose_in(O, OT, T)
        self.gemm(OT, Wr("wo"), D, D, T, Y)

    def final_dbg(self):
        k = self.k
        k.begin()
        s = k.dsem()
        for name in self.dbg:
            if name == "modv":
                o = self.dt("dbg_modv", [128, DEPTH * 48 * 2], kind="ExternalOutput")
                k.dma(o, self.modv[:].rearrange("p a b c -> p (a b c)"), reads=[self.modv], sem=s)
                continue
            src = self.dram[name]
            o = self.dt("dbg_" + name, list(src.shape), kind="ExternalOutput")
            k.dma(o, src.ap(), sem=s, q="sp")
        k.end()


_CACHE = {}
RUN_KW = {}
LAST = {}
RUN_KW = {}
LAST = {}


def run(inputs, layers=(0, 1, 2, 3), dbg=(), cores=NCORES, test=None, extra=None):
    key = (tuple(layers), tuple(dbg), test)
    if key not in _CACHE:
        _CACHE[key] = Prog(list(layers), dbg, test)
    prog = _CACHE[key]
    blobs, lay = pack_host(inputs, list(layers))
    in_maps = []
    for c in range(cores):
        m = {"x": np.ascontiguousarray(inputs["x"][c]),
             "c": np.ascontiguousarray(inputs["c"][c:c + 1]),
             "ctx": np.ascontiguousarray(inputs["ctx"][c]),
             "c_ctx": np.ascontiguousarray(inputs["c_ctx"][None, :])}
        for piece, flat in blobs.items():
            m["blob_" + piece] = flat
        if extra:
            m.update(extra)
        in_maps.append(m)
    res = run_bass_kernel_spmd(prog.nc, in_maps, core_ids=list(range(cores)), **RUN_KW)
    LAST["exec_ns"] = getattr(res, "exec_time_ns", None)
    return res.results


def kernel(**inputs):
    res = run(inputs)
    return np.stack([r["out"] for r in res], 0).astype(np.float32)
```

```python
import math
import numpy as np
from contextlib import ExitStack
import concourse.bass as bass
import concourse.mybir as mybir
from concourse.bass_utils import run_bass_kernel_spmd

F32 = mybir.dt.float32
F32R = mybir.dt.float32r
I32 = mybir.dt.int32
U32 = mybir.dt.uint32
AF = mybir.ActivationFunctionType
ALU = mybir.AluOpType
AX = mybir.AxisListType

NCORES = 8
D = 1024
T = 4096
TC = 256
DEPTH = 4
NE = 16
FF = 2048
ALPHA = (2 * DEPTH) ** 0.25
BW = 1024


class Buf:
    def __init__(self, name, ap=None, nsub=0):
        self.name = name
        self.t = ap
        self.w = []
        self.r = []
        self.kids = [Buf(name + str(i), ap) for i in range(nsub)]

    def __getitem__(self, idx):
        return self.t[idx]

    def ch(self, i):
        return self.kids[i]

    def leaves(self):
        if not self.kids:
            return [self]
        out = []
        for c in self.kids:
            out += c.leaves()
        return out


def _lv(bufs):
    out = []
    for b in bufs:
        out += b.leaves()
    return out


class KB:
    def __init__(self, nc, n_dma_sems=88):
        self.nc = nc
        self.eng = {"pe": nc.tensor, "dve": nc.vector, "act": nc.scalar,
                    "pool": nc.gpsimd, "sp": nc.sync}
        self.esem = {}
        self.cnt = {}
        self.semh = {}
        self.seen = {k: {} for k in self.eng}
        for k in self.eng:
            nm = "e_" + k
            self.semh[nm] = nc.alloc_semaphore(name=nm)
            self.esem[k] = nm
            self.cnt[nm] = 0
        self.free_dma = []
        for i in range(n_dma_sems):
            nm = "d%d" % i
            self.semh[nm] = nc.alloc_semaphore(name=nm)
            self.cnt[nm] = 0
            self.free_dma.append(nm)
        self.phase_sems = []
        self.stack = None
        self.rr = 0
        self.pstack = ExitStack()
        self.semreg = {}

    def begin(self):
        self.stack = ExitStack()
        self.phase_sems = []

    def end(self):
        self.barrier()
        self.stack.close()
        self.stack = None
        for nm in self.phase_sems:
            self.semreg.pop(nm, None)
        self.free_dma = self.phase_sems + self.free_dma
        self.phase_sems = []

    def dsem(self, persistent=False):
        nm = self.free_dma.pop()
        if not persistent:
            self.phase_sems.append(nm)
        return nm

    def tile(self, shape, dtype=F32, name=None, persistent=False, nsub=0):
        st = self.pstack if persistent else self.stack
        t = st.enter_context(self.nc.sbuf_tensor(list(shape), dtype))
        return Buf(name or "t", t, nsub)

    def ptile(self, shape, dtype=F32, name=None, nsub=0):
        t = self.stack.enter_context(self.nc.psum_tensor(list(shape), dtype))
        return Buf(name or "p", t, nsub)

    def _wait(self, ek, toks):
        need = {}
        for (s, v) in toks:
            if v > need.get(s, 0):
                need[s] = v
        for s, v in need.items():
            if self.seen[ek].get(s, 0) >= v:
                continue
            self.eng[ek].wait_ge(self.semh[s], v)
            self.seen[ek][s] = v

    def rnd(self, ek, dst_ap, src_ap, reads, writes):
        if ek == "act":
            return self.op("act", lambda e: e.copy(out=dst_ap, in_=src_ap), reads=reads, writes=writes)
        return self.op(ek, lambda e: e.tensor_copy(out=dst_ap, in_=src_ap), reads=reads, writes=writes)

    def barrier(self):
        toks = [(s, c) for s, c in self.cnt.items() if c > 0]
        for ek in self.eng:
            self._wait(ek, toks)

    def op(self, ek, fn, reads=(), writes=(), accum=False):
        reads = _lv(reads)
        writes = _lv(writes)
        toks = []
        es = self.esem[ek]
        for b in reads:
            toks += b.w
        for b in writes:
            if accum:
                toks += [t for t in b.w if t[0] != es]
            else:
                toks += b.w
            toks += b.r
        self._wait(ek, toks)
        ins = fn(self.eng[ek])
        self.cnt[es] += 1
        tok = (es, self.cnt[es])
        ins.then_inc(self.semh[es], 1)
        for b in reads:
            b.r.append(tok)
        for b in writes:
            b.w = [tok]
            b.r = []
        return ins

    def dma(self, out_ap, in_ap, reads=(), writes=(), sem=None, q=None, fn=None, **kw):
        reads = _lv(reads)
        writes = _lv(writes)
        if q is None:
            q = ("sp", "act")[self.rr % 2]
            self.rr += 1
        toks = []
        for b in reads:
            toks += b.w
        for b in writes:
            toks += [t for t in b.w if t[0] != sem]
            toks += b.r
        self._wait(q, toks)
        if fn is not None:
            ins = fn(self.eng[q])
        else:
            ins = self.eng[q].dma_start(out=out_ap, in_=in_ap, **kw)
        self.cnt[sem] += 16
        tok = (sem, self.cnt[sem])
        ins.then_inc(self.semh[sem], 16)
        reg = self.semreg.setdefault(sem, {})
        for b in reg.values():
            b.w = [tok if t[0] == sem else t for t in b.w]
            b.r = [tok if t[0] == sem else t for t in b.r]
        for b in reads:
            b.r.append(tok)
            reg[id(b)] = b
        for b in writes:
            b.w = [tok]
            b.r = []
            reg[id(b)] = b
        return ins


def _pv_cols():
    cols = {}
    off = 0

    def add(name, n):
        nonlocal off
        cols[name] = (off, n // 128)
        off += n // 128
    for i in range(DEPTH):
        add("mod_b%d" % i, 6 * D)
        for j in range(2):
            add("ln_g%d_%d" % (i, j), D)
            add("ln_b%d_%d" % (i, j), D)
    for j in range(2):
        add("hy_b_out%d" % j, D)
    for n in range(6):
        add("rw_mu%d" % n, D)
    add("fn_bo", D)
    return cols, off


PV_COLS, PV_N = _pv_cols()


def blob_spec(layers):
    sp = {"misc": []}
    m = sp["misc"]
    m.append(("ident", (128, 128)))
    m.append(("pv", (128, 1024)))
    m.append(("pos", (T, D)))
    m.append(("mod_w", (DEPTH * D, 6 * D)))
    m.append(("moe_router", (DEPTH * D, NE)))
    for i in layers:
        sp["moe%d" % i] = [("w1", (NE * D, FF)), ("w3", (NE * D, FF)), ("w2", (NE * FF, D))]
    if 0 in layers or 3 in layers:
        h = [("w_in", (2 * D, 3 * D)), ("b_in", (2, 3 * D)), ("conv_w", (6, 3 * D)), ("conv_b", (2, 3 * D)),
             ("f_w1", (66, 64)), ("hyv", (128, 4)), ("f_w2", (256, 64)), ("f_w3", (128, 4096)),
             ("f_bias", (4, D)), ("w_out", (2 * D, D)),
             ("C2", (64, 64)), ("S2", (64, 64)), ("nS2", (64, 64)), ("RA", (64, 128)), ("RB", (64, 128))]
        for L in (T, TC):
            NH = L // 64
            N1 = 2 * NH
            h += [("zT_%d" % L, (33, L)), ("win_%d" % L, (L, D)), ("F1_%d" % L, (NH, 2 * N1)),
                  ("c_%d" % L, (64, N1)), ("s_%d" % L, (64, N1)), ("cT_%d" % L, (N1, 64)), ("sT_%d" % L, (N1, 64)),
                  ("C1_%d" % L, (N1, NH)), ("nS1_%d" % L, (N1, NH))]
        sp["hy"] = h
    if 1 in layers:
        sp["rw"] = [("wr", (D, D)), ("wk", (D, D)), ("wv", (D, D)), ("wo", (D, D)),
                    ("w1cat", (D, 128)), ("a1cat", (D, 128)), ("g1pad", (D, 256)),
                    ("w2pad0", (128, D)), ("w2pad1", (128, D)), ("a2pad0", (128, D)), ("a2pad1", (128, D)),
                    ("g2pad", (256, D)), ("rows", (9, D)),
                    ("TRI0", (128, 128)), ("TRI1", (128, 128)), ("CHK", (128, 2)),
                    ("MASK0", (64, 512)), ("MASK1", (64, 512)), ("IDN", (64, 64))]
    if 2 in layers:
        sp["fnet"] = [("fn_cs", (128, 256)), ("fn_wo", (D, D)), ("fn_nct", (T, T)), ("fn_nst", (T, T))]
    return sp


def blob_layout(spec):
    lay = {}
    for piece, items in spec.items():
        off = 0
        ent = {}
        for name, (R, C) in items:
            n = R * C
            rows = (n + BW - 1) // BW
            ent[name] = (off, rows, R, C)
            off += rows
        tot = (off + 8 * 16 - 1) // (8 * 16) * (8 * 16)
        lay[piece] = (tot, ent)
    return lay


def grid_pos_embed():
    rows = T // 64
    r_idx = np.repeat(np.arange(rows, dtype=np.float32), 64)
    c_idx = np.tile(np.arange(64, dtype=np.float32), rows)
    quarter = D // 4
    omega = (1.0 / (10000.0 ** (np.arange(quarter, dtype=np.float32) / np.float32(quarter)))).astype(np.float32)

    def emb(p):
        a = (p[:, None] * omega[None, :]).astype(np.float32)
        return np.concatenate([np.sin(a), np.cos(a)], -1)
    return np.concatenate([emb(r_idx), emb(c_idx)], -1).astype(np.float32)


def hy_const_tables():
    out = {}
    n2 = np.arange(64)
    a2 = 2 * np.pi * (np.outer(n2, n2) % 64) / 64.0
    C2, S2 = np.cos(a2), np.sin(a2)
    out["C2"], out["S2"], out["nS2"] = C2, S2, -S2
    out["RA"] = np.concatenate([C2, S2], 1)
    out["RB"] = np.concatenate([-S2, C2], 1)
    for L in (T, TC):
        NH = L // 64
        N1 = 2 * NH
        N = 64 * N1
        a1 = 2 * np.pi * (np.outer(np.arange(NH), np.arange(N1)) % N1) / N1
        out["F1_%d" % L] = np.concatenate([np.cos(a1), -np.sin(a1)], 1)
        at = 2 * np.pi * (np.outer(n2, np.arange(N1)) % N) / N
        out["c_%d" % L], out["s_%d" % L] = np.cos(at), np.sin(at)
        out["cT_%d" % L], out["sT_%d" % L] = np.cos(at).T, np.sin(at).T
        ai = 2 * np.pi * (np.outer(np.arange(N1), np.arange(NH)) % N1) / N1
        out["C1_%d" % L], out["nS1_%d" % L] = np.cos(ai), -np.sin(ai)
        pos = np.arange(L, dtype=np.float32)
        t01 = (pos / np.float32(max(L - 1, 1))).astype(np.float32)
        f = np.linspace(1e-4, 15, 16, dtype=np.float32)
        ang = (f[None, :] * (np.float32(2.0 * math.pi) * pos / np.float32(L))[:, None]).astype(np.float32)
        z = np.concatenate([t01[:, None], np.cos(ang), -np.sin(ang)], -1).astype(np.float32)
        out["zT_%d" % L] = z.T
        deltas = np.linspace(math.log(1e-2) / 1.5, math.log(1e-2) / 0.3, D, dtype=np.float32)
        out["win_%d" % L] = np.exp(-t01[:, None] * np.abs(deltas)[None, :]).astype(np.float32)
    return {k_: np.ascontiguousarray(v, dtype=np.float32) for k_, v in out.items()}


def rw_const_tables():
    out = {}
    i = np.arange(128)
    same = (i[:, None] // 64) == (i[None, :] // 64)
    out["TRI0"] = (same & (i[:, None] <= i[None, :])).astype(np.float32)
    out["TRI1"] = (same & (i[:, None] >= i[None, :])).astype(np.float32)
    out["CHK"] = np.stack([(i < 64), (i >= 64)], 1).astype(np.float32)
    a = np.arange(64)
    for n in range(2):
        before = (a[:, None] < a[None, :]) if n == 0 else (a[:, None] > a[None, :])
        ateq = before | (a[:, None] == a[None, :])
        m = np.concatenate([-before.astype(np.float32), ateq.astype(np.float32), before.astype(np.float32),
                            ateq.astype(np.float32), -before.T.astype(np.float32), np.zeros((64, 192), np.float32)], 1)
        out["MASK%d" % n] = m
    out["IDN"] = np.eye(64, dtype=np.float32)
    return out


def pack_host(inputs, layers):
    spec = blob_spec(layers)
    lay = blob_layout(spec)
    src = {}
    src["ident"] = np.eye(128, dtype=np.float32)
    pv = np.zeros((128, 1024), np.float32)

    def putv(name, v):
        c0, n = PV_COLS[name]
        pv[:, c0:c0 + n] = np.asarray(v, np.float32).reshape(n, 128).T
    for i in range(DEPTH):
        putv("mod_b%d" % i, inputs["mod_b"][i])
        for j in range(2):
            putv("ln_g%d_%d" % (i, j), inputs["ln_g"][i, j])
            putv("ln_b%d_%d" % (i, j), inputs["ln_b"][i, j])
    for j in range(2):
        putv("hy_b_out%d" % j, inputs["hy_b_out"][j])
    for n in range(6):
        putv("rw_mu%d" % n, inputs["rw_mu"][0, n])
    putv("fn_bo", inputs["fn_bo"][0])
    src["pv"] = pv
    src["pos"] = grid_pos_embed()
    src["mod_w"] = inputs["mod_w"].reshape(DEPTH * D, 6 * D)
    src["moe_router"] = inputs["moe_router"].reshape(DEPTH * D, NE)
    if 0 in layers or 3 in layers:
        src["w_in"] = inputs["hy_w_in"].reshape(2 * D, 3 * D)
        src["b_in"] = inputs["hy_b_in"]
        src["conv_w"] = inputs["hy_conv_w"].reshape(6, 3 * D)
        src["conv_b"] = inputs["hy_conv_b"]
        src["f_w1"] = inputs["hy_f_w1"].reshape(66, 64)
        src["hyv"] = np.concatenate([np.stack([inputs["hy_f_b1"][j], inputs["hy_f_freq"][j],
                                               inputs["hy_f_b2"][j, 0], inputs["hy_f_b2"][j, 1]], 1) for j in range(2)], 0)
        src["f_w2"] = inputs["hy_f_w2"].reshape(256, 64)
        src["f_w3"] = inputs["hy_f_w3"].reshape(128, 4096)
        src["f_bias"] = inputs["hy_f_bias"].reshape(4, D)
        src["w_out"] = inputs["hy_w_out"].reshape(2 * D, D)
        src.update(hy_const_tables())
    if 1 in layers:
        src["wr"], src["wk"], src["wv"], src["wo"] = inputs["rw_wr"][0], inputs["rw_wk"][0], inputs["rw_wv"][0], inputs["rw_wo"][0]
        src["w1cat"] = np.concatenate([inputs["rw_w1"][0, 0], inputs["rw_w1"][0, 1]], 1)
        src["a1cat"] = np.concatenate([inputs["rw_a1"][0, 0], inputs["rw_a1"][0, 1]], 1)
        g1 = np.zeros((D, 256), np.float32); g1[:, :160] = inputs["rw_g1"][0]
        g2 = np.zeros((256, D), np.float32); g2[:160] = inputs["rw_g2"][0]
        src["g1pad"], src["g2pad"] = g1, g2
        for n in range(2):
            w2 = np.zeros((128, D), np.float32); w2[n * 64:(n + 1) * 64] = inputs["rw_w2"][0, n]
            a2 = np.zeros((128, D), np.float32); a2[n * 64:(n + 1) * 64] = inputs["rw_a2"][0, n]
            src["w2pad%d" % n], src["a2pad%d" % n] = w2, a2
        src["rows"] = np.stack([inputs["rw_w0"][0, 0], inputs["rw_w0"][0, 1], inputs["rw_a0"][0, 0], inputs["rw_a0"][0, 1],
                                inputs["rw_kk"][0], inputs["rw_ka"][0], inputs["rw_rk"][0], inputs["rw_gn_g"][0], inputs["rw_gn_b"][0]], 0)
        src.update(rw_const_tables())
    if 2 in layers:
        dk = np.outer(np.arange(128), np.arange(128)) % 128
        ang = 2.0 * np.pi * dk / 128.0
        nrm = 1.0 / math.sqrt(T * 128.0)
        src["fn_cs"] = np.concatenate([-np.cos(ang) * nrm, np.sin(ang) * nrm], 1).astype(np.float32)
        tk = (np.outer(np.arange(T, dtype=np.int64), np.arange(T, dtype=np.int64)) % T).astype(np.float64)
        src["fn_nct"] = (-np.cos(2.0 * np.pi * tk / T)).astype(np.float32)
        src["fn_nst"] = (-np.sin(2.0 * np.pi * tk / T)).astype(np.float32)
        del tk
        src["fn_wo"] = inputs["fn_wo"][0]
    blobs = {}
    for piece, (tot, ent) in lay.items():
        flat = np.zeros((tot, BW), np.float32)
        for name, (off, rows, R, C) in ent.items():
            if piece.startswith("moe"):
                i = int(piece[3:])
                a = {"w1": inputs["moe_w1"][i], "w3": inputs["moe_w3"][i], "w2": inputs["moe_w2"][i]}[name]
            else:
                a = src[name]
            a = np.ascontiguousarray(a, dtype=np.float32).reshape(-1)
            flat[off:off + rows].reshape(-1)[:a.size] = a
        blobs[piece] = flat
    return blobs, lay


class Prog:
    def __init__(self, layers, dbg=(), test=None):
        self.test = test
        import os
        self.stop_at = int(os.environ.get("STOP_AT", "0"))
        self.layers = layers
        self.dbg = list(dbg)
        nc = bass.Bass("TRN2", target_bir_lowering=False)
        self.nc = nc
        self.k = KB(nc)
        self.lay = blob_layout(blob_spec(layers))
        self.dram = {}
        self.build()

    def dt(self, name, shape, kind="Internal", dtype=F32):
        t = self.nc.dram_tensor(name, list(shape), dtype, kind=kind)
        self.dram[name] = t
        return t.ap()

    def W(self, piece, name):
        tot, ent = self.lay[piece]
        off, rows, R, C = ent[name]
        g = self.gath[piece]
        v = g[off:off + rows, :]
        if C == BW:
            return v[:R, :]
        if C < BW:
            return v.rearrange("r (a c) -> (r a) c", c=C)[:R, :]
        return v.rearrange("(r a) c -> r (a c)", a=C // BW)[:R, :]

    def build(self):
        nc, k = self.nc, self.k
        self.x_in = self.dt("x", [T, D], kind="ExternalInput")
        self.c_in = self.dt("c", [1, D], kind="ExternalInput")
        self.ctx_in = self.dt("ctx", [TC, D], kind="ExternalInput")
        self.cctx_in = self.dt("c_ctx", [1, D], kind="ExternalInput")
        self.shard = {}
        self.gath = {}
        self.bounce = {}
        for piece, (tot, ent) in self.lay.items():
            self.gath[piece] = self.dt("blob_" + piece, [tot, BW], kind="ExternalInput")
        self.out = self.dt("out", [T, D], kind="ExternalOutput")

        self.ident = k.tile([128, 128], persistent=True)
        self.pv = k.tile([128, 1024], persistent=True)
        self.ones = k.tile([128, 128], persistent=True)
        self.modv = k.tile([128, DEPTH, 48, 2], persistent=True)
        self.modp = k.tile([128, DEPTH, 48, 2], persistent=True)
        self.idxT = k.tile([128, 4, NE], I32, persistent=True)
        self.gateT = k.tile([128, 4, NE], persistent=True)
        self.phase_consts()
        if self.test == "rw":
            injc = self.dt("injc", [TC, D], kind="ExternalInput")
            injl = self.dt("inj", [T, D], kind="ExternalInput")
            HCF = self.dt("HCF", [D, TC]); HLF = self.dt("HLF", [D, T])
            Y = self.dt("YR", [D, T])
            self.transpose_in(injc, HCF, TC)
            self.transpose_in(injl, HLF, T)
            self.rwkv(HCF, HLF, Y)
            self.final_dbg()
            return
        if self.test in ("hy", "hyc"):
            L = T if self.test == "hy" else TC
            inj = self.dt("inj", [L, D], kind="ExternalInput")
            HFM = self.dt("HFM", [D, L])
            Y = self.dt("YH", [D, L])
            self.transpose_in(inj, HFM, L)
            self.hyena(0, HFM, L, Y, "t")
            self.final_dbg()
            return
        if self.test == "fnet":
            inj = self.dt("inj", [T, D], kind="ExternalInput")
            HFM = self.dt("HFM", [D, T])
            Y = self.dt("YF", [D, T])
            self.transpose_in(inj, HFM, T)
            self.fnet(HFM, Y)
            self.final_dbg()
            return
        if self.test == "moe":
            inj = self.dt("inj", [T, D], kind="ExternalInput")
            HFM = self.dt("HFM", [D, T])
            YM = self.dt("YM", [T, D])
            self.transpose_in(inj, HFM, T)
            self.moe(0, HFM, inj, YM, T)
            self.final_dbg()
            return
        self.phase_modvec()
        self.XT = self.dt("XT", [D, T])
        self.XCT = self.dt("XCT", [D, TC])
        XT, XCT = self.XT, self.XCT
        self.transpose_in(self.x_in, XT, T, add=self.W("misc", "pos"))
        self.transpose_in(self.ctx_in, XCT, TC)
        HL = self.dt("HL", [D, T]); HC = self.dt("HC", [D, TC])
        YL = self.dt("YL", [D, T]); YC = self.dt("YC", [D, TC])
        HL2 = self.dt("HL2", [D, T]); HL2TM = self.dt("HL2TM", [T, D])
        HC2 = self.dt("HC2", [D, TC]); HC2TM = self.dt("HC2TM", [TC, D])
        YM = self.dt("YM", [T, D]); YMT = self.dt("YMT", [D, T])
        YMC = self.dt("YMC", [TC, D]); YMCT = self.dt("YMCT", [D, TC])
        self.ln_pass(XT, T, layer=0, first=True, H_out=HL, modj=0, which=0)
        self.ln_pass(XCT, TC, layer=0, first=True, H_out=HC, modj=0, which=1)
        for i in range(DEPTH):
            if i not in self.layers:
                break
            kind = i % 3
            ctx_full = (i == 0)
            if kind == 0:
                self.hyena(i // 3, HL, T, YL, "l%d" % i)
                if ctx_full:
                    self.hyena(i // 3, HC, TC, YC, "c%d" % i)
            elif kind == 1:
                self.rwkv(HC, HL, YL)
            else:
                self.fnet(HL, YL)
            self.ln_pass(XT, T, layer=i, Y=YL, ymul=lambda c, i=i: self.mv(i, 2, c, 0, plus1=True), lnj=0,
                         X_out=XT, H_out=HL2, H_tm=HL2TM, modj=3, which=0)
            self.moe(i, HL2, HL2TM, YM, T)
            self.transpose_in(YM, YMT, T)
            last = (i == DEPTH - 1)
            self.ln_pass(XT, T, layer=i, Y=YMT, ymul=lambda c, i=i: self.mv(i, 5, c, 0, plus1=True), lnj=1,
                         X_out=None if last else XT, X_tm=self.out if last else None,
                         H_out=None if last else HL, do_mod=not last, modj=0, which=0, mod_layer=i + 1)
            if ctx_full:
                self.ln_pass(XCT, TC, layer=i, Y=YC, ymul=lambda c, i=i: self.mv(i, 2, c, 1, plus1=True), lnj=0,
                             X_out=XCT, H_out=HC2, H_tm=HC2TM, modj=3, which=1)
                self.moe(i, HC2, HC2TM, YMC, TC)
                self.transpose_in(YMC, YMCT, TC)
                self.ln_pass(XCT, TC, layer=i, Y=YMCT, ymul=lambda c, i=i: self.mv(i, 5, c, 1, plus1=True), lnj=1,
                             X_out=XCT, H_out=HC, modj=0, which=1, mod_layer=i + 1)
        self.final_dbg()

    def phase_consts(self):
        k = self.k
        k.begin()
        s = k.dsem()
        k.dma(self.ident[:], self.W("misc", "ident"), writes=[self.ident], sem=s)
        k.dma(self.pv[:], self.W("misc", "pv"), writes=[self.pv], sem=s)
        k.op("dve", lambda e: e.memset(self.ones[:], 1.0), writes=[self.ones])
        k.end()

    def pvc(self, name, c=None):
        c0, n = PV_COLS[name]
        if c is None:
            return self.pv[:, c0:c0 + n]
        return self.pv[:, c0 + c:c0 + c + 1]

    def phase_modvec(self):
        k = self.k
        k.begin()
        sc = k.tile([128, 8, 2])
        s = k.dsem()
        if True:
            k.dma(sc[:, :, 0], self.c_in.rearrange("o (c p) -> p (o c)", p=128), writes=[sc], sem=s, allow_slow_non_contiguous=True)
            k.dma(sc[:, :, 1], self.cctx_in.rearrange("o (c p) -> p (o c)", p=128), writes=[sc], sem=s, allow_slow_non_contiguous=True)
        k.op("act", lambda e: e.activation(out=sc[:], in_=sc[:], func=AF.Silu), reads=[sc], writes=[sc])
        mw = self.W("misc", "mod_w")
        wb = [k.tile([128, 8, 1536]) for _ in range(2)]
        ws = [k.dsem() for _ in range(2)]
        ps = [k.ptile([128, 12, 2]) for _ in range(2)]
        it = 0
        for i in range(DEPTH):
            for g in range(4):
                b = wb[it % 2]
                src = mw[i * D:(i + 1) * D, g * 1536:(g + 1) * 1536].rearrange("(c p) n -> p c n", p=128)
                k.dma(b[:], src, writes=[b], sem=ws[it % 2])
                p = ps[it % 2]
                for j in range(12):
                    for c in range(8):
                        k.op("pe", lambda e, j=j, c=c, b=b, p=p: e.matmul(
                            p[:, j, :], b[:, c, j * 128:(j + 1) * 128], sc[:, c, :],
                            start=(c == 0), stop=(c == 7)),
                            reads=[b, sc], writes=[p], accum=True)
                c0, _ = PV_COLS["mod_b%d" % i]
                bias = self.pv[:, c0 + g * 12:c0 + (g + 1) * 12]
                k.op("dve", lambda e, p=p, i=i, g=g, bias=bias: e.tensor_tensor(
                    out=self.modv[:, i, g * 12:(g + 1) * 12, :], in0=p[:],
                    in1=bias.unsqueeze(2).to_broadcast([128, 12, 2]), op=ALU.add),
                    reads=[p, self.pv], writes=[self.modv])
                it += 1
        k.op("dve", lambda e: e.tensor_scalar(out=self.modp[:], in0=self.modv[:], scalar1=1.0,
                                              scalar2=None, op0=ALU.add),
             reads=[self.modv], writes=[self.modp])
        k.end()

    def mv(self, layer, j, c, which=0, plus1=False):
        t = self.modp if plus1 else self.modv
        return t[:, layer, j * 8 + c, which:which + 1]

    def transpose_in(self, src, dstT, Tn, add=None):
        k = self.k
        k.begin()
        nt = Tn // 128
        grp = min(4, nt)
        xin = [k.tile([128, D]) for _ in range(2)]
        ain = [k.tile([128, D]) for _ in range(2)] if add is not None else None
        sx = [k.dsem() for _ in range(2)]
        sa = [k.dsem() for _ in range(2)]
        so = [k.dsem() for _ in range(2)]
        pst = [k.ptile([128, 4, 128]) for _ in range(4)]
        outb = [k.tile([128, 8, 128 * grp], nsub=2 * grp) for _ in range(2)]
        dv = dstT.rearrange("(c p) t -> p c t", p=128)
        pi = 0
        for t in range(nt):
            xb = xin[t % 2]
            k.dma(xb[:], src[t * 128:(t + 1) * 128, :], writes=[xb], sem=sx[t % 2])
            if add is not None:
                ab = ain[t % 2]
                k.dma(ab[:], add[t * 128:(t + 1) * 128, :], writes=[ab], sem=sa[t % 2])
                k.op("dve", lambda e, xb=xb, ab=ab: e.tensor_tensor(out=xb[:], in0=xb[:], in1=ab[:], op=ALU.add),
                     reads=[xb, ab], writes=[xb])
            ob = outb[(t // grp) % 2]
            tt = t % grp
            for h in range(2):
                p = pst[pi % 4]
                pi += 1
                for cc in range(4):
                    c = h * 4 + cc
                    k.op("pe", lambda e, p=p, cc=cc, c=c, xb=xb: e.transpose(
                        p[:, cc, :], xb[:, c * 128:(c + 1) * 128], self.ident[:]),
                        reads=[xb, self.ident], writes=[p], accum=True)
                dst = ob[:, h * 4:(h + 1) * 4, tt * 128:(tt + 1) * 128]
                if h == 0:
                    k.op("act", lambda e, p=p, dst=dst: e.copy(out=dst, in_=p[:]),
                         reads=[p], writes=[ob.ch(h * grp + tt)])
                else:
                    k.op("dve", lambda e, p=p, dst=dst: e.tensor_copy(out=dst, in_=p[:]),
                         reads=[p], writes=[ob.ch(h * grp + tt)])
            if tt == grp - 1:
                g = t // grp
                k.dma(dv[:, :, g * 128 * grp:(g + 1) * 128 * grp], ob[:], reads=[ob], sem=so[g % 2])
        k.end()

    def ln_pass(self, X, Tn, layer, first=False, Y=None, ymul=None, lnj=0, which=0,
                X_out=None, H_out=None, H_tm=None, X_tm=None, modj=0, do_mod=True, Y_tm=False, mod_layer=None):
        k = self.k
        if mod_layer is None:
            mod_layer = layer
        k.begin()
        TT = min(512, Tn)
        ntile = Tn // TT
        xv = X.rearrange("(c p) t -> p c t", p=128)
        xb = [k.tile([128, 8, TT], nsub=8) for _ in range(2)]
        sxs = [k.dsem() for _ in range(2)]
        if not first:
            yv = Y.rearrange("(c p) t -> p c t", p=128)
            yb = [k.tile([128, 8, TT], nsub=8) for _ in range(2)]
            sys_ = [k.dsem() for _ in range(2)]
        sq = [k.tile([128, 8, TT], nsub=8) for _ in range(2)]
        hb = [k.tile([128, 8, TT], nsub=8) for _ in range(2)] if do_mod else None
        so = [k.dsem() for _ in range(2)]
        so2 = [k.dsem() for _ in range(2)]
        st = [k.tile([128, 4, TT], nsub=3) for _ in range(2)]
        ps1 = [k.ptile([128, TT]) for _ in range(2)]
        ps2 = [k.ptile([128, TT]) for _ in range(2)]
        if X_tm is not None or H_tm is not None:
            pst = [k.ptile([128, 4, 128]) for _ in range(2)]
            tmb = [k.tile([128, D], nsub=2) for _ in range(2)]
            stm = [k.dsem() for _ in range(2)]
        self._tmi = 0

        def stats_norm(zb, sqb, sb, p1, p2, eps):
            for c in range(8):
                k.op("act", lambda e, c=c: e.activation(out=sqb[:, c, :], in_=zb[:, c, :], func=AF.Square),
                     reads=[zb.ch(c)], writes=[sqb.ch(c)])
            for c in range(8):
                k.op("pe", lambda e, c=c: e.matmul(p1[:], self.ones[:], zb[:, c, :], start=(c == 0), stop=(c == 7)),
                     reads=[self.ones, zb.ch(c)], writes=[p1], accum=True)
            for c in range(8):
                k.op("pe", lambda e, c=c: e.matmul(p2[:], self.ones[:], sqb[:, c, :], start=(c == 0), stop=(c == 7)),
                     reads=[self.ones, sqb.ch(c)], writes=[p2], accum=True)
            k.op("dve", lambda e: e.tensor_scalar(out=sb[:, 0, :], in0=p1[:], scalar1=1.0 / D, scalar2=None, op0=ALU.mult),
                 reads=[p1], writes=[sb.ch(0)])
            k.op("dve", lambda e: e.tensor_tensor(out=sb[:, 2, :], in0=sb[:, 0, :], in1=sb[:, 0, :], op=ALU.mult),
                 reads=[sb.ch(0)], writes=[sb.ch(2)])
            k.op("dve", lambda e: e.scalar_tensor_tensor(out=sb[:, 1, :], in0=p2[:], scalar=1.0 / D, in1=sb[:, 2, :],
                                                         op0=ALU.mult, op1=ALU.subtract),
                 reads=[p2, sb.ch(2)], writes=[sb.ch(1)])
            k.op("dve", lambda e: e.tensor_scalar(out=sb[:, 1, :], in0=sb[:, 1, :], scalar1=eps, scalar2=None,
                                                  op0=ALU.add),
                 reads=[sb.ch(1)], writes=[sb.ch(1)])
            k.op("act", lambda e: e.sqrt(out=sb[:, 1, :], in_=sb[:, 1, :]),
                 reads=[sb.ch(1)], writes=[sb.ch(1)])
            k.op("dve", lambda e: e.reciprocal(out=sb[:, 1, :], in_=sb[:, 1, :]),
                 reads=[sb.ch(1)], writes=[sb.ch(1)])
            for c in range(8):
                eng = "dve" if c % 2 == 0 else "pool"
                k.op(eng, lambda e, c=c: e.tensor_tensor(out=zb[:, c, :], in0=zb[:, c, :], in1=sb[:, 0, :], op=ALU.subtract),
                     reads=[zb.ch(c), sb.ch(0)], writes=[zb.ch(c)])
                k.op(eng, lambda e, c=c: e.tensor_tensor(out=zb[:, c, :], in0=zb[:, c, :], in1=sb[:, 1, :], op=ALU.mult),
                     reads=[zb.ch(c), sb.ch(1)], writes=[zb.ch(c)])

        def emit_tm(srcb, dst_tm, t0):
            for q in range(TT // 128):
                i = self._tmi
                self._tmi += 1
                tb = tmb[i % 2]
                for h in range(2):
                    p = pst[h]
                    for cc in range(4):
                        c = h * 4 + cc
                        k.op("pe", lambda e, p=p, cc=cc, c=c, q=q: e.transpose(
                            p[:, cc, :], srcb[:, c, q * 128:(q + 1) * 128], self.ident[:]),
                            reads=[srcb.ch(c), self.ident], writes=[p], accum=True)
                    if h == 0:
                        k.op("act", lambda e, p=p, tb=tb, h=h: e.copy(
                            out=tb[:, h * 512:(h + 1) * 512], in_=p[:].rearrange("p a b -> p (a b)")),
                            reads=[p], writes=[tb.ch(h)])
                    else:
                        k.op("dve", lambda e, p=p, tb=tb, h=h: e.tensor_copy(
                            out=tb[:, h * 512:(h + 1) * 512], in_=p[:].rearrange("p a b -> p (a b)")),
                            reads=[p], writes=[tb.ch(h)])
                k.dma(dst_tm[t0 + q * 128:t0 + (q + 1) * 128, :], tb[:], reads=[tb], sem=stm[i % 2])

        for t in range(ntile):
            zb = xb[t % 2]
            sl = slice(t * TT, (t + 1) * TT)
            k.dma(zb[:], xv[:, :, sl], writes=[zb], sem=sxs[t % 2])
            sqb, sb, p1, p2 = sq[t % 2], st[t % 2], ps1[t % 2], ps2[t % 2]
            if not first:
                ybb = yb[t % 2]
                if Y_tm:
                    raise NotImplementedError
                k.dma(ybb[:], yv[:, :, sl], writes=[ybb], sem=sys_[t % 2])
                for c in range(8):
                    k.op("act", lambda e, c=c: e.mul(out=zb[:, c, :], in_=zb[:, c, :], mul=float(ALPHA)),
                         reads=[zb.ch(c)], writes=[zb.ch(c)])
                    k.op("dve", lambda e, c=c: e.scalar_tensor_tensor(
                        out=zb[:, c, :], in0=ybb[:, c, :], scalar=ymul(c), in1=zb[:, c, :],
                        op0=ALU.mult, op1=ALU.add), reads=[ybb.ch(c), zb.ch(c), self.modp], writes=[zb.ch(c)])
                stats_norm(zb, sqb, sb, p1, p2, 1e-5)
                gc0, _ = PV_COLS["ln_g%d_%d" % (layer, lnj)]
                bc0, _ = PV_COLS["ln_b%d_%d" % (layer, lnj)]
                for c in range(8):
                    k.op("act", lambda e, c=c: e.activation(
                        out=zb[:, c, :], in_=zb[:, c, :], func=AF.Identity,
                        scale=self.pv[:, gc0 + c:gc0 + c + 1], bias=self.pv[:, bc0 + c:bc0 + c + 1]),
                        reads=[zb.ch(c), self.pv], writes=[zb.ch(c)])
                if X_out is not None:
                    k.dma(X_out.rearrange("(c p) t -> p c t", p=128)[:, :, sl], zb[:], reads=[zb], sem=so[t % 2])
                if X_tm is not None:
                    emit_tm(zb, X_tm, t * TT)
            if do_mod:
                h = hb[t % 2]
                for c in range(8):
                    k.op("pool", lambda e, c=c: e.tensor_copy(out=h[:, c, :], in_=zb[:, c, :]),
                         reads=[zb.ch(c)], writes=[h.ch(c)])
                stats_norm(h, sqb, sb, p1, p2, 1e-6)
                for c in range(8):
                    k.op("act", lambda e, c=c: e.activation(
                        out=h[:, c, :], in_=h[:, c, :], func=AF.Identity,
                        scale=self.mv(mod_layer, modj + 1, c, which, plus1=True),
                        bias=self.mv(mod_layer, modj, c, which)),
                        reads=[h.ch(c), self.modv, self.modp], writes=[h.ch(c)])
                if H_out is not None:
                    k.dma(H_out.rearrange("(c p) t -> p c t", p=128)[:, :, sl], h[:], reads=[h], sem=so2[t % 2])
                if H_tm is not None:
                    emit_tm(h, H_tm, t * TT)
        k.end()

    def moe(self, layer, HFM, HTM, YM, Tn):
        k, nc = self.k, self.nc
        cap = 2 * Tn // NE
        JP = min(128, cap)
        nch = cap // JP
        ntt = Tn // 128
        piece = "moe%d" % layer
        W1, W3, W2 = self.W(piece, "w1"), self.W(piece, "w3"), self.W(piece, "w2")
        idxT, gateT = self.idxT, self.gateT
        k.begin()
        rw = k.tile([128, 8, NE])
        s = k.dsem()
        rsrc = self.W("misc", "moe_router")[layer * D:(layer + 1) * D, :].rearrange("(c p) e -> p c e", p=128)
        k.dma(rw[:], rsrc, writes=[rw], sem=s)
        zt = k.tile([128, D])
        k.op("pool", lambda e: e.memset(zt[:], 0.0), writes=[zt])
        sz = k.dsem()
        for tt in range(ntt):
            k.dma(YM[tt * 128:(tt + 1) * 128, :], zt[:], reads=[zt], sem=sz)
        TT = min(512, Tn)
        hb = [k.tile([128, 8, TT]) for _ in range(2)]
        sh = [k.dsem() for _ in range(2)]
        hv = HFM.rearrange("(c p) t -> p c t", p=128)
        lg = k.ptile([128, ntt, NE])
        for t in range(Tn // TT):
            b = hb[t % 2]
            k.dma(b[:], hv[:, :, t * TT:(t + 1) * TT], writes=[b], sem=sh[t % 2])
            for q in range(TT // 128):
                tt = t * (TT // 128) + q
                for c in range(8):
                    k.op("pe", lambda e, b=b, c=c, q=q, tt=tt: e.matmul(
                        lg[:, tt, :], b[:, c, q * 128:(q + 1) * 128], rw[:, c, :], start=(c == 0), stop=(c == 7)),
                        reads=[b, rw], writes=[lg], accum=True)
        aff = k.tile([128, ntt, NE])
        mx = k.tile([128, ntt])
        k.op("dve", lambda e: e.tensor_reduce(out=mx[:], in_=lg[:], axis=AX.X, op=ALU.max), reads=[lg], writes=[mx])
        k.op("dve", lambda e: e.tensor_tensor(out=aff[:], in0=lg[:], in1=mx[:].unsqueeze(2).to_broadcast([128, ntt, NE]),
                                              op=ALU.subtract), reads=[lg, mx], writes=[aff])
        k.op("act", lambda e: e.activation(out=aff[:], in_=aff[:], func=AF.Exp), reads=[aff], writes=[aff])
        k.op("dve", lambda e: e.tensor_reduce(out=mx[:], in_=aff[:], axis=AX.X, op=ALU.add), reads=[aff], writes=[mx])
        k.op("dve", lambda e: e.reciprocal(out=mx[:], in_=mx[:]), reads=[mx], writes=[mx])
        k.op("dve", lambda e: e.tensor_tensor(out=aff[:], in0=aff[:], in1=mx[:].unsqueeze(2).to_broadcast([128, ntt, NE]),
                                              op=ALU.mult), reads=[aff, mx], writes=[aff])
        affT = k.tile([NE, Tn])
        pT = [k.ptile([NE, 4, 128]) for _ in range(2)]
        ng = (ntt + 3) // 4
        for g in range(ng):
            p = pT[g % 2]
            n = min(4, ntt - g * 4)
            for q in range(n):
                tt = g * 4 + q
                k.op("pe", lambda e, p=p, q=q, tt=tt: e.transpose(p[:, q, :], aff[:, tt, :], self.ident[:]),
                     reads=[aff, self.ident], writes=[p], accum=True)
            k.op("act", lambda e, p=p, g=g, n=n: e.copy(
                out=affT[:, g * 512:g * 512 + n * 128], in_=p[:, 0:n, :].rearrange("p a b -> p (a b)")),
                reads=[p], writes=[affT], accum=True)
        work = k.tile([NE, Tn])
        vals = k.tile([NE, cap])
        idxu = k.tile([NE, cap], U32)
        k.op("dve", lambda e: e.tensor_copy(out=work[:], in_=affT[:]), reads=[affT], writes=[work])
        for r in range(cap // 8):
            sl = slice(r * 8, (r + 1) * 8)
            k.op("dve", lambda e, sl=sl: e.max(out=vals[:, sl], in_=work[:]), reads=[work], writes=[vals], accum=True)
            k.op("dve", lambda e, sl=sl: e.max_index(out=idxu[:, sl], in_max=vals[:, sl], in_values=work[:]),
                 reads=[work, vals], writes=[idxu], accum=True)
            k.op("dve", lambda e, sl=sl: e.match_replace(out=work[:], in_to_replace=vals[:, sl], in_values=work[:],
                                                         imm_value=-1.0), reads=[vals, work], writes=[work])
        idxf = k.tile([NE, cap])
        k.op("dve", lambda e: e.tensor_copy(out=idxf[:], in_=idxu[:]), reads=[idxu], writes=[idxf])
        pI = k.ptile([128, nch, NE])
        pG = k.ptile([128, nch, NE])
        for ch in range(nch):
            k.op("pe", lambda e, ch=ch: e.transpose(pI[:JP, ch, :], idxf[:, ch * JP:(ch + 1) * JP], self.ident[:NE, :NE]),
                 reads=[idxf, self.ident], writes=[pI], accum=True)
            k.op("pe", lambda e, ch=ch: e.transpose(pG[:JP, ch, :], vals[:, ch * JP:(ch + 1) * JP], self.ident[:NE, :NE]),
                 reads=[vals, self.ident], writes=[pG], accum=True)
        k.op("dve", lambda e: e.tensor_copy(out=idxT[:JP, :nch, :], in_=pI[:JP]), reads=[pI], writes=[idxT])
        k.op("dve", lambda e: e.tensor_copy(out=gateT[:JP, :nch, :], in_=pG[:JP]), reads=[pG], writes=[gateT])
        k.end()
        k.begin()
        Xe = k.tile([128, nch, D], nsub=nch)
        XeT = k.tile([128, 8, cap], F32R, nsub=8)
        heT = k.tile([128, 16, cap], F32R, nsub=16)
        Ye = k.tile([128, nch, D], nsub=nch)
        w1s = [k.tile([128, 8, 256]) for _ in range(2)]
        w3s = [k.tile([128, 8, 256]) for _ in range(2)]
        w2s = [k.tile([128, 8, 256]) for _ in range(2)]
        w1t = [k.tile([128, 8, 256], F32R) for _ in range(2)]
        w3t = [k.tile([128, 8, 256], F32R) for _ in range(2)]
        w2t = [k.tile([128, 16, 256], F32R, nsub=2) for _ in range(1)]
        tmp = [k.tile([128, cap]) for _ in range(2)]
        s1 = [k.dsem() for _ in range(2)]
        s3 = [k.dsem() for _ in range(2)]
        s2 = [k.dsem() for _ in range(2)]
        sg = k.dsem()
        ss = k.dsem()
        ph1 = [k.ptile([128, cap]) for _ in range(2)]
        ph3 = [k.ptile([128, cap]) for _ in range(2)]
        py = [k.ptile([128, 512]) for _ in range(2)]
        pt = [k.ptile([128, 4, 128]) for _ in range(2)]
        i1 = i2 = ip = iy = 0
        for ex in range(NE):
            for ch in range(nch):
                k.dma(None, None, reads=[idxT], writes=[Xe.ch(ch)], sem=sg, q="pool",
                      fn=lambda e, ch=ch, ex=ex: e.indirect_dma_start(
                          out=Xe[:JP, ch, :], out_offset=None, in_=HTM[:, :],
                          in_offset=bass.IndirectOffsetOnAxis(ap=idxT[:JP, ch, ex:ex + 1], axis=0)))
            for dc in range(8):
                for ch in range(nch):
                    if ch % 4 == 0:
                        p = pt[ip % 2]
                        ip += 1
                    k.op("pe", lambda e, p=p, ch=ch, dc=dc: e.transpose(
                        p[:, ch % 4, :JP], Xe[:JP, ch, dc * 128:(dc + 1) * 128], self.ident[:JP, :JP]),
                        reads=[Xe.ch(ch), self.ident], writes=[p], accum=True)
                    if ch % 4 == 3 or ch == nch - 1:
                        c0 = (ch // 4) * 4
                        n = ch - c0 + 1
                        eng = "act" if dc % 2 == 0 else "dve"
                        if eng == "act":
                            k.op("act", lambda e, p=p, dc=dc, c0=c0, n=n: e.copy(
                                out=XeT[:, dc, c0 * JP:(c0 + n) * JP].rearrange("p (a b) -> p a b", a=n),
                                in_=p[:, 0:n, :JP]), reads=[p], writes=[XeT.ch(dc)])
                        else:
                            k.op("dve", lambda e, p=p, dc=dc, c0=c0, n=n: e.tensor_copy(
                                out=XeT[:, dc, c0 * JP:(c0 + n) * JP].rearrange("p (a b) -> p a b", a=n),
                                in_=p[:, 0:n, :JP]), reads=[p], writes=[XeT.ch(dc)])
            for g in range(8):
                a1, a3 = w1t[i1 % 2], w3t[i1 % 2]
                b1, b3 = w1s[i1 % 2], w3s[i1 % 2]
                r0 = ex * D
                k.dma(b1[:], W1[r0:r0 + D, g * 256:(g + 1) * 256].rearrange("(c p) n -> p c n", p=128),
                      writes=[b1], sem=s1[i1 % 2], q="sp")
                k.dma(b3[:], W3[r0:r0 + D, g * 256:(g + 1) * 256].rearrange("(c p) n -> p c n", p=128),
                      writes=[b3], sem=s3[i1 % 2], q="sp")
                k.rnd("act", a1[:], b1[:], [b1], [a1])
                k.rnd("dve", a3[:], b3[:], [b3], [a3])
                i1 += 1
                for fl in range(2):
                    fc = g * 2 + fl
                    p1, p3 = ph1[fc % 2], ph3[fc % 2]
                    for dc in range(8):
                        k.op("pe", lambda e, p1=p1, a1=a1, dc=dc, fl=fl: e.matmul(
                            p1[:], a1[:, dc, fl * 128:(fl + 1) * 128], XeT[:, dc, :], start=(dc == 0), stop=(dc == 7)),
                            reads=[a1, XeT.ch(dc)], writes=[p1], accum=True)
                    for dc in range(8):
                        k.op("pe", lambda e, p3=p3, a3=a3, dc=dc, fl=fl: e.matmul(
                            p3[:], a3[:, dc, fl * 128:(fl + 1) * 128], XeT[:, dc, :], start=(dc == 0), stop=(dc == 7)),
                            reads=[a3, XeT.ch(dc)], writes=[p3], accum=True)
                    tb = tmp[fc % 2]
                    k.op("act", lambda e, tb=tb, p1=p1: e.activation(out=tb[:], in_=p1[:], func=AF.Silu),
                         reads=[p1], writes=[tb])
                    k.op("dve", lambda e, tb=tb, p3=p3, fc=fc: e.tensor_tensor(
                        out=heT[:, fc, :], in0=tb[:], in1=p3[:], op=ALU.mult),
                        reads=[tb, p3], writes=[heT.ch(fc)])
            for q in range(4):
                a2 = w2t[0]
                r0 = ex * FF
                for hf in range(2):
                    b2 = w2s[i2 % 2]
                    k.dma(b2[:], W2[r0 + hf * 1024:r0 + (hf + 1) * 1024, q * 256:(q + 1) * 256].rearrange("(c p) n -> p c n", p=128),
                          writes=[b2], sem=s2[i2 % 2], q="sp")
                    k.rnd("pool", a2[:, hf * 8:(hf + 1) * 8, :], b2[:], [b2], [a2.ch(hf)])
                    i2 += 1
                for ch in range(nch):
                    p = py[iy % 2]
                    iy += 1
                    for fc in range(16):
                        k.op("pe", lambda e, p=p, a2=a2, fc=fc, ch=ch: e.matmul(
                            p[:JP, 0:256], heT[:, fc, ch * JP:(ch + 1) * JP], a2[:, fc, :],
                            start=(fc == 0), stop=(fc == 15)),
                            reads=[a2, heT.ch(fc)], writes=[p], accum=True)
                    k.op("act", lambda e, p=p, ch=ch, q=q, ex=ex: e.activation(
                        out=Ye[:JP, ch, q * 256:(q + 1) * 256], in_=p[:JP, 0:256], func=AF.Identity,
                        scale=gateT[:JP, ch, ex:ex + 1]),
                        reads=[p, gateT], writes=[Ye.ch(ch)], accum=True)
            for ch in range(nch):
                k._wait("pool", [(ss, k.cnt[ss])])
                k.dma(None, None, reads=[idxT, Ye.ch(ch)], sem=ss, q="pool",
                      fn=lambda e, ch=ch, ex=ex: e.indirect_dma_start(
                          out=YM[:, :], out_offset=bass.IndirectOffsetOnAxis(ap=idxT[:JP, ch, ex:ex + 1], axis=0),
                          in_=Ye[:JP, ch, :], in_offset=None, compute_op=ALU.add))
        k.end()

    def gemm(self, XT, Wap, K, N, Tn, out, bias_pv=None, act=None, out_tm=False, bias_row=None):
        k = self.k
        k.begin()
        KC = K // 128
        wt = k.tile([128, KC, N], F32R, nsub=KC)
        ws = [k.tile([128, N]) for _ in range(2)]
        sw = [k.dsem() for _ in range(2)]
        for kc in range(KC):
            w_ = ws[kc % 2]
            k.dma(w_[:], Wap[kc * 128:(kc + 1) * 128, :], writes=[w_], sem=sw[kc % 2])
            k.rnd(("pool", "act")[kc % 2], wt[:, kc, :], w_[:], [w_], [wt.ch(kc)])
        TT = 512 if Tn % 512 == 0 else 256
        xv = XT.rearrange("(c p) t -> p c t", p=128)
        xs = [k.tile([128, KC, TT]) for _ in range(2)]
        xb = [k.tile([128, KC, TT], F32R) for _ in range(2)]
        sx = [k.dsem() for _ in range(2)]
        so = [k.dsem() for _ in range(2)]
        ps = [k.ptile([128, 512]) for _ in range(4)]
        func = act if act is not None else AF.Identity
        ip = io = 0
        if not out_tm:
            ov = out.rearrange("(c p) t -> p c t", p=128)
            ob = [k.tile([128, 4, TT], nsub=4) for _ in range(2)]
            for t in range(Tn // TT):
                x_ = xs[t % 2]
                b = xb[t % 2]
                k.dma(x_[:], xv[:, :, t * TT:(t + 1) * TT], writes=[x_], sem=sx[t % 2])
                k.rnd("pool", b[:], x_[:], [x_], [b])
                for n in range(N // 128):
                    p = ps[ip % 4]
                    ip += 1
                    for kc in range(KC):
                        k.op("pe", lambda e, p=p, kc=kc, n=n, b=b: e.matmul(
                            p[:, :TT], wt[:, kc, n * 128:(n + 1) * 128], b[:, kc, :], start=(kc == 0), stop=(kc == KC - 1)),
                            reads=[wt.ch(kc), b], writes=[p], accum=True)
                    o = ob[io % 2]
                    if bias_pv is not None:
                        c0, _ = PV_COLS[bias_pv]
                        k.op("act", lambda e, p=p, o=o, n=n, c0=c0: e.activation(
                            out=o[:, n % 4, :], in_=p[:, :TT], func=func, bias=self.pv[:, c0 + n:c0 + n + 1]),
                            reads=[p, self.pv], writes=[o.ch(n % 4)])
                    else:
                        k.op("act", lambda e, p=p, o=o, n=n: e.activation(out=o[:, n % 4, :], in_=p[:, :TT], func=func),
                             reads=[p], writes=[o.ch(n % 4)])
                    if n % 4 == 3 or n == N // 128 - 1:
                        n0 = (n // 4) * 4
                        k.dma(ov[:, n0:n + 1, t * TT:(t + 1) * TT], o[:, 0:n - n0 + 1, :], reads=[o], sem=so[io % 2])
                        io += 1
        else:
            brow = None
            if bias_row is not None:
                brow = k.tile([128, N])
                sb_ = k.dsem()
                k.dma(brow[:], bias_row.partition_broadcast(128), writes=[brow], sem=sb_)
            ob = [k.tile([128, 512]) for _ in range(2)]
            for t in range(Tn // TT):
                x_ = xs[t % 2]
                b = xb[t % 2]
                k.dma(x_[:], xv[:, :, t * TT:(t + 1) * TT], writes=[x_], sem=sx[t % 2])
                k.rnd("pool", b[:], x_[:], [x_], [b])
                for q in range(TT // 128):
                    for n in range(N // 512):
                        p = ps[ip % 4]
                        ip += 1
                        for kc in range(KC):
                            k.op("pe", lambda e, p=p, kc=kc, n=n, b=b, q=q: e.matmul(
                                p[:], b[:, kc, q * 128:(q + 1) * 128], wt[:, kc, n * 512:(n + 1) * 512],
                                start=(kc == 0), stop=(kc == KC - 1)), reads=[wt.ch(kc), b], writes=[p], accum=True)
                        o = ob[io % 2]
                        if brow is not None:
                            k.op("dve", lambda e, p=p, o=o, n=n: e.tensor_tensor(
                                out=o[:], in0=p[:], in1=brow[:, n * 512:(n + 1) * 512], op=ALU.add),
                                reads=[p, brow], writes=[o])
                            if act is not None:
                                k.op("act", lambda e, o=o: e.activation(out=o[:], in_=o[:], func=act), reads=[o], writes=[o])
                        else:
                            k.op("act", lambda e, p=p, o=o: e.activation(out=o[:], in_=p[:], func=func), reads=[p], writes=[o])
                        r0 = t * TT + q * 128
                        k.dma(out[r0:r0 + 128, n * 512:(n + 1) * 512], o[:], reads=[o], sem=so[io % 2])
                        io += 1
        k.end()

    def fnet(self, HL, Y):
        k = self.k
        PQ = self.dt("fn_PQ", [2, T, D])
        MX = self.dt("fn_MX", [D, T])
        k.begin()
        cs0 = k.tile([128, 256])
        cs = k.tile([128, 256], F32R)
        s0 = k.dsem()
        k.dma(cs0[:], self.W("fnet", "fn_cs"), writes=[cs0], sem=s0)
        k.rnd("dve", cs[:], cs0[:], [cs0], [cs])
        hv = HL.rearrange("(c p) t -> p c t", p=128)
        hb0 = [k.tile([128, 8, 512]) for _ in range(2)]
        hb = [k.tile([128, 8, 512], F32R) for _ in range(2)]
        sh = [k.dsem() for _ in range(2)]
        ps = [k.ptile([128, 2, 256]) for _ in range(4)]
        pb = [k.tile([128, D], nsub=4) for _ in range(2)]
        qb = [k.tile([128, D], nsub=4) for _ in range(2)]
        so = [k.dsem() for _ in range(2)]
        ip = io = 0
        for t in range(T // 512):
            b0 = hb0[t % 2]
            b = hb[t % 2]
            k.dma(b0[:], hv[:, :, t * 512:(t + 1) * 512], writes=[b0], sem=sh[t % 2])
            k.rnd("pool", b[:], b0[:], [b0], [b])
            for q in range(4):
                po, qo = pb[io % 2], qb[io % 2]
                for g2 in range(4):
                    p = ps[ip % 4]
                    ip += 1
                    for gl in range(2):
                        g = g2 * 2 + gl
                        k.op("pe", lambda e, p=p, gl=gl, g=g, b=b, q=q: e.matmul(
                            p[:, gl, :], b[:, g, q * 128:(q + 1) * 128], cs[:], start=True, stop=True),
                            reads=[b, cs], writes=[p], accum=True)
                    if g2 % 2 == 0:
                        k.op("act", lambda e, p=p, po=po, g2=g2: e.copy(
                            out=po[:, g2 * 256:(g2 + 1) * 256].rearrange("p (a b) -> p a b", a=2), in_=p[:, :, 0:128]),
                            reads=[p], writes=[po.ch(g2)])
                        k.op("act", lambda e, p=p, qo=qo, g2=g2: e.copy(
                            out=qo[:, g2 * 256:(g2 + 1) * 256].rearrange("p (a b) -> p a b", a=2), in_=p[:, :, 128:256]),
                            reads=[p], writes=[qo.ch(g2)])
                    else:
                        k.op("dve", lambda e, p=p, po=po, g2=g2: e.tensor_copy(
                            out=po[:, g2 * 256:(g2 + 1) * 256].rearrange("p (a b) -> p a b", a=2), in_=p[:, :, 0:128]),
                            reads=[p], writes=[po.ch(g2)])
                        k.op("dve", lambda e, p=p, qo=qo, g2=g2: e.tensor_copy(
                            out=qo[:, g2 * 256:(g2 + 1) * 256].rearrange("p (a b) -> p a b", a=2), in_=p[:, :, 128:256]),
                            reads=[p], writes=[qo.ch(g2)])
                r0 = t * 512 + q * 128
                k.dma(PQ[0, r0:r0 + 128, :], po[:], reads=[po], sem=so[io % 2])
                k.dma(PQ[1, r0:r0 + 128, :], qo[:], reads=[qo], sem=so[io % 2])
                io += 1
        k.end()
        if self.test == "fnet" and self.stop_at == 1:
            return
        k.begin()
        CT, ST = self.W("fnet", "fn_nct"), self.W("fnet", "fn_nst")
        ps = [k.ptile([128, 512]) for _ in range(8)]
        pt0 = [k.tile([128, 512]) for _ in range(3)]
        qt0 = [k.tile([128, 512]) for _ in range(3)]
        ct0 = [k.tile([128, 512]) for _ in range(3)]
        st0 = [k.tile([128, 512]) for _ in range(3)]
        pt = [k.tile([128, 512], F32R) for _ in range(3)]
        qt = [k.tile([128, 512], F32R) for _ in range(3)]
        ct = [k.tile([128, 512], F32R) for _ in range(3)]
        st_ = [k.tile([128, 512], F32R) for _ in range(3)]
        sp_ = [k.dsem() for _ in range(3)]
        sq_ = [k.dsem() for _ in range(3)]
        sc_ = [k.dsem() for _ in range(3)]
        ss_ = [k.dsem() for _ in range(3)]
        ob = [k.tile([128, 4, 512], nsub=4) for _ in range(2)]
        so = [k.dsem() for _ in range(2)]
        mv_ = MX.rearrange("(c p) t -> p c t", p=128)
        it = 0
        io = 0
        for kb in range(T // 512):
            for half in range(2):
                pp = [ps[(io % 2) * 4 + j] for j in range(4)]
                for tt in range(T // 128):
                    i = it % 3
                    it += 1
                    k.dma(pt0[i][:], PQ[0, tt * 128:(tt + 1) * 128, half * 512:(half + 1) * 512], writes=[pt0[i]], sem=sp_[i], q="sp")
                    k.dma(qt0[i][:], PQ[1, tt * 128:(tt + 1) * 128, half * 512:(half + 1) * 512], writes=[qt0[i]], sem=sq_[i], q="sp")
                    k.dma(ct0[i][:], CT[tt * 128:(tt + 1) * 128, kb * 512:(kb + 1) * 512], writes=[ct0[i]], sem=sc_[i], q="sp")
                    k.dma(st0[i][:], ST[tt * 128:(tt + 1) * 128, kb * 512:(kb + 1) * 512], writes=[st0[i]], sem=ss_[i], q="sp")
                    k.rnd("pool", pt[i][:], pt0[i][:], [pt0[i]], [pt[i]])
                    k.rnd("dve", qt[i][:], qt0[i][:], [qt0[i]], [qt[i]])
                    k.rnd("pool", ct[i][:], ct0[i][:], [ct0[i]], [ct[i]])
                    k.rnd("act", st_[i][:], st0[i][:], [st0[i]], [st_[i]])
                    for j in range(4):
                        k.op("pe", lambda e, j=j, i=i, tt=tt: e.matmul(
                            pp[j][:], pt[i][:, j * 128:(j + 1) * 128], ct[i][:], start=(tt == 0), stop=False),
                            reads=[pt[i], ct[i]], writes=[pp[j]], accum=True)
                        k.op("pe", lambda e, j=j, i=i, tt=tt: e.matmul(
                            pp[j][:], qt[i][:, j * 128:(j + 1) * 128], st_[i][:], start=False, stop=(tt == T // 128 - 1)),
                            reads=[qt[i], st_[i]], writes=[pp[j]], accum=True)
                o = ob[io % 2]
                for j in range(4):
                    if j % 2 == 0:
                        k.op("act", lambda e, j=j, o=o: e.copy(out=o[:, j, :], in_=pp[j][:]), reads=[pp[j]], writes=[o.ch(j)])
                    else:
                        k.op("dve", lambda e, j=j, o=o: e.tensor_copy(out=o[:, j, :], in_=pp[j][:]), reads=[pp[j]], writes=[o.ch(j)])
                k.dma(mv_[:, half * 4:(half + 1) * 4, kb * 512:(kb + 1) * 512], o[:], reads=[o], sem=so[io % 2])
                io += 1
        k.end()
        if self.test == "fnet" and self.stop_at == 2:
            return
        self.gemm(MX, self.W("fnet", "fn_wo"), D, D, T, Y, bias_pv="fn_bo")

    def hy_tabs(self, L, names, r32=()):
        k = self.k
        NH = L // 64
        N1 = 2 * NH
        shp = {"F1": (NH, 2 * N1), "c": (64, N1), "s": (64, N1), "cT": (N1, 64), "sT": (N1, 64),
               "C2": (64, 64), "S2": (64, 64), "nS2": (64, 64), "RA": (64, 128), "RB": (64, 128),
               "C1": (N1, NH), "nS1": (N1, NH)}
        out = {}
        sm = k.dsem()
        for nm in names:
            r, c = shp[nm]
            t = k.tile([max(r, 1), c])
            key = nm if nm in ("C2", "S2", "nS2", "RA", "RB") else "%s_%d" % (nm, L)
            k.dma(t[:], self.W("hy", key), writes=[t], sem=sm)
            if nm in r32:
                t2 = k.tile([max(r, 1), c], F32R)
                k.rnd("dve", t2[:], t[:], [t], [t2])
                t = t2
            out[nm] = t
        return out

    def hy_filters(self, j, L, HF, RN):
        k = self.k
        N = 2 * L
        k.begin()
        s0 = k.dsem()
        zT = k.tile([33, L])
        w1 = k.tile([33, 64])
        w2 = k.tile([64, 2, 64])
        w3 = k.tile([64, 4096])
        hv = k.tile([64, 4])
        k.dma(zT[:], self.W("hy", "zT_%d" % L), writes=[zT], sem=s0)
        k.dma(w1[:], self.W("hy", "f_w1")[j * 33:(j + 1) * 33, :], writes=[w1], sem=s0)
        for n in range(2):
            k.dma(w2[:, n, :], self.W("hy", "f_w2")[(j * 2 + n) * 64:(j * 2 + n + 1) * 64, :], writes=[w2], sem=s0)
        k.dma(w3[:], self.W("hy", "f_w3")[j * 64:(j + 1) * 64, :], writes=[w3], sem=s0)
        k.dma(hv[:], self.W("hy", "hyv")[j * 64:(j + 1) * 64, :], writes=[hv], sem=s0)
        a = [k.tile([64, L]) for _ in range(3)]
        ps = [k.ptile([128, 512]) for _ in range(2)]
        tmp = [k.tile([64, 512]) for _ in range(2)]
        tmpi = [k.tile([64, 512], I32) for _ in range(2)]
        tmpf = [k.tile([64, 512]) for _ in range(2)]
        CT = min(512, L)
        it = 0
        for layer in range(3):
            for ct in range(L // CT):
                p = ps[it % 2]
                tb = tmp[it % 2]
                it += 1
                sl = slice(ct * CT, (ct + 1) * CT)
                if layer == 0:
                    k.op("pe", lambda e, p=p, sl=sl: e.matmul(p[:64, :CT], w1[:, :], zT[:, sl], start=True, stop=True),
                         reads=[w1, zT], writes=[p])
                else:
                    src = a[layer - 1]
                    k.op("pe", lambda e, p=p, sl=sl, src=src, layer=layer: e.matmul(
                        p[:64, :CT], w2[:, layer - 1, :], src[:, sl], start=True, stop=True),
                        reads=[w2, src], writes=[p])
                bcol = hv[:, 0:1] if layer == 0 else hv[:, 1 + layer:2 + layer]
                k.op("dve", lambda e, p=p, tb=tb, bcol=bcol: e.tensor_scalar(
                    out=tb[:, :CT], in0=p[:64, :CT], scalar1=bcol, scalar2=hv[:, 1:2], op0=ALU.add, op1=ALU.mult),
                    reads=[p, hv], writes=[tb])
                k.op("dve", lambda e, tb=tb: e.tensor_scalar(
                    out=tb[:, :CT], in0=tb[:, :CT], scalar1=1.0 / (2 * math.pi), scalar2=16.0, op0=ALU.mult, op1=ALU.add),
                    reads=[tb], writes=[tb])
                ti, tf = tmpi[it % 2], tmpf[it % 2]
                k.op("dve", lambda e, tb=tb, ti=ti: e.tensor_copy(out=ti[:, :CT], in_=tb[:, :CT]), reads=[tb], writes=[ti])
                k.op("dve", lambda e, tf=tf, ti=ti: e.tensor_copy(out=tf[:, :CT], in_=ti[:, :CT]), reads=[ti], writes=[tf])
                k.op("dve", lambda e, tb=tb, tf=tf: e.tensor_tensor(out=tb[:, :CT], in0=tb[:, :CT], in1=tf[:, :CT], op=ALU.subtract),
                     reads=[tb, tf], writes=[tb])
                k.op("dve", lambda e, tb=tb, tf=tf: e.scalar_tensor_tensor(
                    out=tf[:, :CT], in0=tb[:, :CT], scalar=0.5, in1=tb[:, :CT], op0=ALU.is_gt, op1=ALU.subtract),
                    reads=[tb], writes=[tf])
                dst = a[layer]
                k.op("act", lambda e, tf=tf, dst=dst, sl=sl: e.activation(
                    out=dst[:, sl], in_=tf[:, :CT], func=AF.Sin, scale=-2 * math.pi),
                    reads=[tf], writes=[dst], accum=True)
        a3 = a[2]
        win = [k.tile([128, 512]) for _ in range(2)]
        sw = [k.dsem() for _ in range(2)]
        hs = [k.tile([128, 512]) for _ in range(2)]
        ha = [k.tile([128, 512]) for _ in range(2)]
        so = [k.dsem() for _ in range(2)]
        pn = k.ptile([128, 512])
        nrm = k.tile([128, 8, 512], nsub=8)
        WIN = self.W("hy", "win_%d" % L)
        LT = min(128, L)
        nlt = L // LT
        it = 0
        for ct in range(8):
            dcol = (ct % 2) * 512
            for lt in range(nlt):
                i = it % 2
                it += 1
                p = ps[i]
                k.dma(win[i][:LT, :], WIN[lt * LT:(lt + 1) * LT, dcol:dcol + 512], writes=[win[i]], sem=sw[i])
                k.op("pe", lambda e, p=p, lt=lt, ct=ct: e.matmul(
                    p[:LT, :], a3[:, lt * LT:(lt + 1) * LT], w3[:, ct * 512:(ct + 1) * 512], start=True, stop=True),
                    reads=[a3, w3], writes=[p])
                k.op("dve", lambda e, p=p, i=i: e.tensor_tensor(out=hs[i][:LT, :], in0=p[:LT, :], in1=win[i][:LT, :], op=ALU.mult),
                     reads=[p, win[i]], writes=[hs[i]])
                if ct >= 4 and lt == 0:
                    k.op("dve", lambda e, i=i: e.memset(hs[i][0:1, :], 0.0), reads=[hs[i]], writes=[hs[i]])
                k.dma(HF[lt * LT:(lt + 1) * LT, ct * 512:(ct + 1) * 512], hs[i][:LT, :], reads=[hs[i]], sem=so[i])
                k.op("act", lambda e, i=i: e.activation(out=ha[i][:LT, :], in_=hs[i][:LT, :], func=AF.Abs),
                     reads=[hs[i]], writes=[ha[i]])
                k.op("pe", lambda e, i=i, lt=lt: e.matmul(pn[:, :], self.ones[:LT, :], ha[i][:LT, :],
                                                          start=(lt == 0), stop=(lt == nlt - 1)),
                     reads=[self.ones, ha[i]], writes=[pn], accum=True)
            k.op("act", lambda e, ct=ct: e.copy(out=nrm[:, ct, :], in_=pn[:, :]), reads=[pn], writes=[nrm.ch(ct)])
        rn = k.tile([128, 8, 512], nsub=8)
        for q in range(4):
            k.op("dve", lambda e, q=q: e.tensor_tensor(out=rn[:, q, :], in0=nrm[:, q, :], in1=nrm[:, q + 4, :], op=ALU.add),
                 reads=[nrm.ch(q), nrm.ch(q + 4)], writes=[rn.ch(q)])
            k.op("dve", lambda e, q=q: e.tensor_scalar(out=rn[:, q, :], in0=rn[:, q, :], scalar1=float(N), scalar2=None, op0=ALU.mult),
                 reads=[rn.ch(q)], writes=[rn.ch(q)])
            k.op("dve", lambda e, q=q: e.reciprocal(out=rn[:, q, :], in_=rn[:, q, :]), reads=[rn.ch(q)], writes=[rn.ch(q)])
            k.op("pool", lambda e, q=q: e.tensor_copy(out=rn[:, q + 4, :], in_=rn[:, q, :]), reads=[rn.ch(q)], writes=[rn.ch(q + 4)])
        sr = k.dsem()
        k.dma(RN[0:1, :], rn[0:1, :, :].rearrange("p a b -> p (a b)"), reads=[rn], sem=sr)
        k.end()

    def _fft_fwd(self, L, tb, zs, g0, G, A, Bt, X):
        k = self.k
        NH = L // 64
        N1 = 2 * NH
        for g in range(G):
            k.op("pe", lambda e, g=g: e.matmul(A[:64, g, 0:2 * N1], zs[:NH, :, g0 + g], tb["F1"][:NH, :],
                                               start=True, stop=True),
                 reads=[zs, tb["F1"]], writes=[A], accum=True)
        Ar, Ai = A[:64, :G, 0:N1], A[:64, :G, N1:2 * N1]
        cb = tb["c"][:, :].unsqueeze(1).to_broadcast([64, G, N1])
        sb = tb["s"][:, :].unsqueeze(1).to_broadcast([64, G, N1])
        t1, t2, t3, t4, Br, Bi = Bt
        v = lambda t: t[:64, :G * N1].rearrange("p (g n) -> p g n", g=G)
        k.op("dve", lambda e: e.tensor_tensor(out=v(t1), in0=Ar, in1=cb, op=ALU.mult), reads=[A, tb["c"]], writes=[t1])
        k.op("dve", lambda e: e.tensor_tensor(out=v(t2), in0=Ai, in1=sb, op=ALU.mult), reads=[A, tb["s"]], writes=[t2])
        k.op("dve", lambda e: e.tensor_tensor(out=v(t3), in0=Ai, in1=cb, op=ALU.mult), reads=[A, tb["c"]], writes=[t3])
        k.op("dve", lambda e: e.tensor_tensor(out=v(t4), in0=Ar, in1=sb, op=ALU.mult), reads=[A, tb["s"]], writes=[t4])
        k.op("pool", lambda e: e.tensor_tensor(out=Br[:64, :G * N1], in0=t1[:64, :G * N1], in1=t2[:64, :G * N1], op=ALU.add),
             reads=[t1, t2], writes=[Br])
        k.op("pool", lambda e: e.tensor_tensor(out=Bi[:64, :G * N1], in0=t3[:64, :G * N1], in1=t4[:64, :G * N1], op=ALU.subtract),
             reads=[t3, t4], writes=[Bi])
        W_ = G * N1
        k.op("pe", lambda e: e.matmul(X["r"][:64, :W_], tb["C2"][:, :], Br[:64, :W_], start=True, stop=False),
             reads=[tb["C2"], Br], writes=[X["r"]], accum=True)
        k.op("pe", lambda e: e.matmul(X["r"][:64, :W_], tb["S2"][:, :], Bi[:64, :W_], start=False, stop=True),
             reads=[tb["S2"], Bi], writes=[X["r"]], accum=True)
        k.op("pe", lambda e: e.matmul(X["i"][:64, :W_], tb["C2"][:, :], Bi[:64, :W_], start=True, stop=False),
             reads=[tb["C2"], Bi], writes=[X["i"]], accum=True)
        k.op("pe", lambda e: e.matmul(X["i"][:64, :W_], tb["nS2"][:, :], Br[:64, :W_], start=False, stop=True),
             reads=[tb["nS2"], Br], writes=[X["i"]], accum=True)

    def hy_filter_fft(self, L, HF, RN, SPEC):
        k = self.k
        NH = L // 64
        N1 = 2 * NH
        G = 512 // N1
        k.begin()
        tb = self.hy_tabs(L, ["F1", "c", "s", "C2", "S2", "nS2"], r32=("C2", "S2", "nS2"))
        zs = [k.tile([64, 64, 128]) for _ in range(2)]
        sz = [k.dsem() for _ in range(2)]
        A2 = [k.ptile([128, G, 2 * N1]) for _ in range(2)]
        X = {"r": k.ptile([128, 512]), "i": k.ptile([128, 512])}
        Bt = [k.tile([64, 512]) for _ in range(4)] + [k.tile([64, 512], F32R) for _ in range(2)]
        xo = [[k.tile([64, 512]) for _ in range(2)] for _ in range(2)]
        so = [k.dsem() for _ in range(2)]
        hv = HF.rearrange("(a b) c -> a b c", b=64)
        io = 0
        for cb in range(32):
            z = zs[cb % 2]
            k.dma(z[:NH, :, :], hv[:, :, cb * 128:(cb + 1) * 128], writes=[z], sem=sz[cb % 2])
            for sb in range(128 // G):
                g0 = sb * G
                ch0 = cb * 128 + g0
                self._fft_fwd(L, tb, z, g0, G, A2[io % 2], Bt, X)
                for ri, nm in enumerate(("r", "i")):
                    o = xo[io % 2][ri]
                    if ri == 0:
                        k.op("act", lambda e, o=o, nm=nm: e.copy(out=o[:, :G * N1], in_=X[nm][:64, :G * N1]), reads=[X[nm]], writes=[o])
                    else:
                        k.op("dve", lambda e, o=o, nm=nm: e.tensor_copy(out=o[:, :G * N1], in_=X[nm][:64, :G * N1]), reads=[X[nm]], writes=[o])
                    k.dma(SPEC[ri, :, ch0:ch0 + G, :], o[:, :G * N1].rearrange("p (g n) -> p g n", g=G),
                          reads=[o], sem=so[io % 2])
                io += 1
        k.end()

    def hy_conv(self, L, Z, zc0, XG, xc0, SPEC, o, bias_ap, OUT, RN):
        k = self.k
        NH = L // 64
        N1 = 2 * NH
        G = 4 if N1 == 128 else 8
        k.begin()
        CB = 64
        tb = self.hy_tabs(L, ["F1", "c", "s", "C2", "S2", "nS2", "RA", "RB", "cT", "sT", "C1", "nS1"],
                          r32=("C2", "S2", "nS2"))
        brow = k.tile([64, 1024])
        rnrow = k.tile([64, 1024])
        s0 = k.dsem()
        k.dma(brow[:], bias_ap.partition_broadcast(64), writes=[brow], sem=s0)
        k.dma(rnrow[:], RN[0:1, o * 1024:(o + 1) * 1024].partition_broadcast(64), writes=[rnrow], sem=s0)
        zs = [k.tile([64, 64, CB]) for _ in range(2)]
        xs = [k.tile([64, 64, CB]) for _ in range(2)]
        sz = [k.dsem() for _ in range(2)]
        sx = [k.dsem() for _ in range(2)]
        so = [k.dsem() for _ in range(2)]
        A2 = [k.ptile([128, G, 2 * N1]) for _ in range(2)]
        X = {"r": k.ptile([128, 512]), "i": k.ptile([128, 512])}
        Zp = k.ptile([128, G, 128])
        yp = k.ptile([128, 512])
        Bt = [k.tile([64, 512]) for _ in range(4)] + [k.tile([64, 512], F32R) for _ in range(2)]
        Kt = [[k.tile([64, 512]) for _ in range(4)] for _ in range(2)]
        sk = [k.dsem() for _ in range(2)]
        Kc = [k.tile([64, 512]) for _ in range(2)]
        Yt = [k.tile([64, 512]) for _ in range(6)]
        Zt = [k.tile([128, G * 64]) for _ in range(6)]
        et = [k.tile([64, 64, G]) for _ in range(3)]
        zv = Z.rearrange("(a b) c -> a b c", b=64)
        xv = XG.rearrange("(a b) c -> a b c", b=64)
        ov = OUT.rearrange("(a b) c -> a b c", b=64)
        W_ = G * N1
        ik = 0
        for cb in range(D // CB):
            z, x = zs[cb % 2], xs[cb % 2]
            k.dma(z[:NH, :, :], zv[:, :, zc0 + cb * CB:zc0 + (cb + 1) * CB], writes=[z], sem=sz[cb % 2])
            k.dma(x[:NH, :, :], xv[:, :, xc0 + cb * CB:xc0 + (cb + 1) * CB], writes=[x], sem=sx[cb % 2])
            for sb in range(CB // G):
                g0 = sb * G
                d0 = cb * CB + g0
                kt = Kt[ik % 2]
                for q, (ri, dr) in enumerate(((0, 0), (1, 0), (0, 1), (1, 1))):
                    ch0 = dr * 2048 + o * 1024 + d0
                    k.dma(kt[q][:, :W_].rearrange("p (g n) -> p g n", g=G), SPEC[ri, :, ch0:ch0 + G, :],
                          writes=[kt[q]], sem=sk[ik % 2])
                ik += 1
                self._fft_fwd(L, tb, z, g0, G, A2[ik % 2], Bt, X)
                Kr, Ki = Kc
                k.op("pool", lambda e, kt=kt: e.tensor_tensor(out=Kr[:, :W_], in0=kt[0][:, :W_], in1=kt[2][:, :W_], op=ALU.add),
                     reads=[kt[0], kt[2]], writes=[Kr])
                k.op("pool", lambda e, kt=kt: e.tensor_tensor(out=Ki[:, :W_], in0=kt[1][:, :W_], in1=kt[3][:, :W_], op=ALU.subtract),
                     reads=[kt[1], kt[3]], writes=[Ki])
                y1, y2, y3, y4, Yr, Yi = Yt
                k.op("dve", lambda e: e.tensor_tensor(out=y1[:, :W_], in0=X["r"][:64, :W_], in1=Kr[:, :W_], op=ALU.mult), reads=[X["r"], Kr], writes=[y1])
                k.op("dve", lambda e: e.tensor_tensor(out=y2[:, :W_], in0=X["i"][:64, :W_], in1=Ki[:, :W_], op=ALU.mult), reads=[X["i"], Ki], writes=[y2])
                k.op("dve", lambda e: e.tensor_tensor(out=y3[:, :W_], in0=X["r"][:64, :W_], in1=Ki[:, :W_], op=ALU.mult), reads=[X["r"], Ki], writes=[y3])
                k.op("dve", lambda e: e.tensor_tensor(out=y4[:, :W_], in0=X["i"][:64, :W_], in1=Kr[:, :W_], op=ALU.mult), reads=[X["i"], Kr], writes=[y4])
                k.op("pool", lambda e: e.tensor_tensor(out=Yr[:, :W_], in0=y1[:, :W_], in1=y2[:, :W_], op=ALU.subtract), reads=[y1, y2], writes=[Yr])
                k.op("pool", lambda e: e.tensor_tensor(out=Yi[:, :W_], in0=y3[:, :W_], in1=y4[:, :W_], op=ALU.add), reads=[y3, y4], writes=[Yi])
                for g in range(G):
                    k.op("pe", lambda e, g=g: e.matmul(Zp[:N1, g, :], Yr[:, g * N1:(g + 1) * N1], tb["RA"][:, :], start=True, stop=False),
                         reads=[Yr, tb["RA"]], writes=[Zp], accum=True)
                    k.op("pe", lambda e, g=g: e.matmul(Zp[:N1, g, :], Yi[:, g * N1:(g + 1) * N1], tb["RB"][:, :], start=False, stop=True),
                         reads=[Yi, tb["RB"]], writes=[Zp], accum=True)
                Zr, Zi = Zp[:N1, :, 0:64], Zp[:N1, :, 64:128]
                cT = tb["cT"][:N1, :].unsqueeze(1).to_broadcast([N1, G, 64])
                sT = tb["sT"][:N1, :].unsqueeze(1).to_broadcast([N1, G, 64])
                u1, u2, u3, u4, Zr2, Zi2 = Zt
                v = lambda t: t[:N1, :].rearrange("p (g n) -> p g n", g=G)
                k.op("dve", lambda e: e.tensor_tensor(out=v(u1), in0=Zr, in1=cT, op=ALU.mult), reads=[Zp, tb["cT"]], writes=[u1])
                k.op("dve", lambda e: e.tensor_tensor(out=v(u2), in0=Zi, in1=sT, op=ALU.mult), reads=[Zp, tb["sT"]], writes=[u2])
                k.op("dve", lambda e: e.tensor_tensor(out=v(u3), in0=Zr, in1=sT, op=ALU.mult), reads=[Zp, tb["sT"]], writes=[u3])
                k.op("dve", lambda e: e.tensor_tensor(out=v(u4), in0=Zi, in1=cT, op=ALU.mult), reads=[Zp, tb["cT"]], writes=[u4])
                k.op("pool", lambda e: e.tensor_tensor(out=Zr2[:N1, :], in0=u1[:N1, :], in1=u2[:N1, :], op=ALU.subtract), reads=[u1, u2], writes=[Zr2])
                k.op("pool", lambda e: e.tensor_tensor(out=Zi2[:N1, :], in0=u3[:N1, :], in1=u4[:N1, :], op=ALU.add), reads=[u3, u4], writes=[Zi2])
                k.op("pe", lambda e: e.matmul(yp[:NH, :G * 64], tb["C1"][:N1, :NH], Zr2[:N1, :], start=True, stop=False),
                     reads=[tb["C1"], Zr2], writes=[yp], accum=True)
                k.op("pe", lambda e: e.matmul(yp[:NH, :G * 64], tb["nS1"][:N1, :NH], Zi2[:N1, :], start=False, stop=True),
                     reads=[tb["nS1"], Zi2], writes=[yp], accum=True)
                e1, e2, e3 = et
                bv = brow[:NH, d0:d0 + G].unsqueeze(1).to_broadcast([NH, 64, G])
                rv = rnrow[:NH, d0:d0 + G].unsqueeze(1).to_broadcast([NH, 64, G])
                k.op("pool", lambda e, z=z, g0=g0, bv=bv: e.tensor_tensor(out=e1[:NH, :, :], in0=z[:NH, :, g0:g0 + G], in1=bv, op=ALU.mult),
                     reads=[z, brow], writes=[e1])
                k.op("dve", lambda e, rv=rv: e.tensor_tensor(out=e3[:NH, :, :], in0=yp[:NH, :G * 64].rearrange("p (g n) -> p n g", g=G),
                                                             in1=rv, op=ALU.mult), reads=[yp, rnrow], writes=[e3])
                k.op("pool", lambda e: e.tensor_tensor(out=e2[:NH, :, :], in0=e3[:NH, :, :], in1=e1[:NH, :, :], op=ALU.add),
                     reads=[e3, e1], writes=[e2])
                k.op("pool", lambda e, x=x, g0=g0: e.tensor_tensor(out=x[:NH, :, g0:g0 + G], in0=x[:NH, :, g0:g0 + G], in1=e2[:NH, :, :], op=ALU.mult),
                     reads=[x, e2], writes=[x])
            k.dma(ov[:, :, cb * CB:(cb + 1) * CB], x[:NH, :, :], reads=[x], sem=so[cb % 2])
        k.end()

    def hy_conv3(self, U, L, j, UC):
        k = self.k
        k.begin()
        C3 = 3 * D
        wr = k.tile([128, 3, C3])
        br = k.tile([128, C3])
        s0 = k.dsem()
        for r in range(3):
            k.dma(wr[:, r, :], self.W("hy", "conv_w")[j * 3 + r:j * 3 + r + 1, :].partition_broadcast(128), writes=[wr], sem=s0)
        k.dma(br[:], self.W("hy", "conv_b")[j:j + 1, :].partition_broadcast(128), writes=[br], sem=s0)
        ub = [[k.tile([128, C3]) for _ in range(3)] for _ in range(2)]
        su = [[k.dsem() for _ in range(3)] for _ in range(2)]
        so = [k.dsem() for _ in range(2)]
        nt = L // 128
        for t in range(nt):
            u0, u1, u2 = ub[t % 2]
            s_ = su[t % 2]
            t0 = t * 128
            if t == 0:
                k.op("pool", lambda e, u0=u0: e.memset(u0[:], 0.0), writes=[u0])
                k.dma(u0[1:128, :], U[0:127, :], writes=[u0], sem=s_[0])
            else:
                k.dma(u0[:], U[t0 - 1:t0 + 127, :], writes=[u0], sem=s_[0])
            k.dma(u1[:], U[t0:t0 + 128, :], writes=[u1], sem=s_[1])
            if t == nt - 1:
                k.op("pool", lambda e, u2=u2: e.memset(u2[:], 0.0), writes=[u2])
                k.dma(u2[0:127, :], U[t0 + 1:t0 + 128, :], writes=[u2], sem=s_[2])
            else:
                k.dma(u2[:], U[t0 + 1:t0 + 129, :], writes=[u2], sem=s_[2])
            k.op("pool", lambda e, u0=u0: e.tensor_tensor(out=u0[:], in0=u0[:], in1=wr[:, 0, :], op=ALU.mult), reads=[u0, wr], writes=[u0])
            k.op("dve", lambda e, u1=u1: e.tensor_tensor(out=u1[:], in0=u1[:], in1=wr[:, 1, :], op=ALU.mult), reads=[u1, wr], writes=[u1])
            k.op("pool", lambda e, u2=u2: e.tensor_tensor(out=u2[:], in0=u2[:], in1=wr[:, 2, :], op=ALU.mult), reads=[u2, wr], writes=[u2])
            k.op("dve", lambda e, u0=u0, u1=u1: e.tensor_tensor(out=u1[:], in0=u1[:], in1=u0[:], op=ALU.add), reads=[u0, u1], writes=[u1])
            k.op("pool", lambda e, u2=u2: e.tensor_tensor(out=u2[:], in0=u2[:], in1=br[:], op=ALU.add), reads=[u2, br], writes=[u2])
            k.op("dve", lambda e, u1=u1, u2=u2: e.tensor_tensor(out=u1[:], in0=u1[:], in1=u2[:], op=ALU.add), reads=[u1, u2], writes=[u1])
            k.dma(UC[t0:t0 + 128, :], u1[:], reads=[u1], sem=so[t % 2])
        k.end()

    def hyena(self, j, HLFM, L, Y, tag):
        N1 = L // 32
        U = self.dt("hyU" + tag, [L, 3 * D])
        UC = self.dt("hyUC" + tag, [L, 3 * D])
        HF = self.dt("hyHF" + tag, [L, 4096])
        RN = self.dt("hyRN" + tag, [1, 4096])
        SPEC = self.dt("hySP" + tag, [2, 64, 4096, N1])
        Z1 = self.dt("hyZ1" + tag, [L, D])
        Z2 = self.dt("hyZ2" + tag, [L, D])
        Z2T = self.dt("hyZ2T" + tag, [D, L])
        self.hy_filters(j, L, HF, RN)
        if self.stop_at == 1:
            return
        self.hy_filter_fft(L, HF, RN, SPEC)
        if self.stop_at == 2:
            return
        for q3 in range(3):
            self.gemm(HLFM, self.W("hy", "w_in")[j * D:(j + 1) * D, q3 * D:(q3 + 1) * D], D, D, L, U[:, q3 * D:(q3 + 1) * D],
                      out_tm=True, bias_row=self.W("hy", "b_in")[j:j + 1, q3 * D:(q3 + 1) * D])
        self.hy_conv3(U, L, j, UC)
        if self.stop_at == 3:
            return
        fb = self.W("hy", "f_bias")
        self.hy_conv(L, UC, 0, UC, D, SPEC, 0, fb[j * 2:j * 2 + 1, :], Z1, RN)
        if self.stop_at == 4:
            return
        self.hy_conv(L, Z1, 0, UC, 2 * D, SPEC, 1, fb[j * 2 + 1:j * 2 + 2, :], Z2, RN)
        self.transpose_in(Z2, Z2T, L)
        self.gemm(Z2T, self.W("hy", "w_out")[j * D:(j + 1) * D, :], D, D, L, Y, bias_pv="hy_b_out%d" % j)

    def rwkv(self, HC, HL, Y):
        k = self.k
        S = TC + T
        NT = S // 128
        NCH = S // 64
        H = 16
        Wr = lambda nm: self.W("rw", nm)
        rows = Wr("rows")
        XN = self.dt("rwXN", [6, D, S])
        k.begin()
        hh = [k.tile([128, T + 2]) for _ in range(2)]
        sh = [k.dsem() for _ in range(2)]
        xx = k.tile([128, T])
        xo = [k.tile([128, T]) for _ in range(2)]
        so = [k.dsem() for _ in range(2)]
        it = io = 0
        for (src, Tn, c0) in ((HC, TC, 0), (HL, T, TC)):
            for c in range(8):
                hb = hh[it % 2]
                it += 1
                k.op("pool", lambda e, hb=hb: e.memset(hb[:], 0.0), writes=[hb])
                k.dma(hb[:, 1:Tn + 1], src[c * 128:(c + 1) * 128, :], writes=[hb], sem=sh[it % 2])
                k.op("dve", lambda e, hb=hb, Tn=Tn: e.tensor_tensor(out=xx[:, :Tn], in0=hb[:, 0:Tn], in1=hb[:, 2:Tn + 2], op=ALU.add),
                     reads=[hb], writes=[xx])
                k.op("dve", lambda e, hb=hb, Tn=Tn: e.scalar_tensor_tensor(out=xx[:, :Tn], in0=xx[:, :Tn], scalar=0.5, in1=hb[:, 1:Tn + 1],
                                                                       op0=ALU.mult, op1=ALU.subtract), reads=[hb, xx], writes=[xx])
                for n in range(6):
                    o = xo[io % 2]
                    mu = self.pvc("rw_mu%d" % n, c)
                    eng = "dve"
                    k.op(eng, lambda e, o=o, hb=hb, Tn=Tn, mu=mu: e.scalar_tensor_tensor(
                        out=o[:, :Tn], in0=xx[:, :Tn], scalar=mu, in1=hb[:, 1:Tn + 1], op0=ALU.mult, op1=ALU.add),
                        reads=[xx, hb, self.pv], writes=[o])
                    k.dma(XN[n, c * 128:(c + 1) * 128, c0:c0 + Tn], o[:, :Tn], reads=[o], sem=so[io % 2])
                    io += 1
        k.end()
        Rm = self.dt("rwR", [S, D]); Km = self.dt("rwK", [S, D]); Vm = self.dt("rwV", [S, D])
        SW = [self.dt("rwSW%d" % n, [S, D]) for n in range(2)]
        Am = [self.dt("rwA%d" % n, [S, D]) for n in range(2)]
        Gm = self.dt("rwG", [S, D])
        HW = self.dt("rwHW", [128, S]); HA = self.dt("rwHA", [128, S]); HG = self.dt("rwHG", [256, S])
        self.gemm(XN[0], Wr("wr"), D, D, S, Rm, out_tm=True)
        self.gemm(XN[2], Wr("wk"), D, D, S, Km, out_tm=True)
        self.gemm(XN[3], Wr("wv"), D, D, S, Vm, out_tm=True)
        self.gemm(XN[1], Wr("w1cat"), D, 128, S, HW, act=AF.Tanh)
        self.gemm(XN[4], Wr("a1cat"), D, 128, S, HA)
        self.gemm(XN[5], Wr("g1pad"), D, 256, S, HG, act=AF.Sigmoid)
        for n in range(2):
            self.gemm(HW, Wr("w2pad%d" % n), 128, D, S, SW[n], out_tm=True, bias_row=rows[n:n + 1, :], act=AF.Sigmoid)
            self.gemm(HA, Wr("a2pad%d" % n), 128, D, S, Am[n], out_tm=True, bias_row=rows[2 + n:3 + n, :], act=AF.Sigmoid)
        self.gemm(HG, Wr("g2pad"), 256, D, S, Gm, out_tm=True)
        XT = self.dt("rwXT", [2, NT, 64, H, 4, 128])
        KD = self.dt("rwKD", [2, S, D]); NAD = self.dt("rwNAD", [2, S, D])
        PCf = self.dt("rwPC", [2, NCH, 64, H])
        BON = self.dt("rwBON", [S, D])
        k.begin()
        s0 = k.dsem()
        rw = k.tile([128, 3, D])
        for i_, r_ in enumerate((4, 5, 6)):
            k.dma(rw[:, i_, :], rows[r_:r_ + 1, :].partition_broadcast(128), writes=[rw], sem=s0)
        tri = [k.tile([128, 128]) for _ in range(2)]
        for n in range(2):
            k.dma(tri[n][:], Wr("TRI%d" % n), writes=[tri[n]], sem=s0)
        chk = k.tile([128, 2])
        k.dma(chk[:], Wr("CHK"), writes=[chk], sem=s0)
        inp = {nm: k.tile([128, D]) for nm in ("r", "k", "v", "sw0", "sw1", "a0", "a1")}
        sin = {nm: k.dsem() for nm in inp}
        srcs = {"r": Rm, "k": Km, "v": Vm, "sw0": SW[0], "sw1": SW[1], "a0": Am[0], "a1": Am[1]}
        wk_ = {nm: k.tile([128, D]) for nm in ("kkq", "kk", "t1", "t2", "lw", "kdir", "kd", "nad", "kt", "rt", "ksum")}
        sm = k.tile([128, H])
        pl = k.ptile([128, 2, 512])
        ptp = [k.ptile([64, 4, 128]) for _ in range(2)]
        ppc = k.ptile([64, H, 2])
        xts = k.tile([64, H, 4, 128])
        pcs = k.tile([64, 2, H])
        sxo = k.dsem(); sko = k.dsem(); sno = k.dsem(); spo = k.dsem(); sbo = k.dsem()
        v3 = lambda t: t[:].rearrange("p (h c) -> p h c", h=H)
        itp = 0
        for tt in range(NT):
            r0 = tt * 128
            for nm in inp:
                k.dma(inp[nm][:], srcs[nm][r0:r0 + 128, :], writes=[inp[nm]], sem=sin[nm])
            R_, K_, V_ = inp["r"], inp["k"], inp["v"]
            kkq, kk, t1, t2, lw, kdir, kd, nad, kt, rt, ksum = (wk_[n_] for n_ in ("kkq", "kk", "t1", "t2", "lw", "kdir", "kd", "nad", "kt", "rt", "ksum"))
            k.op("dve", lambda e: e.tensor_tensor(out=kkq[:], in0=K_[:], in1=rw[:, 0, :], op=ALU.mult), reads=[K_, rw], writes=[kkq])
            k.op("pool", lambda e: e.tensor_tensor(out=t1[:], in0=kkq[:], in1=kkq[:], op=ALU.mult), reads=[kkq], writes=[t1])
            k.op("dve", lambda e: e.tensor_reduce(out=sm[:], in_=v3(t1), axis=AX.X, op=ALU.add), reads=[t1], writes=[sm])
            k.op("dve", lambda e: e.tensor_scalar(out=sm[:], in0=sm[:], scalar1=1e-24, scalar2=None, op0=ALU.max), reads=[sm], writes=[sm])
            k.op("act", lambda e: e.sqrt(out=sm[:], in_=sm[:]), reads=[sm], writes=[sm])
            k.op("dve", lambda e: e.reciprocal(out=sm[:], in_=sm[:]), reads=[sm], writes=[sm])
            k.op("dve", lambda e: e.tensor_tensor(out=v3(kk), in0=v3(kkq), in1=sm[:].unsqueeze(2).to_broadcast([128, H, 64]), op=ALU.mult),
                 reads=[kkq, sm], writes=[kk])
            for n in range(2):
                SWt, At = inp["sw%d" % n], inp["a%d" % n]
                k.op("act", lambda e, SWt=SWt: e.mul(out=lw[:], in_=SWt[:], mul=-0.6065306597126334), reads=[SWt], writes=[lw])
                k.op("dve", lambda e, At=At: e.scalar_tensor_tensor(out=t1[:], in0=At[:], scalar=-1.0, in1=rw[:, 1, :], op0=ALU.add, op1=ALU.mult),
                     reads=[At, rw], writes=[t1])
                k.op("dve", lambda e: e.scalar_tensor_tensor(out=kdir[:], in0=t1[:], scalar=1.0, in1=K_[:], op0=ALU.add, op1=ALU.mult),
                     reads=[t1, K_], writes=[kdir])
                if n == 0:
                    k.op("pool", lambda e: e.tensor_copy(out=ksum[:], in_=kdir[:]), reads=[kdir], writes=[ksum])
                else:
                    k.op("pool", lambda e: e.tensor_tensor(out=ksum[:], in0=ksum[:], in1=kdir[:], op=ALU.add), reads=[kdir, ksum], writes=[ksum])
                k.op("pool", lambda e, At=At: e.tensor_tensor(out=nad[:], in0=kk[:], in1=At[:], op=ALU.mult), reads=[kk, At], writes=[nad])
                for hf in range(2):
                    k.op("pe", lambda e, hf=hf, n=n: e.matmul(pl[:, hf, :], tri[n][:], lw[:, hf * 512:(hf + 1) * 512], start=True, stop=True),
                         reads=[tri[n], lw], writes=[pl], accum=True)
                plv = pl[:].rearrange("p a b -> p (a b)")
                k.op("dve", lambda e: e.tensor_tensor(out=t2[:], in0=plv, in1=lw[:], op=ALU.subtract), reads=[pl, lw], writes=[t2])
                k.op("act", lambda e: e.activation(out=t2[:], in_=t2[:], func=AF.Exp), reads=[t2], writes=[t2])
                k.op("pool", lambda e: e.tensor_tensor(out=kt[:], in0=kk[:], in1=t2[:], op=ALU.mult), reads=[kk, t2], writes=[kt])
                k.op("act", lambda e: e.activation(out=t1[:], in_=plv, func=AF.Exp), reads=[pl], writes=[t1])
                k.op("dve", lambda e: e.tensor_tensor(out=rt[:], in0=R_[:], in1=t1[:], op=ALU.mult), reads=[R_, t1], writes=[rt])
                k.op("act", lambda e: e.activation(out=t2[:], in_=plv, func=AF.Exp, scale=-1.0), reads=[pl], writes=[t2])
                k.op("dve", lambda e: e.tensor_tensor(out=kd[:], in0=kdir[:], in1=t2[:], op=ALU.mult), reads=[kdir, t2], writes=[kd])
                k.op("dve", lambda e: e.scalar_tensor_tensor(out=nad[:], in0=nad[:], scalar=-1.0, in1=t2[:], op0=ALU.mult, op1=ALU.mult),
                     reads=[nad, t2], writes=[nad])
                k.dma(KD[n, r0:r0 + 128, :], kd[:], reads=[kd], sem=sko)
                k.dma(NAD[n, r0:r0 + 128, :], nad[:], reads=[nad], sem=sno)
                for h in range(H):
                    k.op("pe", lambda e, h=h: e.matmul(ppc[:, h, :], lw[:, h * 64:(h + 1) * 64], chk[:], start=True, stop=True),
                         reads=[lw, chk], writes=[ppc], accum=True)
                k.op("act", lambda e: e.activation(out=pcs[:].rearrange("p c h -> p h c"), in_=ppc[:], func=AF.Exp), reads=[ppc], writes=[pcs])
                for cc in range(2):
                    k.dma(PCf[n, tt * 2 + cc, :, :], pcs[:, cc, :], reads=[pcs], sem=spo)
                for q, srct in enumerate((kt, rt, kd, nad)):
                    for hg in range(4):
                        p = ptp[itp % 2]
                        itp += 1
                        for hl in range(4):
                            h = hg * 4 + hl
                            k.op("pe", lambda e, p=p, hl=hl, h=h, srct=srct: e.transpose(p[:, hl, :], srct[:, h * 64:(h + 1) * 64], self.ident[:]),
                                 reads=[srct, self.ident], writes=[p], accum=True)
                        if itp % 2 == 0:
                            k.op("act", lambda e, p=p, hg=hg, q=q: e.copy(out=xts[:, hg * 4:(hg + 1) * 4, q, :], in_=p[:]), reads=[p], writes=[xts], accum=True)
                        else:
                            k.op("dve", lambda e, p=p, hg=hg, q=q: e.tensor_copy(out=xts[:, hg * 4:(hg + 1) * 4, q, :], in_=p[:]), reads=[p], writes=[xts], accum=True)
                k._wait("sp", [(k.esem["act"], k.cnt[k.esem["act"]]), (k.esem["dve"], k.cnt[k.esem["dve"]])])
                k.dma(XT[n, tt], xts[:], reads=[xts], sem=sxo, q="sp")
            k.op("dve", lambda e: e.tensor_tensor(out=t1[:], in0=ksum[:], in1=rw[:, 2, :], op=ALU.mult), reads=[ksum, rw], writes=[t1])
            k.op("dve", lambda e: e.tensor_tensor(out=t1[:], in0=t1[:], in1=R_[:], op=ALU.mult), reads=[t1, R_], writes=[t1])
            k.op("dve", lambda e: e.tensor_reduce(out=sm[:], in_=v3(t1), axis=AX.X, op=ALU.add), reads=[t1], writes=[sm])
            k.op("dve", lambda e: e.tensor_tensor(out=v3(t2), in0=v3(V_), in1=sm[:].unsqueeze(2).to_broadcast([128, H, 64]), op=ALU.mult),
                 reads=[V_, sm], writes=[t2])
            k.dma(BON[r0:r0 + 128, :], t2[:], reads=[t2], sem=sbo)
        k.end()
        if self.stop_at == 3:
            return
        CH = self.dt("rwCH", [2, NCH, 64, H, 256])
        k.begin()
        s0 = k.dsem()
        msk = [k.tile([64, 512]) for _ in range(2)]
        for n in range(2):
            k.dma(msk[n][:], Wr("MASK%d" % n), writes=[msk[n]], sem=s0)
        idn = k.tile([64, 64])
        k.dma(idn[:], Wr("IDN"), writes=[idn], sem=s0)
        xt = [k.tile([64, H, 4, 128]) for _ in range(2)]
        sx = [k.dsem() for _ in range(2)]
        sc = [k.tile([64, H, 320]) for _ in range(2)]
        psc = k.ptile([64, 2, 512])
        pN = k.ptile([64, H, 64]); pL = k.ptile([64, H, 64]); pX = k.ptile([64, H, 64])
        Nb = [k.tile([64, H, 64]) for _ in range(2)]
        Lb = [k.tile([64, H, 64]) for _ in range(2)]
        ILb = k.tile([64, H, 64])
        Xb = [k.tile([64, H, 64]) for _ in range(2)]
        so1 = [k.dsem() for _ in range(2)]
        so2 = [k.dsem() for _ in range(2)]
        idb = idn[:].unsqueeze(1).to_broadcast([64, H, 64])
        ci = 0
        for tt in range(NT):
            for n in range(2):
                x = xt[(tt * 2 + n) % 2]
                k.dma(x[:], XT[n, tt], writes=[x], sem=sx[(tt * 2 + n) % 2])
                for cc in range(2):
                    cs = slice(cc * 64, (cc + 1) * 64)
                    s_ = sc[ci % 2]
                    for hp in range(8):
                        for hl in range(2):
                            h = hp * 2 + hl
                            k.op("pe", lambda e, hl=hl, h=h, x=x, cs=cs: e.matmul(
                                psc[:, hl, 0:128].rearrange("p (a b) -> p a b", a=2), x[:, h, 3, cs], x[:, h, 0:2, cs], start=True, stop=True),
                                reads=[x], writes=[psc], accum=True)
                            k.op("pe", lambda e, hl=hl, h=h, x=x, cs=cs: e.matmul(
                                psc[:, hl, 128:256].rearrange("p (a b) -> p a b", a=2), x[:, h, 2, cs], x[:, h, 0:2, cs], start=True, stop=True),
                                reads=[x], writes=[psc], accum=True)
                            k.op("pe", lambda e, hl=hl, h=h, x=x, cs=cs: e.matmul(
                                psc[:, hl, 256:320], x[:, h, 0, cs], x[:, h, 3, cs], start=True, stop=True),
                                reads=[x], writes=[psc], accum=True)
                        k.op("dve", lambda e, hp=hp, s_=s_, n=n: e.tensor_tensor(
                            out=s_[:, hp * 2:hp * 2 + 2, :], in0=psc[:, :, 0:320],
                            in1=msk[n][:, 0:320].unsqueeze(1).to_broadcast([64, 2, 320]), op=ALU.mult),
                            reads=[psc, msk[n]], writes=[s_], accum=True)
                    N2, L2 = s_[:, :, 0:64], s_[:, :, 256:320]
                    Xc = Xb[0]
                    k.op("pool", lambda e, Xc=Xc, N2=N2: e.tensor_tensor(out=Xc[:], in0=idb, in1=N2, op=ALU.subtract), reads=[idn, s_], writes=[Xc])
                    Ncur, Lcur, Nbuf, Lbuf = N2, L2, s_, s_
                    for j in range(1, 6):
                        Ln = Lb[j % 2]
                        for h in range(H):
                            k.op("pe", lambda e, h=h, Ncur=Ncur, Lcur=Lcur: e.matmul(pL[:, h, :], Ncur[:, h, :], Lcur[:, h, :], start=True, stop=True),
                                 reads=[Nbuf, Lbuf], writes=[pL], accum=True)
                        if j < 5:
                            Nn = Nb[j % 2]
                            for h in range(H):
                                k.op("pe", lambda e, h=h, Ncur=Ncur, Lcur=Lcur: e.matmul(pN[:, h, :], Lcur[:, h, :], Ncur[:, h, :], start=True, stop=True),
                                     reads=[Nbuf, Lbuf], writes=[pN], accum=True)
                            k.op("act", lambda e, Nn=Nn: e.copy(out=Nn[:], in_=pN[:]), reads=[pN], writes=[Nn])
                        k.op("dve", lambda e: e.tensor_tensor(out=ILb[:], in0=pL[:], in1=idb, op=ALU.add), reads=[pL, idn], writes=[ILb])
                        if j < 5:
                            k.op("dve", lambda e, Ln=Ln: e.tensor_copy(out=Ln[:], in_=pL[:]), reads=[pL], writes=[Ln])
                        Xn = Xb[j % 2]
                        for h in range(H):
                            k.op("pe", lambda e, h=h, Xc=Xc: e.matmul(pX[:, h, :], ILb[:, h, :], Xc[:, h, :], start=True, stop=True),
                                 reads=[ILb, Xc], writes=[pX], accum=True)
                        k.op("act", lambda e, Xn=Xn: e.copy(out=Xn[:], in_=pX[:]), reads=[pX], writes=[Xn])
                        Xc = Xn
                        if j < 5:
                            Ncur, Lcur, Nbuf, Lbuf = Nn[:], Ln[:], Nn, Ln
                    chn = tt * 2 + cc
                    k.dma(CH[n, chn, :, :, 0:64], Xc[:], reads=[Xc], sem=so1[ci % 2])
                    k.dma(CH[n, chn, :, :, 64:256], s_[:, :, 64:256], reads=[s_], sem=so2[ci % 2])
                    ci += 1
        k.end()
        if self.stop_at == 4:
            return
        YW = self.dt("rwYW", [2, S, D])
        k.begin()
        M = [k.tile([64, H, 64]) for _ in range(2)]
        for n in range(2):
            k.op("pool", lambda e, n=n: e.memset(M[n][:], 0.0), writes=[M[n]])
        ktrt = [k.tile([64, H, 2, 64]) for _ in range(2)]
        cht = [k.tile([64, H, 256]) for _ in range(2)]
        kdt = [k.tile([64, D]) for _ in range(2)]
        nadt = [k.tile([64, D]) for _ in range(2)]
        vt = [k.tile([64, D]) for _ in range(2)]
        pct = [k.tile([64, H]) for _ in range(2)]
        sl_ = [[k.dsem() for _ in range(6)] for _ in range(2)]
        W0s = k.tile([64, H, 64]); Us = k.tile([64, H, 64])
        Ys = [k.tile([64, H, 64]) for _ in range(2)]
        sy = [k.dsem() for _ in range(2)]
        pW = k.ptile([64, H, 64]); pU = k.ptile([64, H, 64]); pY = k.ptile([64, H, 64]); pM = k.ptile([64, H, 64])
        order = {0: list(range(NCH)), 1: list(range(TC // 64 - 1, -1, -1)) + list(range(NCH - 1, TC // 64 - 1, -1))}
        for step in range(NCH):
            for n in range(2):
                chn = order[n][step]
                tt, cc = chn // 2, chn % 2
                cs = slice(cc * 64, (cc + 1) * 64)
                r0 = chn * 64
                b = n
                sl = sl_[b]
                k.dma(ktrt[b][:], XT[n, tt, :, :, 0:2, cs], writes=[ktrt[b]], sem=sl[0], q="sp")
                k.dma(cht[b][:], CH[n, chn], writes=[cht[b]], sem=sl[1], q="act")
                k.dma(kdt[b][:], KD[n, r0:r0 + 64, :], writes=[kdt[b]], sem=sl[2], q="sp")
                k.dma(nadt[b][:], NAD[n, r0:r0 + 64, :], writes=[nadt[b]], sem=sl[3], q="act")
                k.dma(vt[b][:], Vm[r0:r0 + 64, :], writes=[vt[b]], sem=sl[4], q="sp")
                k.dma(pct[b][:], PCf[n, chn], writes=[pct[b]], sem=sl[5], q="act")
                kr, ch_, kd_, nad_, v_, pc_, Mn = ktrt[b], cht[b], kdt[b], nadt[b], vt[b], pct[b], M[n]
                for h in range(H):
                    hs = slice(h * 64, (h + 1) * 64)
                    k.op("pe", lambda e, h=h: e.matmul(pW[:, h, :], kr[:, h, 0, :], Mn[:, h, :], start=True, stop=False),
                         reads=[kr, Mn], writes=[pW], accum=True)
                    k.op("pe", lambda e, h=h, hs=hs: e.matmul(pW[:, h, :], ch_[:, h, 128:192], v_[:, hs], start=False, stop=True),
                         reads=[ch_, v_], writes=[pW], accum=True)
                k.op("act", lambda e: e.copy(out=W0s[:], in_=pW[:]), reads=[pW], writes=[W0s])
                for h in range(H):
                    k.op("pe", lambda e, h=h: e.matmul(pU[:, h, :], ch_[:, h, 0:64], W0s[:, h, :], start=True, stop=True),
                         reads=[ch_, W0s], writes=[pU], accum=True)
                k.op("dve", lambda e: e.tensor_copy(out=Us[:], in_=pU[:]), reads=[pU], writes=[Us])
                if chn >= TC // 64:
                    ys = Ys[n]
                    for h in range(H):
                        hs = slice(h * 64, (h + 1) * 64)
                        k.op("pe", lambda e, h=h: e.matmul(pY[:, h, :], kr[:, h, 1, :], Mn[:, h, :], start=True, stop=False),
                             reads=[kr, Mn], writes=[pY], accum=True)
                        k.op("pe", lambda e, h=h, hs=hs: e.matmul(pY[:, h, :], ch_[:, h, 192:256], v_[:, hs], start=False, stop=False),
                             reads=[ch_, v_], writes=[pY], accum=True)
                        k.op("pe", lambda e, h=h: e.matmul(pY[:, h, :], ch_[:, h, 64:128], Us[:, h, :], start=False, stop=True),
                             reads=[ch_, Us], writes=[pY], accum=True)
                    k.op("act", lambda e, ys=ys: e.copy(out=ys[:], in_=pY[:]), reads=[pY], writes=[ys])
                    k.dma(YW[n, r0:r0 + 64, :], ys[:].rearrange("p h c -> p (h c)"), reads=[ys], sem=sy[n])
                for h in range(H):
                    hs = slice(h * 64, (h + 1) * 64)
                    k.op("pe", lambda e, h=h, hs=hs: e.matmul(pM[:, h, :], kd_[:, hs], v_[:, hs], start=True, stop=False),
                         reads=[kd_, v_], writes=[pM], accum=True)
                    k.op("pe", lambda e, h=h, hs=hs: e.matmul(pM[:, h, :], nad_[:, hs], Us[:, h, :], start=False, stop=True),
                         reads=[nad_, Us], writes=[pM], accum=True)
                k.op("dve", lambda e, Mn=Mn: e.tensor_tensor(out=Mn[:], in0=Mn[:], in1=pM[:], op=ALU.add), reads=[Mn, pM], writes=[Mn])
                k.op("dve", lambda e, Mn=Mn, pc_=pc_: e.tensor_tensor(out=Mn[:], in0=Mn[:], in1=pc_[:].unsqueeze(2).to_broadcast([64, H, 64]), op=ALU.mult),
                     reads=[Mn, pc_], writes=[Mn])
        k.end()
        if self.stop_at == 5:
            return
        O = self.dt("rwO", [T, D])
        k.begin()
        s0 = k.dsem()
        gw = k.tile([128, 2, D])
        for i_, r_ in enumerate((7, 8)):
            k.dma(gw[:, i_, :], rows[r_:r_ + 1, :].partition_broadcast(128), writes=[gw], sem=s0)
        ya = [k.tile([128, D]) for _ in range(2)]
        yb_ = [k.tile([128, D]) for _ in range(2)]
        bo = [k.tile([128, D]) for _ in range(2)]
        gt = [k.tile([128, D]) for _ in range(2)]
        ss = [[k.dsem() for _ in range(4)] for _ in range(2)]
        sq_ = k.tile([128, D])
        st1 = k.tile([128, H]); st2 = k.tile([128, H])
        so = [k.dsem() for _ in range(2)]
        for t in range(T // 128):
            b = t % 2
            r0 = TC + t * 128
            k.dma(ya[b][:], YW[0, r0:r0 + 128, :], writes=[ya[b]], sem=ss[b][0])
            k.dma(yb_[b][:], YW[1, r0:r0 + 128, :], writes=[yb_[b]], sem=ss[b][1])
            k.dma(bo[b][:], BON[r0:r0 + 128, :], writes=[bo[b]], sem=ss[b][2])
            k.dma(gt[b][:], Gm[r0:r0 + 128, :], writes=[gt[b]], sem=ss[b][3])
            w = ya[b]
            w3_ = w[:].rearrange("p (h c) -> p h c", h=H)
            k.op("dve", lambda e, w=w, b=b: e.tensor_tensor(out=w[:], in0=w[:], in1=yb_[b][:], op=ALU.add), reads=[w, yb_[b]], writes=[w])
            k.op("dve", lambda e, w3_=w3_: e.tensor_reduce(out=st1[:], in_=w3_, axis=AX.X, op=ALU.add), reads=[w], writes=[st1])
            k.op("dve", lambda e: e.tensor_scalar(out=st1[:], in0=st1[:], scalar1=1.0 / 64, scalar2=None, op0=ALU.mult), reads=[st1], writes=[st1])
            k.op("dve", lambda e, w3_=w3_: e.tensor_tensor(out=w3_, in0=w3_, in1=st1[:].unsqueeze(2).to_broadcast([128, H, 64]), op=ALU.subtract),
                 reads=[w, st1], writes=[w])
            k.op("pool", lambda e, w=w: e.tensor_tensor(out=sq_[:], in0=w[:], in1=w[:], op=ALU.mult), reads=[w], writes=[sq_])
            k.op("dve", lambda e: e.tensor_reduce(out=st2[:], in_=sq_[:].rearrange("p (h c) -> p h c", h=H), axis=AX.X, op=ALU.add), reads=[sq_], writes=[st2])
            k.op("dve", lambda e: e.tensor_scalar(out=st2[:], in0=st2[:], scalar1=1.0 / 64, scalar2=64e-5, op0=ALU.mult, op1=ALU.add), reads=[st2], writes=[st2])
            k.op("act", lambda e: e.sqrt(out=st2[:], in_=st2[:]), reads=[st2], writes=[st2])
            k.op("dve", lambda e: e.reciprocal(out=st2[:], in_=st2[:]), reads=[st2], writes=[st2])
            k.op("dve", lambda e, w3_=w3_: e.tensor_tensor(out=w3_, in0=w3_, in1=st2[:].unsqueeze(2).to_broadcast([128, H, 64]), op=ALU.mult),
                 reads=[w, st2], writes=[w])
            k.op("pool", lambda e, w=w: e.tensor_tensor(out=w[:], in0=w[:], in1=gw[:, 0, :], op=ALU.mult), reads=[w, gw], writes=[w])
            k.op("pool", lambda e, w=w: e.tensor_tensor(out=w[:], in0=w[:], in1=gw[:, 1, :], op=ALU.add), reads=[w, gw], writes=[w])
            k.op("dve", lambda e, w=w, b=b: e.tensor_tensor(out=w[:], in0=w[:], in1=bo[b][:], op=ALU.add), reads=[w, bo[b]], writes=[w])
            k.op("dve", lambda e, w=w, b=b: e.tensor_tensor(out=w[:], in0=w[:], in1=gt[b][:], op=ALU.mult), reads=[w, gt[b]], writes=[w])
            k.dma(O[t * 128:(t + 1) * 128, :], w[:], reads=[w], sem=so[b])
        k.end()
        OT = self.dt("rwOT", [D, T])
        self.transpose_in(O, OT, T)
        self.gemm(OT, Wr("wo"), D, D, T, Y)

    def final_dbg(self):
        k = self.k
        k.begin()
        s = k.dsem()
        for name in self.dbg:
            if name == "modv":
                o = self.dt("dbg_modv", [128, DEPTH * 48 * 2], kind="ExternalOutput")
                k.dma(o, self.modv[:].rearrange("p a b c -> p (a b c)"), reads=[self.modv], sem=s)
                continue
            src = self.dram[name]
            o = self.dt("dbg_" + name, list(src.shape), kind="ExternalOutput")
            k.dma(o, src.ap(), sem=s, q="sp")
        k.end()


_CACHE = {}
RUN_KW = {}
LAST = {}
RUN_KW = {}
LAST = {}


def run(inputs, layers=(0, 1, 2, 3), dbg=(), cores=NCORES, test=None, extra=None):
    key = (tuple(layers), tuple(dbg), test)
    if key not in _CACHE:
        _CACHE[key] = Prog(list(layers), dbg, test)
    prog = _CACHE[key]
    blobs, lay = pack_host(inputs, list(layers))
    in_maps = []
    for c in range(cores):
        m = {"x": np.ascontiguousarray(inputs["x"][c]),
             "c": np.ascontiguousarray(inputs["c"][c:c + 1]),
             "ctx": np.ascontiguousarray(inputs["ctx"][c]),
             "c_ctx": np.ascontiguousarray(inputs["c_ctx"][None, :])}
        for piece, flat in blobs.items():
            m["blob_" + piece] = flat
        if extra:
            m.update(extra)
        in_maps.append(m)
    res = run_bass_kernel_spmd(prog.nc, in_maps, core_ids=list(range(cores)), **RUN_KW)
    LAST["exec_ns"] = getattr(res, "exec_time_ns", None)
    return res.results


def kernel(**inputs):
    res = run(inputs)
    return np.stack([r["out"] for r in res], 0).astype(np.float32)
```
